# Optimizing a Trainium2 kernel written in Bass

```python
import math, functools
import jax, jax.numpy as jnp
from jax import lax
import numpy as np

D_MODEL = 1024
BATCH = 4
SEQ = 4096
DEPTH = 1

MIX_WIDTH = D_MODEL
HEAD_DIM = 64
ATTN_WIDTH = D_MODEL // 2
N_HEADS_ATTN = ATTN_WIDTH // HEAD_DIM
POOL_WIDTH = D_MODEL // 4
POOL_WINDOWS = (2, 4, 8, 16)
POOL_GROUP = POOL_WIDTH // len(POOL_WINDOWS)
MEM_WIDTH = D_MODEL // 4
MEM_HEADS = 4
MEM_HEAD_DIM = MEM_WIDTH // MEM_HEADS
N_MEM = 256
IN_WIDTH = 3 * ATTN_WIDTH + POOL_WIDTH + MEM_WIDTH

DILATED_PATTERNS = ((128, 1), (512, 4), (2048, 16))
BLOCK = 128

N_GROUPS = 8
EXPERTS_PER_GROUP = 8
N_EXPERTS = N_GROUPS * EXPERTS_PER_GROUP
TOP_K_INNER = 2
EXPERT_HIDDEN = D_MODEL // 2
ROUTE_BLOCK = 128

EPS = 1e-6
NEG_INF = -1e30

kernel_name = "hymba_dilated_pool_memory_hiermoe"


def _rms_norm(x, g):
    xf = x.astype(jnp.float32)
    y = xf * lax.rsqrt(jnp.mean(xf * xf, axis=-1, keepdims=True) + EPS)
    return (y * g.astype(jnp.float32)).astype(x.dtype)


def _banded_window_attention(q, k, v, steps):
    N, L, H, hd = q.shape
    nb = -(-L // BLOCK)
    pad = nb * BLOCK - L

    def blocks(t):
        return jnp.pad(t, ((0, 0), (0, pad), (0, 0), (0, 0))).reshape(N, nb, BLOCK, H, hd)

    def band(t):
        prev = jnp.concatenate([jnp.zeros_like(t[:, :1]), t[:, :-1]], axis=1)
        return jnp.concatenate([prev, t], axis=2)

    qb = blocks(q)
    kk = band(blocks(k))
    vv = band(blocks(v))
    s = jnp.einsum('nbqhd,nbkhd->nbhqk', qb, kk).astype(jnp.float32) * (hd ** -0.5)
    qi = jnp.arange(BLOCK)[:, None]
    kj = jnp.arange(2 * BLOCK)[None, :]
    dist = qi + BLOCK - kj
    in_band = (dist >= 0) & (dist <= steps)
    has_prev = (jnp.arange(nb) > 0)[:, None, None] | (kj >= BLOCK)[None]
    mask = in_band[None] & has_prev
    s = jnp.where(mask[None, :, None], s, NEG_INF)
    m = jnp.max(s, axis=-1, keepdims=True)
    p = jnp.exp(s - m)
    den = jnp.sum(p, axis=-1, keepdims=True)
    o = jnp.einsum('nbhqk,nbkhd->nbqhd', p, vv.astype(jnp.float32))
    o = o / jnp.transpose(den[..., 0], (0, 1, 3, 2))[..., None]
    lse = jnp.transpose((m + jnp.log(den))[..., 0], (0, 1, 3, 2))
    o = o.reshape(N, nb * BLOCK, H, hd)[:, :L]
    lse = lse.reshape(N, nb * BLOCK, H)[:, :L]
    return o, lse


def _dilated_attention_mixture(q, k, v):
    B, S, H, hd = q.shape
    outs, lses = [], []
    for window, dil in DILATED_PATTERNS:
        L = S // dil
        qr = q.reshape(B, L, dil, H, hd).transpose(0, 2, 1, 3, 4).reshape(B * dil, L, H, hd)
        kr = k.reshape(B, L, dil, H, hd).transpose(0, 2, 1, 3, 4).reshape(B * dil, L, H, hd)
        vr = v.reshape(B, L, dil, H, hd).transpose(0, 2, 1, 3, 4).reshape(B * dil, L, H, hd)
        o, lse = _banded_window_attention(qr, kr, vr, window // dil)
        outs.append(o.reshape(B, dil, L, H, hd).transpose(0, 2, 1, 3, 4).reshape(B, S, H, hd))
        lses.append(lse.reshape(B, dil, L, H).transpose(0, 2, 1, 3).reshape(B, S, H))
    alpha = jax.nn.softmax(jnp.stack(lses, axis=0), axis=0)
    return jnp.sum(alpha[..., None] * jnp.stack(outs, axis=0), axis=0)


def _multiscale_pool(u, proj, scale):
    B, S, C = u.shape
    uf = u.astype(jnp.float32)
    c0 = jnp.concatenate([jnp.zeros((B, 1, C), jnp.float32), jnp.cumsum(uf, axis=1)], axis=1)
    groups = []
    for gi, w in enumerate(POOL_WINDOWS):
        sl = slice(gi * POOL_GROUP, (gi + 1) * POOL_GROUP)
        cg = c0[:, :, sl]
        upper = cg[:, 1:]
        lower = jnp.concatenate([jnp.zeros((B, w - 1, POOL_GROUP), jnp.float32), cg[:, :S + 1 - w]], axis=1)
        cnt = jnp.minimum(jnp.arange(1, S + 1), w).astype(jnp.float32)[None, :, None]
        groups.append((upper - lower) / cnt - uf[:, :, sl])
    pooled = jnp.stack(groups, axis=2)
    y = jnp.einsum('bsgc,gce->bsge', pooled, proj.astype(jnp.float32)).reshape(B, S, C)
    return y * scale.astype(jnp.float32)


def _memory_attention(qm, mem, mem_norm, w_mem_kv, mq_norm, mk_norm):
    B, S, _ = qm.shape
    mn = _rms_norm(mem, mem_norm)
    kv = jnp.einsum('bmd,de->bme', mn, w_mem_kv)
    km = kv[..., :MEM_WIDTH].reshape(B, -1, MEM_HEADS, MEM_HEAD_DIM)
    vm = kv[..., MEM_WIDTH:].reshape(B, -1, MEM_HEADS, MEM_HEAD_DIM)
    q = _rms_norm(qm.reshape(B, S, MEM_HEADS, MEM_HEAD_DIM), mq_norm)
    km = _rms_norm(km, mk_norm)
    s = jnp.einsum('bshd,bmhd->bhsm', q, km).astype(jnp.float32) * (MEM_HEAD_DIM ** -0.5)
    p = jax.nn.softmax(s, axis=-1)
    return jnp.einsum('bhsm,bmhd->bshd', p, vm.astype(jnp.float32)).reshape(B, S, MEM_WIDTH)


def _hier_moe(h, w_group, b_group, w_router, b_router, w1, w3, w2):
    T, D = h.shape
    lg = (jnp.einsum('td,dg->tg', h, w_group) + b_group).astype(jnp.float32)
    pg = jax.nn.softmax(lg, axis=-1)
    g_sel = jnp.argmax(lg, axis=-1)
    w_g = jnp.take_along_axis(pg, g_sel[:, None], axis=-1)[:, 0]
    le = (jnp.einsum('td,gde->tge', h, w_router) + b_router).astype(jnp.float32)
    le = jnp.take_along_axis(le, g_sel[:, None, None], axis=1)[:, 0]
    top_v, top_i = lax.top_k(le, TOP_K_INNER)
    gates = w_g[:, None] * jax.nn.softmax(top_v, axis=-1)
    eid = (g_sel[:, None] * EXPERTS_PER_GROUP + top_i).reshape(-1).astype(jnp.int32)
    tok = jnp.repeat(jnp.arange(T, dtype=jnp.int32), TOP_K_INNER)
    gate = gates.reshape(-1)
    order = jnp.argsort(eid)
    eid_s, tok_s, gate_s = eid[order], tok[order], gate[order]
    counts = jnp.bincount(eid, length=N_EXPERTS)
    starts = jnp.cumsum(counts) - counts
    padded = ((counts + ROUTE_BLOCK - 1) // ROUTE_BLOCK) * ROUTE_BLOCK
    pends = jnp.cumsum(padded)
    pstarts = pends - padded
    n_assign = T * TOP_K_INNER
    rank = jnp.arange(n_assign, dtype=jnp.int32) - starts[eid_s]
    dest = pstarts[eid_s] + rank
    R = n_assign + N_EXPERTS * ROUTE_BLOCK
    n_blk = R // ROUTE_BLOCK
    row_tok = jnp.full((R,), T, jnp.int32).at[dest].set(tok_s)
    row_gate = jnp.zeros((R,), jnp.float32).at[dest].set(gate_s)
    blk_exp = jnp.minimum(jnp.searchsorted(pends, jnp.arange(n_blk) * ROUTE_BLOCK, side='right'),
                          N_EXPERTS - 1).astype(jnp.int32)
    h_pad = jnp.concatenate([h, jnp.zeros((1, D), h.dtype)], axis=0)
    xr = h_pad[row_tok].reshape(n_blk, ROUTE_BLOCK, D)

    def expert_block(args):
        xb, e = args
        a = jnp.einsum('rd,df->rf', xb, w1[e])
        b = jnp.einsum('rd,df->rf', xb, w3[e])
        return jnp.einsum('rf,fd->rd', jax.nn.silu(a) * b, w2[e])

    yb = lax.map(expert_block, (xr, blk_exp)).reshape(R, D).astype(jnp.float32)
    y = jax.ops.segment_sum(yb * row_gate[:, None], row_tok, num_segments=T + 1)[:T]
    return y


def _layer(x, mem, attn_norm, w_in, q_norm, k_norm, pool_proj, pool_scale, mem_norm, w_mem_kv,
           mq_norm, mk_norm, w_out, ffn_norm, w_group, b_group, w_router, b_router, w1, w3, w2):
    B, S, D = x.shape
    h = _rms_norm(x, attn_norm)
    z = jnp.einsum('bsd,de->bse', h, w_in)
    a0, a1, a2, a3 = ATTN_WIDTH, 2 * ATTN_WIDTH, 3 * ATTN_WIDTH, 3 * ATTN_WIDTH + POOL_WIDTH
    q = _rms_norm(z[..., :a0].reshape(B, S, N_HEADS_ATTN, HEAD_DIM), q_norm)
    k = _rms_norm(z[..., a0:a1].reshape(B, S, N_HEADS_ATTN, HEAD_DIM), k_norm)
    v = z[..., a1:a2].reshape(B, S, N_HEADS_ATTN, HEAD_DIM)
    u = z[..., a2:a3]
    qm = z[..., a3:]
    y_attn = _dilated_attention_mixture(q, k, v).reshape(B, S, ATTN_WIDTH)
    y_pool = _multiscale_pool(u, pool_proj, pool_scale)
    y_mem = _memory_attention(qm, mem, mem_norm, w_mem_kv, mq_norm, mk_norm)
    mix = jnp.concatenate([y_attn, y_pool, y_mem], axis=-1).astype(x.dtype)
    x = x + jnp.einsum('bse,ed->bsd', mix, w_out)
    h2 = _rms_norm(x, ffn_norm).reshape(B * S, D)
    y = _hier_moe(h2, w_group, b_group, w_router, b_router, w1, w3, w2)
    return x + y.reshape(B, S, D).astype(x.dtype)


def setup_inputs(seed: int = 0) -> dict:
    key = jax.random.key(seed)
    ks = jax.random.split(key, 24)
    f32 = jnp.float32
    L = DEPTH

    def nrm(k, shape, scale):
        return jax.random.normal(k, shape, f32) * scale

    def gain(k, n):
        return 1.0 + 0.02 * jax.random.normal(k, (L, n), f32)

    return {
        "x": jax.random.normal(ks[0], (BATCH, SEQ, D_MODEL), f32),
        "mem": jax.random.normal(ks[1], (BATCH, N_MEM, D_MODEL), f32),
        "attn_norm": gain(ks[2], D_MODEL),
        "w_in": nrm(ks[3], (L, D_MODEL, IN_WIDTH), D_MODEL ** -0.5),
        "q_norm": gain(ks[4], HEAD_DIM),
        "k_norm": gain(ks[5], HEAD_DIM),
        "pool_proj": nrm(ks[6], (L, len(POOL_WINDOWS), POOL_GROUP, POOL_GROUP), POOL_GROUP ** -0.5),
        "pool_scale": gain(ks[7], POOL_WIDTH),
        "mem_norm": gain(ks[8], D_MODEL),
        "w_mem_kv": nrm(ks[9], (L, D_MODEL, 2 * MEM_WIDTH), D_MODEL ** -0.5),
        "mq_norm": gain(ks[10], MEM_HEAD_DIM),
        "mk_norm": gain(ks[11], MEM_HEAD_DIM),
        "w_out": nrm(ks[12], (L, MIX_WIDTH, D_MODEL), MIX_WIDTH ** -0.5),
        "ffn_norm": gain(ks[13], D_MODEL),
        "w_group": nrm(ks[14], (L, D_MODEL, N_GROUPS), D_MODEL ** -0.5),
        "b_group": nrm(ks[15], (L, N_GROUPS), 0.01),
        "w_router": nrm(ks[16], (L, N_GROUPS, D_MODEL, EXPERTS_PER_GROUP), D_MODEL ** -0.5),
        "b_router": nrm(ks[17], (L, N_GROUPS, EXPERTS_PER_GROUP), 0.01),
        "w1": nrm(ks[18], (L, N_EXPERTS, D_MODEL, EXPERT_HIDDEN), D_MODEL ** -0.5),
        "w3": nrm(ks[19], (L, N_EXPERTS, D_MODEL, EXPERT_HIDDEN), D_MODEL ** -0.5),
        "w2": nrm(ks[20], (L, N_EXPERTS, EXPERT_HIDDEN, D_MODEL), EXPERT_HIDDEN ** -0.5),
    }


def reference(x, mem, attn_norm, w_in, q_norm, k_norm, pool_proj, pool_scale, mem_norm, w_mem_kv,
              mq_norm, mk_norm, w_out, ffn_norm, w_group, b_group, w_router, b_router, w1, w3, w2):
    for l in range(DEPTH):
        x = _layer(x, mem, attn_norm[l], w_in[l], q_norm[l], k_norm[l], pool_proj[l], pool_scale[l],
                   mem_norm[l], w_mem_kv[l], mq_norm[l], mk_norm[l], w_out[l], ffn_norm[l],
                   w_group[l], b_group[l], w_router[l], b_router[l], w1[l], w3[l], w2[l])
    return x
```

```python
import numpy as np
import concourse.bass as bass
import concourse.mybir as mybir
from concourse.bass_utils import run_bass_kernel_spmd

F32 = mybir.dt.float32
BF16 = mybir.dt.bfloat16
I32 = mybir.dt.int32
AF = mybir.ActivationFunctionType
ALU = mybir.AluOpType
AX = mybir.AxisListType

PE, ACT, DVE, POOL, SP = "pe", "act", "dve", "pool", "sp"
ENGS = (PE, ACT, DVE, POOL, SP)
RING = 12


class Op:
    __slots__ = ("eng", "fn", "deps", "dma", "need", "seq", "di", "name")

    def __init__(self, eng, fn, dma, name):
        self.eng, self.fn, self.dma, self.name = eng, fn, dma, name
        self.deps, self.need, self.seq, self.di = [], False, 0, -1


class Prog:
    def __init__(self):
        self.ops = {e: [] for e in ENGS}
        self.lastw = {}
        self.readers = {}

    def add(self, eng, fn, r=(), w=(), dma=False, name=""):
        op = Op(eng, fn, dma, name)
        deps = {}
        for k in r:
            lw = self.lastw.get(k)
            if lw is not None:
                deps[id(lw)] = (lw, True)
        for k in w:
            lw = self.lastw.get(k)
            if lw is not None:
                deps[id(lw)] = (lw, True)
            for rd in self.readers.get(k, ()):
                if id(rd) not in deps:
                    deps[id(rd)] = (rd, False)
        for k in r:
            self.readers.setdefault(k, []).append(op)
        for k in w:
            self.lastw[k] = op
            self.readers[k] = []
        op.deps = [d for d in deps.values() if d[0] is not op]
        self.ops[eng].append(op)
        return op

    @staticmethod
    def _needs_wait(o, d, true_dep):
        if d.dma:
            return True
        if d.eng != o.eng:
            return True
        if o.eng == PE:
            return False
        if o.dma:
            return True
        return true_dep

    def emit(self, nc):
        for e in ENGS:
            for o in self.ops[e]:
                for d, t in o.deps:
                    if self._needs_wait(o, d, t):
                        d.need = True
        for e in ENGS:
            c = 0
            n = 0
            for o in self.ops[e]:
                if o.dma:
                    o.di = n
                    n += 1
                elif o.need:
                    c += 1
                    o.seq = c
        from contextlib import ExitStack

        with ExitStack() as st:
            esem = {e: st.enter_context(nc.semaphore("c_" + e)) for e in (PE, ACT, DVE, POOL)}
            rsem = {
                e: [st.enter_context(nc.semaphore("r_%s%d" % (e, i))) for i in range(RING)]
                for e in (SP, POOL, ACT)
            }
            block = st.enter_context(nc.Block())

            def run(ename, eng):
                seen = {}

                def wait(sem, val):
                    if seen.get(sem.num, 0) < val:
                        eng.wait_ge(sem, val)
                        seen[sem.num] = val

                ndma = 0
                for o in self.ops[ename]:
                    for d, t in o.deps:
                        if not self._needs_wait(o, d, t):
                            continue
                        if d.dma:
                            wait(rsem[d.eng][d.di % RING], 16 * (d.di // RING + 1))
                        else:
                            wait(esem[d.eng], d.seq)
                    if o.dma:
                        if o.di >= RING:
                            wait(rsem[ename][o.di % RING], 16 * (o.di // RING))
                        ins = o.fn(eng)
                        ins.then_inc(rsem[ename][o.di % RING], 16)
                        ndma = o.di + 1
                    else:
                        ins = o.fn(eng)
                        if o.need:
                            ins.then_inc(esem[ename], 1)
                for i in range(max(0, ndma - RING), ndma):
                    wait(rsem[ename][i % RING], 16 * (i // RING + 1))

            block.tensor(lambda eng: run(PE, eng))
            block.scalar(lambda eng: run(ACT, eng))
            block.vector(lambda eng: run(DVE, eng))
            block.gpsimd(lambda eng: run(POOL, eng))
            block.sync(lambda eng: run(SP, eng))


class Mem:
    def __init__(self, nc, P, total_bytes):
        self.P = P
        self.t = nc.alloc_sbuf_tensor("arena", [128, total_bytes // 4], F32)
        self.regs = {}
        self.top = 0
        self.total = total_bytes

    def region(self, name, size):
        size = (size + 63) // 64 * 64
        assert self.top + size <= self.total, (name, self.top, size)
        self.regs[name] = dict(off=self.top, size=size, cur=0, keys=[], old=[])
        self.top += size

    def reset(self, name):
        r = self.regs[name]
        r["old"] = r["old"] + r["keys"]
        r["keys"] = []
        r["cur"] = 0

    def alloc(self, reg, key, shape, dt, keys=None):
        r = self.regs[reg]
        isz = 2 if dt == BF16 else 4
        n = 1
        for s in shape[1:]:
            n *= s
        nb = (n * isz + 63) // 64 * 64
        assert r["cur"] + nb <= r["size"], (reg, key, r["cur"], nb, r["size"])
        off = r["off"] + r["cur"]
        r["cur"] += nb
        keys = list(keys) if keys else [key]
        r["keys"].extend(keys)
        olds = []
        for ok in r["old"]:
            lw = self.P.lastw.get(ok)
            if lw is not None:
                olds.append(lw)
            olds.extend(self.P.readers.get(ok, ()))
        if olds:
            for kk in keys:
                self.P.readers.setdefault(kk, []).extend(olds)
        ap = self.t[:, off // 4:(off + n * isz + 3) // 4]
        if dt == BF16:
            ap = ap.bitcast(BF16)
        ap = ap[:, 0:n]
        if len(shape) == 3:
            ap = ap.rearrange("p (a b) -> p a b", a=shape[1])
        elif len(shape) == 4:
            ap = ap.rearrange("p (a b c) -> p a b c", a=shape[1], b=shape[2])
        return ap


NCF = 1416
NCB = 1280
CF_GATTN, CF_GMEM, CF_GQ, CF_GK, CF_GMQ, CF_GMK = 0, 8, 16, 17, 18, 19
CF_INVW, CF_PSC, CF_IC16, CF_HV, CF_EPS = 20, 22, 24, 56, 57
CF_ONES, CF_OFFS, CF_BIAS, CF_IDF, CF_GFFN = 64, 128, 192, 264, 392
CB_ID, CB_MASK, CB_BD, CB_ONES, CB_TRI, CB_PBD = 0, 128, 640, 768, 896, 1024
EPS = 1e-6
EXPERT_CAP = 128


def build_program(stage=99, dumps=()):
    nc = bass.Bass("TRN2", target_bir_lowering=False)
    P = Prog()
    D = {}

    def din(name, shape, dt=F32):
        D[name] = nc.dram_tensor(name, shape, dt, kind="ExternalInput").ap()

    din("xo", [2048, 1024]); din("xh", [2048, 1024]); din("mem", [256, 1024])
    din("w_in", [1024, 2048]); din("w_out", [1024, 1024]); din("w_kv", [1024, 512])
    din("w_r", [1024, 72]); din("cf", [128, NCF]); din("cb", [128, NCB])
    if stage >= 5:
        din("w1", [64, 1024, 512]); din("w3", [64, 1024, 512]); din("w2", [64, 512, 1024])
    out = nc.dram_tensor("out", [2048, 1024], F32, kind="ExternalOutput").ap()
    xg = nc.dram_tensor("xg", [64 * EXPERT_CAP, 1024], BF16, kind="Internal").ap()
    yg = nc.dram_tensor("yg", [64 * EXPERT_CAP, 1024], F32, kind="Internal").ap()
    xmid = nc.dram_tensor("xmid", [2048, 1024], F32, kind="Internal").ap()
    dump_out = {}

    M = Mem(nc, P, 203 * 1024)
    M.region("const", 12 * 1024)
    M.region("wout", 16 * 1024)
    M.region("r1", 32 * 1024)
    M.region("qt", 16 * 1024)
    M.region("kt", 32 * 1024)
    M.region("vt", 32 * 1024)
    M.region("r2", 36 * 1024)
    M.region("ut", 16640)
    M.region("qmt", 8 * 1024)
    PS = [nc.alloc_psum_tensor("ps%d" % i, [128, 512], F32) for i in range(8)]

    def psf(i):
        return PS[i][:, :]

    def psb(i):
        return PS[i][:, :].bitcast(BF16)

    def pk(i):
        return "ps%d" % i

    A = P.add
    CF = M.alloc("const", "cf", [128, NCF], F32)
    CB = M.alloc("const", "cb", [128, NCB], BF16)
    ZT = M.alloc("const", "zt", [128, 1024], BF16)
    IDB = CB[:, CB_ID:CB_ID + 128]
    MASK4 = CB[:, CB_MASK:CB_MASK + 512]
    BD64 = CB[:, CB_BD:CB_BD + 128]
    ONESB = CB[:, CB_ONES:CB_ONES + 128]
    TRI = CB[:, CB_TRI:CB_TRI + 128]
    EPSC = CF[:, CF_EPS:CF_EPS + 1]
    IDF = CF[:, CF_IDF:CF_IDF + 128]
    ONES64 = CF[:, CF_ONES:CF_ONES + 64]

    A(SP, lambda e: e.dma_start(out=CF, in_=D["cf"]), w=["cf"], dma=True)
    A(POOL, lambda e: e.dma_start(out=CB, in_=D["cb"]), w=["cb"], dma=True)
    WIN = M.alloc("r1", "win", [128, 8, 2048], BF16, keys=["win%d" % i for i in range(4)])
    win_keys = []
    for i in range(4):
        key = "win%d" % i
        win_keys.append(key)
        A(POOL, lambda e, i=i: e.dma_start(
            out=WIN[:, 2 * i:2 * i + 2, :],
            in_=D["w_in"][256 * i:256 * i + 256, :].rearrange("(k p) c -> p k c", p=128)),
          w=[key], dma=True)
    A(DVE, lambda e: e.memset(ZT, 0.0), w=["zt"])
    for i in range(64):
        A(SP, lambda e, i=i: e.dma_start(out=xg[i * 128:(i + 1) * 128, :], in_=ZT), r=["zt"], w=["xg"], dma=True)

    QT = M.alloc("qt", "qt", [128, 4, 2048], BF16, keys=["qt%d_%d" % (c, g) for c in range(4) for g in range(4)])
    KT = M.alloc("kt", "kt", [128, 4, 4096], BF16, keys=["kt%d_%d" % (c, g) for c in range(4) for g in range(8)])
    VT = M.alloc("vt", "vt", [128, 4, 4096], BF16, keys=["vt%d_%d" % (c, g) for c in range(4) for g in range(8)])
    UT = M.alloc("ut", "ut", [128, 2, 2064], F32, keys=["ut0", "ut1"])
    QMT = M.alloc("qmt", "qmt", [128, 2, 2048], BF16, keys=["qmt0", "qmt1"])
    XT = [M.alloc("r2", "xt%d" % i, [128, 1024], F32) for i in range(2)]
    XN = [M.alloc("r2", "xn%d" % i, [128, 1024], BF16) for i in range(2)]
    HTG = [M.alloc("r2", "htg%d" % i, [128, 8, 512], BF16, keys=["htg%d_%d" % (i, t) for t in range(4)]) for i in range(2)]
    SQZ = [M.alloc("r2", "sqz%d" % i, [128, 512], BF16) for i in range(2)]
    RS = [M.alloc("r2", "rs%d" % i, [128, 512], F32) for i in range(2)]
    ST4 = [M.alloc("r2", "st4_%d" % i, [128, 4], F32) for i in range(2)]

    def norm_tile(src_ap, slot, gain_col, dst_fn, dkeys, ti):
        xt, xn, st = XT[slot], XN[slot], ST4[slot]
        kx, kn, ks = "xt%d" % slot, "xn%d" % slot, "st4_%d" % slot
        tp = ti % 2
        A(SP, lambda e: e.dma_start(out=xt, in_=src_ap), w=[kx], dma=True)
        A(ACT, lambda e: e.activation(out=xn, in_=xt, func=AF.Square, accum_out=st[:, 0:1]), r=[kx], w=[kn, ks])
        A(ACT, lambda e: e.activation(out=st[:, 1:2], in_=st[:, 0:1], func=AF.Sqrt, bias=EPSC, scale=1.0 / 1024), r=[ks, "cf"], w=[ks])
        A(DVE, lambda e: e.reciprocal(out=st[:, 2:3], in_=st[:, 1:2]), r=[ks], w=[ks])
        A(ACT, lambda e: e.activation(out=xn, in_=xt, func=AF.Copy, scale=st[:, 2:3]), r=[kx, ks], w=[kn])

        def tr(e):
            for k in range(8):
                ins = e.transpose(out=psb(tp)[:, k * 128:(k + 1) * 128], in_=xn[:, k * 128:(k + 1) * 128], identity=IDB)
            return ins
        A(PE, tr, r=[kn, "cb"], w=[pk(tp)])
        g = CF[:, gain_col:gain_col + 8].unsqueeze(2).broadcast_to([128, 8, 128])
        A(DVE, lambda e: e.tensor_tensor(out=dst_fn(), in0=psb(tp).rearrange("p (k t) -> p k t", k=8), in1=g, op=ALU.mult),
          r=[pk(tp), "cf"], w=dkeys)

    def head_norm(zbank, zkey, n, gain_col, dst, dkeys, i):
        sq, rs = SQZ[i % 2], RS[i % 2]
        ksq, krs = "sqz%d" % (i % 2), "rs%d" % (i % 2)
        mb = 4 + i % 2
        A(ACT, lambda e: e.activation(out=sq[:, 0:n], in_=psf(zbank)[:, 0:n], func=AF.Square), r=[zkey], w=[ksq])
        A(PE, lambda e: e.matmul(psf(mb)[:, 0:n], lhsT=BD64, rhs=sq[:, 0:n], start=True, stop=True), r=[ksq, "cb"], w=[pk(mb)])
        A(ACT, lambda e: e.activation(out=rs[:, 0:n], in_=psf(mb)[:, 0:n], func=AF.Sqrt, bias=EPSC, scale=1.0), r=[pk(mb), "cf"], w=[krs])
        A(DVE, lambda e: e.reciprocal(out=rs[:, 0:n], in_=rs[:, 0:n]), r=[krs], w=[krs])
        A(DVE, lambda e: e.scalar_tensor_tensor(out=dst, in0=psf(zbank)[:, 0:n], scalar=CF[:, gain_col:gain_col + 1], in1=rs[:, 0:n],
                                                 op0=ALU.mult, op1=ALU.mult), r=[zkey, krs, "cf"], w=dkeys)

    ti = 0
    zi = 0
    hn = 0
    for g in range(8):
        hb = HTG[g % 2]
        hkeys = ["htg%d_%d" % (g % 2, t) for t in range(4)]
        for t in range(4):
            src = (D["xh"] if g < 4 else D["xo"])[((g % 4) * 4 + t) * 128:((g % 4) * 4 + t + 1) * 128, :]
            norm_tile(src, ti % 2, CF_GATTN, lambda hb=hb, t=t: hb[:, :, t * 128:(t + 1) * 128], [hkeys[t]], ti)
            ti += 1
        chunks = list(range(4, 12))
        if g >= 3:
            chunks += [12, 13]
        if g >= 4:
            chunks += [0, 1, 2, 3, 14, 15]
        for c in chunks:
            zb = 2 + zi % 2
            zi += 1

            def proj(e, c=c, zb=zb, hb=hb):
                for k in range(8):
                    ins = e.matmul(psf(zb), lhsT=WIN[:, k, c * 128:(c + 1) * 128], rhs=hb[:, k, :], start=(k == 0), stop=(k == 7))
                return ins
            A(PE, proj, r=win_keys + hkeys, w=[pk(zb)])
            if 8 <= c < 12:
                A(ACT, lambda e, c=c, zb=zb, g=g: e.activation(out=VT[:, c - 8, g * 512:(g + 1) * 512], in_=psf(zb), func=AF.Copy),
                  r=[pk(zb)], w=["vt%d_%d" % (c - 8, g)])
            elif c in (12, 13):
                if g == 3:
                    A(DVE, lambda e, c=c, zb=zb: e.tensor_copy(out=UT[:, c - 12, 0:16], in_=psf(zb)[:, 496:512]), r=[pk(zb)], w=["ut%d" % (c - 12)])
                else:
                    A(DVE, lambda e, c=c, zb=zb, g=g: e.tensor_copy(out=UT[:, c - 12, 16 + (g - 4) * 512:16 + (g - 3) * 512], in_=psf(zb)),
                      r=[pk(zb)], w=["ut%d" % (c - 12)])
            elif c < 4:
                head_norm(zb, pk(zb), 512, CF_GQ, QT[:, c, (g - 4) * 512:(g - 3) * 512], ["qt%d_%d" % (c, g - 4)], hn); hn += 1
            elif c < 8:
                head_norm(zb, pk(zb), 512, CF_GK, KT[:, c - 4, g * 512:(g + 1) * 512], ["kt%d_%d" % (c - 4, g)], hn); hn += 1
            else:
                head_norm(zb, pk(zb), 512, CF_GMQ, QMT[:, c - 14, (g - 4) * 512:(g - 3) * 512], ["qmt%d" % (c - 14)], hn); hn += 1

    def add_dump(name, ap, keys, shape, dt):
        t = nc.dram_tensor("dump_" + name, shape, dt, kind="ExternalOutput").ap()
        dump_out[name] = t
        A(SP, lambda e: e.dma_start(out=t, in_=ap), r=keys, w=["dump_" + name], dma=True)

    vt_keys = lambda c: ["vt%d_%d" % (c, g) for g in range(8)]
    kt_keys = lambda c: ["kt%d_%d" % (c, g) for g in range(8)]
    qt_keys = lambda c: ["qt%d_%d" % (c, g) for g in range(4)]
    if "qkv" in dumps:
        add_dump("qt", QT, sum([qt_keys(c) for c in range(4)], []), [128, 4, 2048], BF16)
        add_dump("kt", KT, sum([kt_keys(c) for c in range(4)], []), [128, 4, 4096], BF16)
        add_dump("vt", VT, sum([vt_keys(c) for c in range(4)], []), [128, 4, 4096], BF16)
        add_dump("ut", UT, ["ut0", "ut1"], [128, 2, 2064], F32)
        add_dump("qmt", QMT, ["qmt0", "qmt1"], [128, 2, 2048], BF16)
    if stage <= 1:
        P.emit(nc)
        return nc, dump_out
    return build_rest(nc, P, M, D, A, CF, CB, PS, psf, psb, pk, out, xg, yg, xmid, stage, dumps, dump_out, add_dump,
                      dict(QT=QT, KT=KT, VT=VT, UT=UT, QMT=QMT, WIN=WIN, vt_keys=vt_keys, kt_keys=kt_keys, qt_keys=qt_keys,
                           norm_tile=norm_tile, head_norm=head_norm, XT=XT, XN=XN, ST4=ST4, SQZ=SQZ, RS=RS, win_keys=win_keys))


POOL_WINDOWS = (2, 4, 8, 16)


def prep_inputs(inp):
    f = np.float32
    x = np.ascontiguousarray(inp["x"], dtype=f)
    mem = np.ascontiguousarray(inp["mem"], dtype=f)
    cb = np.zeros((128, NCB), f)
    k = np.arange(128)[:, None]
    q = np.arange(128)[None, :]
    cb[:, CB_ID:CB_ID + 128] = np.eye(128, dtype=f)
    mprev = (k >= q).astype(f)
    mcur = (k <= q).astype(f)
    cb[:, CB_MASK:CB_MASK + 512] = np.concatenate([mprev, mcur, mprev, mcur], axis=1)
    bd = np.zeros((128, 128), f)
    bd[:64, :64] = 1.0 / 64
    bd[64:, 64:] = 1.0 / 64
    cb[:, CB_BD:CB_BD + 128] = bd
    cb[:, CB_ONES:CB_ONES + 128] = 1.0
    cb[:, CB_TRI:CB_TRI + 128] = (k < q).astype(f)
    pp = np.asarray(inp["pool_proj"], dtype=f)[0]
    for c in range(2):
        cb[0:64, CB_PBD + c * 128:CB_PBD + c * 128 + 64] = pp[2 * c]
        cb[64:128, CB_PBD + c * 128 + 64:CB_PBD + c * 128 + 128] = pp[2 * c + 1]
    w_r = np.concatenate([np.asarray(inp["w_group"], f)[0],
                          np.transpose(np.asarray(inp["w_router"], f)[0], (1, 0, 2)).reshape(1024, 64)], axis=1)
    w_r = np.ascontiguousarray(w_r)
    shared = dict(
        w_in=np.ascontiguousarray(inp["w_in"][0], dtype=f), w_out=np.ascontiguousarray(inp["w_out"][0], dtype=f),
        w_kv=np.ascontiguousarray(inp["w_mem_kv"][0], dtype=f), w_r=w_r,
        w1=np.ascontiguousarray(inp["w1"][0], dtype=f), w3=np.ascontiguousarray(inp["w3"][0], dtype=f),
        w2=np.ascontiguousarray(inp["w2"][0], dtype=f), cb=cb)
    p = np.arange(128)
    in_maps = []
    for c in range(8):
        b, half = c // 2, c % 2
        cf = np.zeros((128, NCF), f)
        cf[:, CF_GATTN:CF_GATTN + 8] = np.asarray(inp["attn_norm"], f)[0].reshape(8, 128).T
        cf[:, CF_GMEM:CF_GMEM + 8] = np.asarray(inp["mem_norm"], f)[0].reshape(8, 128).T
        cf[:, CF_GQ] = np.tile(np.asarray(inp["q_norm"], f)[0], 2)
        cf[:, CF_GK] = np.tile(np.asarray(inp["k_norm"], f)[0], 2)
        cf[:, CF_GMQ] = np.tile(np.asarray(inp["mq_norm"], f)[0], 2)
        cf[:, CF_GMK] = np.tile(np.asarray(inp["mk_norm"], f)[0], 2)
        for ch in range(2):
            w = np.array([POOL_WINDOWS[2 * ch + pi // 64] for pi in range(128)], f)
            cf[:, CF_INVW + ch] = 1.0 / w
            cf[:, CF_PSC + ch] = np.asarray(inp["pool_scale"], f)[0, ch * 128:(ch + 1) * 128]
            for t in range(16):
                pos = t + 2048 * half
                cf[:, CF_IC16 + ch * 16 + t] = 1.0 / np.minimum(pos + 1, w)
        cf[:, CF_HV] = float(half)
        cf[:, CF_EPS] = EPS
        cf[:, CF_ONES:CF_ONES + 64] = 1.0
        cf[:, CF_OFFS:CF_OFFS + 64] = (np.arange(64) * EXPERT_CAP).astype(f)[None, :]
        cf[:, CF_BIAS:CF_BIAS + 8] = np.asarray(inp["b_group"], f)[0][None, :]
        cf[:, CF_BIAS + 8:CF_BIAS + 72] = np.asarray(inp["b_router"], f)[0].reshape(64)[None, :]
        cf[:, CF_IDF:CF_IDF + 128] = np.eye(128, dtype=f)
        cf[:, CF_GFFN:CF_GFFN + 1024] = np.asarray(inp["ffn_norm"], f)[0][None, :]
        xo = x[b, half * 2048:(half + 1) * 2048]
        xh = x[b, 0:2048] if half == 1 else np.zeros((2048, 1024), f)
        m = dict(shared)
        m.update(xo=np.ascontiguousarray(xo), xh=np.ascontiguousarray(xh), mem=mem[b], cf=cf)
        in_maps.append(m)
    return in_maps


_NC_CACHE = {}


def kernel(**inputs):
    in_maps = prep_inputs(inputs)
    if "nc" not in _NC_CACHE:
        _NC_CACHE["nc"] = build_program()[0]
    res = run_bass_kernel_spmd(_NC_CACHE["nc"], in_maps, core_ids=list(range(8)))
    outs = [np.asarray(r["out"], dtype=np.float32) for r in res.results]
    return np.stack([np.concatenate([outs[2 * b], outs[2 * b + 1]], axis=0) for b in range(4)], axis=0)


def build_rest(nc, P, M, D, A, CF, CB, PS, psf, psb, pk, out, xg, yg, xmid, stage, dumps, dump_out, add_dump, S):
    QT, KT, VT, UT, QMT = S["QT"], S["KT"], S["VT"], S["UT"], S["QMT"]
    vt_keys, kt_keys, qt_keys = S["vt_keys"], S["kt_keys"], S["qt_keys"]
    IDB = CB[:, CB_ID:CB_ID + 128]
    MASK4 = CB[:, CB_MASK:CB_MASK + 512]
    ONESB = CB[:, CB_ONES:CB_ONES + 128]
    TRI = CB[:, CB_TRI:CB_TRI + 128]
    EPSC = CF[:, CF_EPS:CF_EPS + 1]
    IDF = CF[:, CF_IDF:CF_IDF + 128]
    ONES64 = CF[:, CF_ONES:CF_ONES + 64]
    HV = CF[:, CF_HV:CF_HV + 1]

    WOUT = M.alloc("wout", "wout", [128, 8, 1024], BF16)
    A(POOL, lambda e: e.dma_start(out=WOUT, in_=D["w_out"].rearrange("(k p) c -> p k c", p=128)), w=["wout"], dma=True)

    M.reset("r1")
    MIXT = M.alloc("r1", "mixt", [128, 8, 2048], BF16, keys=["mixt%d" % i for i in range(8)])
    M.reset("r2")
    TA = M.alloc("r2", "ta", [128, 2064], F32)
    TB = M.alloc("r2", "tb", [128, 2064], F32)
    PL = M.alloc("r2", "pl", [128, 2, 2048], BF16, keys=["pl0", "pl1"])
    T16 = M.alloc("r2", "t16", [128, 16], F32)
    IC = CF[:, CF_IC16:CF_IC16 + 32].rearrange("p (c t) -> p c t", c=2)
    N = 2064
    for c in range(2):
        U = UT[:, c, :]
        uk = "ut%d" % c
        A(POOL, lambda e, U=U: e.tensor_tensor(out=TA[:, 1:N], in0=U[:, 1:N], in1=U[:, 0:N - 1], op=ALU.add), r=[uk], w=["ta"])
        if c == 0:
            A(POOL, lambda e: e.tensor_tensor(out=TB[64:128, 3:N], in0=TA[64:128, 3:N], in1=TA[64:128, 1:N - 2], op=ALU.add), r=["ta"], w=["tb"])
        else:
            A(POOL, lambda e: e.tensor_tensor(out=TB[:, 3:N], in0=TA[:, 3:N], in1=TA[:, 1:N - 2], op=ALU.add), r=["ta"], w=["tb"])
            A(DVE, lambda e: e.tensor_tensor(out=TA[:, 7:N], in0=TB[:, 7:N], in1=TB[:, 3:N - 4], op=ALU.add), r=["tb"], w=["ta"])
            A(DVE, lambda e: e.tensor_tensor(out=TB[64:128, 15:N], in0=TA[64:128, 15:N], in1=TA[64:128, 7:N - 8], op=ALU.add), r=["ta"], w=["tb"])
        for (lo, hi, T, tk) in ((0, 64, TA, "ta"), (64, 128, TB, "tb")):
            A(DVE, lambda e, lo=lo, hi=hi, T=T, U=U, c=c: e.scalar_tensor_tensor(
                out=PL[lo:hi, c, :], in0=T[lo:hi, 16:N], scalar=CF[lo:hi, CF_INVW + c:CF_INVW + c + 1], in1=U[lo:hi, 16:N],
                op0=ALU.mult, op1=ALU.subtract), r=[tk, uk, "cf"], w=["pl%d" % c])
            A(DVE, lambda e, lo=lo, hi=hi, T=T, c=c: e.tensor_tensor(out=T16[lo:hi, :], in0=T[lo:hi, 16:32], in1=IC[lo:hi, c, :], op=ALU.mult),
              r=[tk, "cf"], w=["t16"])
        A(DVE, lambda e, U=U, c=c: e.tensor_tensor(out=PL[:, c, 0:16], in0=T16, in1=U[:, 16:32], op=ALU.subtract), r=["t16", uk], w=["pl%d" % c])
        for tg in range(4):
            zb = 2 + tg % 2
            A(PE, lambda e, c=c, tg=tg, zb=zb: e.matmul(psf(zb), lhsT=CB[:, CB_PBD + c * 128:CB_PBD + (c + 1) * 128],
                                                         rhs=PL[:, c, tg * 512:(tg + 1) * 512], start=True, stop=True),
              r=["pl%d" % c, "cb"], w=[pk(zb)])
            A(ACT, lambda e, c=c, tg=tg, zb=zb: e.activation(out=MIXT[:, 4 + c, tg * 512:(tg + 1) * 512], in_=psf(zb), func=AF.Copy,
                                                             scale=CF[:, CF_PSC + c:CF_PSC + c + 1]),
              r=[pk(zb), "cf"], w=["mixt%d" % (4 + c)])

    M.reset("r2")
    M.reset("ut")
    XT = [M.alloc("r2", "xt%d" % i, [128, 1024], F32) for i in range(2)]
    XN = [M.alloc("r2", "xn%d" % i, [128, 1024], BF16) for i in range(2)]
    SQZ = [M.alloc("r2", "sqz%d" % i, [128, 512], BF16) for i in range(2)]
    RS = [M.alloc("r2", "rs%d" % i, [128, 512], F32) for i in range(2)]
    ST4 = [M.alloc("r2", "st4_%d" % i, [128, 4], F32) for i in range(2)]
    MEMT = M.alloc("r2", "memt", [128, 8, 256], BF16, keys=["memt0", "memt1"])
    KMT = M.alloc("r2", "kmt", [128, 2, 256], BF16, keys=["kmt0", "kmt1"])
    VMA = M.alloc("r2", "vma", [128, 2, 4, 65], BF16)
    PTM = [M.alloc("r2", "ptm%d" % i, [128, 512], BF16) for i in range(2)]
    ACM = [M.alloc("r2", "acm%d" % i, [128, 512], F32) for i in range(2)]
    WKV = M.alloc("ut", "wkv", [128, 8, 512], BF16)
    TMPO = [M.alloc("ut", "tmpo%d" % i, [128, 2048], BF16) for i in range(2)]
    S["XT"], S["XN"], S["ST4"], S["SQZ"], S["RS"] = XT, XN, ST4, SQZ, RS
    A(POOL, lambda e: e.dma_start(out=WKV, in_=D["w_kv"].rearrange("(k p) c -> p k c", p=128)), w=["wkv"], dma=True)
    A(DVE, lambda e: e.memset(VMA[:, :, :, 64:65], 1.0), w=["vma"])

    def norm_tile(src_ap, slot, gain_col, dst, dkeys, ti):
        xt, xn, st = XT[slot], XN[slot], ST4[slot]
        kx, kn, ks = "xt%d" % slot, "xn%d" % slot, "st4_%d" % slot
        tp = ti % 2
        A(SP, lambda e: e.dma_start(out=xt, in_=src_ap), w=[kx], dma=True)
        A(ACT, lambda e: e.activation(out=xn, in_=xt, func=AF.Square, accum_out=st[:, 0:1]), r=[kx], w=[kn, ks])
        A(ACT, lambda e: e.activation(out=st[:, 1:2], in_=st[:, 0:1], func=AF.Sqrt, bias=EPSC, scale=1.0 / 1024), r=[ks, "cf"], w=[ks])
        A(DVE, lambda e: e.reciprocal(out=st[:, 2:3], in_=st[:, 1:2]), r=[ks], w=[ks])
        A(ACT, lambda e: e.activation(out=xn, in_=xt, func=AF.Copy, scale=st[:, 2:3]), r=[kx, ks], w=[kn])

        def tr(e):
            for k in range(8):
                ins = e.transpose(out=psb(tp)[:, k * 128:(k + 1) * 128], in_=xn[:, k * 128:(k + 1) * 128], identity=IDB)
            return ins
        A(PE, tr, r=[kn, "cb"], w=[pk(tp)])
        g = CF[:, gain_col:gain_col + 8].unsqueeze(2).broadcast_to([128, 8, 128])
        A(DVE, lambda e: e.tensor_tensor(out=dst, in0=psb(tp).rearrange("p (k t) -> p k t", k=8), in1=g, op=ALU.mult),
          r=[pk(tp), "cf"], w=dkeys)

    def head_norm(zbank, zkey, n, gain_col, dst, dkeys, i):
        sq, rs = SQZ[i % 2], RS[i % 2]
        ksq, krs = "sqz%d" % (i % 2), "rs%d" % (i % 2)
        mb = 4 + i % 2
        BD64 = CB[:, CB_BD:CB_BD + 128]
        A(ACT, lambda e: e.activation(out=sq[:, 0:n], in_=psf(zbank)[:, 0:n], func=AF.Square), r=[zkey], w=[ksq])
        A(PE, lambda e: e.matmul(psf(mb)[:, 0:n], lhsT=BD64, rhs=sq[:, 0:n], start=True, stop=True), r=[ksq, "cb"], w=[pk(mb)])
        A(ACT, lambda e: e.activation(out=rs[:, 0:n], in_=psf(mb)[:, 0:n], func=AF.Sqrt, bias=EPSC, scale=1.0), r=[pk(mb), "cf"], w=[krs])
        A(DVE, lambda e: e.reciprocal(out=rs[:, 0:n], in_=rs[:, 0:n]), r=[krs], w=[krs])
        A(DVE, lambda e: e.scalar_tensor_tensor(out=dst, in0=psf(zbank)[:, 0:n], scalar=CF[:, gain_col:gain_col + 1], in1=rs[:, 0:n],
                                                 op0=ALU.mult, op1=ALU.mult), r=[zkey, krs, "cf"], w=dkeys)

    for mt in range(2):
        norm_tile(D["mem"][mt * 128:(mt + 1) * 128, :], mt, CF_GMEM, MEMT[:, :, mt * 128:(mt + 1) * 128], ["memt%d" % mt], mt)
    mkeys = ["memt0", "memt1"]
    for c in range(2):
        zb = 2 + c

        def kproj(e, c=c, zb=zb):
            for k in range(8):
                ins = e.matmul(psf(zb)[:, 0:256], lhsT=WKV[:, k, c * 128:(c + 1) * 128], rhs=MEMT[:, k, :], start=(k == 0), stop=(k == 7))
            return ins
        A(PE, kproj, r=["wkv"] + mkeys, w=[pk(zb)])
        head_norm(zb, pk(zb), 256, CF_GMK, KMT[:, c, :], ["kmt%d" % c], c)
    for mt in range(2):
        zb = 2 + mt

        def vproj(e, mt=mt, zb=zb):
            for k in range(8):
                ins = e.matmul(psf(zb)[:, 0:256], lhsT=MEMT[:, k, mt * 128:(mt + 1) * 128], rhs=WKV[:, k, 256:512], start=(k == 0), stop=(k == 7))
            return ins
        A(PE, vproj, r=["wkv", "memt%d" % mt], w=[pk(zb)])
        A(ACT, lambda e, mt=mt, zb=zb: e.activation(out=VMA[:, mt, :, 0:64], in_=psf(zb)[:, 0:256].rearrange("p (h d) -> p h d", h=4), func=AF.Copy),
          r=[pk(zb)], w=["vma"])

    def normalize_out(acc_ap_fn, acc_keys, tmpo, tkey, bcb):
        for tg in range(4):
            a = acc_ap_fn(tg)
            A(DVE, lambda e, a=a: e.reciprocal(out=a[64:65, :], in_=a[64:65, :]), r=acc_keys, w=acc_keys)
            A(PE, lambda e, a=a: e.matmul(psf(bcb)[0:64, :], lhsT=ONES64[64:65, :], rhs=a[64:65, :], start=True, stop=True), r=acc_keys + ["cf"], w=[pk(bcb)])
            A(DVE, lambda e, a=a, tg=tg: e.tensor_tensor(out=tmpo[0:64, tg * 512:(tg + 1) * 512], in0=a[0:64, :], in1=psf(bcb)[0:64, :], op=ALU.mult),
              r=acc_keys + [pk(bcb)], w=[tkey])

    si = 0
    for mh in range(4):
        c, b0 = mh // 2, 64 * (mh % 2)
        tmpo, tkey = TMPO[mh % 2], "tmpo%d" % (mh % 2)
        for tg in range(4):
            ob = tg % 2
            for mt in range(2):
                sb = 6 + si % 2
                ptm, pkey = PTM[si % 2], "ptm%d" % (si % 2)
                si += 1
                A(PE, lambda e, c=c, b0=b0, mt=mt, tg=tg, sb=sb: e.matmul(
                    psf(sb), lhsT=KMT[b0:b0 + 64, c, mt * 128:(mt + 1) * 128], rhs=QMT[b0:b0 + 64, c, tg * 512:(tg + 1) * 512], start=True, stop=True),
                  r=["kmt%d" % c, "qmt%d" % c], w=[pk(sb)])
                A(ACT, lambda e, sb=sb, ptm=ptm: e.activation(out=ptm, in_=psf(sb), func=AF.Exp, scale=0.125), r=[pk(sb)], w=[pkey])
                A(PE, lambda e, mt=mt, mh=mh, ob=ob, ptm=ptm: e.matmul(psf(ob)[0:65, :], lhsT=VMA[:, mt, mh, :], rhs=ptm, start=(mt == 0), stop=(mt == 1)),
                  r=["vma", pkey], w=[pk(ob)])
            acm, akey = ACM[tg % 2], "acm%d" % (tg % 2)
            A(ACT, lambda e, ob=ob, acm=acm: e.activation(out=acm[0:65, :], in_=psf(ob)[0:65, :], func=AF.Copy), r=[pk(ob)], w=[akey])
            a = acm
            bcb = 4 + tg % 2
            A(DVE, lambda e, a=a: e.reciprocal(out=a[64:65, :], in_=a[64:65, :]), r=[akey], w=[akey])
            A(PE, lambda e, a=a, bcb=bcb: e.matmul(psf(bcb)[0:64, :], lhsT=ONES64[64:65, :], rhs=a[64:65, :], start=True, stop=True), r=[akey, "cf"], w=[pk(bcb)])
            A(DVE, lambda e, a=a, tg=tg, bcb=bcb, tmpo=tmpo: e.tensor_tensor(out=tmpo[0:64, tg * 512:(tg + 1) * 512], in0=a[0:64, :], in1=psf(bcb)[0:64, :], op=ALU.mult),
              r=[akey, pk(bcb)], w=[tkey])
        A(SP, lambda e, b0=b0, c=c, tmpo=tmpo: e.dma_start(out=MIXT[b0:b0 + 64, 6 + c, :], in_=tmpo[0:64, :]), r=[tkey], w=["mixt%d" % (6 + c)], dma=True)

    if "mix" in dumps and stage == 2:
        add_dump("mixt", MIXT, ["mixt%d" % i for i in range(8)], [128, 8, 2048], BF16)
    if stage <= 2:
        P.emit(nc)
        return nc, dump_out
    return build_attn(nc, P, M, D, A, CF, CB, PS, psf, psb, pk, out, xg, yg, xmid, stage, dumps, dump_out, add_dump, S, MIXT, WOUT)


def build_attn(nc, P, M, D, A, CF, CB, PS, psf, psb, pk, out, xg, yg, xmid, stage, dumps, dump_out, add_dump, S, MIXT, WOUT):
    QT, KT, VT = S["QT"], S["KT"], S["VT"]
    vt_keys, kt_keys, qt_keys = S["vt_keys"], S["kt_keys"], S["qt_keys"]
    IDB = CB[:, CB_ID:CB_ID + 128]
    MASK4 = CB[:, CB_MASK:CB_MASK + 512]
    ONES64 = CF[:, CF_ONES:CF_ONES + 64]
    HV = CF[:, CF_HV:CF_HV + 1]
    M.reset("r2"); M.reset("ut"); M.reset("qmt")
    import os as _os
    DILS = tuple(int(v) for v in _os.environ.get('ATTN_DILS', '1,4,16').split(','))
    NHP = int(_os.environ.get('ATTN_HP', '4'))
    LVL = int(_os.environ.get('ATTN_LEVEL', '9'))
    VA = [M.alloc("r2" if p < 3 else "ut", "va%d" % p, [128, 32, 2, 65], BF16) for p in range(3)]
    PT = [M.alloc("r2", "pt%d" % i, [128, 512], BF16) for i in range(4)]
    ACC = M.alloc("ut", "acc", [128, 2, 2048], F32, keys=["acc0", "acc1"])
    TMPO = [M.alloc("qmt", "tmpo%d" % i, [128, 2048], BF16) for i in range(2)]
    for p, d in enumerate(DILS):
        nb = 32 // d
        A(DVE, lambda e, p=p: e.memset(VA[p][:, :, :, 64:65], 1.0), w=["va%d" % p])
        v = VA[p][:, :, :, 64:65].rearrange("p (r b) h o -> p r b (h o)", r=d)[:, :, 0:nb // 2, :]
        A(DVE, lambda e, v=v: e.tensor_scalar(out=v, in0=v, scalar1=HV, scalar2=None, op0=ALU.mult), r=["cf"], w=["va%d" % p])

    sti = 0
    pti = 0
    mi = 0
    tmi = 0
    for hp in range(NHP):
        c = hp
        for p, d in enumerate(DILS):
            nb = 32 // d
            va, vak = VA[p], "va%d" % p
            for grp in range(4 if LVL >= 1 else 0):
                def vtr(e, grp=grp, d=d, nb=nb, c=c):
                    for j in range(8):
                        kb = grp * 8 + j
                        r, b = kb // nb, kb % nb
                        s0 = d * 128 * b + r
                        ins = e.transpose(out=psb(6)[:, j * 128:(j + 1) * 128], in_=VT[:, c, s0:s0 + 127 * d + 1:d], identity=IDB)
                    return ins
                A(PE, vtr, r=vt_keys(c) + ["cb"], w=[pk(6)])
                eng = ACT if grp % 2 == 0 else DVE
                if eng == ACT:
                    A(ACT, lambda e, grp=grp, va=va: e.activation(out=va[:, grp * 8:grp * 8 + 8, :, 0:64],
                                                                 in_=psb(6).rearrange("p (k h d) -> p k h d", k=8, h=2), func=AF.Copy),
                      r=[pk(6)], w=[vak])
                else:
                    A(DVE, lambda e, grp=grp, va=va: e.tensor_copy(out=va[:, grp * 8:grp * 8 + 8, :, 0:64],
                                                                  in_=psb(6).rearrange("p (k h d) -> p k h d", k=8, h=2)),
                      r=[pk(6)], w=[vak])
            for qg in range(4 if LVL >= 2 else 0):
                for jp in range(2):
                    sset = sti % 2
                    sti += 1
                    blk = []
                    for jj in range(2):
                        j = 2 * jp + jj
                        if d == 1:
                            r, b = 0, 16 + 4 * qg + j
                        elif d == 4:
                            r, b = j, 4 + qg
                        else:
                            r, b = 4 * qg + j, 1
                        kbc = r * nb + b
                        kc0 = d * 128 * b + r
                        blk.append((j, kbc, kbc - 1, kc0, d * 128 * (b - 1) + r, kc0 - 2048))
                    pts = [(PT[2 * sset + hh], "pt%d" % (2 * sset + hh)) for hh in range(2)]

                    def qk(e, sset=sset, c=c, d=d, blk=blk):
                        for jj, (j, kbc, kbp, kc0, kp0, q0) in enumerate(blk):
                            for hh in range(2):
                                for kv, k0 in enumerate((kp0, kc0)):
                                    ins = e.matmul(psf(2 * sset + hh)[:, (jj * 2 + kv) * 128:(jj * 2 + kv + 1) * 128],
                                                   lhsT=KT[hh * 64:hh * 64 + 64, c, k0:k0 + 127 * d + 1:d],
                                                   rhs=QT[hh * 64:hh * 64 + 64, c, q0:q0 + 127 * d + 1:d], start=True, stop=True)
                        return ins
                    A(PE, qk, r=kt_keys(c) + qt_keys(c), w=[pk(2 * sset), pk(2 * sset + 1)])
                    for hh in range(2):
                        pt, ptk = pts[hh]
                        A(ACT, lambda e, sb=2 * sset + hh, pt=pt: e.activation(out=pt, in_=psf(sb), func=AF.Exp, scale=0.125), r=[pk(2 * sset + hh)], w=[ptk])
                        meng = DVE if mi % 2 == 0 else POOL
                        mi += 1
                        A(meng, lambda e, pt=pt: e.tensor_tensor(out=pt, in0=pt, in1=MASK4, op=ALU.mult), r=[ptk, "cb"], w=[ptk])

                    def pv(e, pts=pts, va=va, blk=blk):
                        for jj, (j, kbc, kbp, kc0, kp0, q0) in enumerate(blk):
                            for hh in range(2):
                                pt = pts[hh][0]
                                e.matmul(psf(4 + hh)[0:65, j * 128:(j + 1) * 128], lhsT=va[:, kbp, hh, :], rhs=pt[:, (jj * 2) * 128:(jj * 2 + 1) * 128],
                                         start=True, stop=False)
                                ins = e.matmul(psf(4 + hh)[0:65, j * 128:(j + 1) * 128], lhsT=va[:, kbc, hh, :], rhs=pt[:, (jj * 2 + 1) * 128:(jj * 2 + 2) * 128],
                                               start=False, stop=True)
                        return ins
                    if LVL >= 3:
                        A(PE, pv, r=[vak, pts[0][1], pts[1][1]], w=[pk(4), pk(5)])
                for hh in range(2 if LVL >= 4 else 0):
                    ob = 4 + hh
                    ak = "acc%d" % hh
                    if d == 1:
                        A(ACT, lambda e, ob=ob, hh=hh, qg=qg: e.activation(out=ACC[0:65, hh, qg * 512:(qg + 1) * 512], in_=psf(ob)[0:65, :], func=AF.Copy),
                          r=[pk(ob)], w=[ak])
                    else:
                        if d == 4:
                            dst = ACC[0:65, hh, qg * 512:(qg + 1) * 512].rearrange("p (i r) -> p r i", r=4)
                        else:
                            dst = ACC[0:65, hh, :].rearrange("p (i r) -> p r i", r=16)[:, 4 * qg:4 * qg + 4, :]
                        src = psf(ob)[0:65, :].rearrange("p (r i) -> p r i", r=4)
                        A(DVE, lambda e, dst=dst, src=src: e.tensor_tensor(out=dst, in0=src, in1=dst, op=ALU.add), r=[pk(ob), ak], w=[ak])
        for hh in range(2 if LVL >= 5 else 0):
            ak = "acc%d" % hh
            tmpo, tkey = TMPO[tmi % 2], "tmpo%d" % (tmi % 2)
            tmi += 1
            for tg in range(4):
                a = ACC[:, hh, tg * 512:(tg + 1) * 512]
                A(DVE, lambda e, a=a: e.reciprocal(out=a[64:65, :], in_=a[64:65, :]), r=[ak], w=[ak])
                A(PE, lambda e, a=a: e.matmul(psf(7)[0:64, :], lhsT=ONES64[64:65, :], rhs=a[64:65, :], start=True, stop=True), r=[ak, "cf"], w=[pk(7)])
                A(DVE, lambda e, a=a, tg=tg, tmpo=tmpo: e.tensor_tensor(out=tmpo[0:64, tg * 512:(tg + 1) * 512], in0=a[0:64, :], in1=psf(7)[0:64, :], op=ALU.mult),
                  r=[ak, pk(7)], w=[tkey])
            A(SP, lambda e, hh=hh, hp=hp, tmpo=tmpo: e.dma_start(out=MIXT[hh * 64:hh * 64 + 64, hp, :], in_=tmpo[0:64, :]), r=[tkey], w=["mixt%d" % hp], dma=True)

    if "mix" in dumps and stage == 3:
        add_dump("mixt", MIXT, ["mixt%d" % i for i in range(8)], [128, 8, 2048], BF16)
    if stage <= 3:
        P.emit(nc)
        return nc, dump_out
    return build_ffn(nc, P, M, D, A, CF, CB, PS, psf, psb, pk, out, xg, yg, xmid, stage, dumps, dump_out, add_dump, S, MIXT, WOUT)


def build_ffn(nc, P, M, D, A, CF, CB, PS, psf, psb, pk, out, xg, yg, xmid, stage, dumps, dump_out, add_dump, S, MIXT, WOUT):
    IDB = CB[:, CB_ID:CB_ID + 128]
    ONESB = CB[:, CB_ONES:CB_ONES + 128]
    TRI = CB[:, CB_TRI:CB_TRI + 128]
    EPSC = CF[:, CF_EPS:CF_EPS + 1]
    IDF = CF[:, CF_IDF:CF_IDF + 128]
    GFFN = CF[:, CF_GFFN:CF_GFFN + 1024]
    BIASR = CF[:, CF_BIAS:CF_BIAS + 72]
    OFFS = CF[:, CF_OFFS:CF_OFFS + 64]
    M.reset("kt"); M.reset("qt")
    XT = [M.alloc("kt", "xt%d" % i, [128, 1024], F32) for i in range(2)]
    XM = [M.alloc("kt", "xm%d" % i, [128, 1024], F32) for i in range(2)]
    H2 = M.alloc("kt", "h2", [128, 1024], F32)
    H2B = [M.alloc("kt", "h2b%d" % i, [128, 1024], BF16) for i in range(2)]
    H2T = M.alloc("kt", "h2t", [128, 8, 128], F32, keys=["h2t0", "h2t1"])
    WR = M.alloc("qt", "wr", [128, 8, 72], F32)
    AB = M.alloc("qt", "ab", [128, 16, 64], BF16, keys=["ab%d" % i for i in range(16)])
    G12 = M.alloc("qt", "g12", [128, 2, 16], F32)
    RT = M.alloc("qt", "rt", [128, 1024], F32)
    DEST = [[nc.alloc_sbuf_tensor("dest%d_%d" % (a, tt), [128, 1], I32) for tt in range(16)] for a in range(2)]
    A(SP, lambda e: e.dma_start(out=WR, in_=D["w_r"].rearrange("(k p) c -> p k c", p=128)), w=["wr"], dma=True)

    def rt(o, n):
        return RT[:, o:o + n]
    L, OHG, E8, T64, LSEL, OH1, L2, OH2 = rt(0, 72), rt(72, 8), rt(80, 8), rt(88, 64), rt(152, 8), rt(160, 8), rt(168, 8), rt(176, 8)
    A1F, A2F, RB, SM = rt(184, 64), rt(248, 64), rt(312, 64), rt(376, 16)
    RK = "rt"

    for tt in range(16):
        sl = tt % 2
        xt, xm, h2b = XT[sl], XM[sl], H2B[sl]
        kx, km, kb = "xt%d" % sl, "xm%d" % sl, "h2b%d" % sl
        tok = slice(tt * 128, (tt + 1) * 128)
        A(SP, lambda e, xt=xt, tok=tok: e.dma_start(out=xt, in_=D["xo"][tok, :]), w=[kx], dma=True)
        for half in range(2):
            def oproj(e, half=half, tok=tok):
                for k in range(8):
                    ins = e.matmul(psf(half), lhsT=MIXT[:, k, tok], rhs=WOUT[:, k, half * 512:(half + 1) * 512], start=(k == 0), stop=(k == 7))
                return ins
            A(PE, oproj, r=["wout"] + ["mixt%d" % i for i in range(8)], w=[pk(half)])
            A(DVE, lambda e, half=half, xt=xt, xm=xm: e.tensor_tensor(out=xm[:, half * 512:(half + 1) * 512], in0=psf(half), in1=xt[:, half * 512:(half + 1) * 512], op=ALU.add),
              r=[pk(half), kx], w=[km])
        A(SP, lambda e, xm=xm, tok=tok: e.dma_start(out=xmid[tok, :], in_=xm), r=[km], w=["xmid%d" % tt], dma=True)
        A(ACT, lambda e, xm=xm: e.activation(out=H2, in_=xm, func=AF.Square, accum_out=SM[:, 0:1]), r=[km], w=["h2", "sm"])
        A(ACT, lambda e: e.activation(out=SM[:, 1:2], in_=SM[:, 0:1], func=AF.Sqrt, bias=EPSC, scale=1.0 / 1024), r=["sm", "cf"], w=["sm"])
        A(DVE, lambda e: e.reciprocal(out=SM[:, 2:3], in_=SM[:, 1:2]), r=["sm"], w=["sm"])
        A(DVE, lambda e, xm=xm: e.scalar_tensor_tensor(out=H2, in0=xm, scalar=SM[:, 2:3], in1=GFFN, op0=ALU.mult, op1=ALU.mult), r=[km, "sm", "cf"], w=["h2"])
        A(ACT, lambda e, h2b=h2b: e.activation(out=h2b, in_=H2, func=AF.Copy), r=["h2"], w=[kb])
        for hb in range(2):
            def tr(e, hb=hb):
                for k in range(4):
                    kk = hb * 4 + k
                    ins = e.transpose(out=psf(2 + hb)[:, k * 128:(k + 1) * 128], in_=H2[:, kk * 128:(kk + 1) * 128], identity=IDF)
                return ins
            A(PE, tr, r=["h2", "cf"], w=[pk(2 + hb)])
            if hb == 0:
                A(ACT, lambda e: e.activation(out=H2T[:, 0:4, :], in_=psf(2).rearrange("p (k t) -> p k t", k=4), func=AF.Copy), r=[pk(2)], w=["h2t0"])
            else:
                A(DVE, lambda e: e.tensor_copy(out=H2T[:, 4:8, :], in_=psf(3).rearrange("p (k t) -> p k t", k=4)), r=[pk(3)], w=["h2t1"])

        def rmm(e):
            for k in range(8):
                ins = e.matmul(psf(6)[:, 0:72], lhsT=H2T[:, k, :], rhs=WR[:, k, :], start=(k == 0), stop=(k == 7))
            return ins
        A(PE, rmm, r=["h2t0", "h2t1", "wr"], w=[pk(6)])
        V = lambda fn, r=(), w=(): A(DVE, fn, r=list(r) + [RK], w=list(w) + [RK])
        V(lambda e: e.tensor_tensor(out=L, in0=psf(6)[:, 0:72], in1=BIASR, op=ALU.add), r=[pk(6), "cf"])
        V(lambda e: e.tensor_reduce(out=SM[:, 4:5], in_=L[:, 0:8], axis=AX.X, op=ALU.max), r=["sm"], w=["sm"])
        V(lambda e: e.tensor_scalar(out=OHG, in0=L[:, 0:8], scalar1=SM[:, 4:5], scalar2=None, op0=ALU.is_equal), r=["sm"])
        V(lambda e: e.tensor_scalar(out=SM[:, 5:6], in0=SM[:, 4:5], scalar1=-1.0, scalar2=None, op0=ALU.mult), r=["sm"], w=["sm"])
        A(ACT, lambda e: e.activation(out=E8, in_=L[:, 0:8], func=AF.Exp, bias=SM[:, 5:6], scale=1.0, accum_out=SM[:, 6:7]), r=[RK, "sm"], w=[RK, "sm"])
        V(lambda e: e.reciprocal(out=SM[:, 7:8], in_=SM[:, 6:7]), r=["sm"], w=["sm"])
        V(lambda e: e.tensor_tensor(out=T64.rearrange("p (g x) -> p g x", g=8), in0=L[:, 8:72].rearrange("p (g x) -> p g x", g=8),
                                    in1=OHG.unsqueeze(2).broadcast_to([128, 8, 8]), op=ALU.mult))
        V(lambda e: e.tensor_reduce(out=LSEL, in_=T64.rearrange("p (g x) -> p x g", g=8), axis=AX.X, op=ALU.add))
        V(lambda e: e.tensor_reduce(out=SM[:, 8:9], in_=LSEL, axis=AX.X, op=ALU.max), r=["sm"], w=["sm"])
        V(lambda e: e.tensor_scalar(out=OH1, in0=LSEL, scalar1=SM[:, 8:9], scalar2=None, op0=ALU.is_equal), r=["sm"])
        V(lambda e: e.scalar_tensor_tensor(out=L2, in0=OH1, scalar=-1e30, in1=LSEL, op0=ALU.mult, op1=ALU.add))
        V(lambda e: e.tensor_reduce(out=SM[:, 9:10], in_=L2, axis=AX.X, op=ALU.max), r=["sm"], w=["sm"])
        V(lambda e: e.tensor_scalar(out=OH2, in0=L2, scalar1=SM[:, 9:10], scalar2=None, op0=ALU.is_equal), r=["sm"])
        V(lambda e: e.tensor_tensor(out=SM[:, 10:11], in0=SM[:, 9:10], in1=SM[:, 8:9], op=ALU.subtract), r=["sm"], w=["sm"])
        A(ACT, lambda e: e.activation(out=SM[:, 11:12], in_=SM[:, 10:11], func=AF.Exp), r=["sm"], w=["sm"])
        V(lambda e: e.tensor_scalar(out=SM[:, 12:13], in0=SM[:, 11:12], scalar1=1.0, scalar2=None, op0=ALU.add), r=["sm"], w=["sm"])
        V(lambda e: e.reciprocal(out=SM[:, 13:14], in_=SM[:, 12:13]), r=["sm"], w=["sm"])
        V(lambda e, tt=tt: e.tensor_tensor(out=G12[:, 0, tt:tt + 1], in0=SM[:, 7:8], in1=SM[:, 13:14], op=ALU.mult), r=["sm"], w=["g12"])
        V(lambda e, tt=tt: e.tensor_tensor(out=G12[:, 1, tt:tt + 1], in0=SM[:, 7:8], in1=G12[:, 0, tt:tt + 1], op=ALU.subtract), r=["sm", "g12"], w=["g12"])
        V(lambda e: e.tensor_tensor(out=A1F.rearrange("p (g x) -> p g x", g=8), in0=OHG.unsqueeze(2).broadcast_to([128, 8, 8]),
                                    in1=OH1.unsqueeze(1).broadcast_to([128, 8, 8]), op=ALU.mult))
        V(lambda e: e.tensor_tensor(out=A2F.rearrange("p (g x) -> p g x", g=8), in0=OHG.unsqueeze(2).broadcast_to([128, 8, 8]),
                                    in1=OH2.unsqueeze(1).broadcast_to([128, 8, 8]), op=ALU.mult))
        V(lambda e, tt=tt: e.tensor_tensor(out=AB[:, tt, :], in0=A1F, in1=A2F, op=ALU.add), w=["ab%d" % tt])

        def rank(e, tt=tt):
            for t2 in range(tt):
                e.matmul(psf(7)[:, 0:64], lhsT=ONESB, rhs=AB[:, t2, :], start=(t2 == 0), stop=False)
            return e.matmul(psf(7)[:, 0:64], lhsT=TRI, rhs=AB[:, tt, :], start=(tt == 0), stop=True)
        A(PE, rank, r=["ab%d" % i for i in range(tt + 1)] + ["cb"], w=[pk(7)])
        V(lambda e: e.scalar_tensor_tensor(out=RB, in0=psf(7)[:, 0:64], scalar=float(EXPERT_CAP - 1), in1=OFFS, op0=ALU.min, op1=ALU.add), r=[pk(7), "cf"])
        for a, AF_ in enumerate((A1F, A2F)):
            V(lambda e, AF_=AF_: e.tensor_tensor(out=T64, in0=AF_, in1=RB, op=ALU.mult))
            V(lambda e: e.tensor_reduce(out=SM[:, 14:15], in_=T64, axis=AX.X, op=ALU.add), r=["sm"], w=["sm"])
            dk = "dest%d_%d" % (a, tt)
            V(lambda e, a=a, tt=tt: e.tensor_copy(out=DEST[a][tt][:, :], in_=SM[:, 14:15]), r=["sm"], w=[dk])
            A(POOL, lambda e, a=a, tt=tt, h2b=h2b: e.indirect_dma_start(
                out=xg, out_offset=bass.IndirectOffsetOnAxis(ap=DEST[a][tt][:, 0:1], axis=0), in_=h2b, in_offset=None,
                bounds_check=None, oob_is_err=False), r=[dk, kb, "xg"], w=["xgs%d_%d" % (a, tt)], dma=True)

    if "route" in dumps:
        add_dump("g12", G12, ["g12"], [128, 2, 16], F32)
        for a in range(2):
            for tt in (0, 15):
                add_dump("dest%d_%d" % (a, tt), DEST[a][tt][:, :], ["dest%d_%d" % (a, tt)], [128, 1], I32)
        add_dump("xm", XM[1], ["xm1"], [128, 1024], F32)
    if stage <= 4:
        P.emit(nc)
        return nc, dump_out

    M.reset("vt"); M.reset("r2"); M.reset("ut"); M.reset("qmt"); M.reset("r1")
    NW = 3
    W1 = [M.alloc("vt" if i == 0 else "r2", "w1_%d" % i, [128, 8, 512], BF16) for i in range(NW)]
    W3 = [M.alloc("vt" if i == 0 else "r2", "w3_%d" % i, [128, 8, 512], BF16) for i in range(NW)]
    W2 = [M.alloc("vt" if i == 0 else "ut", "w2_%d" % i, [128, 4, 1024], BF16) for i in range(NW)]
    XE = [M.alloc("vt", "xe%d" % i, [128, 1024], BF16) for i in range(2)]
    XET = [M.alloc("vt", "xet%d" % i, [128, 8, 128], BF16) for i in range(2)]
    SA = M.alloc("qmt", "sa", [128, 512], F32)
    HM = M.alloc("qmt", "hm", [128, 512], BF16)
    HMT = M.alloc("qmt", "hmt", [128, 4, 128], BF16)
    YE = [M.alloc("r1", "ye%d" % i, [128, 1024], F32) for i in range(2)]

    def wload(e_):
        i = e_ % NW
        A(POOL, lambda e, i=i, e_=e_: e.dma_start(out=W1[i], in_=D["w1"][e_].rearrange("(k p) f -> p k f", p=128)), w=["w1_%d" % i], dma=True)
        A(POOL, lambda e, i=i, e_=e_: e.dma_start(out=W3[i], in_=D["w3"][e_].rearrange("(k p) f -> p k f", p=128)), w=["w3_%d" % i], dma=True)
        A(POOL, lambda e, i=i, e_=e_: e.dma_start(out=W2[i], in_=D["w2"][e_].rearrange("(k p) f -> p k f", p=128)), w=["w2_%d" % i], dma=True)

    NEXP = 64
    for e_ in range(min(NW - 1, NEXP)):
        wload(e_)
    for e_ in range(NEXP):
        if e_ + NW - 1 < NEXP:
            wload(e_ + NW - 1)
        i = e_ % NW
        s2 = e_ % 2
        xe, xet, ye = XE[s2], XET[s2], YE[s2]
        kxe, kxt, kye = "xe%d" % s2, "xet%d" % s2, "ye%d" % s2
        tb = 0 if s2 == 0 else 6
        ab = 1 if s2 == 0 else 7
        A(SP, lambda e, xe=xe, e_=e_: e.dma_start(out=xe, in_=xg[e_ * 128:(e_ + 1) * 128, :]), r=["xg"] + ["xgs%d_%d" % (a, t) for a in range(2) for t in range(16)], w=[kxe], dma=True)

        def trx(e, xe=xe, tb=tb):
            for k in range(8):
                ins = e.transpose(out=psb(tb)[:, k * 128:(k + 1) * 128], in_=xe[:, k * 128:(k + 1) * 128], identity=IDB)
            return ins
        A(PE, trx, r=[kxe, "cb"], w=[pk(tb)])
        A(ACT, lambda e, xet=xet, tb=tb: e.activation(out=xet, in_=psb(tb).rearrange("p (k t) -> p k t", k=8), func=AF.Copy), r=[pk(tb)], w=[kxt])

        def up(e, W, bank, xet=xet):
            for k in range(8):
                ins = e.matmul(psf(bank), lhsT=xet[:, k, :], rhs=W[:, k, :], start=(k == 0), stop=(k == 7))
            return ins
        A(PE, lambda e, i=i, ab=ab, up=up: up(e, W1[i], ab), r=[kxt, "w1_%d" % i], w=[pk(ab)])
        A(PE, lambda e, i=i, up=up: up(e, W3[i], 2), r=[kxt, "w3_%d" % i], w=[pk(2)])
        A(ACT, lambda e, ab=ab: e.activation(out=SA, in_=psf(ab), func=AF.Silu), r=[pk(ab)], w=["sa"])
        A(DVE, lambda e: e.tensor_tensor(out=HM, in0=SA, in1=psf(2), op=ALU.mult), r=["sa", pk(2)], w=["hm"])

        def trh(e):
            for k in range(4):
                ins = e.transpose(out=psb(3)[:, k * 128:(k + 1) * 128], in_=HM[:, k * 128:(k + 1) * 128], identity=IDB)
            return ins
        A(PE, trh, r=["hm", "cb"], w=[pk(3)])
        A(DVE, lambda e: e.tensor_copy(out=HMT, in_=psb(3)[:, 0:512].rearrange("p (k t) -> p k t", k=4)), r=[pk(3)], w=["hmt"])
        for half in range(2):
            def dn(e, i=i, half=half):
                for k in range(4):
                    ins = e.matmul(psf(4 + half), lhsT=HMT[:, k, :], rhs=W2[i][:, k, half * 512:(half + 1) * 512], start=(k == 0), stop=(k == 3))
                return ins
            A(PE, dn, r=["hmt", "w2_%d" % i], w=[pk(4 + half)])
        A(ACT, lambda e, ye=ye: e.activation(out=ye[:, 0:512], in_=psf(4), func=AF.Copy), r=[pk(4)], w=[kye + "a"])
        A(DVE, lambda e, ye=ye: e.tensor_copy(out=ye[:, 512:1024], in_=psf(5)), r=[pk(5)], w=[kye + "b"])
        A(SP, lambda e, ye=ye, e_=e_: e.dma_start(out=yg[e_ * 128:(e_ + 1) * 128, :], in_=ye), r=[kye + "a", kye + "b"], w=["yg%d" % e_], dma=True)

    if stage <= 5:
        P.emit(nc)
        return nc, dump_out

    M.reset("kt")
    Y1 = [M.alloc("kt", "y1_%d" % i, [128, 1024], F32) for i in range(2)]
    Y2 = [M.alloc("kt", "y2_%d" % i, [128, 1024], F32) for i in range(2)]
    XM2 = [M.alloc("kt", "xm2_%d" % i, [128, 1024], F32) for i in range(2)]
    OT_ = [M.alloc("kt", "ot_%d" % i, [128, 1024], F32) for i in range(2)]
    for tt in range(16):
        sl = tt % 2
        tok = slice(tt * 128, (tt + 1) * 128)
        for a, Y in enumerate((Y1, Y2)):
            A(POOL, lambda e, a=a, tt=tt, y=Y[sl]: e.indirect_dma_start(
                out=y, out_offset=None, in_=yg, in_offset=bass.IndirectOffsetOnAxis(ap=DEST[a][tt][:, 0:1], axis=0),
                bounds_check=None, oob_is_err=False), r=["dest%d_%d" % (a, tt)] + ["yg%d" % i for i in range(64)], w=["y%d_%d" % (a + 1, sl)], dma=True)
        A(SP, lambda e, sl=sl, tok=tok: e.dma_start(out=XM2[sl], in_=xmid[tok, :]), r=["xmid%d" % tt], w=["xm2_%d" % sl], dma=True)
        A(DVE, lambda e, sl=sl, tt=tt: e.scalar_tensor_tensor(out=OT_[sl], in0=Y1[sl], scalar=G12[:, 0, tt:tt + 1], in1=XM2[sl], op0=ALU.mult, op1=ALU.add),
          r=["y1_%d" % sl, "xm2_%d" % sl, "g12"], w=["ot_%d" % sl])
        A(DVE, lambda e, sl=sl, tt=tt: e.scalar_tensor_tensor(out=OT_[sl], in0=Y2[sl], scalar=G12[:, 1, tt:tt + 1], in1=OT_[sl], op0=ALU.mult, op1=ALU.add),
          r=["y2_%d" % sl, "ot_%d" % sl, "g12"], w=["ot_%d" % sl])
        A(SP, lambda e, sl=sl, tok=tok: e.dma_start(out=out[tok, :], in_=OT_[sl]), r=["ot_%d" % sl], w=["out"], dma=True)
    if "moe" in dumps:
        allxg = ["xg"] + ["xgs%d_%d" % (a, t) for a in range(2) for t in range(16)]
        for ee in (0, 37):
            add_dump("xg%d" % ee, xg[ee * 128:(ee + 1) * 128, :], allxg, [128, 1024], BF16)
            add_dump("yg%d" % ee, yg[ee * 128:(ee + 1) * 128, :], ["yg%d" % ee], [128, 1024], F32)
        add_dump("g12", G12, ["g12"], [128, 2, 16], F32)
        for a in range(2):
            for tt in range(16):
                add_dump("dest%d_%d" % (a, tt), DEST[a][tt][:, :], ["dest%d_%d" % (a, tt)], [128, 1], I32)
        add_dump("xmid", xmid, ["xmid%d" % t for t in range(16)], [2048, 1024], F32)
    P.emit(nc)
    return nc, dump_out
```

```python
import numpy as np
import concourse.bass as bass
import concourse.mybir as mybir
from concourse.bass_utils import run_bass_kernel_spmd

F32 = mybir.dt.float32
BF16 = mybir.dt.bfloat16
I32 = mybir.dt.int32
AF = mybir.ActivationFunctionType
ALU = mybir.AluOpType
AX = mybir.AxisListType

PE, ACT, DVE, POOL, SP = "pe", "act", "dve", "pool", "sp"
ENGS = (PE, ACT, DVE, POOL, SP)
RING = 12


class Op:
    __slots__ = ("eng", "fn", "deps", "dma", "need", "seq", "di", "name")

    def __init__(self, eng, fn, dma, name):
        self.eng, self.fn, self.dma, self.name = eng, fn, dma, name
        self.deps, self.need, self.seq, self.di = [], False, 0, -1


class Prog:
    def __init__(self):
        self.ops = {e: [] for e in ENGS}
        self.lastw = {}
        self.readers = {}

    def add(self, eng, fn, r=(), w=(), dma=False, name=""):
        op = Op(eng, fn, dma, name)
        deps = {}
        for k in r:
            lw = self.lastw.get(k)
            if lw is not None:
                deps[id(lw)] = (lw, True)
        for k in w:
            lw = self.lastw.get(k)
            if lw is not None:
                deps[id(lw)] = (lw, True)
            for rd in self.readers.get(k, ()):
                if id(rd) not in deps:
                    deps[id(rd)] = (rd, False)
        for k in r:
            self.readers.setdefault(k, []).append(op)
        for k in w:
            self.lastw[k] = op
            self.readers[k] = []
        op.deps = [d for d in deps.values() if d[0] is not op]
        self.ops[eng].append(op)
        return op

    @staticmethod
    def _needs_wait(o, d, true_dep):
        if d.dma:
            return True
        if d.eng != o.eng:
            return True
        if o.eng == PE:
            return False
        if o.dma:
            return True
        return true_dep

    def emit(self, nc):
        for e in ENGS:
            for o in self.ops[e]:
                for d, t in o.deps:
                    if self._needs_wait(o, d, t):
                        d.need = True
        for e in ENGS:
            c = 0
            n = 0
            for o in self.ops[e]:
                if o.dma:
                    o.di = n
                    n += 1
                elif o.need:
                    c += 1
                    o.seq = c
        from contextlib import ExitStack

        with ExitStack() as st:
            esem = {e: st.enter_context(nc.semaphore("c_" + e)) for e in (PE, ACT, DVE, POOL)}
            rsem = {
                e: [st.enter_context(nc.semaphore("r_%s%d" % (e, i))) for i in range(RING)]
                for e in (SP, POOL, ACT)
            }
            block = st.enter_context(nc.Block())

            def run(ename, eng):
                seen = {}

                def wait(sem, val):
                    if seen.get(sem.num, 0) < val:
                        eng.wait_ge(sem, val)
                        seen[sem.num] = val

                ndma = 0
                for o in self.ops[ename]:
                    for d, t in o.deps:
                        if not self._needs_wait(o, d, t):
                            continue
                        if d.dma:
                            wait(rsem[d.eng][d.di % RING], 16 * (d.di // RING + 1))
                        else:
                            wait(esem[d.eng], d.seq)
                    if o.dma:
                        if o.di >= RING:
                            wait(rsem[ename][o.di % RING], 16 * (o.di // RING))
                        ins = o.fn(eng)
                        ins.then_inc(rsem[ename][o.di % RING], 16)
                        ndma = o.di + 1
                    else:
                        ins = o.fn(eng)
                        if o.need:
                            ins.then_inc(esem[ename], 1)
                for i in range(max(0, ndma - RING), ndma):
                    wait(rsem[ename][i % RING], 16 * (i // RING + 1))

            block.tensor(lambda eng: run(PE, eng))
            block.scalar(lambda eng: run(ACT, eng))
            block.vector(lambda eng: run(DVE, eng))
            block.gpsimd(lambda eng: run(POOL, eng))
            block.sync(lambda eng: run(SP, eng))


class Mem:
    def __init__(self, nc, P, total_bytes):
        self.P = P
        self.t = nc.alloc_sbuf_tensor("arena", [128, total_bytes // 4], F32)
        self.regs = {}
        self.top = 0
        self.total = total_bytes

    def region(self, name, size):
        size = (size + 63) // 64 * 64
        assert self.top + size <= self.total, (name, self.top, size)
        self.regs[name] = dict(off=self.top, size=size, cur=0, keys=[], old=[])
        self.top += size

    def reset(self, name):
        r = self.regs[name]
        r["old"] = r["old"] + r["keys"]
        r["keys"] = []
        r["cur"] = 0

    def alloc(self, reg, key, shape, dt, keys=None):
        r = self.regs[reg]
        isz = 2 if dt == BF16 else 4
        n = 1
        for s in shape[1:]:
            n *= s
        nb = (n * isz + 63) // 64 * 64
        assert r["cur"] + nb <= r["size"], (reg, key, r["cur"], nb, r["size"])
        off = r["off"] + r["cur"]
        r["cur"] += nb
        keys = list(keys) if keys else [key]
        r["keys"].extend(keys)
        olds = []
        for ok in r["old"]:
            lw = self.P.lastw.get(ok)
            if lw is not None:
                olds.append(lw)
            olds.extend(self.P.readers.get(ok, ()))
        if olds:
            for kk in keys:
                self.P.readers.setdefault(kk, []).extend(olds)
        ap = self.t[:, off // 4:(off + n * isz + 3) // 4]
        if dt == BF16:
            ap = ap.bitcast(BF16)
        ap = ap[:, 0:n]
        if len(shape) == 3:
            ap = ap.rearrange("p (a b) -> p a b", a=shape[1])
        elif len(shape) == 4:
            ap = ap.rearrange("p (a b c) -> p a b c", a=shape[1], b=shape[2])
        return ap


NCF = 1416
NCB = 1280
CF_GATTN, CF_GMEM, CF_GQ, CF_GK, CF_GMQ, CF_GMK = 0, 8, 16, 17, 18, 19
CF_INVW, CF_PSC, CF_IC16, CF_HV, CF_EPS = 20, 22, 24, 56, 57
CF_ONES, CF_OFFS, CF_BIAS, CF_IDF, CF_GFFN = 64, 128, 192, 264, 392
CB_ID, CB_MASK, CB_BD, CB_ONES, CB_TRI, CB_PBD = 0, 128, 640, 768, 896, 1024
EPS = 1e-6
EXPERT_CAP = 128


def build_program(stage=99, dumps=()):
    nc = bass.Bass("TRN2", target_bir_lowering=False)
    P = Prog()
    D = {}

    def din(name, shape, dt=F32):
        D[name] = nc.dram_tensor(name, shape, dt, kind="ExternalInput").ap()

    din("xo", [2048, 1024]); din("xh", [2048, 1024]); din("mem", [256, 1024])
    din("w_in", [1024, 2048]); din("w_out", [1024, 1024]); din("w_kv", [1024, 512])
    din("w_r", [1024, 72]); din("cf", [128, NCF]); din("cb", [128, NCB])
    if stage >= 5:
        din("w1", [64, 1024, 512]); din("w3", [64, 1024, 512]); din("w2", [64, 512, 1024])
    out = nc.dram_tensor("out", [2048, 1024], F32, kind="ExternalOutput").ap()
    xg = nc.dram_tensor("xg", [64 * EXPERT_CAP, 1024], BF16, kind="Internal").ap()
    yg = nc.dram_tensor("yg", [64 * EXPERT_CAP, 1024], F32, kind="Internal").ap()
    xmid = nc.dram_tensor("xmid", [2048, 1024], F32, kind="Internal").ap()
    dump_out = {}

    M = Mem(nc, P, 203 * 1024)
    M.region("const", 12 * 1024)
    M.region("wout", 16 * 1024)
    M.region("r1", 32 * 1024)
    M.region("qt", 16 * 1024)
    M.region("kt", 32 * 1024)
    M.region("vt", 32 * 1024)
    M.region("r2", 36 * 1024)
    M.region("ut", 16640)
    M.region("qmt", 8 * 1024)
    PS = [nc.alloc_psum_tensor("ps%d" % i, [128, 512], F32) for i in range(8)]

    def psf(i):
        return PS[i][:, :]

    def psb(i):
        return PS[i][:, :].bitcast(BF16)

    def pk(i):
        return "ps%d" % i

    A = P.add
    CF = M.alloc("const", "cf", [128, NCF], F32)
    CB = M.alloc("const", "cb", [128, NCB], BF16)
    ZT = M.alloc("const", "zt", [128, 1024], BF16)
    IDB = CB[:, CB_ID:CB_ID + 128]
    MASK4 = CB[:, CB_MASK:CB_MASK + 512]
    BD64 = CB[:, CB_BD:CB_BD + 128]
    ONESB = CB[:, CB_ONES:CB_ONES + 128]
    TRI = CB[:, CB_TRI:CB_TRI + 128]
    EPSC = CF[:, CF_EPS:CF_EPS + 1]
    IDF = CF[:, CF_IDF:CF_IDF + 128]
    ONES64 = CF[:, CF_ONES:CF_ONES + 64]

    A(SP, lambda e: e.dma_start(out=CF, in_=D["cf"]), w=["cf"], dma=True)
    A(POOL, lambda e: e.dma_start(out=CB, in_=D["cb"]), w=["cb"], dma=True)
    WIN = M.alloc("r1", "win", [128, 8, 2048], BF16, keys=["win%d" % i for i in range(4)])
    win_keys = []
    for i in range(4):
        key = "win%d" % i
        win_keys.append(key)
        A(POOL, lambda e, i=i: e.dma_start(
            out=WIN[:, 2 * i:2 * i + 2, :],
            in_=D["w_in"][256 * i:256 * i + 256, :].rearrange("(k p) c -> p k c", p=128)),
          w=[key], dma=True)
    A(DVE, lambda e: e.memset(ZT, 0.0), w=["zt"])
    ZT4 = ZT.unsqueeze(1).broadcast_to([128, 4, 1024])

    QT = M.alloc("qt", "qt", [128, 4, 2048], BF16, keys=["qt%d_%d" % (c, g) for c in range(4) for g in range(4)])
    KT = M.alloc("kt", "kt", [128, 4, 4096], BF16, keys=["kt%d_%d" % (c, g) for c in range(4) for g in range(8)])
    VT = M.alloc("vt", "vt", [128, 4, 4096], BF16, keys=["vt%d_%d" % (c, g) for c in range(4) for g in range(8)])
    UT = M.alloc("ut", "ut", [128, 2, 2064], F32, keys=["ut0", "ut1"])
    QMT = M.alloc("qmt", "qmt", [128, 2, 2048], BF16, keys=["qmt0", "qmt1"])
    XT = [M.alloc("r2", "xt%d" % i, [128, 1024], F32) for i in range(2)]
    XN = [M.alloc("r2", "xn%d" % i, [128, 1024], BF16) for i in range(2)]
    HTG = [M.alloc("r2", "htg%d" % i, [128, 8, 512], BF16, keys=["htg%d_%d" % (i, t) for t in range(4)]) for i in range(2)]
    SQZ = [M.alloc("r2", "sqz%d" % i, [128, 512], BF16) for i in range(2)]
    RS = [M.alloc("r2", "rs%d" % i, [128, 512], F32) for i in range(2)]
    ST4 = [M.alloc("r2", "st4_%d" % i, [128, 4], F32) for i in range(2)]

    def norm_tile(src_ap, slot, gain_col, dst_fn, dkeys, ti):
        xt, xn, st = XT[slot], XN[slot], ST4[slot]
        kx, kn, ks = "xt%d" % slot, "xn%d" % slot, "st4_%d" % slot
        tp = ti % 2
        A(SP, lambda e: e.dma_start(out=xt, in_=src_ap), w=[kx], dma=True)
        A(ACT, lambda e: e.activation(out=xn, in_=xt, func=AF.Square, accum_out=st[:, 0:1]), r=[kx], w=[kn, ks])
        A(ACT, lambda e: e.activation(out=st[:, 1:2], in_=st[:, 0:1], func=AF.Ln, bias=EPSC, scale=1.0 / 1024), r=[ks, "cf"], w=[ks])
        A(ACT, lambda e: e.activation(out=st[:, 2:3], in_=st[:, 1:2], func=AF.Exp, scale=-0.5), r=[ks], w=[ks])
        A(ACT, lambda e: e.activation(out=xn, in_=xt, func=AF.Copy, scale=st[:, 2:3]), r=[kx, ks], w=[kn])

        def tr(e):
            for k in range(8):
                ins = e.transpose(out=psb(tp)[:, k * 128:(k + 1) * 128], in_=xn[:, k * 128:(k + 1) * 128], identity=IDB)
            return ins
        A(PE, tr, r=[kn, "cb"], w=[pk(tp)])
        g = CF[:, gain_col:gain_col + 8].unsqueeze(2).broadcast_to([128, 8, 128])
        A(DVE, lambda e: e.tensor_tensor(out=dst_fn(), in0=psb(tp).rearrange("p (k t) -> p k t", k=8), in1=g, op=ALU.mult),
          r=[pk(tp), "cf"], w=dkeys)

    def head_norm(zbank, zkey, n, gain_col, dst, dkeys, i):
        sq, rs = SQZ[i % 2], RS[i % 2]
        ksq, krs = "sqz%d" % (i % 2), "rs%d" % (i % 2)
        mb = 4 + i % 2
        A(ACT, lambda e: e.activation(out=sq[:, 0:n], in_=psf(zbank)[:, 0:n], func=AF.Square), r=[zkey], w=[ksq])
        A(PE, lambda e: e.matmul(psf(mb)[:, 0:n], lhsT=BD64, rhs=sq[:, 0:n], start=True, stop=True), r=[ksq, "cb"], w=[pk(mb)])
        A(ACT, lambda e: e.activation(out=rs[:, 0:n], in_=psf(mb)[:, 0:n], func=AF.Ln, bias=EPSC, scale=1.0), r=[pk(mb), "cf"], w=[krs])
        A(ACT, lambda e: e.activation(out=rs[:, 0:n], in_=rs[:, 0:n], func=AF.Exp, scale=-0.5), r=[krs], w=[krs])
        A(DVE, lambda e: e.scalar_tensor_tensor(out=dst, in0=psf(zbank)[:, 0:n], scalar=CF[:, gain_col:gain_col + 1], in1=rs[:, 0:n],
                                                 op0=ALU.mult, op1=ALU.mult), r=[zkey, krs, "cf"], w=dkeys)

    cnt = dict(ti=0, zi=0, hn=0)

    def tile_task(g, t):
        hb = HTG[g % 2]
        src = (D["xh"] if g < 4 else D["xo"])[((g % 4) * 4 + t) * 128:((g % 4) * 4 + t + 1) * 128, :]
        norm_tile(src, cnt["ti"] % 2, CF_GATTN, lambda: hb[:, :, t * 128:(t + 1) * 128], ["htg%d_%d" % (g % 2, t)], cnt["ti"])
        cnt["ti"] += 1

    ZB = (2, 3, 6, 7)

    def chunk_part1(g, c):
        hb = HTG[g % 2]
        hkeys = ["htg%d_%d" % (g % 2, t) for t in range(4)]
        zb = ZB[cnt["zi"] % 4]
        cnt["zi"] += 1

        def proj(e):
            for k in range(8):
                ins = e.matmul(psf(zb), lhsT=WIN[:, k, c * 128:(c + 1) * 128], rhs=hb[:, k, :], start=(k == 0), stop=(k == 7))
            return ins
        A(PE, proj, r=win_keys + hkeys, w=[pk(zb)])
        st = dict(g=g, c=c, zb=zb)
        if c < 8 or c >= 14:
            i = cnt["hn"]
            cnt["hn"] += 1
            st["i"] = i
            sq, ksq = SQZ[i % 2], "sqz%d" % (i % 2)
            A(ACT, lambda e: e.activation(out=sq, in_=psf(zb), func=AF.Square), r=[pk(zb)], w=[ksq])
        return st

    def chunk_part2(st):
        g, c, zb = st["g"], st["c"], st["zb"]
        if 8 <= c < 12:
            A(ACT, lambda e: e.activation(out=VT[:, c - 8, g * 512:(g + 1) * 512], in_=psf(zb), func=AF.Copy),
              r=[pk(zb)], w=["vt%d_%d" % (c - 8, g)])
            return
        if c in (12, 13):
            if g == 3:
                A(DVE, lambda e: e.tensor_copy(out=UT[:, c - 12, 0:16], in_=psf(zb)[:, 496:512]), r=[pk(zb)], w=["ut%d" % (c - 12)])
            else:
                A(DVE, lambda e: e.tensor_copy(out=UT[:, c - 12, 16 + (g - 4) * 512:16 + (g - 3) * 512], in_=psf(zb)),
                  r=[pk(zb)], w=["ut%d" % (c - 12)])
            return
        if c < 4:
            gain_col, dst, dkeys = CF_GQ, QT[:, c, (g - 4) * 512:(g - 3) * 512], ["qt%d_%d" % (c, g - 4)]
        elif c < 8:
            gain_col, dst, dkeys = CF_GK, KT[:, c - 4, g * 512:(g + 1) * 512], ["kt%d_%d" % (c - 4, g)]
        else:
            gain_col, dst, dkeys = CF_GMQ, QMT[:, c - 14, (g - 4) * 512:(g - 3) * 512], ["qmt%d" % (c - 14)]
        i = st["i"]
        sq, rs = SQZ[i % 2], RS[i % 2]
        ksq, krs = "sqz%d" % (i % 2), "rs%d" % (i % 2)
        mb = 4 + i % 2
        A(PE, lambda e: e.matmul(psf(mb), lhsT=BD64, rhs=sq, start=True, stop=True), r=[ksq, "cb"], w=[pk(mb)])
        A(ACT, lambda e: e.activation(out=rs, in_=psf(mb), func=AF.Ln, bias=EPSC, scale=1.0), r=[pk(mb), "cf"], w=[krs])
        A(ACT, lambda e: e.activation(out=rs, in_=rs, func=AF.Exp, scale=-0.5), r=[krs], w=[krs])
        A(DVE, lambda e: e.scalar_tensor_tensor(out=dst, in0=psf(zb), scalar=CF[:, gain_col:gain_col + 1], in1=rs,
                                                 op0=ALU.mult, op1=ALU.mult), r=[pk(zb), krs, "cf"], w=dkeys)

    def chunks_of(g):
        k_, v_ = list(range(4, 8)), list(range(8, 12))
        ch = []
        if g >= 4:
            extra = [0, 1, 2, 3, 14, 15, 12, 13]
        elif g == 3:
            extra = [12, 13]
        else:
            extra = []
        normed = k_ + [c for c in extra if c < 4 or c >= 14]
        cheap = v_ + [c for c in extra if c in (12, 13)]
        while normed or cheap:
            if normed:
                ch.append(normed.pop(0))
            if cheap:
                ch.append(cheap.pop(0))
        return ch

    for t in range(4):
        tile_task(0, t)
    pending = None
    for g in range(8):
        ch = chunks_of(g)
        per = (len(ch) + 3) // 4
        nt = 0
        for i, c in enumerate(ch):
            st = chunk_part1(g, c)
            if pending is not None:
                chunk_part2(pending)
            pending = st
            if g + 1 < 8 and (i + 1) % per == 0 and nt < 4:
                tile_task(g + 1, nt)
                nt += 1
        while g + 1 < 8 and nt < 4:
            tile_task(g + 1, nt)
            nt += 1
    chunk_part2(pending)

    def add_dump(name, ap, keys, shape, dt):
        t = nc.dram_tensor("dump_" + name, shape, dt, kind="ExternalOutput").ap()
        dump_out[name] = t
        A(SP, lambda e: e.dma_start(out=t, in_=ap), r=keys, w=["dump_" + name], dma=True)

    vt_keys = lambda c: ["vt%d_%d" % (c, g) for g in range(8)]
    kt_keys = lambda c: ["kt%d_%d" % (c, g) for g in range(8)]
    qt_keys = lambda c: ["qt%d_%d" % (c, g) for g in range(4)]
    if "qkv" in dumps:
        add_dump("qt", QT, sum([qt_keys(c) for c in range(4)], []), [128, 4, 2048], BF16)
        add_dump("kt", KT, sum([kt_keys(c) for c in range(4)], []), [128, 4, 4096], BF16)
        add_dump("vt", VT, sum([vt_keys(c) for c in range(4)], []), [128, 4, 4096], BF16)
        add_dump("ut", UT, ["ut0", "ut1"], [128, 2, 2064], F32)
        add_dump("qmt", QMT, ["qmt0", "qmt1"], [128, 2, 2048], BF16)
    if stage <= 1:
        P.emit(nc)
        return nc, dump_out
    return build_rest(nc, P, M, D, A, CF, CB, PS, psf, psb, pk, out, xg, yg, xmid, stage, dumps, dump_out, add_dump,
                      dict(QT=QT, KT=KT, VT=VT, UT=UT, QMT=QMT, WIN=WIN, vt_keys=vt_keys, kt_keys=kt_keys, qt_keys=qt_keys,
                           norm_tile=norm_tile, head_norm=head_norm, ZT4=ZT4, XT=XT, XN=XN, ST4=ST4, SQZ=SQZ, RS=RS, win_keys=win_keys))


POOL_WINDOWS = (2, 4, 8, 16)


def prep_inputs(inp):
    f = np.float32
    x = np.ascontiguousarray(inp["x"], dtype=f)
    mem = np.ascontiguousarray(inp["mem"], dtype=f)
    cb = np.zeros((128, NCB), f)
    k = np.arange(128)[:, None]
    q = np.arange(128)[None, :]
    cb[:, CB_ID:CB_ID + 128] = np.eye(128, dtype=f)
    mprev = (k >= q).astype(f)
    mcur = (k <= q).astype(f)
    cb[:, CB_MASK:CB_MASK + 512] = np.concatenate([mprev, mcur, mprev, mcur], axis=1)
    bd = np.zeros((128, 128), f)
    bd[:64, :64] = 1.0 / 64
    bd[64:, 64:] = 1.0 / 64
    cb[:, CB_BD:CB_BD + 128] = bd
    cb[:, CB_ONES:CB_ONES + 128] = 1.0
    cb[:, CB_TRI:CB_TRI + 128] = (k < q).astype(f)
    pp = np.asarray(inp["pool_proj"], dtype=f)[0]
    for c in range(2):
        cb[0:64, CB_PBD + c * 128:CB_PBD + c * 128 + 64] = pp[2 * c]
        cb[64:128, CB_PBD + c * 128 + 64:CB_PBD + c * 128 + 128] = pp[2 * c + 1]
    w_r = np.concatenate([np.asarray(inp["w_group"], f)[0],
                          np.transpose(np.asarray(inp["w_router"], f)[0], (1, 0, 2)).reshape(1024, 64)], axis=1)
    w_r = np.ascontiguousarray(w_r)
    shared = dict(
        w_in=np.ascontiguousarray(inp["w_in"][0], dtype=f), w_out=np.ascontiguousarray(inp["w_out"][0], dtype=f),
        w_kv=np.ascontiguousarray(inp["w_mem_kv"][0], dtype=f), w_r=w_r,
        w1=np.ascontiguousarray(inp["w1"][0], dtype=f), w3=np.ascontiguousarray(inp["w3"][0], dtype=f),
        w2=np.ascontiguousarray(inp["w2"][0], dtype=f), cb=cb)
    p = np.arange(128)
    in_maps = []
    for c in range(8):
        b, half = c // 2, c % 2
        cf = np.zeros((128, NCF), f)
        cf[:, CF_GATTN:CF_GATTN + 8] = np.asarray(inp["attn_norm"], f)[0].reshape(8, 128).T
        cf[:, CF_GMEM:CF_GMEM + 8] = np.asarray(inp["mem_norm"], f)[0].reshape(8, 128).T
        cf[:, CF_GQ] = np.tile(np.asarray(inp["q_norm"], f)[0], 2)
        cf[:, CF_GK] = np.tile(np.asarray(inp["k_norm"], f)[0], 2)
        cf[:, CF_GMQ] = np.tile(np.asarray(inp["mq_norm"], f)[0], 2)
        cf[:, CF_GMK] = np.tile(np.asarray(inp["mk_norm"], f)[0], 2)
        for ch in range(2):
            w = np.array([POOL_WINDOWS[2 * ch + pi // 64] for pi in range(128)], f)
            cf[:, CF_INVW + ch] = 1.0 / w
            cf[:, CF_PSC + ch] = np.asarray(inp["pool_scale"], f)[0, ch * 128:(ch + 1) * 128]
            for t in range(16):
                pos = t + 2048 * half
                cf[:, CF_IC16 + ch * 16 + t] = 1.0 / np.minimum(pos + 1, w)
        cf[:, CF_HV] = float(half)
        cf[:, CF_EPS] = EPS
        cf[:, CF_ONES:CF_ONES + 64] = 1.0
        cf[:, CF_OFFS:CF_OFFS + 64] = (np.arange(64) * EXPERT_CAP).astype(f)[None, :]
        cf[:, CF_BIAS:CF_BIAS + 8] = np.asarray(inp["b_group"], f)[0][None, :]
        cf[:, CF_BIAS + 8:CF_BIAS + 72] = np.asarray(inp["b_router"], f)[0].reshape(64)[None, :]
        cf[:, CF_IDF:CF_IDF + 128] = np.eye(128, dtype=f)
        cf[:, CF_GFFN:CF_GFFN + 1024] = np.asarray(inp["ffn_norm"], f)[0][None, :]
        xo = x[b, half * 2048:(half + 1) * 2048]
        xh = x[b, 0:2048] if half == 1 else np.zeros((2048, 1024), f)
        m = dict(shared)
        m.update(xo=np.ascontiguousarray(xo), xh=np.ascontiguousarray(xh), mem=mem[b], cf=cf)
        in_maps.append(m)
    return in_maps


_NC_CACHE = {}


def kernel(**inputs):
    in_maps = prep_inputs(inputs)
    if "nc" not in _NC_CACHE:
        _NC_CACHE["nc"] = build_program()[0]
    res = run_bass_kernel_spmd(_NC_CACHE["nc"], in_maps, core_ids=list(range(8)))
    outs = [np.asarray(r["out"], dtype=np.float32) for r in res.results]
    return np.stack([np.concatenate([outs[2 * b], outs[2 * b + 1]], axis=0) for b in range(4)], axis=0)


def build_rest(nc, P, M, D, A, CF, CB, PS, psf, psb, pk, out, xg, yg, xmid, stage, dumps, dump_out, add_dump, S):
    QT, KT, VT, UT, QMT = S["QT"], S["KT"], S["VT"], S["UT"], S["QMT"]
    vt_keys, kt_keys, qt_keys = S["vt_keys"], S["kt_keys"], S["qt_keys"]
    IDB = CB[:, CB_ID:CB_ID + 128]
    MASK4 = CB[:, CB_MASK:CB_MASK + 512]
    ONESB = CB[:, CB_ONES:CB_ONES + 128]
    TRI = CB[:, CB_TRI:CB_TRI + 128]
    EPSC = CF[:, CF_EPS:CF_EPS + 1]
    IDF = CF[:, CF_IDF:CF_IDF + 128]
    ONES64 = CF[:, CF_ONES:CF_ONES + 64]
    HV = CF[:, CF_HV:CF_HV + 1]

    WOUT = M.alloc("wout", "wout", [128, 8, 1024], BF16)
    A(POOL, lambda e: e.dma_start(out=WOUT, in_=D["w_out"].rearrange("(k p) c -> p k c", p=128)), w=["wout"], dma=True)

    M.reset("r1")
    MIXT = M.alloc("r1", "mixt", [128, 8, 2048], BF16, keys=["mixt%d" % i for i in range(8)])
    M.reset("r2")
    TA = M.alloc("r2", "ta", [128, 2064], F32)
    TB = M.alloc("r2", "tb", [128, 2064], F32)
    PL = M.alloc("r2", "pl", [128, 2, 2048], BF16, keys=["pl0", "pl1"])
    T16 = M.alloc("r2", "t16", [128, 16], F32)
    IC = CF[:, CF_IC16:CF_IC16 + 32].rearrange("p (c t) -> p c t", c=2)
    N = 2064
    for c in range(2):
        U = UT[:, c, :]
        uk = "ut%d" % c
        A(POOL, lambda e, U=U: e.tensor_tensor(out=TA[:, 1:N], in0=U[:, 1:N], in1=U[:, 0:N - 1], op=ALU.add), r=[uk], w=["ta"])
        if c == 0:
            A(POOL, lambda e: e.tensor_tensor(out=TB[64:128, 3:N], in0=TA[64:128, 3:N], in1=TA[64:128, 1:N - 2], op=ALU.add), r=["ta"], w=["tb"])
        else:
            A(POOL, lambda e: e.tensor_tensor(out=TB[:, 3:N], in0=TA[:, 3:N], in1=TA[:, 1:N - 2], op=ALU.add), r=["ta"], w=["tb"])
            A(DVE, lambda e: e.tensor_tensor(out=TA[:, 7:N], in0=TB[:, 7:N], in1=TB[:, 3:N - 4], op=ALU.add), r=["tb"], w=["ta"])
            A(DVE, lambda e: e.tensor_tensor(out=TB[64:128, 15:N], in0=TA[64:128, 15:N], in1=TA[64:128, 7:N - 8], op=ALU.add), r=["ta"], w=["tb"])
        for (lo, hi, T, tk) in ((0, 64, TA, "ta"), (64, 128, TB, "tb")):
            A(DVE, lambda e, lo=lo, hi=hi, T=T, U=U, c=c: e.scalar_tensor_tensor(
                out=PL[lo:hi, c, :], in0=T[lo:hi, 16:N], scalar=CF[lo:hi, CF_INVW + c:CF_INVW + c + 1], in1=U[lo:hi, 16:N],
                op0=ALU.mult, op1=ALU.subtract), r=[tk, uk, "cf"], w=["pl%d" % c])
            A(DVE, lambda e, lo=lo, hi=hi, T=T, c=c: e.tensor_tensor(out=T16[lo:hi, :], in0=T[lo:hi, 16:32], in1=IC[lo:hi, c, :], op=ALU.mult),
              r=[tk, "cf"], w=["t16"])
        A(DVE, lambda e, U=U, c=c: e.tensor_tensor(out=PL[:, c, 0:16], in0=T16, in1=U[:, 16:32], op=ALU.subtract), r=["t16", uk], w=["pl%d" % c])
        for tg in range(4):
            zb = 2 + tg % 2
            A(PE, lambda e, c=c, tg=tg, zb=zb: e.matmul(psf(zb), lhsT=CB[:, CB_PBD + c * 128:CB_PBD + (c + 1) * 128],
                                                         rhs=PL[:, c, tg * 512:(tg + 1) * 512], start=True, stop=True),
              r=["pl%d" % c, "cb"], w=[pk(zb)])
            A(ACT, lambda e, c=c, tg=tg, zb=zb: e.activation(out=MIXT[:, 4 + c, tg * 512:(tg + 1) * 512], in_=psf(zb), func=AF.Copy,
                                                             scale=CF[:, CF_PSC + c:CF_PSC + c + 1]),
              r=[pk(zb), "cf"], w=["mixt%d" % (4 + c)])

    M.reset("r2")
    M.reset("ut")
    XT = [M.alloc("r2", "xt%d" % i, [128, 1024], F32) for i in range(2)]
    XN = [M.alloc("r2", "xn%d" % i, [128, 1024], BF16) for i in range(2)]
    SQZ = [M.alloc("r2", "sqz%d" % i, [128, 512], BF16) for i in range(2)]
    RS = [M.alloc("r2", "rs%d" % i, [128, 512], F32) for i in range(2)]
    ST4 = [M.alloc("r2", "st4_%d" % i, [128, 4], F32) for i in range(2)]
    MEMT = M.alloc("r2", "memt", [128, 8, 256], BF16, keys=["memt0", "memt1"])
    KMT = M.alloc("r2", "kmt", [128, 2, 256], BF16, keys=["kmt0", "kmt1"])
    VMA = M.alloc("r2", "vma", [128, 2, 4, 65], BF16)
    PTM = [M.alloc("r2", "ptm%d" % i, [128, 512], BF16) for i in range(2)]
    ACM = [M.alloc("r2", "acm%d" % i, [128, 512], F32) for i in range(2)]
    WKV = M.alloc("ut", "wkv", [128, 8, 512], BF16)
    TMPO = [M.alloc("ut", "tmpo%d" % i, [128, 2048], BF16) for i in range(2)]
    S["XT"], S["XN"], S["ST4"], S["SQZ"], S["RS"] = XT, XN, ST4, SQZ, RS
    A(POOL, lambda e: e.dma_start(out=WKV, in_=D["w_kv"].rearrange("(k p) c -> p k c", p=128)), w=["wkv"], dma=True)
    A(DVE, lambda e: e.memset(VMA[:, :, :, 64:65], 1.0), w=["vma"])

    def norm_tile(src_ap, slot, gain_col, dst, dkeys, ti):
        xt, xn, st = XT[slot], XN[slot], ST4[slot]
        kx, kn, ks = "xt%d" % slot, "xn%d" % slot, "st4_%d" % slot
        tp = ti % 2
        A(SP, lambda e: e.dma_start(out=xt, in_=src_ap), w=[kx], dma=True)
        A(ACT, lambda e: e.activation(out=xn, in_=xt, func=AF.Square, accum_out=st[:, 0:1]), r=[kx], w=[kn, ks])
        A(ACT, lambda e: e.activation(out=st[:, 1:2], in_=st[:, 0:1], func=AF.Ln, bias=EPSC, scale=1.0 / 1024), r=[ks, "cf"], w=[ks])
        A(ACT, lambda e: e.activation(out=st[:, 2:3], in_=st[:, 1:2], func=AF.Exp, scale=-0.5), r=[ks], w=[ks])
        A(ACT, lambda e: e.activation(out=xn, in_=xt, func=AF.Copy, scale=st[:, 2:3]), r=[kx, ks], w=[kn])

        def tr(e):
            for k in range(8):
                ins = e.transpose(out=psb(tp)[:, k * 128:(k + 1) * 128], in_=xn[:, k * 128:(k + 1) * 128], identity=IDB)
            return ins
        A(PE, tr, r=[kn, "cb"], w=[pk(tp)])
        g = CF[:, gain_col:gain_col + 8].unsqueeze(2).broadcast_to([128, 8, 128])
        A(DVE, lambda e: e.tensor_tensor(out=dst, in0=psb(tp).rearrange("p (k t) -> p k t", k=8), in1=g, op=ALU.mult),
          r=[pk(tp), "cf"], w=dkeys)

    def head_norm(zbank, zkey, n, gain_col, dst, dkeys, i):
        sq, rs = SQZ[i % 2], RS[i % 2]
        ksq, krs = "sqz%d" % (i % 2), "rs%d" % (i % 2)
        mb = 4 + i % 2
        BD64 = CB[:, CB_BD:CB_BD + 128]
        A(ACT, lambda e: e.activation(out=sq[:, 0:n], in_=psf(zbank)[:, 0:n], func=AF.Square), r=[zkey], w=[ksq])
        A(PE, lambda e: e.matmul(psf(mb)[:, 0:n], lhsT=BD64, rhs=sq[:, 0:n], start=True, stop=True), r=[ksq, "cb"], w=[pk(mb)])
        A(ACT, lambda e: e.activation(out=rs[:, 0:n], in_=psf(mb)[:, 0:n], func=AF.Ln, bias=EPSC, scale=1.0), r=[pk(mb), "cf"], w=[krs])
        A(ACT, lambda e: e.activation(out=rs[:, 0:n], in_=rs[:, 0:n], func=AF.Exp, scale=-0.5), r=[krs], w=[krs])
        A(DVE, lambda e: e.scalar_tensor_tensor(out=dst, in0=psf(zbank)[:, 0:n], scalar=CF[:, gain_col:gain_col + 1], in1=rs[:, 0:n],
                                                 op0=ALU.mult, op1=ALU.mult), r=[zkey, krs, "cf"], w=dkeys)

    for mt in range(2):
        norm_tile(D["mem"][mt * 128:(mt + 1) * 128, :], mt, CF_GMEM, MEMT[:, :, mt * 128:(mt + 1) * 128], ["memt%d" % mt], mt)
    mkeys = ["memt0", "memt1"]
    for c in range(2):
        zb = 2 + c

        def kproj(e, c=c, zb=zb):
            for k in range(8):
                ins = e.matmul(psf(zb)[:, 0:256], lhsT=WKV[:, k, c * 128:(c + 1) * 128], rhs=MEMT[:, k, :], start=(k == 0), stop=(k == 7))
            return ins
        A(PE, kproj, r=["wkv"] + mkeys, w=[pk(zb)])
        head_norm(zb, pk(zb), 256, CF_GMK, KMT[:, c, :], ["kmt%d" % c], c)
    for mt in range(2):
        zb = 2 + mt

        def vproj(e, mt=mt, zb=zb):
            for k in range(8):
                ins = e.matmul(psf(zb)[:, 0:256], lhsT=MEMT[:, k, mt * 128:(mt + 1) * 128], rhs=WKV[:, k, 256:512], start=(k == 0), stop=(k == 7))
            return ins
        A(PE, vproj, r=["wkv", "memt%d" % mt], w=[pk(zb)])
        A(ACT, lambda e, mt=mt, zb=zb: e.activation(out=VMA[:, mt, :, 0:64], in_=psf(zb)[:, 0:256].rearrange("p (h d) -> p h d", h=4), func=AF.Copy),
          r=[pk(zb)], w=["vma"])

    def normalize_out(acc_ap_fn, acc_keys, tmpo, tkey, bcb):
        for tg in range(4):
            a = acc_ap_fn(tg)
            A(DVE, lambda e, a=a: e.reciprocal(out=a[64:65, :], in_=a[64:65, :]), r=acc_keys, w=acc_keys)
            A(PE, lambda e, a=a: e.matmul(psf(bcb)[0:64, :], lhsT=ONES64[64:65, :], rhs=a[64:65, :], start=True, stop=True), r=acc_keys + ["cf"], w=[pk(bcb)])
            A(DVE, lambda e, a=a, tg=tg: e.tensor_tensor(out=tmpo[0:64, tg * 512:(tg + 1) * 512], in0=a[0:64, :], in1=psf(bcb)[0:64, :], op=ALU.mult),
              r=acc_keys + [pk(bcb)], w=[tkey])

    its = [(mh, tg) for mh in range(4) for tg in range(4)]

    def MF1(it):
        mh, tg = its[it]
        c, b0 = mh // 2, 64 * (mh % 2)
        sbs = (2, 3) if it % 2 == 0 else (6, 7)
        for mt in range(2):
            sb = sbs[mt]
            ptm, pkey = PTM[mt], "ptm%d" % mt
            A(PE, lambda e, mt=mt, sb=sb: e.matmul(
                psf(sb), lhsT=KMT[b0:b0 + 64, c, mt * 128:(mt + 1) * 128], rhs=QMT[b0:b0 + 64, c, tg * 512:(tg + 1) * 512], start=True, stop=True),
              r=["kmt%d" % c, "qmt%d" % c], w=[pk(sb)])
            A(ACT, lambda e, sb=sb, ptm=ptm: e.activation(out=ptm, in_=psf(sb), func=AF.Exp, scale=0.125), r=[pk(sb)], w=[pkey])

    def MF2(it):
        mh, tg = its[it]
        ob = it % 2
        for mt in range(2):
            ptm, pkey = PTM[mt], "ptm%d" % mt
            A(PE, lambda e, mt=mt, ptm=ptm: e.matmul(psf(ob)[0:65, :], lhsT=VMA[:, mt, mh, :], rhs=ptm, start=(mt == 0), stop=(mt == 1)),
              r=["vma", pkey], w=[pk(ob)])
        acm, akey = ACM[it % 2], "acm%d" % (it % 2)
        A(ACT, lambda e: e.activation(out=acm[0:65, :], in_=psf(ob)[0:65, :], func=AF.Copy), r=[pk(ob)], w=[akey])

    def MB(it):
        mh, tg = its[it]
        c, b0 = mh // 2, 64 * (mh % 2)
        tmpo, tkey = TMPO[mh % 2], "tmpo%d" % (mh % 2)
        acm, akey = ACM[it % 2], "acm%d" % (it % 2)
        bcb = 4 + it % 2
        A(ACT, lambda e: e.activation(out=acm[64:65, :], in_=acm[64:65, :], func=AF.Ln), r=[akey], w=[akey])
        A(ACT, lambda e: e.activation(out=acm[64:65, :], in_=acm[64:65, :], func=AF.Exp, scale=-1.0), r=[akey], w=[akey])
        A(PE, lambda e: e.matmul(psf(bcb)[0:64, :], lhsT=ONES64[64:65, :], rhs=acm[64:65, :], start=True, stop=True), r=[akey, "cf"], w=[pk(bcb)])
        A(DVE, lambda e: e.tensor_tensor(out=tmpo[0:64, tg * 512:(tg + 1) * 512], in0=acm[0:64, :], in1=psf(bcb)[0:64, :], op=ALU.mult),
          r=[akey, pk(bcb)], w=[tkey])
        if tg == 3:
            A(SP, lambda e: e.dma_start(out=MIXT[b0:b0 + 64, 6 + c, :], in_=tmpo[0:64, :]), r=[tkey], w=["mixt%d" % (6 + c)], dma=True)

    PTM = [M.alloc("r2", "ptm%d" % i, [128, 512], BF16) for i in range(2)] if False else PTM
    MF1(0)
    for it in range(16):
        MF2(it)
        if it + 1 < 16:
            MF1(it + 1)
        if it >= 1:
            MB(it - 1)
    MB(15)

    if "mix" in dumps and stage == 2:
        add_dump("mixt", MIXT, ["mixt%d" % i for i in range(8)], [128, 8, 2048], BF16)
    if stage <= 2:
        P.emit(nc)
        return nc, dump_out
    return build_attn(nc, P, M, D, A, CF, CB, PS, psf, psb, pk, out, xg, yg, xmid, stage, dumps, dump_out, add_dump, S, MIXT, WOUT)


def build_attn(nc, P, M, D, A, CF, CB, PS, psf, psb, pk, out, xg, yg, xmid, stage, dumps, dump_out, add_dump, S, MIXT, WOUT):
    QT, KT, VT = S["QT"], S["KT"], S["VT"]
    vt_keys, kt_keys, qt_keys = S["vt_keys"], S["kt_keys"], S["qt_keys"]
    IDB = CB[:, CB_ID:CB_ID + 128]
    MASK4 = CB[:, CB_MASK:CB_MASK + 512]
    ONES64 = CF[:, CF_ONES:CF_ONES + 64]
    HV = CF[:, CF_HV:CF_HV + 1]
    M.reset("r2"); M.reset("ut"); M.reset("qmt")
    import os as _os
    DILS = tuple(int(v) for v in _os.environ.get('ATTN_DILS', '1,4,16').split(','))
    NHP = int(_os.environ.get('ATTN_HP', '4'))
    LVL = int(_os.environ.get('ATTN_LEVEL', '9'))
    VA = [M.alloc("r2" if p < 3 else "ut", "va%d" % p, [128, 32, 2, 65], BF16) for p in range(3)]
    PT = [M.alloc("r2", "pt%d" % i, [128, 512], BF16) for i in range(4)]
    ACC = M.alloc("ut", "acc", [128, 2, 2048], F32, keys=["acc_q%d" % i for i in range(4)])
    TMPO = [M.alloc("qmt", "tmpo%d" % i, [128, 2048], BF16) for i in range(2)]
    for p, d in enumerate(DILS):
        nb = 32 // d
        A(DVE, lambda e, p=p: e.memset(VA[p][:, :, :, 64:65], 1.0), w=["va%d" % p])
        v = VA[p][:, :, :, 64:65].rearrange("p (r b) h o -> p r b (h o)", r=d)[:, :, 0:nb // 2, :]
        A(DVE, lambda e, v=v: e.tensor_scalar(out=v, in0=v, scalar1=HV, scalar2=None, op0=ALU.mult), r=["cf"], w=["va%d" % p])

    for i in range(16):
        A(SP, lambda e, i=i: e.dma_start(out=xg[i * 512:(i + 1) * 512, :].rearrange("(p r) d -> p r d", p=128), in_=S["ZT4"]),
          r=["zt"], w=["xg"], dma=True)

    units = [(hp, p, d) for hp in range(NHP) for p, d in enumerate(DILS)]
    NU = len(units)
    akeys = ["acc_q%d" % i for i in range(4)]

    def vprep_group(u, grp):
        hp, p, d = units[u]
        c, nb, va, vak = hp, 32 // d, VA[p], "va%d" % p

        def vtr(e):
            for j in range(8):
                kb = grp * 8 + j
                r, b = kb // nb, kb % nb
                s0 = d * 128 * b + r
                ins = e.transpose(out=psb(6)[:, j * 128:(j + 1) * 128], in_=VT[:, c, s0:s0 + 127 * d + 1:d], identity=IDB)
            return ins
        A(PE, vtr, r=vt_keys(c) + ["cb"], w=[pk(6)])
        src = psb(6).rearrange("p (k h d) -> p k h d", k=8, h=2)
        dst = va[:, grp * 8:grp * 8 + 8, :, 0:64]
        if grp % 2 == 0:
            A(ACT, lambda e: e.activation(out=dst, in_=src, func=AF.Copy), r=[pk(6)], w=[vak])
        else:
            A(DVE, lambda e: e.tensor_copy(out=dst, in_=src), r=[pk(6)], w=[vak])

    steps = []
    for u, (hp, p, d) in enumerate(units):
        nb = 32 // d
        for qg in range(4):
            for jp in range(2):
                blk = []
                for jj in range(2):
                    j = 2 * jp + jj
                    if d == 1:
                        r, b = 0, 16 + 4 * qg + j
                    elif d == 4:
                        r, b = j, 4 + qg
                    else:
                        r, b = 4 * qg + j, 1
                    kbc = r * nb + b
                    kc0 = d * 128 * b + r
                    blk.append((j, kbc, kbc - 1, kc0, d * 128 * (b - 1) + r, kc0 - 2048))
                steps.append(dict(u=u, hp=hp, p=p, d=d, qg=qg, jp=jp, blk=blk, n=len(steps)))
    mi = [0]

    def F1(st):
        n, c, d, blk = st["n"], st["hp"], st["d"], st["blk"]
        sset = n % 2
        pts = [(PT[2 * sset + hh], "pt%d" % (2 * sset + hh)) for hh in range(2)]
        st["pts"] = pts

        def qk(e):
            for jj, (j, kbc, kbp, kc0, kp0, q0) in enumerate(blk):
                for hh in range(2):
                    for kv, k0 in enumerate((kp0, kc0)):
                        ins = e.matmul(psf(2 * sset + hh)[:, (jj * 2 + kv) * 128:(jj * 2 + kv + 1) * 128],
                                       lhsT=KT[hh * 64:hh * 64 + 64, c, k0:k0 + 127 * d + 1:d],
                                       rhs=QT[hh * 64:hh * 64 + 64, c, q0:q0 + 127 * d + 1:d], start=True, stop=True)
            return ins
        A(PE, qk, r=kt_keys(c) + qt_keys(c), w=[pk(2 * sset), pk(2 * sset + 1)])
        for hh in range(2):
            pt, ptk = pts[hh]
            A(ACT, lambda e, sb=2 * sset + hh, pt=pt: e.activation(out=pt, in_=psf(sb), func=AF.Exp, scale=0.125), r=[pk(2 * sset + hh)], w=[ptk])
            meng = DVE if mi[0] % 2 == 0 else POOL
            mi[0] += 1
            A(meng, lambda e, pt=pt: e.tensor_tensor(out=pt, in0=pt, in1=MASK4, op=ALU.mult), r=[ptk, "cb"], w=[ptk])

    def F2(st):
        n, p, d, blk, qg, jp, pts = st["n"], st["p"], st["d"], st["blk"], st["qg"], st["jp"], st["pts"]
        va, vak = VA[p], "va%d" % p
        ob = 4 + n % 2

        def pv(e):
            for jj, (j, kbc, kbp, kc0, kp0, q0) in enumerate(blk):
                for hh in range(2):
                    pt = pts[hh][0]
                    o = psf(ob)[0:65, (hh * 2 + jj) * 128:(hh * 2 + jj + 1) * 128]
                    e.matmul(o, lhsT=va[:, kbp, hh, :], rhs=pt[:, (jj * 2) * 128:(jj * 2 + 1) * 128], start=True, stop=False)
                    ins = e.matmul(o, lhsT=va[:, kbc, hh, :], rhs=pt[:, (jj * 2 + 1) * 128:(jj * 2 + 2) * 128], start=False, stop=True)
            return ins
        A(PE, pv, r=[vak, pts[0][1], pts[1][1]], w=[pk(ob)])
        if d == 1:
            t0 = (qg * 4 + jp * 2) * 128
            dst = ACC[0:65, :, t0:t0 + 256]
            src = psf(ob)[0:65, :].rearrange("p (h t) -> p h t", h=2)
            A(ACT, lambda e: e.activation(out=dst, in_=src, func=AF.Copy), r=[pk(ob)], w=[akeys[qg]])
        else:
            if d == 4:
                dst = ACC[0:65, :, qg * 512:(qg + 1) * 512].rearrange("p h (i r) -> p h r i", r=4)[:, :, 2 * jp:2 * jp + 2, :]
                ks = [akeys[qg]]
            else:
                dst = ACC[0:65, :, :].rearrange("p h (i r) -> p h r i", r=16)[:, :, 4 * qg + 2 * jp:4 * qg + 2 * jp + 2, :]
                ks = akeys
            src = psf(ob)[0:65, :].rearrange("p (h r i) -> p h r i", h=2, r=2)
            A(DVE, lambda e: e.tensor_tensor(out=dst, in0=src, in1=dst, op=ALU.add), r=[pk(ob)] + ks, w=ks)

    tmi = [0]

    bci = [0]

    def NORM(hp):
        for hh in range(2):
            a = ACC[64:65, hh, :]
            A(ACT, lambda e, a=a: e.activation(out=a, in_=a, func=AF.Ln), r=akeys, w=akeys)
            A(ACT, lambda e, a=a: e.activation(out=a, in_=a, func=AF.Exp, scale=-1.0), r=akeys, w=akeys)
        for hh in range(2):
            tmpo, tkey = TMPO[tmi[0] % 2], "tmpo%d" % (tmi[0] % 2)
            tmi[0] += 1
            for tg in range(4):
                a = ACC[:, hh, tg * 512:(tg + 1) * 512]
                bb = 6 + bci[0] % 2
                bci[0] += 1
                A(PE, lambda e, a=a, bb=bb: e.matmul(psf(bb)[0:64, :], lhsT=ONES64[64:65, :], rhs=a[64:65, :], start=True, stop=True), r=[akeys[tg], "cf"], w=[pk(bb)])
                A(DVE, lambda e, a=a, tg=tg, tmpo=tmpo, bb=bb: e.tensor_tensor(out=tmpo[0:64, tg * 512:(tg + 1) * 512], in0=a[0:64, :], in1=psf(bb)[0:64, :], op=ALU.mult),
                  r=[akeys[tg], pk(bb)], w=[tkey])
            A(SP, lambda e, hh=hh, hp=hp, tmpo=tmpo: e.dma_start(out=MIXT[hh * 64:hh * 64 + 64, hp, :], in_=tmpo[0:64, :]), r=[tkey], w=["mixt%d" % hp], dma=True)

    for grp in range(4):
        vprep_group(0, grp)
    F1(steps[0])
    for n, st in enumerate(steps):
        if n + 1 < len(steps):
            F1(steps[n + 1])
        F2(st)
        sl = n % 8
        if sl in (1, 3, 5, 7) and st["u"] + 1 < NU:
            vprep_group(st["u"] + 1, sl // 2)
        if sl == 7 and st["p"] == len(DILS) - 1:
            NORM(st["hp"])

    if "mix" in dumps and stage == 3:
        add_dump("mixt", MIXT, ["mixt%d" % i for i in range(8)], [128, 8, 2048], BF16)
    if stage <= 3:
        P.emit(nc)
        return nc, dump_out
    return build_ffn(nc, P, M, D, A, CF, CB, PS, psf, psb, pk, out, xg, yg, xmid, stage, dumps, dump_out, add_dump, S, MIXT, WOUT)


def build_ffn(nc, P, M, D, A, CF, CB, PS, psf, psb, pk, out, xg, yg, xmid, stage, dumps, dump_out, add_dump, S, MIXT, WOUT):
    IDB = CB[:, CB_ID:CB_ID + 128]
    ONESB = CB[:, CB_ONES:CB_ONES + 128]
    TRI = CB[:, CB_TRI:CB_TRI + 128]
    EPSC = CF[:, CF_EPS:CF_EPS + 1]
    IDF = CF[:, CF_IDF:CF_IDF + 128]
    GFFN = CF[:, CF_GFFN:CF_GFFN + 1024]
    BIASR = CF[:, CF_BIAS:CF_BIAS + 72]
    OFFS = CF[:, CF_OFFS:CF_OFFS + 64]
    M.reset("kt"); M.reset("qt")
    XT = [M.alloc("kt", "xt%d" % i, [128, 1024], F32) for i in range(2)]
    XM = [M.alloc("kt", "xm%d" % i, [128, 1024], F32) for i in range(2)]
    H2 = M.alloc("kt", "h2", [128, 1024], F32)
    H2B = [M.alloc("kt", "h2b%d" % i, [128, 1024], BF16) for i in range(2)]
    H2T = M.alloc("kt", "h2t", [128, 8, 128], F32, keys=["h2t0", "h2t1"])
    WR = M.alloc("qt", "wr", [128, 8, 72], F32)
    AB = M.alloc("qt", "ab", [128, 16, 64], BF16, keys=["ab%d" % i for i in range(16)])
    G12 = M.alloc("qt", "g12", [128, 2, 16], F32)
    RT = M.alloc("qt", "rt", [128, 1024], F32)
    DEST = [[nc.alloc_sbuf_tensor("dest%d_%d" % (a, tt), [128, 1], I32) for tt in range(16)] for a in range(2)]
    A(SP, lambda e: e.dma_start(out=WR, in_=D["w_r"].rearrange("(k p) c -> p k c", p=128)), w=["wr"], dma=True)

    def rt(o, n):
        return RT[:, o:o + n]
    L, OHG, E8, T64, LSEL, OH1, L2, OH2 = rt(0, 72), rt(72, 8), rt(80, 8), rt(88, 64), rt(152, 8), rt(160, 8), rt(168, 8), rt(176, 8)
    A1F, A2F, RB, SM = rt(184, 64), rt(248, 64), rt(312, 64), rt(376, 16)
    RK = "rt"

    for tt in range(16):
        sl = tt % 2
        xt, xm, h2b = XT[sl], XM[sl], H2B[sl]
        kx, km, kb = "xt%d" % sl, "xm%d" % sl, "h2b%d" % sl
        tok = slice(tt * 128, (tt + 1) * 128)
        A(SP, lambda e, xt=xt, tok=tok: e.dma_start(out=xt, in_=D["xo"][tok, :]), w=[kx], dma=True)
        for half in range(2):
            def oproj(e, half=half, tok=tok):
                for k in range(8):
                    ins = e.matmul(psf(half), lhsT=MIXT[:, k, tok], rhs=WOUT[:, k, half * 512:(half + 1) * 512], start=(k == 0), stop=(k == 7))
                return ins
            A(PE, oproj, r=["wout"] + ["mixt%d" % i for i in range(8)], w=[pk(half)])
            A(DVE, lambda e, half=half, xt=xt, xm=xm: e.tensor_tensor(out=xm[:, half * 512:(half + 1) * 512], in0=psf(half), in1=xt[:, half * 512:(half + 1) * 512], op=ALU.add),
              r=[pk(half), kx], w=[km])
        A(SP, lambda e, xm=xm, tok=tok: e.dma_start(out=xmid[tok, :], in_=xm), r=[km], w=["xmid%d" % tt], dma=True)
        A(ACT, lambda e, xm=xm: e.activation(out=H2, in_=xm, func=AF.Square, accum_out=SM[:, 0:1]), r=[km], w=["h2", "sm"])
        A(ACT, lambda e: e.activation(out=SM[:, 1:2], in_=SM[:, 0:1], func=AF.Sqrt, bias=EPSC, scale=1.0 / 1024), r=["sm", "cf"], w=["sm"])
        A(DVE, lambda e: e.reciprocal(out=SM[:, 2:3], in_=SM[:, 1:2]), r=["sm"], w=["sm"])
        A(DVE, lambda e, xm=xm: e.scalar_tensor_tensor(out=H2, in0=xm, scalar=SM[:, 2:3], in1=GFFN, op0=ALU.mult, op1=ALU.mult), r=[km, "sm", "cf"], w=["h2"])
        A(ACT, lambda e, h2b=h2b: e.activation(out=h2b, in_=H2, func=AF.Copy), r=["h2"], w=[kb])
        for hb in range(2):
            def tr(e, hb=hb):
                for k in range(4):
                    kk = hb * 4 + k
                    ins = e.transpose(out=psf(2 + hb)[:, k * 128:(k + 1) * 128], in_=H2[:, kk * 128:(kk + 1) * 128], identity=IDF)
                return ins
            A(PE, tr, r=["h2", "cf"], w=[pk(2 + hb)])
            if hb == 0:
                A(ACT, lambda e: e.activation(out=H2T[:, 0:4, :], in_=psf(2).rearrange("p (k t) -> p k t", k=4), func=AF.Copy), r=[pk(2)], w=["h2t0"])
            else:
                A(DVE, lambda e: e.tensor_copy(out=H2T[:, 4:8, :], in_=psf(3).rearrange("p (k t) -> p k t", k=4)), r=[pk(3)], w=["h2t1"])

        def rmm(e):
            for k in range(8):
                ins = e.matmul(psf(6)[:, 0:72], lhsT=H2T[:, k, :], rhs=WR[:, k, :], start=(k == 0), stop=(k == 7))
            return ins
        A(PE, rmm, r=["h2t0", "h2t1", "wr"], w=[pk(6)])
        V = lambda fn, r=(), w=(): A(DVE, fn, r=list(r) + [RK], w=list(w) + [RK])
        V(lambda e: e.tensor_tensor(out=L, in0=psf(6)[:, 0:72], in1=BIASR, op=ALU.add), r=[pk(6), "cf"])
        V(lambda e: e.tensor_reduce(out=SM[:, 4:5], in_=L[:, 0:8], axis=AX.X, op=ALU.max), r=["sm"], w=["sm"])
        V(lambda e: e.tensor_scalar(out=OHG, in0=L[:, 0:8], scalar1=SM[:, 4:5], scalar2=None, op0=ALU.is_equal), r=["sm"])
        V(lambda e: e.tensor_scalar(out=SM[:, 5:6], in0=SM[:, 4:5], scalar1=-1.0, scalar2=None, op0=ALU.mult), r=["sm"], w=["sm"])
        A(ACT, lambda e: e.activation(out=E8, in_=L[:, 0:8], func=AF.Exp, bias=SM[:, 5:6], scale=1.0, accum_out=SM[:, 6:7]), r=[RK, "sm"], w=[RK, "sm"])
        V(lambda e: e.reciprocal(out=SM[:, 7:8], in_=SM[:, 6:7]), r=["sm"], w=["sm"])
        V(lambda e: e.tensor_tensor(out=T64.rearrange("p (g x) -> p g x", g=8), in0=L[:, 8:72].rearrange("p (g x) -> p g x", g=8),
                                    in1=OHG.unsqueeze(2).broadcast_to([128, 8, 8]), op=ALU.mult))
        V(lambda e: e.tensor_reduce(out=LSEL, in_=T64.rearrange("p (g x) -> p x g", g=8), axis=AX.X, op=ALU.add))
        V(lambda e: e.tensor_reduce(out=SM[:, 8:9], in_=LSEL, axis=AX.X, op=ALU.max), r=["sm"], w=["sm"])
        V(lambda e: e.tensor_scalar(out=OH1, in0=LSEL, scalar1=SM[:, 8:9], scalar2=None, op0=ALU.is_equal), r=["sm"])
        V(lambda e: e.scalar_tensor_tensor(out=L2, in0=OH1, scalar=-1e30, in1=LSEL, op0=ALU.mult, op1=ALU.add))
        V(lambda e: e.tensor_reduce(out=SM[:, 9:10], in_=L2, axis=AX.X, op=ALU.max), r=["sm"], w=["sm"])
        V(lambda e: e.tensor_scalar(out=OH2, in0=L2, scalar1=SM[:, 9:10], scalar2=None, op0=ALU.is_equal), r=["sm"])
        V(lambda e: e.tensor_tensor(out=SM[:, 10:11], in0=SM[:, 9:10], in1=SM[:, 8:9], op=ALU.subtract), r=["sm"], w=["sm"])
        A(ACT, lambda e: e.activation(out=SM[:, 11:12], in_=SM[:, 10:11], func=AF.Exp), r=["sm"], w=["sm"])
        V(lambda e: e.tensor_scalar(out=SM[:, 12:13], in0=SM[:, 11:12], scalar1=1.0, scalar2=None, op0=ALU.add), r=["sm"], w=["sm"])
        V(lambda e: e.reciprocal(out=SM[:, 13:14], in_=SM[:, 12:13]), r=["sm"], w=["sm"])
        V(lambda e, tt=tt: e.tensor_tensor(out=G12[:, 0, tt:tt + 1], in0=SM[:, 7:8], in1=SM[:, 13:14], op=ALU.mult), r=["sm"], w=["g12"])
        V(lambda e, tt=tt: e.tensor_tensor(out=G12[:, 1, tt:tt + 1], in0=SM[:, 7:8], in1=G12[:, 0, tt:tt + 1], op=ALU.subtract), r=["sm", "g12"], w=["g12"])
        V(lambda e: e.tensor_tensor(out=A1F.rearrange("p (g x) -> p g x", g=8), in0=OHG.unsqueeze(2).broadcast_to([128, 8, 8]),
                                    in1=OH1.unsqueeze(1).broadcast_to([128, 8, 8]), op=ALU.mult))
        V(lambda e: e.tensor_tensor(out=A2F.rearrange("p (g x) -> p g x", g=8), in0=OHG.unsqueeze(2).broadcast_to([128, 8, 8]),
                                    in1=OH2.unsqueeze(1).broadcast_to([128, 8, 8]), op=ALU.mult))
        V(lambda e, tt=tt: e.tensor_tensor(out=AB[:, tt, :], in0=A1F, in1=A2F, op=ALU.add), w=["ab%d" % tt])

        def rank(e, tt=tt):
            for t2 in range(tt):
                e.matmul(psf(7)[:, 0:64], lhsT=ONESB, rhs=AB[:, t2, :], start=(t2 == 0), stop=False)
            return e.matmul(psf(7)[:, 0:64], lhsT=TRI, rhs=AB[:, tt, :], start=(tt == 0), stop=True)
        A(PE, rank, r=["ab%d" % i for i in range(tt + 1)] + ["cb"], w=[pk(7)])
        V(lambda e: e.scalar_tensor_tensor(out=RB, in0=psf(7)[:, 0:64], scalar=float(EXPERT_CAP - 1), in1=OFFS, op0=ALU.min, op1=ALU.add), r=[pk(7), "cf"])
        for a, AF_ in enumerate((A1F, A2F)):
            V(lambda e, AF_=AF_: e.tensor_tensor(out=T64, in0=AF_, in1=RB, op=ALU.mult))
            V(lambda e: e.tensor_reduce(out=SM[:, 14:15], in_=T64, axis=AX.X, op=ALU.add), r=["sm"], w=["sm"])
            dk = "dest%d_%d" % (a, tt)
            V(lambda e, a=a, tt=tt: e.tensor_copy(out=DEST[a][tt][:, :], in_=SM[:, 14:15]), r=["sm"], w=[dk])
            A(POOL, lambda e, a=a, tt=tt, h2b=h2b: e.indirect_dma_start(
                out=xg, out_offset=bass.IndirectOffsetOnAxis(ap=DEST[a][tt][:, 0:1], axis=0), in_=h2b, in_offset=None,
                bounds_check=None, oob_is_err=False), r=[dk, kb, "xg"], w=["xgs%d_%d" % (a, tt)], dma=True)

    if "route" in dumps:
        add_dump("g12", G12, ["g12"], [128, 2, 16], F32)
        for a in range(2):
            for tt in (0, 15):
                add_dump("dest%d_%d" % (a, tt), DEST[a][tt][:, :], ["dest%d_%d" % (a, tt)], [128, 1], I32)
        add_dump("xm", XM[1], ["xm1"], [128, 1024], F32)
    if stage <= 4:
        P.emit(nc)
        return nc, dump_out

    M.reset("vt"); M.reset("r2"); M.reset("ut"); M.reset("qmt"); M.reset("r1")
    NW = 3
    W1 = [M.alloc("vt" if i == 0 else "r2", "w1_%d" % i, [128, 8, 512], BF16) for i in range(NW)]
    W3 = [M.alloc("vt" if i == 0 else "r2", "w3_%d" % i, [128, 8, 512], BF16) for i in range(NW)]
    W2 = [M.alloc("vt" if i == 0 else "ut", "w2_%d" % i, [128, 4, 1024], BF16) for i in range(NW)]
    XE = [M.alloc("vt", "xe%d" % i, [128, 1024], BF16) for i in range(2)]
    XET = [M.alloc("vt", "xet%d" % i, [128, 8, 128], BF16) for i in range(2)]
    SA = M.alloc("qmt", "sa", [128, 512], F32)
    HM = M.alloc("qmt", "hm", [128, 512], BF16)
    HMT = M.alloc("qmt", "hmt", [128, 4, 128], BF16)
    YE = [M.alloc("r1", "ye%d" % i, [128, 1024], F32) for i in range(2)]

    def wload(e_):
        i = e_ % NW
        A(POOL, lambda e, i=i, e_=e_: e.dma_start(out=W1[i], in_=D["w1"][e_].rearrange("(k p) f -> p k f", p=128)), w=["w1_%d" % i], dma=True)
        A(POOL, lambda e, i=i, e_=e_: e.dma_start(out=W3[i], in_=D["w3"][e_].rearrange("(k p) f -> p k f", p=128)), w=["w3_%d" % i], dma=True)
        A(POOL, lambda e, i=i, e_=e_: e.dma_start(out=W2[i], in_=D["w2"][e_].rearrange("(k p) f -> p k f", p=128)), w=["w2_%d" % i], dma=True)

    NEXP = 64
    for e_ in range(min(NW - 1, NEXP)):
        wload(e_)
    for e_ in range(NEXP):
        if e_ + NW - 1 < NEXP:
            wload(e_ + NW - 1)
        i = e_ % NW
        s2 = e_ % 2
        xe, xet, ye = XE[s2], XET[s2], YE[s2]
        kxe, kxt, kye = "xe%d" % s2, "xet%d" % s2, "ye%d" % s2
        tb = 0 if s2 == 0 else 6
        ab = 1 if s2 == 0 else 7
        A(SP, lambda e, xe=xe, e_=e_: e.dma_start(out=xe, in_=xg[e_ * 128:(e_ + 1) * 128, :]), r=["xg"] + ["xgs%d_%d" % (a, t) for a in range(2) for t in range(16)], w=[kxe], dma=True)

        def trx(e, xe=xe, tb=tb):
            for k in range(8):
                ins = e.transpose(out=psb(tb)[:, k * 128:(k + 1) * 128], in_=xe[:, k * 128:(k + 1) * 128], identity=IDB)
            return ins
        A(PE, trx, r=[kxe, "cb"], w=[pk(tb)])
        A(ACT, lambda e, xet=xet, tb=tb: e.activation(out=xet, in_=psb(tb).rearrange("p (k t) -> p k t", k=8), func=AF.Copy), r=[pk(tb)], w=[kxt])

        def up(e, W, bank, xet=xet):
            for k in range(8):
                ins = e.matmul(psf(bank), lhsT=xet[:, k, :], rhs=W[:, k, :], start=(k == 0), stop=(k == 7))
            return ins
        A(PE, lambda e, i=i, ab=ab, up=up: up(e, W1[i], ab), r=[kxt, "w1_%d" % i], w=[pk(ab)])
        A(PE, lambda e, i=i, up=up: up(e, W3[i], 2), r=[kxt, "w3_%d" % i], w=[pk(2)])
        A(ACT, lambda e, ab=ab: e.activation(out=SA, in_=psf(ab), func=AF.Silu), r=[pk(ab)], w=["sa"])
        A(DVE, lambda e: e.tensor_tensor(out=HM, in0=SA, in1=psf(2), op=ALU.mult), r=["sa", pk(2)], w=["hm"])

        def trh(e):
            for k in range(4):
                ins = e.transpose(out=psb(3)[:, k * 128:(k + 1) * 128], in_=HM[:, k * 128:(k + 1) * 128], identity=IDB)
            return ins
        A(PE, trh, r=["hm", "cb"], w=[pk(3)])
        A(DVE, lambda e: e.tensor_copy(out=HMT, in_=psb(3)[:, 0:512].rearrange("p (k t) -> p k t", k=4)), r=[pk(3)], w=["hmt"])
        for half in range(2):
            def dn(e, i=i, half=half):
                for k in range(4):
                    ins = e.matmul(psf(4 + half), lhsT=HMT[:, k, :], rhs=W2[i][:, k, half * 512:(half + 1) * 512], start=(k == 0), stop=(k == 3))
                return ins
            A(PE, dn, r=["hmt", "w2_%d" % i], w=[pk(4 + half)])
        A(ACT, lambda e, ye=ye: e.activation(out=ye[:, 0:512], in_=psf(4), func=AF.Copy), r=[pk(4)], w=[kye + "a"])
        A(DVE, lambda e, ye=ye: e.tensor_copy(out=ye[:, 512:1024], in_=psf(5)), r=[pk(5)], w=[kye + "b"])
        A(SP, lambda e, ye=ye, e_=e_: e.dma_start(out=yg[e_ * 128:(e_ + 1) * 128, :], in_=ye), r=[kye + "a", kye + "b"], w=["yg%d" % e_], dma=True)

    if stage <= 5:
        P.emit(nc)
        return nc, dump_out

    M.reset("kt")
    Y1 = [M.alloc("kt", "y1_%d" % i, [128, 1024], F32) for i in range(2)]
    Y2 = [M.alloc("kt", "y2_%d" % i, [128, 1024], F32) for i in range(2)]
    XM2 = [M.alloc("kt", "xm2_%d" % i, [128, 1024], F32) for i in range(2)]
    OT_ = [M.alloc("kt", "ot_%d" % i, [128, 1024], F32) for i in range(2)]
    for tt in range(16):
        sl = tt % 2
        tok = slice(tt * 128, (tt + 1) * 128)
        for a, Y in enumerate((Y1, Y2)):
            A(POOL, lambda e, a=a, tt=tt, y=Y[sl]: e.indirect_dma_start(
                out=y, out_offset=None, in_=yg, in_offset=bass.IndirectOffsetOnAxis(ap=DEST[a][tt][:, 0:1], axis=0),
                bounds_check=None, oob_is_err=False), r=["dest%d_%d" % (a, tt)] + ["yg%d" % i for i in range(64)], w=["y%d_%d" % (a + 1, sl)], dma=True)
        A(SP, lambda e, sl=sl, tok=tok: e.dma_start(out=XM2[sl], in_=xmid[tok, :]), r=["xmid%d" % tt], w=["xm2_%d" % sl], dma=True)
        A(DVE, lambda e, sl=sl, tt=tt: e.scalar_tensor_tensor(out=OT_[sl], in0=Y1[sl], scalar=G12[:, 0, tt:tt + 1], in1=XM2[sl], op0=ALU.mult, op1=ALU.add),
          r=["y1_%d" % sl, "xm2_%d" % sl, "g12"], w=["ot_%d" % sl])
        A(DVE, lambda e, sl=sl, tt=tt: e.scalar_tensor_tensor(out=OT_[sl], in0=Y2[sl], scalar=G12[:, 1, tt:tt + 1], in1=OT_[sl], op0=ALU.mult, op1=ALU.add),
          r=["y2_%d" % sl, "ot_%d" % sl, "g12"], w=["ot_%d" % sl])
        A(SP, lambda e, sl=sl, tok=tok: e.dma_start(out=out[tok, :], in_=OT_[sl]), r=["ot_%d" % sl], w=["out"], dma=True)
    if "moe" in dumps:
        allxg = ["xg"] + ["xgs%d_%d" % (a, t) for a in range(2) for t in range(16)]
        for ee in (0, 37):
            add_dump("xg%d" % ee, xg[ee * 128:(ee + 1) * 128, :], allxg, [128, 1024], BF16)
            add_dump("yg%d" % ee, yg[ee * 128:(ee + 1) * 128, :], ["yg%d" % ee], [128, 1024], F32)
        add_dump("g12", G12, ["g12"], [128, 2, 16], F32)
        for a in range(2):
            for tt in range(16):
                add_dump("dest%d_%d" % (a, tt), DEST[a][tt][:, :], ["dest%d_%d" % (a, tt)], [128, 1], I32)
        add_dump("xmid", xmid, ["xmid%d" % t for t in range(16)], [2048, 1024], F32)
    P.emit(nc)
    return nc, dump_out
```

```python
import numpy as np
import concourse.bass as bass
import concourse.mybir as mybir
from concourse.bass_utils import run_bass_kernel_spmd

F32 = mybir.dt.float32
BF16 = mybir.dt.bfloat16
I32 = mybir.dt.int32
AF = mybir.ActivationFunctionType
ALU = mybir.AluOpType
AX = mybir.AxisListType

PE, ACT, DVE, POOL, SP = "pe", "act", "dve", "pool", "sp"
ENGS = (PE, ACT, DVE, POOL, SP)
RING = 12


class Op:
    __slots__ = ("eng", "fn", "deps", "dma", "need", "seq", "di", "name")

    def __init__(self, eng, fn, dma, name):
        self.eng, self.fn, self.dma, self.name = eng, fn, dma, name
        self.deps, self.need, self.seq, self.di = [], False, 0, -1


class Prog:
    def __init__(self):
        self.ops = {e: [] for e in ENGS}
        self.lastw = {}
        self.readers = {}

    def add(self, eng, fn, r=(), w=(), dma=False, name=""):
        op = Op(eng, fn, dma, name)
        deps = {}
        for k in r:
            lw = self.lastw.get(k)
            if lw is not None:
                deps[id(lw)] = (lw, True)
        for k in w:
            lw = self.lastw.get(k)
            if lw is not None:
                deps[id(lw)] = (lw, True)
            for rd in self.readers.get(k, ()):
                if id(rd) not in deps:
                    deps[id(rd)] = (rd, False)
        for k in r:
            self.readers.setdefault(k, []).append(op)
        for k in w:
            self.lastw[k] = op
            self.readers[k] = []
        op.deps = [d for d in deps.values() if d[0] is not op]
        self.ops[eng].append(op)
        return op

    @staticmethod
    def _needs_wait(o, d, true_dep):
        if d.dma:
            return True
        if d.eng != o.eng:
            return True
        if o.eng == PE:
            return False
        if o.dma:
            return True
        return true_dep

    def emit(self, nc):
        for e in ENGS:
            for o in self.ops[e]:
                for d, t in o.deps:
                    if self._needs_wait(o, d, t):
                        d.need = True
        for e in ENGS:
            c = 0
            n = 0
            for o in self.ops[e]:
                if o.dma:
                    o.di = n
                    n += 1
                elif o.need:
                    c += 1
                    o.seq = c
        from contextlib import ExitStack

        with ExitStack() as st:
            esem = {e: st.enter_context(nc.semaphore("c_" + e)) for e in (PE, ACT, DVE, POOL)}
            rsem = {
                e: [st.enter_context(nc.semaphore("r_%s%d" % (e, i))) for i in range(RING)]
                for e in (SP, POOL, ACT)
            }
            block = st.enter_context(nc.Block())

            def run(ename, eng):
                seen = {}

                def wait(sem, val):
                    if seen.get(sem.num, 0) < val:
                        eng.wait_ge(sem, val)
                        seen[sem.num] = val

                ndma = 0
                for o in self.ops[ename]:
                    for d, t in o.deps:
                        if not self._needs_wait(o, d, t):
                            continue
                        if d.dma:
                            wait(rsem[d.eng][d.di % RING], 16 * (d.di // RING + 1))
                        else:
                            wait(esem[d.eng], d.seq)
                    if o.dma:
                        if o.di >= RING:
                            wait(rsem[ename][o.di % RING], 16 * (o.di // RING))
                        ins = o.fn(eng)
                        ins.then_inc(rsem[ename][o.di % RING], 16)
                        ndma = o.di + 1
                    else:
                        ins = o.fn(eng)
                        if o.need:
                            ins.then_inc(esem[ename], 1)
                for i in range(max(0, ndma - RING), ndma):
                    wait(rsem[ename][i % RING], 16 * (i // RING + 1))

            block.tensor(lambda eng: run(PE, eng))
            block.scalar(lambda eng: run(ACT, eng))
            block.vector(lambda eng: run(DVE, eng))
            block.gpsimd(lambda eng: run(POOL, eng))
            block.sync(lambda eng: run(SP, eng))


class Mem:
    def __init__(self, nc, P, total_bytes):
        self.P = P
        self.t = nc.alloc_sbuf_tensor("arena", [128, total_bytes // 4], F32)
        self.regs = {}
        self.top = 0
        self.total = total_bytes

    def region(self, name, size):
        size = (size + 63) // 64 * 64
        assert self.top + size <= self.total, (name, self.top, size)
        self.regs[name] = dict(off=self.top, size=size, cur=0, keys=[], old=[])
        self.top += size

    def reset(self, name):
        r = self.regs[name]
        r["old"] = r["old"] + r["keys"]
        r["keys"] = []
        r["cur"] = 0

    def alloc(self, reg, key, shape, dt, keys=None):
        r = self.regs[reg]
        isz = 2 if dt == BF16 else 4
        n = 1
        for s in shape[1:]:
            n *= s
        nb = (n * isz + 63) // 64 * 64
        assert r["cur"] + nb <= r["size"], (reg, key, r["cur"], nb, r["size"])
        off = r["off"] + r["cur"]
        r["cur"] += nb
        keys = list(keys) if keys else [key]
        r["keys"].extend(keys)
        olds = []
        for ok in r["old"]:
            lw = self.P.lastw.get(ok)
            if lw is not None:
                olds.append(lw)
            olds.extend(self.P.readers.get(ok, ()))
        if olds:
            for kk in keys:
                self.P.readers.setdefault(kk, []).extend(olds)
        ap = self.t[:, off // 4:(off + n * isz + 3) // 4]
        if dt == BF16:
            ap = ap.bitcast(BF16)
        ap = ap[:, 0:n]
        if len(shape) == 3:
            ap = ap.rearrange("p (a b) -> p a b", a=shape[1])
        elif len(shape) == 4:
            ap = ap.rearrange("p (a b c) -> p a b c", a=shape[1], b=shape[2])
        return ap


NCF = 1416
NCB = 1280
CF_GATTN, CF_GMEM, CF_GQ, CF_GK, CF_GMQ, CF_GMK = 0, 8, 16, 17, 18, 19
CF_INVW, CF_PSC, CF_IC16, CF_HV, CF_EPS = 20, 22, 24, 56, 57
CF_ONES, CF_OFFS, CF_BIAS, CF_IDF, CF_GFFN = 64, 128, 192, 264, 392
CB_ID, CB_MASK, CB_BD, CB_ONES, CB_TRI, CB_PBD = 0, 128, 640, 768, 896, 1024
EPS = 1e-6
EXPERT_CAP = 128


def build_program(stage=99, dumps=()):
    nc = bass.Bass("TRN2", target_bir_lowering=False)
    P = Prog()
    D = {}

    def din(name, shape, dt=F32):
        D[name] = nc.dram_tensor(name, shape, dt, kind="ExternalInput").ap()

    din("xo", [2048, 1024]); din("xh", [2048, 1024]); din("mem", [256, 1024])
    din("w_in", [1024, 2048]); din("w_out", [1024, 1024]); din("w_kv", [1024, 512])
    din("w_r", [1024, 72]); din("cf", [128, NCF]); din("cb", [128, NCB])
    if stage >= 5:
        din("w1", [64, 1024, 512]); din("w3", [64, 1024, 512]); din("w2", [64, 512, 1024])
    out = nc.dram_tensor("out", [2048, 1024], F32, kind="ExternalOutput").ap()
    xg = nc.dram_tensor("xg", [64 * EXPERT_CAP, 1024], BF16, kind="Internal").ap()
    yg = nc.dram_tensor("yg", [64 * EXPERT_CAP, 1024], F32, kind="Internal").ap()
    xmid = nc.dram_tensor("xmid", [2048, 1024], F32, kind="Internal").ap()
    dump_out = {}

    M = Mem(nc, P, 203 * 1024)
    M.region("const", 12 * 1024)
    M.region("wout", 16 * 1024)
    M.region("r1", 32 * 1024)
    M.region("qt", 16 * 1024)
    M.region("kt", 32 * 1024)
    M.region("vt", 32 * 1024)
    M.region("r2", 36 * 1024)
    M.region("ut", 16640)
    M.region("qmt", 8 * 1024)
    PS = [nc.alloc_psum_tensor("ps%d" % i, [128, 512], F32) for i in range(8)]

    def psf(i):
        return PS[i][:, :]

    def psb(i):
        return PS[i][:, :].bitcast(BF16)

    def pk(i):
        return "ps%d" % i

    A = P.add
    CF = M.alloc("const", "cf", [128, NCF], F32)
    CB = M.alloc("const", "cb", [128, NCB], BF16)
    ZT = M.alloc("const", "zt", [128, 1024], BF16)
    IDB = CB[:, CB_ID:CB_ID + 128]
    MASK4 = CB[:, CB_MASK:CB_MASK + 512]
    BD64 = CB[:, CB_BD:CB_BD + 128]
    ONESB = CB[:, CB_ONES:CB_ONES + 128]
    TRI = CB[:, CB_TRI:CB_TRI + 128]
    EPSC = CF[:, CF_EPS:CF_EPS + 1]
    IDF = CF[:, CF_IDF:CF_IDF + 128]
    ONES64 = CF[:, CF_ONES:CF_ONES + 64]

    A(SP, lambda e: e.dma_start(out=CF, in_=D["cf"]), w=["cf"], dma=True)
    A(POOL, lambda e: e.dma_start(out=CB, in_=D["cb"]), w=["cb"], dma=True)
    WIN = M.alloc("r1", "win", [128, 8, 2048], BF16, keys=["win%d" % i for i in range(4)])
    win_keys = []
    for i in range(4):
        key = "win%d" % i
        win_keys.append(key)
        A(POOL, lambda e, i=i: e.dma_start(
            out=WIN[:, 2 * i:2 * i + 2, :],
            in_=D["w_in"][256 * i:256 * i + 256, :].rearrange("(k p) c -> p k c", p=128)),
          w=[key], dma=True)
    A(DVE, lambda e: e.memset(ZT, 0.0), w=["zt"])
    ZT4 = ZT.unsqueeze(1).broadcast_to([128, 4, 1024])

    QT = M.alloc("qt", "qt", [128, 4, 2048], BF16, keys=["qt%d_%d" % (c, g) for c in range(4) for g in range(4)])
    KT = M.alloc("kt", "kt", [128, 4, 4096], BF16, keys=["kt%d_%d" % (c, g) for c in range(4) for g in range(8)])
    VT = M.alloc("vt", "vt", [128, 4, 4096], BF16, keys=["vt%d_%d" % (c, g) for c in range(4) for g in range(8)])
    UT = M.alloc("ut", "ut", [128, 2, 2064], F32, keys=["ut0", "ut1"])
    QMT = M.alloc("qmt", "qmt", [128, 2, 2048], BF16, keys=["qmt0", "qmt1"])
    XT = [M.alloc("r2", "xt%d" % i, [128, 1024], F32) for i in range(2)]
    XN = [M.alloc("r2", "xn%d" % i, [128, 1024], BF16) for i in range(2)]
    HTG = [M.alloc("r2", "htg%d" % i, [128, 8, 512], BF16, keys=["htg%d_%d" % (i, t) for t in range(4)]) for i in range(2)]
    SQZ = [M.alloc("r2", "sqz%d" % i, [128, 512], BF16) for i in range(2)]
    RS = [M.alloc("r2", "rs%d" % i, [128, 512], F32) for i in range(2)]
    ST4 = [M.alloc("r2", "st4_%d" % i, [128, 4], F32) for i in range(2)]

    def norm_tile(src_ap, slot, gain_col, dst_fn, dkeys, ti):
        xt, xn, st = XT[slot], XN[slot], ST4[slot]
        kx, kn, ks = "xt%d" % slot, "xn%d" % slot, "st4_%d" % slot
        tp = ti % 2
        A(SP, lambda e: e.dma_start(out=xt, in_=src_ap), w=[kx], dma=True)
        A(ACT, lambda e: e.activation(out=xn, in_=xt, func=AF.Square, accum_out=st[:, 0:1]), r=[kx], w=[kn, ks])
        A(ACT, lambda e: e.activation(out=st[:, 1:2], in_=st[:, 0:1], func=AF.Ln, bias=EPSC, scale=1.0 / 1024), r=[ks, "cf"], w=[ks])
        A(ACT, lambda e: e.activation(out=st[:, 2:3], in_=st[:, 1:2], func=AF.Exp, scale=-0.5), r=[ks], w=[ks])
        A(ACT, lambda e: e.activation(out=xn, in_=xt, func=AF.Copy, scale=st[:, 2:3]), r=[kx, ks], w=[kn])

        def tr(e):
            for k in range(8):
                ins = e.transpose(out=psb(tp)[:, k * 128:(k + 1) * 128], in_=xn[:, k * 128:(k + 1) * 128], identity=IDB)
            return ins
        A(PE, tr, r=[kn, "cb"], w=[pk(tp)])
        g = CF[:, gain_col:gain_col + 8].unsqueeze(2).broadcast_to([128, 8, 128])
        A(DVE, lambda e: e.tensor_tensor(out=dst_fn(), in0=psb(tp).rearrange("p (k t) -> p k t", k=8), in1=g, op=ALU.mult),
          r=[pk(tp), "cf"], w=dkeys)

    def head_norm(zbank, zkey, n, gain_col, dst, dkeys, i):
        sq, rs = SQZ[i % 2], RS[i % 2]
        ksq, krs = "sqz%d" % (i % 2), "rs%d" % (i % 2)
        mb = 4 + i % 2
        A(ACT, lambda e: e.activation(out=sq[:, 0:n], in_=psf(zbank)[:, 0:n], func=AF.Square), r=[zkey], w=[ksq])
        A(PE, lambda e: e.matmul(psf(mb)[:, 0:n], lhsT=BD64, rhs=sq[:, 0:n], start=True, stop=True), r=[ksq, "cb"], w=[pk(mb)])
        A(ACT, lambda e: e.activation(out=rs[:, 0:n], in_=psf(mb)[:, 0:n], func=AF.Ln, bias=EPSC, scale=1.0), r=[pk(mb), "cf"], w=[krs])
        A(ACT, lambda e: e.activation(out=rs[:, 0:n], in_=rs[:, 0:n], func=AF.Exp, scale=-0.5), r=[krs], w=[krs])
        A(DVE, lambda e: e.scalar_tensor_tensor(out=dst, in0=psf(zbank)[:, 0:n], scalar=CF[:, gain_col:gain_col + 1], in1=rs[:, 0:n],
                                                 op0=ALU.mult, op1=ALU.mult), r=[zkey, krs, "cf"], w=dkeys)

    cnt = dict(ti=0, zi=0, hn=0)

    def tile_task(g, t):
        hb = HTG[g % 2]
        src = (D["xh"] if g < 4 else D["xo"])[((g % 4) * 4 + t) * 128:((g % 4) * 4 + t + 1) * 128, :]
        norm_tile(src, cnt["ti"] % 2, CF_GATTN, lambda: hb[:, :, t * 128:(t + 1) * 128], ["htg%d_%d" % (g % 2, t)], cnt["ti"])
        cnt["ti"] += 1

    ZB = (2, 3, 6, 7)

    def chunk_part1(g, c):
        hb = HTG[g % 2]
        hkeys = ["htg%d_%d" % (g % 2, t) for t in range(4)]
        zb = ZB[cnt["zi"] % 4]
        cnt["zi"] += 1

        def proj(e):
            for k in range(8):
                ins = e.matmul(psf(zb), lhsT=WIN[:, k, c * 128:(c + 1) * 128], rhs=hb[:, k, :], start=(k == 0), stop=(k == 7))
            return ins
        A(PE, proj, r=win_keys + hkeys, w=[pk(zb)])
        st = dict(g=g, c=c, zb=zb)
        if c < 8 or c >= 14:
            i = cnt["hn"]
            cnt["hn"] += 1
            st["i"] = i
            sq, ksq = SQZ[i % 2], "sqz%d" % (i % 2)
            A(ACT, lambda e: e.activation(out=sq, in_=psf(zb), func=AF.Square), r=[pk(zb)], w=[ksq])
        return st

    def chunk_part2(st):
        g, c, zb = st["g"], st["c"], st["zb"]
        if 8 <= c < 12:
            A(ACT, lambda e: e.activation(out=VT[:, c - 8, g * 512:(g + 1) * 512], in_=psf(zb), func=AF.Copy),
              r=[pk(zb)], w=["vt%d_%d" % (c - 8, g)])
            return
        if c in (12, 13):
            if g == 3:
                A(DVE, lambda e: e.tensor_copy(out=UT[:, c - 12, 0:16], in_=psf(zb)[:, 496:512]), r=[pk(zb)], w=["ut%d" % (c - 12)])
            else:
                A(DVE, lambda e: e.tensor_copy(out=UT[:, c - 12, 16 + (g - 4) * 512:16 + (g - 3) * 512], in_=psf(zb)),
                  r=[pk(zb)], w=["ut%d" % (c - 12)])
            return
        if c < 4:
            gain_col, dst, dkeys = CF_GQ, QT[:, c, (g - 4) * 512:(g - 3) * 512], ["qt%d_%d" % (c, g - 4)]
        elif c < 8:
            gain_col, dst, dkeys = CF_GK, KT[:, c - 4, g * 512:(g + 1) * 512], ["kt%d_%d" % (c - 4, g)]
        else:
            gain_col, dst, dkeys = CF_GMQ, QMT[:, c - 14, (g - 4) * 512:(g - 3) * 512], ["qmt%d" % (c - 14)]
        i = st["i"]
        sq, rs = SQZ[i % 2], RS[i % 2]
        ksq, krs = "sqz%d" % (i % 2), "rs%d" % (i % 2)
        mb = 4 + i % 2
        A(PE, lambda e: e.matmul(psf(mb), lhsT=BD64, rhs=sq, start=True, stop=True), r=[ksq, "cb"], w=[pk(mb)])
        A(ACT, lambda e: e.activation(out=rs, in_=psf(mb), func=AF.Ln, bias=EPSC, scale=1.0), r=[pk(mb), "cf"], w=[krs])
        A(ACT, lambda e: e.activation(out=rs, in_=rs, func=AF.Exp, scale=-0.5), r=[krs], w=[krs])
        A(DVE, lambda e: e.scalar_tensor_tensor(out=dst, in0=psf(zb), scalar=CF[:, gain_col:gain_col + 1], in1=rs,
                                                 op0=ALU.mult, op1=ALU.mult), r=[pk(zb), krs, "cf"], w=dkeys)

    def chunks_of(g):
        k_, v_ = list(range(4, 8)), list(range(8, 12))
        ch = []
        if g >= 4:
            extra = [0, 1, 2, 3, 14, 15, 12, 13]
        elif g == 3:
            extra = [12, 13]
        else:
            extra = []
        normed = k_ + [c for c in extra if c < 4 or c >= 14]
        cheap = v_ + [c for c in extra if c in (12, 13)]
        while normed or cheap:
            if normed:
                ch.append(normed.pop(0))
            if cheap:
                ch.append(cheap.pop(0))
        return ch

    for t in range(4):
        tile_task(0, t)
    pending = None
    for g in range(8):
        ch = chunks_of(g)
        per = (len(ch) + 3) // 4
        nt = 0
        for i, c in enumerate(ch):
            st = chunk_part1(g, c)
            if pending is not None:
                chunk_part2(pending)
            pending = st
            if g + 1 < 8 and (i + 1) % per == 0 and nt < 4:
                tile_task(g + 1, nt)
                nt += 1
        while g + 1 < 8 and nt < 4:
            tile_task(g + 1, nt)
            nt += 1
    chunk_part2(pending)

    def add_dump(name, ap, keys, shape, dt):
        t = nc.dram_tensor("dump_" + name, shape, dt, kind="ExternalOutput").ap()
        dump_out[name] = t
        A(SP, lambda e: e.dma_start(out=t, in_=ap), r=keys, w=["dump_" + name], dma=True)

    vt_keys = lambda c: ["vt%d_%d" % (c, g) for g in range(8)]
    kt_keys = lambda c: ["kt%d_%d" % (c, g) for g in range(8)]
    qt_keys = lambda c: ["qt%d_%d" % (c, g) for g in range(4)]
    if "qkv" in dumps:
        add_dump("qt", QT, sum([qt_keys(c) for c in range(4)], []), [128, 4, 2048], BF16)
        add_dump("kt", KT, sum([kt_keys(c) for c in range(4)], []), [128, 4, 4096], BF16)
        add_dump("vt", VT, sum([vt_keys(c) for c in range(4)], []), [128, 4, 4096], BF16)
        add_dump("ut", UT, ["ut0", "ut1"], [128, 2, 2064], F32)
        add_dump("qmt", QMT, ["qmt0", "qmt1"], [128, 2, 2048], BF16)
    if stage <= 1:
        P.emit(nc)
        return nc, dump_out
    return build_rest(nc, P, M, D, A, CF, CB, PS, psf, psb, pk, out, xg, yg, xmid, stage, dumps, dump_out, add_dump,
                      dict(QT=QT, KT=KT, VT=VT, UT=UT, QMT=QMT, WIN=WIN, vt_keys=vt_keys, kt_keys=kt_keys, qt_keys=qt_keys,
                           norm_tile=norm_tile, head_norm=head_norm, ZT4=ZT4, XT=XT, XN=XN, ST4=ST4, SQZ=SQZ, RS=RS, win_keys=win_keys))


POOL_WINDOWS = (2, 4, 8, 16)


def prep_inputs(inp):
    f = np.float32
    x = np.ascontiguousarray(inp["x"], dtype=f)
    mem = np.ascontiguousarray(inp["mem"], dtype=f)
    cb = np.zeros((128, NCB), f)
    k = np.arange(128)[:, None]
    q = np.arange(128)[None, :]
    cb[:, CB_ID:CB_ID + 128] = np.eye(128, dtype=f)
    mprev = (k >= q).astype(f)
    mcur = (k <= q).astype(f)
    cb[:, CB_MASK:CB_MASK + 512] = np.concatenate([mprev, mcur, mprev, mcur], axis=1)
    bd = np.zeros((128, 128), f)
    bd[:64, :64] = 1.0 / 64
    bd[64:, 64:] = 1.0 / 64
    cb[:, CB_BD:CB_BD + 128] = bd
    cb[:, CB_ONES:CB_ONES + 128] = 1.0
    cb[:, CB_TRI:CB_TRI + 128] = (k < q).astype(f)
    pp = np.asarray(inp["pool_proj"], dtype=f)[0]
    for c in range(2):
        cb[0:64, CB_PBD + c * 128:CB_PBD + c * 128 + 64] = pp[2 * c]
        cb[64:128, CB_PBD + c * 128 + 64:CB_PBD + c * 128 + 128] = pp[2 * c + 1]
    w_r = np.concatenate([np.asarray(inp["w_group"], f)[0],
                          np.transpose(np.asarray(inp["w_router"], f)[0], (1, 0, 2)).reshape(1024, 64)], axis=1)
    w_r = np.ascontiguousarray(w_r)
    shared = dict(
        w_in=np.ascontiguousarray(inp["w_in"][0], dtype=f), w_out=np.ascontiguousarray(inp["w_out"][0], dtype=f),
        w_kv=np.ascontiguousarray(inp["w_mem_kv"][0], dtype=f), w_r=w_r,
        w1=np.ascontiguousarray(inp["w1"][0], dtype=f), w3=np.ascontiguousarray(inp["w3"][0], dtype=f),
        w2=np.ascontiguousarray(inp["w2"][0], dtype=f), cb=cb)
    p = np.arange(128)
    in_maps = []
    for c in range(8):
        b, half = c // 2, c % 2
        cf = np.zeros((128, NCF), f)
        cf[:, CF_GATTN:CF_GATTN + 8] = np.asarray(inp["attn_norm"], f)[0].reshape(8, 128).T
        cf[:, CF_GMEM:CF_GMEM + 8] = np.asarray(inp["mem_norm"], f)[0].reshape(8, 128).T
        cf[:, CF_GQ] = np.tile(np.asarray(inp["q_norm"], f)[0], 2)
        cf[:, CF_GK] = np.tile(np.asarray(inp["k_norm"], f)[0], 2)
        cf[:, CF_GMQ] = np.tile(np.asarray(inp["mq_norm"], f)[0], 2)
        cf[:, CF_GMK] = np.tile(np.asarray(inp["mk_norm"], f)[0], 2)
        for ch in range(2):
            w = np.array([POOL_WINDOWS[2 * ch + pi // 64] for pi in range(128)], f)
            cf[:, CF_INVW + ch] = 1.0 / w
            cf[:, CF_PSC + ch] = np.asarray(inp["pool_scale"], f)[0, ch * 128:(ch + 1) * 128]
            for t in range(16):
                pos = t + 2048 * half
                cf[:, CF_IC16 + ch * 16 + t] = 1.0 / np.minimum(pos + 1, w)
        cf[:, CF_HV] = float(half)
        cf[:, CF_EPS] = EPS
        cf[:, CF_ONES:CF_ONES + 64] = 1.0
        cf[:, CF_OFFS:CF_OFFS + 64] = (np.arange(64) * EXPERT_CAP).astype(f)[None, :]
        cf[:, CF_BIAS:CF_BIAS + 8] = np.asarray(inp["b_group"], f)[0][None, :]
        cf[:, CF_BIAS + 8:CF_BIAS + 72] = np.asarray(inp["b_router"], f)[0].reshape(64)[None, :]
        cf[:, CF_IDF:CF_IDF + 128] = np.eye(128, dtype=f)
        cf[:, CF_GFFN:CF_GFFN + 1024] = np.asarray(inp["ffn_norm"], f)[0][None, :]
        xo = x[b, half * 2048:(half + 1) * 2048]
        xh = x[b, 0:2048] if half == 1 else np.zeros((2048, 1024), f)
        m = dict(shared)
        m.update(xo=np.ascontiguousarray(xo), xh=np.ascontiguousarray(xh), mem=mem[b], cf=cf)
        in_maps.append(m)
    return in_maps


_NC_CACHE = {}


def kernel(**inputs):
    in_maps = prep_inputs(inputs)
    if "nc" not in _NC_CACHE:
        _NC_CACHE["nc"] = build_program()[0]
    res = run_bass_kernel_spmd(_NC_CACHE["nc"], in_maps, core_ids=list(range(8)))
    outs = [np.asarray(r["out"], dtype=np.float32) for r in res.results]
    return np.stack([np.concatenate([outs[2 * b], outs[2 * b + 1]], axis=0) for b in range(4)], axis=0)


def build_rest(nc, P, M, D, A, CF, CB, PS, psf, psb, pk, out, xg, yg, xmid, stage, dumps, dump_out, add_dump, S):
    QT, KT, VT, UT, QMT = S["QT"], S["KT"], S["VT"], S["UT"], S["QMT"]
    vt_keys, kt_keys, qt_keys = S["vt_keys"], S["kt_keys"], S["qt_keys"]
    IDB = CB[:, CB_ID:CB_ID + 128]
    MASK4 = CB[:, CB_MASK:CB_MASK + 512]
    ONESB = CB[:, CB_ONES:CB_ONES + 128]
    TRI = CB[:, CB_TRI:CB_TRI + 128]
    EPSC = CF[:, CF_EPS:CF_EPS + 1]
    IDF = CF[:, CF_IDF:CF_IDF + 128]
    ONES64 = CF[:, CF_ONES:CF_ONES + 64]
    HV = CF[:, CF_HV:CF_HV + 1]

    WOUT = M.alloc("wout", "wout", [128, 8, 1024], BF16)
    A(POOL, lambda e: e.dma_start(out=WOUT, in_=D["w_out"].rearrange("(k p) c -> p k c", p=128)), w=["wout"], dma=True)

    M.reset("r1")
    MIXT = M.alloc("r1", "mixt", [128, 8, 2048], BF16, keys=["mixt%d" % i for i in range(8)])
    M.reset("r2")
    TA = M.alloc("r2", "ta", [128, 2064], F32)
    TB = M.alloc("r2", "tb", [128, 2064], F32)
    PL = M.alloc("r2", "pl", [128, 2, 2048], BF16, keys=["pl0", "pl1"])
    T16 = M.alloc("r2", "t16", [128, 16], F32)
    IC = CF[:, CF_IC16:CF_IC16 + 32].rearrange("p (c t) -> p c t", c=2)
    N = 2064
    for c in range(2):
        U = UT[:, c, :]
        uk = "ut%d" % c
        A(POOL, lambda e, U=U: e.tensor_tensor(out=TA[:, 1:N], in0=U[:, 1:N], in1=U[:, 0:N - 1], op=ALU.add), r=[uk], w=["ta"])
        if c == 0:
            A(POOL, lambda e: e.tensor_tensor(out=TB[64:128, 3:N], in0=TA[64:128, 3:N], in1=TA[64:128, 1:N - 2], op=ALU.add), r=["ta"], w=["tb"])
        else:
            A(POOL, lambda e: e.tensor_tensor(out=TB[:, 3:N], in0=TA[:, 3:N], in1=TA[:, 1:N - 2], op=ALU.add), r=["ta"], w=["tb"])
            A(DVE, lambda e: e.tensor_tensor(out=TA[:, 7:N], in0=TB[:, 7:N], in1=TB[:, 3:N - 4], op=ALU.add), r=["tb"], w=["ta"])
            A(DVE, lambda e: e.tensor_tensor(out=TB[64:128, 15:N], in0=TA[64:128, 15:N], in1=TA[64:128, 7:N - 8], op=ALU.add), r=["ta"], w=["tb"])
        for (lo, hi, T, tk) in ((0, 64, TA, "ta"), (64, 128, TB, "tb")):
            A(DVE, lambda e, lo=lo, hi=hi, T=T, U=U, c=c: e.scalar_tensor_tensor(
                out=PL[lo:hi, c, :], in0=T[lo:hi, 16:N], scalar=CF[lo:hi, CF_INVW + c:CF_INVW + c + 1], in1=U[lo:hi, 16:N],
                op0=ALU.mult, op1=ALU.subtract), r=[tk, uk, "cf"], w=["pl%d" % c])
            A(DVE, lambda e, lo=lo, hi=hi, T=T, c=c: e.tensor_tensor(out=T16[lo:hi, :], in0=T[lo:hi, 16:32], in1=IC[lo:hi, c, :], op=ALU.mult),
              r=[tk, "cf"], w=["t16"])
        A(DVE, lambda e, U=U, c=c: e.tensor_tensor(out=PL[:, c, 0:16], in0=T16, in1=U[:, 16:32], op=ALU.subtract), r=["t16", uk], w=["pl%d" % c])
        for tg in range(4):
            zb = 2 + tg % 2
            A(PE, lambda e, c=c, tg=tg, zb=zb: e.matmul(psf(zb), lhsT=CB[:, CB_PBD + c * 128:CB_PBD + (c + 1) * 128],
                                                         rhs=PL[:, c, tg * 512:(tg + 1) * 512], start=True, stop=True),
              r=["pl%d" % c, "cb"], w=[pk(zb)])
            A(ACT, lambda e, c=c, tg=tg, zb=zb: e.activation(out=MIXT[:, 4 + c, tg * 512:(tg + 1) * 512], in_=psf(zb), func=AF.Copy,
                                                             scale=CF[:, CF_PSC + c:CF_PSC + c + 1]),
              r=[pk(zb), "cf"], w=["mixt%d" % (4 + c)])

    M.reset("r2")
    M.reset("ut")
    XT = [M.alloc("r2", "xt%d" % i, [128, 1024], F32) for i in range(2)]
    XN = [M.alloc("r2", "xn%d" % i, [128, 1024], BF16) for i in range(2)]
    SQZ = [M.alloc("r2", "sqz%d" % i, [128, 512], BF16) for i in range(2)]
    RS = [M.alloc("r2", "rs%d" % i, [128, 512], F32) for i in range(2)]
    ST4 = [M.alloc("r2", "st4_%d" % i, [128, 4], F32) for i in range(2)]
    MEMT = M.alloc("r2", "memt", [128, 8, 256], BF16, keys=["memt0", "memt1"])
    KMT = M.alloc("r2", "kmt", [128, 2, 256], BF16, keys=["kmt0", "kmt1"])
    VMA = M.alloc("r2", "vma", [128, 2, 4, 65], BF16)
    PTM = [M.alloc("r2", "ptm%d" % i, [128, 512], BF16) for i in range(2)]
    ACM = [M.alloc("r2", "acm%d" % i, [128, 512], F32) for i in range(2)]
    WKV = M.alloc("ut", "wkv", [128, 8, 512], BF16)
    TMPO = [M.alloc("ut", "tmpo%d" % i, [128, 2048], BF16) for i in range(2)]
    S["XT"], S["XN"], S["ST4"], S["SQZ"], S["RS"] = XT, XN, ST4, SQZ, RS
    A(POOL, lambda e: e.dma_start(out=WKV, in_=D["w_kv"].rearrange("(k p) c -> p k c", p=128)), w=["wkv"], dma=True)
    A(DVE, lambda e: e.memset(VMA[:, :, :, 64:65], 1.0), w=["vma"])

    def norm_tile(src_ap, slot, gain_col, dst, dkeys, ti):
        xt, xn, st = XT[slot], XN[slot], ST4[slot]
        kx, kn, ks = "xt%d" % slot, "xn%d" % slot, "st4_%d" % slot
        tp = ti % 2
        A(SP, lambda e: e.dma_start(out=xt, in_=src_ap), w=[kx], dma=True)
        A(ACT, lambda e: e.activation(out=xn, in_=xt, func=AF.Square, accum_out=st[:, 0:1]), r=[kx], w=[kn, ks])
        A(ACT, lambda e: e.activation(out=st[:, 1:2], in_=st[:, 0:1], func=AF.Ln, bias=EPSC, scale=1.0 / 1024), r=[ks, "cf"], w=[ks])
        A(ACT, lambda e: e.activation(out=st[:, 2:3], in_=st[:, 1:2], func=AF.Exp, scale=-0.5), r=[ks], w=[ks])
        A(ACT, lambda e: e.activation(out=xn, in_=xt, func=AF.Copy, scale=st[:, 2:3]), r=[kx, ks], w=[kn])

        def tr(e):
            for k in range(8):
                ins = e.transpose(out=psb(tp)[:, k * 128:(k + 1) * 128], in_=xn[:, k * 128:(k + 1) * 128], identity=IDB)
            return ins
        A(PE, tr, r=[kn, "cb"], w=[pk(tp)])
        g = CF[:, gain_col:gain_col + 8].unsqueeze(2).broadcast_to([128, 8, 128])
        A(DVE, lambda e: e.tensor_tensor(out=dst, in0=psb(tp).rearrange("p (k t) -> p k t", k=8), in1=g, op=ALU.mult),
          r=[pk(tp), "cf"], w=dkeys)

    def head_norm(zbank, zkey, n, gain_col, dst, dkeys, i):
        sq, rs = SQZ[i % 2], RS[i % 2]
        ksq, krs = "sqz%d" % (i % 2), "rs%d" % (i % 2)
        mb = 4 + i % 2
        BD64 = CB[:, CB_BD:CB_BD + 128]
        A(ACT, lambda e: e.activation(out=sq[:, 0:n], in_=psf(zbank)[:, 0:n], func=AF.Square), r=[zkey], w=[ksq])
        A(PE, lambda e: e.matmul(psf(mb)[:, 0:n], lhsT=BD64, rhs=sq[:, 0:n], start=True, stop=True), r=[ksq, "cb"], w=[pk(mb)])
        A(ACT, lambda e: e.activation(out=rs[:, 0:n], in_=psf(mb)[:, 0:n], func=AF.Ln, bias=EPSC, scale=1.0), r=[pk(mb), "cf"], w=[krs])
        A(ACT, lambda e: e.activation(out=rs[:, 0:n], in_=rs[:, 0:n], func=AF.Exp, scale=-0.5), r=[krs], w=[krs])
        A(DVE, lambda e: e.scalar_tensor_tensor(out=dst, in0=psf(zbank)[:, 0:n], scalar=CF[:, gain_col:gain_col + 1], in1=rs[:, 0:n],
                                                 op0=ALU.mult, op1=ALU.mult), r=[zkey, krs, "cf"], w=dkeys)

    for mt in range(2):
        norm_tile(D["mem"][mt * 128:(mt + 1) * 128, :], mt, CF_GMEM, MEMT[:, :, mt * 128:(mt + 1) * 128], ["memt%d" % mt], mt)
    mkeys = ["memt0", "memt1"]
    for c in range(2):
        zb = 2 + c

        def kproj(e, c=c, zb=zb):
            for k in range(8):
                ins = e.matmul(psf(zb)[:, 0:256], lhsT=WKV[:, k, c * 128:(c + 1) * 128], rhs=MEMT[:, k, :], start=(k == 0), stop=(k == 7))
            return ins
        A(PE, kproj, r=["wkv"] + mkeys, w=[pk(zb)])
        head_norm(zb, pk(zb), 256, CF_GMK, KMT[:, c, :], ["kmt%d" % c], c)
    for mt in range(2):
        zb = 2 + mt

        def vproj(e, mt=mt, zb=zb):
            for k in range(8):
                ins = e.matmul(psf(zb)[:, 0:256], lhsT=MEMT[:, k, mt * 128:(mt + 1) * 128], rhs=WKV[:, k, 256:512], start=(k == 0), stop=(k == 7))
            return ins
        A(PE, vproj, r=["wkv", "memt%d" % mt], w=[pk(zb)])
        A(ACT, lambda e, mt=mt, zb=zb: e.activation(out=VMA[:, mt, :, 0:64], in_=psf(zb)[:, 0:256].rearrange("p (h d) -> p h d", h=4), func=AF.Copy),
          r=[pk(zb)], w=["vma"])

    def normalize_out(acc_ap_fn, acc_keys, tmpo, tkey, bcb):
        for tg in range(4):
            a = acc_ap_fn(tg)
            A(DVE, lambda e, a=a: e.reciprocal(out=a[64:65, :], in_=a[64:65, :]), r=acc_keys, w=acc_keys)
            A(PE, lambda e, a=a: e.matmul(psf(bcb)[0:64, :], lhsT=ONES64[64:65, :], rhs=a[64:65, :], start=True, stop=True), r=acc_keys + ["cf"], w=[pk(bcb)])
            A(DVE, lambda e, a=a, tg=tg: e.tensor_tensor(out=tmpo[0:64, tg * 512:(tg + 1) * 512], in0=a[0:64, :], in1=psf(bcb)[0:64, :], op=ALU.mult),
              r=acc_keys + [pk(bcb)], w=[tkey])

    its = [(mh, tg) for mh in range(4) for tg in range(4)]

    def MF1(it):
        mh, tg = its[it]
        c, b0 = mh // 2, 64 * (mh % 2)
        sbs = (2, 3) if it % 2 == 0 else (6, 7)
        for mt in range(2):
            sb = sbs[mt]
            ptm, pkey = PTM[mt], "ptm%d" % mt
            A(PE, lambda e, mt=mt, sb=sb: e.matmul(
                psf(sb), lhsT=KMT[b0:b0 + 64, c, mt * 128:(mt + 1) * 128], rhs=QMT[b0:b0 + 64, c, tg * 512:(tg + 1) * 512], start=True, stop=True),
              r=["kmt%d" % c, "qmt%d" % c], w=[pk(sb)])
            A(ACT, lambda e, sb=sb, ptm=ptm: e.activation(out=ptm, in_=psf(sb), func=AF.Exp, scale=0.125), r=[pk(sb)], w=[pkey])

    def MF2(it):
        mh, tg = its[it]
        ob = it % 2
        for mt in range(2):
            ptm, pkey = PTM[mt], "ptm%d" % mt
            A(PE, lambda e, mt=mt, ptm=ptm: e.matmul(psf(ob)[0:65, :], lhsT=VMA[:, mt, mh, :], rhs=ptm, start=(mt == 0), stop=(mt == 1)),
              r=["vma", pkey], w=[pk(ob)])
        acm, akey = ACM[it % 2], "acm%d" % (it % 2)
        A(ACT, lambda e: e.activation(out=acm[0:65, :], in_=psf(ob)[0:65, :], func=AF.Copy), r=[pk(ob)], w=[akey])

    def MB(it):
        mh, tg = its[it]
        c, b0 = mh // 2, 64 * (mh % 2)
        tmpo, tkey = TMPO[mh % 2], "tmpo%d" % (mh % 2)
        acm, akey = ACM[it % 2], "acm%d" % (it % 2)
        bcb = 4 + it % 2
        A(ACT, lambda e: e.activation(out=acm[64:65, :], in_=acm[64:65, :], func=AF.Ln), r=[akey], w=[akey])
        A(ACT, lambda e: e.activation(out=acm[64:65, :], in_=acm[64:65, :], func=AF.Exp, scale=-1.0), r=[akey], w=[akey])
        A(PE, lambda e: e.matmul(psf(bcb)[0:64, :], lhsT=ONES64[64:65, :], rhs=acm[64:65, :], start=True, stop=True), r=[akey, "cf"], w=[pk(bcb)])
        A(DVE, lambda e: e.tensor_tensor(out=tmpo[0:64, tg * 512:(tg + 1) * 512], in0=acm[0:64, :], in1=psf(bcb)[0:64, :], op=ALU.mult),
          r=[akey, pk(bcb)], w=[tkey])
        if tg == 3:
            A(SP, lambda e: e.dma_start(out=MIXT[b0:b0 + 64, 6 + c, :], in_=tmpo[0:64, :]), r=[tkey], w=["mixt%d" % (6 + c)], dma=True)

    PTM = [M.alloc("r2", "ptm%d" % i, [128, 512], BF16) for i in range(2)] if False else PTM
    MF1(0)
    for it in range(16):
        MF2(it)
        if it + 1 < 16:
            MF1(it + 1)
        if it >= 1:
            MB(it - 1)
    MB(15)

    if "mix" in dumps and stage == 2:
        add_dump("mixt", MIXT, ["mixt%d" % i for i in range(8)], [128, 8, 2048], BF16)
    if stage <= 2:
        P.emit(nc)
        return nc, dump_out
    return build_attn(nc, P, M, D, A, CF, CB, PS, psf, psb, pk, out, xg, yg, xmid, stage, dumps, dump_out, add_dump, S, MIXT, WOUT)


def build_attn(nc, P, M, D, A, CF, CB, PS, psf, psb, pk, out, xg, yg, xmid, stage, dumps, dump_out, add_dump, S, MIXT, WOUT):
    QT, KT, VT = S["QT"], S["KT"], S["VT"]
    vt_keys, kt_keys, qt_keys = S["vt_keys"], S["kt_keys"], S["qt_keys"]
    IDB = CB[:, CB_ID:CB_ID + 128]
    MASK4 = CB[:, CB_MASK:CB_MASK + 512]
    ONES64 = CF[:, CF_ONES:CF_ONES + 64]
    HV = CF[:, CF_HV:CF_HV + 1]
    M.reset("r2"); M.reset("ut"); M.reset("qmt")
    import os as _os
    DILS = tuple(int(v) for v in _os.environ.get('ATTN_DILS', '1,4,16').split(','))
    NHP = int(_os.environ.get('ATTN_HP', '4'))
    LVL = int(_os.environ.get('ATTN_LEVEL', '9'))
    VA = [M.alloc("r2" if p < 3 else "ut", "va%d" % p, [128, 32, 2, 65], BF16) for p in range(3)]
    PT = [M.alloc("r2", "pt%d" % i, [128, 512], BF16) for i in range(4)]
    ACC = M.alloc("ut", "acc", [128, 2, 2048], F32, keys=["acc_q%d" % i for i in range(4)])
    TMPO = [M.alloc("qmt", "tmpo%d" % i, [128, 2048], BF16) for i in range(2)]
    for p, d in enumerate(DILS):
        nb = 32 // d
        A(DVE, lambda e, p=p: e.memset(VA[p][:, :, :, 64:65], 1.0), w=["va%d" % p])
        v = VA[p][:, :, :, 64:65].rearrange("p (r b) h o -> p r b (h o)", r=d)[:, :, 0:nb // 2, :]
        A(DVE, lambda e, v=v: e.tensor_scalar(out=v, in0=v, scalar1=HV, scalar2=None, op0=ALU.mult), r=["cf"], w=["va%d" % p])

    for i in range(16):
        A(SP, lambda e, i=i: e.dma_start(out=xg[i * 512:(i + 1) * 512, :].rearrange("(p r) d -> p r d", p=128), in_=S["ZT4"]),
          r=["zt"], w=["xg"], dma=True)

    units = [(hp, p, d) for hp in range(NHP) for p, d in enumerate(DILS)]
    NU = len(units)
    akeys = ["acc_q%d" % i for i in range(4)]

    def vprep_group(u, grp):
        hp, p, d = units[u]
        c, nb, va, vak = hp, 32 // d, VA[p], "va%d" % p

        def vtr(e):
            for j in range(8):
                kb = grp * 8 + j
                r, b = kb // nb, kb % nb
                s0 = d * 128 * b + r
                ins = e.transpose(out=psb(6)[:, j * 128:(j + 1) * 128], in_=VT[:, c, s0:s0 + 127 * d + 1:d], identity=IDB)
            return ins
        A(PE, vtr, r=vt_keys(c) + ["cb"], w=[pk(6)])
        src = psb(6).rearrange("p (k h d) -> p k h d", k=8, h=2)
        dst = va[:, grp * 8:grp * 8 + 8, :, 0:64]
        if grp % 2 == 0:
            A(ACT, lambda e: e.activation(out=dst, in_=src, func=AF.Copy), r=[pk(6)], w=[vak])
        else:
            A(DVE, lambda e: e.tensor_copy(out=dst, in_=src), r=[pk(6)], w=[vak])

    steps = []
    for u, (hp, p, d) in enumerate(units):
        nb = 32 // d
        for qg in range(4):
            for jp in range(2):
                blk = []
                for jj in range(2):
                    j = 2 * jp + jj
                    if d == 1:
                        r, b = 0, 16 + 4 * qg + j
                    elif d == 4:
                        r, b = j, 4 + qg
                    else:
                        r, b = 4 * qg + j, 1
                    kbc = r * nb + b
                    kc0 = d * 128 * b + r
                    blk.append((j, kbc, kbc - 1, kc0, d * 128 * (b - 1) + r, kc0 - 2048))
                steps.append(dict(u=u, hp=hp, p=p, d=d, qg=qg, jp=jp, blk=blk, n=len(steps)))
    mi = [0]

    def F1(st):
        n, c, d, blk = st["n"], st["hp"], st["d"], st["blk"]
        sset = n % 2
        pts = [(PT[2 * sset + hh], "pt%d" % (2 * sset + hh)) for hh in range(2)]
        st["pts"] = pts

        def qk(e):
            for jj, (j, kbc, kbp, kc0, kp0, q0) in enumerate(blk):
                for hh in range(2):
                    for kv, k0 in enumerate((kp0, kc0)):
                        ins = e.matmul(psf(2 * sset + hh)[:, (jj * 2 + kv) * 128:(jj * 2 + kv + 1) * 128],
                                       lhsT=KT[hh * 64:hh * 64 + 64, c, k0:k0 + 127 * d + 1:d],
                                       rhs=QT[hh * 64:hh * 64 + 64, c, q0:q0 + 127 * d + 1:d], start=True, stop=True)
            return ins
        A(PE, qk, r=kt_keys(c) + qt_keys(c), w=[pk(2 * sset), pk(2 * sset + 1)])
        for hh in range(2):
            pt, ptk = pts[hh]
            A(ACT, lambda e, sb=2 * sset + hh, pt=pt: e.activation(out=pt, in_=psf(sb), func=AF.Exp, scale=0.125), r=[pk(2 * sset + hh)], w=[ptk])
            meng = DVE if mi[0] % 2 == 0 else POOL
            mi[0] += 1
            A(meng, lambda e, pt=pt: e.tensor_tensor(out=pt, in0=pt, in1=MASK4, op=ALU.mult), r=[ptk, "cb"], w=[ptk])

    def F2(st):
        n, p, d, blk, qg, jp, pts = st["n"], st["p"], st["d"], st["blk"], st["qg"], st["jp"], st["pts"]
        va, vak = VA[p], "va%d" % p
        ob = 4 + n % 2

        def pv(e):
            for jj, (j, kbc, kbp, kc0, kp0, q0) in enumerate(blk):
                for hh in range(2):
                    pt = pts[hh][0]
                    o = psf(ob)[0:65, (hh * 2 + jj) * 128:(hh * 2 + jj + 1) * 128]
                    e.matmul(o, lhsT=va[:, kbp, hh, :], rhs=pt[:, (jj * 2) * 128:(jj * 2 + 1) * 128], start=True, stop=False)
                    ins = e.matmul(o, lhsT=va[:, kbc, hh, :], rhs=pt[:, (jj * 2 + 1) * 128:(jj * 2 + 2) * 128], start=False, stop=True)
            return ins
        A(PE, pv, r=[vak, pts[0][1], pts[1][1]], w=[pk(ob)])
        if d == 1:
            t0 = (qg * 4 + jp * 2) * 128
            dst = ACC[0:65, :, t0:t0 + 256]
            src = psf(ob)[0:65, :].rearrange("p (h t) -> p h t", h=2)
            A(ACT, lambda e: e.activation(out=dst, in_=src, func=AF.Copy), r=[pk(ob)], w=[akeys[qg]])
        else:
            if d == 4:
                dst = ACC[0:65, :, qg * 512:(qg + 1) * 512].rearrange("p h (i r) -> p h r i", r=4)[:, :, 2 * jp:2 * jp + 2, :]
                ks = [akeys[qg]]
            else:
                dst = ACC[0:65, :, :].rearrange("p h (i r) -> p h r i", r=16)[:, :, 4 * qg + 2 * jp:4 * qg + 2 * jp + 2, :]
                ks = akeys
            src = psf(ob)[0:65, :].rearrange("p (h r i) -> p h r i", h=2, r=2)
            A(DVE, lambda e: e.tensor_tensor(out=dst, in0=src, in1=dst, op=ALU.add), r=[pk(ob)] + ks, w=ks)

    tmi = [0]

    bci = [0]

    def NORM(hp):
        for hh in range(2):
            a = ACC[64:65, hh, :]
            A(ACT, lambda e, a=a: e.activation(out=a, in_=a, func=AF.Ln), r=akeys, w=akeys)
            A(ACT, lambda e, a=a: e.activation(out=a, in_=a, func=AF.Exp, scale=-1.0), r=akeys, w=akeys)
        for hh in range(2):
            tmpo, tkey = TMPO[tmi[0] % 2], "tmpo%d" % (tmi[0] % 2)
            tmi[0] += 1
            for tg in range(4):
                a = ACC[:, hh, tg * 512:(tg + 1) * 512]
                bb = 6 + bci[0] % 2
                bci[0] += 1
                A(PE, lambda e, a=a, bb=bb: e.matmul(psf(bb)[0:64, :], lhsT=ONES64[64:65, :], rhs=a[64:65, :], start=True, stop=True), r=[akeys[tg], "cf"], w=[pk(bb)])
                A(DVE, lambda e, a=a, tg=tg, tmpo=tmpo, bb=bb: e.tensor_tensor(out=tmpo[0:64, tg * 512:(tg + 1) * 512], in0=a[0:64, :], in1=psf(bb)[0:64, :], op=ALU.mult),
                  r=[akeys[tg], pk(bb)], w=[tkey])
            A(SP, lambda e, hh=hh, hp=hp, tmpo=tmpo: e.dma_start(out=MIXT[hh * 64:hh * 64 + 64, hp, :], in_=tmpo[0:64, :]), r=[tkey], w=["mixt%d" % hp], dma=True)

    for grp in range(4):
        vprep_group(0, grp)
    F1(steps[0])
    for n, st in enumerate(steps):
        if n + 1 < len(steps):
            F1(steps[n + 1])
        F2(st)
        sl = n % 8
        if sl in (1, 3, 5, 7) and st["u"] + 1 < NU:
            vprep_group(st["u"] + 1, sl // 2)
        if sl == 7 and st["p"] == len(DILS) - 1:
            NORM(st["hp"])

    if "mix" in dumps and stage == 3:
        add_dump("mixt", MIXT, ["mixt%d" % i for i in range(8)], [128, 8, 2048], BF16)
    if stage <= 3:
        P.emit(nc)
        return nc, dump_out
    return build_ffn(nc, P, M, D, A, CF, CB, PS, psf, psb, pk, out, xg, yg, xmid, stage, dumps, dump_out, add_dump, S, MIXT, WOUT)


def build_ffn(nc, P, M, D, A, CF, CB, PS, psf, psb, pk, out, xg, yg, xmid, stage, dumps, dump_out, add_dump, S, MIXT, WOUT):
    IDB = CB[:, CB_ID:CB_ID + 128]
    ONESB = CB[:, CB_ONES:CB_ONES + 128]
    TRI = CB[:, CB_TRI:CB_TRI + 128]
    EPSC = CF[:, CF_EPS:CF_EPS + 1]
    IDF = CF[:, CF_IDF:CF_IDF + 128]
    GFFN = CF[:, CF_GFFN:CF_GFFN + 1024]
    BIASR = CF[:, CF_BIAS:CF_BIAS + 72]
    OFFS = CF[:, CF_OFFS:CF_OFFS + 64]
    M.reset("kt"); M.reset("qt"); M.reset("r2")
    XT = [M.alloc("kt", "xt%d" % i, [128, 1024], F32) for i in range(2)]
    XM = [M.alloc("kt", "xm%d" % i, [128, 1024], F32, keys=["xm%d_0" % i, "xm%d_1" % i]) for i in range(2)]
    H2 = [M.alloc("kt", "h2_%d" % i, [128, 1024], F32) for i in range(2)]
    H2T = [M.alloc("kt", "h2t%d" % i, [128, 8, 128], F32, keys=["h2t%d_0" % i, "h2t%d_1" % i]) for i in range(2)]
    NHB = 12
    H2B = [M.alloc("r2", "h2b%d" % i, [128, 1024], BF16) for i in range(NHB)]
    WR = M.alloc("qt", "wr", [128, 8, 72], F32)
    AB = M.alloc("qt", "ab", [128, 16, 64], BF16, keys=["ab%d" % i for i in range(4)])
    G12 = M.alloc("qt", "g12", [128, 2, 16], F32)
    SMS = M.alloc("qt", "sms", [128, 2, 4], F32, keys=["sms0", "sms1"])
    NT = 4
    L4 = [M.alloc("r2", "l4_%d" % i, [128, NT, 72], F32, keys=["l4_%d_%d" % (i, j) for j in range(NT)]) for i in range(2)]
    RT = M.alloc("r2", "rt", [128, 1400], F32)
    DEST = [[nc.alloc_sbuf_tensor("dest%d_%d" % (a, tt), [128, 1], I32) for tt in range(16)] for a in range(2)]
    A(SP, lambda e: e.dma_start(out=WR, in_=D["w_r"].rearrange("(k p) c -> p k c", p=128)), w=["wr"], dma=True)

    _o = [0]

    def rt(n, shape=None):
        ap = RT[:, _o[0]:_o[0] + n]
        _o[0] += n
        if shape:
            ap = ap.rearrange("p (a b) -> p a b", a=shape[0]) if len(shape) == 2 else ap.rearrange("p (a b c) -> p a b c", a=shape[0], b=shape[1])
        return ap
    OHG, D8, E8, LSEL, OH1, L2, OH2 = [rt(NT * 8, (NT, 8)) for _ in range(7)]
    T64, A1F, A2F, RB = [rt(NT * 64, (NT, 8, 8)) for _ in range(4)]
    SM = rt(16 * NT, (16, NT))
    RK = "rt"

    def S1(tt):
        sl = tt % 2
        xt, xm, h2, h2b = XT[sl], XM[sl], H2[sl], H2B[tt % NHB]
        kx, km, kh, kb = "xt%d" % sl, "xm%d" % sl, "h2_%d" % sl, "h2b%d" % (tt % NHB)
        sm, ksm = SMS[:, sl, :], "sms%d" % sl
        tok = slice(tt * 128, (tt + 1) * 128)
        ob = (0, 1) if sl == 0 else (4, 5)
        A(SP, lambda e: e.dma_start(out=xt, in_=D["xo"][tok, :]), w=[kx], dma=True)
        for half in range(2):
            def oproj(e, half=half):
                for k in range(8):
                    e.matmul(psf(ob[half]), lhsT=MIXT[:, k, tok], rhs=WOUT[:, k, half * 512:(half + 1) * 512], start=(k == 0), stop=False)
                return e.matmul(psf(ob[half]), lhsT=IDF, rhs=xt[:, half * 512:(half + 1) * 512], start=False, stop=True)
            A(PE, oproj, r=["wout", kx, "cf"] + ["mixt%d" % i for i in range(8)], w=[pk(ob[half])])
            A(ACT, lambda e, half=half: e.activation(out=xm[:, half * 512:(half + 1) * 512], in_=psf(ob[half]), func=AF.Copy), r=[pk(ob[half])], w=[km + "_%d" % half])
        kms = [km + "_0", km + "_1"]
        A(SP, lambda e: e.dma_start(out=xmid[tok, :], in_=xm), r=kms, w=["xmid%d" % tt], dma=True)
        A(ACT, lambda e: e.activation(out=h2, in_=xm, func=AF.Square, accum_out=sm[:, 0:1]), r=kms, w=[kh, ksm])
        A(ACT, lambda e: e.activation(out=sm[:, 1:2], in_=sm[:, 0:1], func=AF.Ln, bias=EPSC, scale=1.0 / 1024), r=[ksm, "cf"], w=[ksm])
        A(ACT, lambda e: e.activation(out=sm[:, 2:3], in_=sm[:, 1:2], func=AF.Exp, scale=-0.5), r=[ksm], w=[ksm])

    def S1c(tt):
        sl = tt % 2
        xm, h2, h2b = XM[sl], H2[sl], H2B[tt % NHB]
        km, kh, kb = "xm%d" % sl, "h2_%d" % sl, "h2b%d" % (tt % NHB)
        sm, ksm = SMS[:, sl, :], "sms%d" % sl
        kms = [km + "_0", km + "_1"]
        A(DVE, lambda e: e.scalar_tensor_tensor(out=h2, in0=xm, scalar=sm[:, 2:3], in1=GFFN, op0=ALU.mult, op1=ALU.mult), r=kms + [ksm, "cf"], w=[kh])
        A(ACT, lambda e: e.activation(out=h2b, in_=h2, func=AF.Copy), r=[kh], w=[kb])

    def S1b(tt):
        sl = tt % 2
        h2, h2t, kh = H2[sl], H2T[sl], "h2_%d" % sl
        for hb in range(2):
            def tr(e, hb=hb):
                for k in range(4):
                    kk = hb * 4 + k
                    ins = e.transpose(out=psf(2 + hb)[:, k * 128:(k + 1) * 128], in_=h2[:, kk * 128:(kk + 1) * 128], identity=IDF)
                return ins
            A(PE, tr, r=[kh, "cf"], w=[pk(2 + hb)])
            A(ACT, lambda e, hb=hb: e.activation(out=h2t[:, 4 * hb:4 * hb + 4, :], in_=psf(2 + hb).rearrange("p (k t) -> p k t", k=4), func=AF.Copy),
              r=[pk(2 + hb)], w=["h2t%d_%d" % (sl, hb)])

        def rmm(e):
            for k in range(8):
                ins = e.matmul(psf(6)[:, 0:72], lhsT=h2t[:, k, :], rhs=WR[:, k, :], start=(k == 0), stop=(k == 7))
            return ins
        A(PE, rmm, r=["h2t%d_0" % sl, "h2t%d_1" % sl, "wr"], w=[pk(6)])
        b, i = tt // NT, tt % NT
        A(DVE, lambda e: e.tensor_tensor(out=L4[b % 2][:, i, :], in0=psf(6)[:, 0:72], in1=BIASR, op=ALU.add), r=[pk(6), "cf"], w=["l4_%d_%d" % (b % 2, i)])

    V = lambda fn, r=(), w=(): A(DVE, fn, r=list(r) + [RK], w=list(w) + [RK])
    bc3 = lambda ap: ap.unsqueeze(2).broadcast_to([128, NT, 8])
    bcE = lambda ap: ap.unsqueeze(3).broadcast_to([128, NT, 8, 8])
    bcG = lambda ap: ap.unsqueeze(2).broadcast_to([128, NT, 8, 8])
    R_ = lambda i: SM[:, i, :]

    def S2B(b):
        l4 = L4[b % 2]
        lk = ["l4_%d_%d" % (b % 2, j) for j in range(NT)]
        LG_ = l4[:, :, 0:8]
        LE_ = l4[:, :, 8:72].rearrange("p t (g x) -> p t g x", g=8)
        V(lambda e: e.tensor_reduce(out=R_(0), in_=LG_, axis=AX.X, op=ALU.max), r=lk)
        V(lambda e: e.tensor_tensor(out=OHG, in0=LG_, in1=bc3(R_(0)), op=ALU.is_equal), r=lk)
        V(lambda e: e.tensor_tensor(out=D8, in0=LG_, in1=bc3(R_(0)), op=ALU.subtract), r=lk)
        A(ACT, lambda e: e.activation(out=E8, in_=D8, func=AF.Exp), r=[RK], w=[RK])
        V(lambda e: e.tensor_reduce(out=R_(1), in_=E8, axis=AX.X, op=ALU.add))
        V(lambda e: e.reciprocal(out=R_(2), in_=R_(1)))
        V(lambda e: e.tensor_tensor(out=T64, in0=LE_, in1=bcE(OHG), op=ALU.mult), r=lk)
        V(lambda e: e.tensor_reduce(out=LSEL, in_=T64.rearrange("p t g x -> p t x g"), axis=AX.X, op=ALU.add))
        V(lambda e: e.tensor_reduce(out=R_(3), in_=LSEL, axis=AX.X, op=ALU.max))
        V(lambda e: e.tensor_tensor(out=OH1, in0=LSEL, in1=bc3(R_(3)), op=ALU.is_equal))
        V(lambda e: e.scalar_tensor_tensor(out=L2, in0=OH1, scalar=-1e30, in1=LSEL, op0=ALU.mult, op1=ALU.add))
        V(lambda e: e.tensor_reduce(out=R_(4), in_=L2, axis=AX.X, op=ALU.max))
        V(lambda e: e.tensor_tensor(out=OH2, in0=L2, in1=bc3(R_(4)), op=ALU.is_equal))
        V(lambda e: e.tensor_tensor(out=R_(5), in0=R_(4), in1=R_(3), op=ALU.subtract))
        A(ACT, lambda e: e.activation(out=R_(6), in_=R_(5), func=AF.Exp), r=[RK], w=[RK])
        V(lambda e: e.tensor_tensor(out=A1F, in0=bcE(OHG), in1=bcG(OH1), op=ALU.mult))
        V(lambda e: e.tensor_tensor(out=A2F, in0=bcE(OHG), in1=bcG(OH2), op=ALU.mult))
        V(lambda e: e.tensor_tensor(out=AB[:, b * NT:(b + 1) * NT, :].rearrange("p t (g x) -> p t g x", g=8), in0=A1F, in1=A2F, op=ALU.add), w=["ab%d" % b])
        V(lambda e: e.tensor_scalar(out=R_(7), in0=R_(6), scalar1=1.0, scalar2=None, op0=ALU.add))
        V(lambda e: e.reciprocal(out=R_(8), in_=R_(7)))
        V(lambda e: e.tensor_tensor(out=G12[:, 0, b * NT:(b + 1) * NT], in0=R_(2), in1=R_(8), op=ALU.mult), w=["g12"])
        V(lambda e: e.tensor_tensor(out=G12[:, 1, b * NT:(b + 1) * NT], in0=R_(2), in1=G12[:, 0, b * NT:(b + 1) * NT], op=ALU.subtract), r=["g12"], w=["g12"])

    def S3B(b):
        def rank(e):
            for i in range(NT):
                tt = b * NT + i
                o = psf(7)[:, i * 64:(i + 1) * 64]
                for t2 in range(tt):
                    e.matmul(o, lhsT=ONESB, rhs=AB[:, t2, :], start=(t2 == 0), stop=False)
                ins = e.matmul(o, lhsT=TRI, rhs=AB[:, tt, :], start=(tt == 0), stop=True)
            return ins
        A(PE, rank, r=["ab%d" % i for i in range(b + 1)] + ["cb"], w=[pk(7)])
        V(lambda e: e.scalar_tensor_tensor(out=RB.rearrange("p t g x -> p t (g x)"), in0=psf(7)[:, 0:NT * 64].rearrange("p (t x) -> p t x", t=NT), scalar=float(EXPERT_CAP - 1),
                                           in1=OFFS.unsqueeze(1).broadcast_to([128, NT, 64]), op0=ALU.min, op1=ALU.add), r=[pk(7), "cf"])
        for a, AF_ in enumerate((A1F, A2F)):
            V(lambda e, AF_=AF_: e.tensor_tensor(out=T64, in0=AF_, in1=RB, op=ALU.mult))
            V(lambda e, a=a: e.tensor_reduce(out=R_(9 + a), in_=T64.rearrange("p t g x -> p t (g x)"), axis=AX.X, op=ALU.add))
            for i in range(NT):
                tt = b * NT + i
                dk = "dest%d_%d" % (a, tt)
                V(lambda e, a=a, i=i, tt=tt: e.tensor_copy(out=DEST[a][tt][:, :], in_=R_(9 + a)[:, i:i + 1]), w=[dk])
        for i in range(NT):
            tt = b * NT + i
            h2b, kb = H2B[tt % NHB], "h2b%d" % (tt % NHB)
            for a in range(2):
                dk = "dest%d_%d" % (a, tt)
                A(POOL, lambda e, a=a, tt=tt, h2b=h2b: e.indirect_dma_start(
                    out=xg, out_offset=bass.IndirectOffsetOnAxis(ap=DEST[a][tt][:, 0:1], axis=0), in_=h2b, in_offset=None,
                    bounds_check=None, oob_is_err=False), r=[dk, kb, "xg"], w=["xgs%d_%d" % (a, tt)], dma=True)

    S1(0)
    S1c(0)
    S1(1)
    S1b(0)
    S1c(1)
    for tt in range(16):
        if tt + 2 < 16:
            S1(tt + 2)
        if tt + 1 < 16:
            S1b(tt + 1)
        if tt + 2 < 16:
            S1c(tt + 2)
        if tt % NT == 1 and tt >= NT:
            S2B(tt // NT - 1)
        if tt % NT == 2 and tt >= NT:
            S3B(tt // NT - 1)
    S2B(3)
    S3B(3)

    if "route" in dumps:
        add_dump("g12", G12, ["g12"], [128, 2, 16], F32)
        for a in range(2):
            for tt in (0, 15):
                add_dump("dest%d_%d" % (a, tt), DEST[a][tt][:, :], ["dest%d_%d" % (a, tt)], [128, 1], I32)
        add_dump("xm", XM[1], ["xm1_0", "xm1_1"], [128, 1024], F32)
    if stage <= 4:
        P.emit(nc)
        return nc, dump_out

    M.reset("vt"); M.reset("r2"); M.reset("ut"); M.reset("qmt"); M.reset("r1")
    NW = 3
    W1 = [M.alloc("vt" if i == 0 else "r2", "w1_%d" % i, [128, 8, 512], BF16) for i in range(NW)]
    W3 = [M.alloc("vt" if i == 0 else "r2", "w3_%d" % i, [128, 8, 512], BF16) for i in range(NW)]
    W2 = [M.alloc("vt" if i == 0 else "ut", "w2_%d" % i, [128, 4, 1024], BF16) for i in range(NW)]
    XE = [M.alloc("vt", "xe%d" % i, [128, 1024], BF16) for i in range(2)]
    XET = [M.alloc("vt", "xet%d" % i, [128, 8, 128], BF16) for i in range(2)]
    SA = M.alloc("qmt", "sa", [128, 512], F32)
    HM = M.alloc("qmt", "hm", [128, 512], BF16)
    HMT = M.alloc("qmt", "hmt", [128, 4, 128], BF16)
    YE = [M.alloc("r1", "ye%d" % i, [128, 1024], F32) for i in range(2)]

    def wload(e_):
        i = e_ % NW
        A(POOL, lambda e, i=i, e_=e_: e.dma_start(out=W1[i], in_=D["w1"][e_].rearrange("(k p) f -> p k f", p=128)), w=["w1_%d" % i], dma=True)
        A(POOL, lambda e, i=i, e_=e_: e.dma_start(out=W3[i], in_=D["w3"][e_].rearrange("(k p) f -> p k f", p=128)), w=["w3_%d" % i], dma=True)
        A(POOL, lambda e, i=i, e_=e_: e.dma_start(out=W2[i], in_=D["w2"][e_].rearrange("(k p) f -> p k f", p=128)), w=["w2_%d" % i], dma=True)

    NEXP = 64
    for e_ in range(min(NW - 1, NEXP)):
        wload(e_)
    for e_ in range(NEXP):
        if e_ + NW - 1 < NEXP:
            wload(e_ + NW - 1)
        i = e_ % NW
        s2 = e_ % 2
        xe, xet, ye = XE[s2], XET[s2], YE[s2]
        kxe, kxt, kye = "xe%d" % s2, "xet%d" % s2, "ye%d" % s2
        tb = 0 if s2 == 0 else 6
        ab = 1 if s2 == 0 else 7
        A(SP, lambda e, xe=xe, e_=e_: e.dma_start(out=xe, in_=xg[e_ * 128:(e_ + 1) * 128, :]), r=["xg"] + ["xgs%d_%d" % (a, t) for a in range(2) for t in range(16)], w=[kxe], dma=True)

        def trx(e, xe=xe, tb=tb):
            for k in range(8):
                ins = e.transpose(out=psb(tb)[:, k * 128:(k + 1) * 128], in_=xe[:, k * 128:(k + 1) * 128], identity=IDB)
            return ins
        A(PE, trx, r=[kxe, "cb"], w=[pk(tb)])
        A(ACT, lambda e, xet=xet, tb=tb: e.activation(out=xet, in_=psb(tb).rearrange("p (k t) -> p k t", k=8), func=AF.Copy), r=[pk(tb)], w=[kxt])

        def up(e, W, bank, xet=xet):
            for k in range(8):
                ins = e.matmul(psf(bank), lhsT=xet[:, k, :], rhs=W[:, k, :], start=(k == 0), stop=(k == 7))
            return ins
        A(PE, lambda e, i=i, ab=ab, up=up: up(e, W1[i], ab), r=[kxt, "w1_%d" % i], w=[pk(ab)])
        A(PE, lambda e, i=i, up=up: up(e, W3[i], 2), r=[kxt, "w3_%d" % i], w=[pk(2)])
        A(ACT, lambda e, ab=ab: e.activation(out=SA, in_=psf(ab), func=AF.Silu), r=[pk(ab)], w=["sa"])
        A(DVE, lambda e: e.tensor_tensor(out=HM, in0=SA, in1=psf(2), op=ALU.mult), r=["sa", pk(2)], w=["hm"])

        def trh(e):
            for k in range(4):
                ins = e.transpose(out=psb(3)[:, k * 128:(k + 1) * 128], in_=HM[:, k * 128:(k + 1) * 128], identity=IDB)
            return ins
        A(PE, trh, r=["hm", "cb"], w=[pk(3)])
        A(DVE, lambda e: e.tensor_copy(out=HMT, in_=psb(3)[:, 0:512].rearrange("p (k t) -> p k t", k=4)), r=[pk(3)], w=["hmt"])
        for half in range(2):
            def dn(e, i=i, half=half):
                for k in range(4):
                    ins = e.matmul(psf(4 + half), lhsT=HMT[:, k, :], rhs=W2[i][:, k, half * 512:(half + 1) * 512], start=(k == 0), stop=(k == 3))
                return ins
            A(PE, dn, r=["hmt", "w2_%d" % i], w=[pk(4 + half)])
        A(ACT, lambda e, ye=ye: e.activation(out=ye[:, 0:512], in_=psf(4), func=AF.Copy), r=[pk(4)], w=[kye + "a"])
        A(DVE, lambda e, ye=ye: e.tensor_copy(out=ye[:, 512:1024], in_=psf(5)), r=[pk(5)], w=[kye + "b"])
        A(SP, lambda e, ye=ye, e_=e_: e.dma_start(out=yg[e_ * 128:(e_ + 1) * 128, :], in_=ye), r=[kye + "a", kye + "b"], w=["yg%d" % e_], dma=True)

    if stage <= 5:
        P.emit(nc)
        return nc, dump_out

    M.reset("kt")
    Y1 = [M.alloc("kt", "y1_%d" % i, [128, 1024], F32) for i in range(2)]
    Y2 = [M.alloc("kt", "y2_%d" % i, [128, 1024], F32) for i in range(2)]
    XM2 = [M.alloc("kt", "xm2_%d" % i, [128, 1024], F32) for i in range(2)]
    OT_ = [M.alloc("kt", "ot_%d" % i, [128, 1024], F32) for i in range(2)]
    for tt in range(16):
        sl = tt % 2
        tok = slice(tt * 128, (tt + 1) * 128)
        for a, Y in enumerate((Y1, Y2)):
            A(POOL, lambda e, a=a, tt=tt, y=Y[sl]: e.indirect_dma_start(
                out=y, out_offset=None, in_=yg, in_offset=bass.IndirectOffsetOnAxis(ap=DEST[a][tt][:, 0:1], axis=0),
                bounds_check=None, oob_is_err=False), r=["dest%d_%d" % (a, tt)] + ["yg%d" % i for i in range(64)], w=["y%d_%d" % (a + 1, sl)], dma=True)
        A(SP, lambda e, sl=sl, tok=tok: e.dma_start(out=XM2[sl], in_=xmid[tok, :]), r=["xmid%d" % tt], w=["xm2_%d" % sl], dma=True)
        A(DVE, lambda e, sl=sl, tt=tt: e.scalar_tensor_tensor(out=OT_[sl], in0=Y1[sl], scalar=G12[:, 0, tt:tt + 1], in1=XM2[sl], op0=ALU.mult, op1=ALU.add),
          r=["y1_%d" % sl, "xm2_%d" % sl, "g12"], w=["ot_%d" % sl])
        A(DVE, lambda e, sl=sl, tt=tt: e.scalar_tensor_tensor(out=OT_[sl], in0=Y2[sl], scalar=G12[:, 1, tt:tt + 1], in1=OT_[sl], op0=ALU.mult, op1=ALU.add),
          r=["y2_%d" % sl, "ot_%d" % sl, "g12"], w=["ot_%d" % sl])
        A(SP, lambda e, sl=sl, tok=tok: e.dma_start(out=out[tok, :], in_=OT_[sl]), r=["ot_%d" % sl], w=["out"], dma=True)
    if "moe" in dumps:
        allxg = ["xg"] + ["xgs%d_%d" % (a, t) for a in range(2) for t in range(16)]
        for ee in (0, 37):
            add_dump("xg%d" % ee, xg[ee * 128:(ee + 1) * 128, :], allxg, [128, 1024], BF16)
            add_dump("yg%d" % ee, yg[ee * 128:(ee + 1) * 128, :], ["yg%d" % ee], [128, 1024], F32)
        add_dump("g12", G12, ["g12"], [128, 2, 16], F32)
        for a in range(2):
            for tt in range(16):
                add_dump("dest%d_%d" % (a, tt), DEST[a][tt][:, :], ["dest%d_%d" % (a, tt)], [128, 1], I32)
        add_dump("xmid", xmid, ["xmid%d" % t for t in range(16)], [2048, 1024], F32)
    P.emit(nc)
    return nc, dump_out
```

```python
import numpy as np
import concourse.bass as bass
import concourse.mybir as mybir
from concourse.bass_utils import run_bass_kernel_spmd

F32 = mybir.dt.float32
BF16 = mybir.dt.bfloat16
I32 = mybir.dt.int32
AF = mybir.ActivationFunctionType
ALU = mybir.AluOpType
AX = mybir.AxisListType

PE, ACT, DVE, POOL, SP = "pe", "act", "dve", "pool", "sp"
ENGS = (PE, ACT, DVE, POOL, SP)
RING = 12


class Op:
    __slots__ = ("eng", "fn", "deps", "dma", "need", "seq", "di", "name")

    def __init__(self, eng, fn, dma, name):
        self.eng, self.fn, self.dma, self.name = eng, fn, dma, name
        self.deps, self.need, self.seq, self.di = [], False, 0, -1


class Prog:
    def __init__(self):
        self.ops = {e: [] for e in ENGS}
        self.lastw = {}
        self.readers = {}

    def add(self, eng, fn, r=(), w=(), dma=False, name=""):
        op = Op(eng, fn, dma, name)
        deps = {}
        for k in r:
            lw = self.lastw.get(k)
            if lw is not None:
                deps[id(lw)] = (lw, True)
        for k in w:
            lw = self.lastw.get(k)
            if lw is not None:
                deps[id(lw)] = (lw, True)
            for rd in self.readers.get(k, ()):
                if id(rd) not in deps:
                    deps[id(rd)] = (rd, False)
        for k in r:
            self.readers.setdefault(k, []).append(op)
        for k in w:
            self.lastw[k] = op
            self.readers[k] = []
        op.deps = [d for d in deps.values() if d[0] is not op]
        self.ops[eng].append(op)
        return op

    @staticmethod
    def _needs_wait(o, d, true_dep):
        if d.dma:
            return True
        if d.eng != o.eng:
            return True
        if o.eng == PE:
            return False
        if o.dma:
            return True
        return true_dep

    def emit(self, nc):
        for e in ENGS:
            for o in self.ops[e]:
                for d, t in o.deps:
                    if self._needs_wait(o, d, t):
                        d.need = True
        for e in ENGS:
            c = 0
            n = 0
            for o in self.ops[e]:
                if o.dma:
                    o.di = n
                    n += 1
                elif o.need:
                    c += 1
                    o.seq = c
        from contextlib import ExitStack

        with ExitStack() as st:
            esem = {e: st.enter_context(nc.semaphore("c_" + e)) for e in (PE, ACT, DVE, POOL)}
            rsem = {
                e: [st.enter_context(nc.semaphore("r_%s%d" % (e, i))) for i in range(RING)]
                for e in (SP, POOL, ACT)
            }
            block = st.enter_context(nc.Block())

            def run(ename, eng):
                seen = {}

                def wait(sem, val):
                    if seen.get(sem.num, 0) < val:
                        eng.wait_ge(sem, val)
                        seen[sem.num] = val

                ndma = 0
                for o in self.ops[ename]:
                    for d, t in o.deps:
                        if not self._needs_wait(o, d, t):
                            continue
                        if d.dma:
                            wait(rsem[d.eng][d.di % RING], 16 * (d.di // RING + 1))
                        else:
                            wait(esem[d.eng], d.seq)
                    if o.dma:
                        if o.di >= RING:
                            wait(rsem[ename][o.di % RING], 16 * (o.di // RING))
                        ins = o.fn(eng)
                        ins.then_inc(rsem[ename][o.di % RING], 16)
                        ndma = o.di + 1
                    else:
                        ins = o.fn(eng)
                        if o.need:
                            ins.then_inc(esem[ename], 1)
                for i in range(max(0, ndma - RING), ndma):
                    wait(rsem[ename][i % RING], 16 * (i // RING + 1))

            block.tensor(lambda eng: run(PE, eng))
            block.scalar(lambda eng: run(ACT, eng))
            block.vector(lambda eng: run(DVE, eng))
            block.gpsimd(lambda eng: run(POOL, eng))
            block.sync(lambda eng: run(SP, eng))


class Mem:
    def __init__(self, nc, P, total_bytes):
        self.P = P
        self.t = nc.alloc_sbuf_tensor("arena", [128, total_bytes // 4], F32)
        self.regs = {}
        self.top = 0
        self.total = total_bytes

    def region(self, name, size):
        size = (size + 63) // 64 * 64
        assert self.top + size <= self.total, (name, self.top, size)
        self.regs[name] = dict(off=self.top, size=size, cur=0, keys=[], old=[])
        self.top += size

    def reset(self, name):
        r = self.regs[name]
        r["old"] = r["old"] + r["keys"]
        r["keys"] = []
        r["cur"] = 0

    def alloc(self, reg, key, shape, dt, keys=None):
        r = self.regs[reg]
        isz = 2 if dt == BF16 else 4
        n = 1
        for s in shape[1:]:
            n *= s
        nb = (n * isz + 63) // 64 * 64
        assert r["cur"] + nb <= r["size"], (reg, key, r["cur"], nb, r["size"])
        off = r["off"] + r["cur"]
        r["cur"] += nb
        keys = list(keys) if keys else [key]
        r["keys"].extend(keys)
        olds = []
        for ok in r["old"]:
            lw = self.P.lastw.get(ok)
            if lw is not None:
                olds.append(lw)
            olds.extend(self.P.readers.get(ok, ()))
        if olds:
            for kk in keys:
                self.P.readers.setdefault(kk, []).extend(olds)
        ap = self.t[:, off // 4:(off + n * isz + 3) // 4]
        if dt == BF16:
            ap = ap.bitcast(BF16)
        ap = ap[:, 0:n]
        if len(shape) == 3:
            ap = ap.rearrange("p (a b) -> p a b", a=shape[1])
        elif len(shape) == 4:
            ap = ap.rearrange("p (a b c) -> p a b c", a=shape[1], b=shape[2])
        return ap


NCF = 1416
NCB = 1280
CF_GATTN, CF_GMEM, CF_GQ, CF_GK, CF_GMQ, CF_GMK = 0, 8, 16, 17, 18, 19
CF_INVW, CF_PSC, CF_IC16, CF_HV, CF_EPS = 20, 22, 24, 56, 57
CF_ONES, CF_OFFS, CF_BIAS, CF_IDF, CF_GFFN = 64, 128, 192, 264, 392
CB_ID, CB_MASK, CB_BD, CB_ONES, CB_TRI, CB_PBD = 0, 128, 640, 768, 896, 1024
EPS = 1e-6
EXPERT_CAP = 128


def build_program(stage=99, dumps=()):
    nc = bass.Bass("TRN2", target_bir_lowering=False)
    P = Prog()
    D = {}

    def din(name, shape, dt=F32):
        D[name] = nc.dram_tensor(name, shape, dt, kind="ExternalInput").ap()

    din("xo", [2048, 1024]); din("xh", [2048, 1024]); din("mem", [256, 1024])
    din("w_in", [1024, 2048]); din("w_out", [1024, 1024]); din("w_kv", [1024, 512])
    din("w_r", [1024, 72]); din("cf", [128, NCF]); din("cb", [128, NCB])
    if stage >= 5:
        din("w1", [64, 1024, 512]); din("w3", [64, 1024, 512]); din("w2", [64, 512, 1024])
    out = nc.dram_tensor("out", [2048, 1024], F32, kind="ExternalOutput").ap()
    xg = nc.dram_tensor("xg", [64 * EXPERT_CAP, 1024], BF16, kind="Internal").ap()
    yg = nc.dram_tensor("yg", [64 * EXPERT_CAP, 1024], BF16, kind="Internal").ap()
    xmid = nc.dram_tensor("xmid", [2048, 1024], F32, kind="Internal").ap()
    dump_out = {}

    M = Mem(nc, P, 203 * 1024)
    M.region("const", 12 * 1024)
    M.region("wout", 16 * 1024)
    M.region("r1", 32 * 1024)
    M.region("qt", 16 * 1024)
    M.region("kt", 32 * 1024)
    M.region("vt", 32 * 1024)
    M.region("r2", 36 * 1024)
    M.region("ut", 16640)
    M.region("qmt", 8 * 1024)
    PS = [nc.alloc_psum_tensor("ps%d" % i, [128, 512], F32) for i in range(8)]

    def psf(i):
        return PS[i][:, :]

    def psb(i):
        return PS[i][:, :].bitcast(BF16)

    def pk(i):
        return "ps%d" % i

    A = P.add
    CF = M.alloc("const", "cf", [128, NCF], F32)
    CB = M.alloc("const", "cb", [128, NCB], BF16)
    ZT = M.alloc("const", "zt", [128, 1024], BF16)
    IDB = CB[:, CB_ID:CB_ID + 128]
    MASK4 = CB[:, CB_MASK:CB_MASK + 512]
    BD64 = CB[:, CB_BD:CB_BD + 128]
    ONESB = CB[:, CB_ONES:CB_ONES + 128]
    TRI = CB[:, CB_TRI:CB_TRI + 128]
    EPSC = CF[:, CF_EPS:CF_EPS + 1]
    IDF = CF[:, CF_IDF:CF_IDF + 128]
    ONES64 = CF[:, CF_ONES:CF_ONES + 64]

    A(SP, lambda e: e.dma_start(out=CF, in_=D["cf"]), w=["cf"], dma=True)
    A(POOL, lambda e: e.dma_start(out=CB, in_=D["cb"]), w=["cb"], dma=True)
    WIN = M.alloc("r1", "win", [128, 8, 2048], BF16, keys=["win%d" % i for i in range(4)])
    win_keys = []
    for i in range(4):
        key = "win%d" % i
        win_keys.append(key)
        A(POOL, lambda e, i=i: e.dma_start(
            out=WIN[:, 2 * i:2 * i + 2, :],
            in_=D["w_in"][256 * i:256 * i + 256, :].rearrange("(k p) c -> p k c", p=128)),
          w=[key], dma=True)
    A(DVE, lambda e: e.memset(ZT, 0.0), w=["zt"])
    ZT4 = ZT.unsqueeze(1).broadcast_to([128, 4, 1024])

    QT = M.alloc("qt", "qt", [128, 4, 2048], BF16, keys=["qt%d_%d" % (c, g) for c in range(4) for g in range(4)])
    KT = M.alloc("kt", "kt", [128, 4, 4096], BF16, keys=["kt%d_%d" % (c, g) for c in range(4) for g in range(8)])
    VT = M.alloc("vt", "vt", [128, 4, 4096], BF16, keys=["vt%d_%d" % (c, g) for c in range(4) for g in range(8)])
    UT = M.alloc("ut", "ut", [128, 2, 2064], F32, keys=["ut0", "ut1"])
    QMT = M.alloc("qmt", "qmt", [128, 2, 2048], BF16, keys=["qmt0", "qmt1"])
    XT = [M.alloc("r2", "xt%d" % i, [128, 1024], F32) for i in range(2)]
    XN = [M.alloc("r2", "xn%d" % i, [128, 1024], BF16) for i in range(2)]
    HTG = [M.alloc("r2", "htg%d" % i, [128, 8, 512], BF16, keys=["htg%d_%d" % (i, t) for t in range(4)]) for i in range(2)]
    SQZ = [M.alloc("r2", "sqz%d" % i, [128, 512], BF16) for i in range(2)]
    RS = [M.alloc("r2", "rs%d" % i, [128, 512], F32) for i in range(2)]
    ST4 = [M.alloc("r2", "st4_%d" % i, [128, 4], F32) for i in range(2)]

    def norm_tile(src_ap, slot, gain_col, dst_fn, dkeys, ti):
        xt, xn, st = XT[slot], XN[slot], ST4[slot]
        kx, kn, ks = "xt%d" % slot, "xn%d" % slot, "st4_%d" % slot
        tp = ti % 2
        A(SP, lambda e: e.dma_start(out=xt, in_=src_ap), w=[kx], dma=True)
        A(ACT, lambda e: e.activation(out=xn, in_=xt, func=AF.Square, accum_out=st[:, 0:1]), r=[kx], w=[kn, ks])
        A(ACT, lambda e: e.activation(out=st[:, 1:2], in_=st[:, 0:1], func=AF.Ln, bias=EPSC, scale=1.0 / 1024), r=[ks, "cf"], w=[ks])
        A(ACT, lambda e: e.activation(out=st[:, 2:3], in_=st[:, 1:2], func=AF.Exp, scale=-0.5), r=[ks], w=[ks])
        A(ACT, lambda e: e.activation(out=xn, in_=xt, func=AF.Copy, scale=st[:, 2:3]), r=[kx, ks], w=[kn])

        def tr(e):
            for k in range(8):
                ins = e.transpose(out=psb(tp)[:, k * 128:(k + 1) * 128], in_=xn[:, k * 128:(k + 1) * 128], identity=IDB)
            return ins
        A(PE, tr, r=[kn, "cb"], w=[pk(tp)])
        g = CF[:, gain_col:gain_col + 8].unsqueeze(2).broadcast_to([128, 8, 128])
        A(DVE, lambda e: e.tensor_tensor(out=dst_fn(), in0=psb(tp).rearrange("p (k t) -> p k t", k=8), in1=g, op=ALU.mult),
          r=[pk(tp), "cf"], w=dkeys)

    def head_norm(zbank, zkey, n, gain_col, dst, dkeys, i):
        sq, rs = SQZ[i % 2], RS[i % 2]
        ksq, krs = "sqz%d" % (i % 2), "rs%d" % (i % 2)
        mb = 4 + i % 2
        A(ACT, lambda e: e.activation(out=sq[:, 0:n], in_=psf(zbank)[:, 0:n], func=AF.Square), r=[zkey], w=[ksq])
        A(PE, lambda e: e.matmul(psf(mb)[:, 0:n], lhsT=BD64, rhs=sq[:, 0:n], start=True, stop=True), r=[ksq, "cb"], w=[pk(mb)])
        A(ACT, lambda e: e.activation(out=rs[:, 0:n], in_=psf(mb)[:, 0:n], func=AF.Ln, bias=EPSC, scale=1.0), r=[pk(mb), "cf"], w=[krs])
        A(ACT, lambda e: e.activation(out=rs[:, 0:n], in_=rs[:, 0:n], func=AF.Exp, scale=-0.5), r=[krs], w=[krs])
        A(DVE, lambda e: e.scalar_tensor_tensor(out=dst, in0=psf(zbank)[:, 0:n], scalar=CF[:, gain_col:gain_col + 1], in1=rs[:, 0:n],
                                                 op0=ALU.mult, op1=ALU.mult), r=[zkey, krs, "cf"], w=dkeys)

    cnt = dict(ti=0, zi=0, hn=0)

    def tile_task(g, t):
        hb = HTG[g % 2]
        src = (D["xh"] if g < 4 else D["xo"])[((g % 4) * 4 + t) * 128:((g % 4) * 4 + t + 1) * 128, :]
        norm_tile(src, cnt["ti"] % 2, CF_GATTN, lambda: hb[:, :, t * 128:(t + 1) * 128], ["htg%d_%d" % (g % 2, t)], cnt["ti"])
        cnt["ti"] += 1

    ZB = (2, 3, 6, 7)

    def chunk_part1(g, c):
        hb = HTG[g % 2]
        hkeys = ["htg%d_%d" % (g % 2, t) for t in range(4)]
        zb = ZB[cnt["zi"] % 4]
        cnt["zi"] += 1

        def proj(e):
            for k in range(8):
                ins = e.matmul(psf(zb), lhsT=WIN[:, k, c * 128:(c + 1) * 128], rhs=hb[:, k, :], start=(k == 0), stop=(k == 7))
            return ins
        A(PE, proj, r=win_keys + hkeys, w=[pk(zb)])
        st = dict(g=g, c=c, zb=zb)
        if c < 8 or c >= 14:
            i = cnt["hn"]
            cnt["hn"] += 1
            st["i"] = i
            sq, ksq = SQZ[i % 2], "sqz%d" % (i % 2)
            A(ACT, lambda e: e.activation(out=sq, in_=psf(zb), func=AF.Square), r=[pk(zb)], w=[ksq])
        return st

    def chunk_part2(st):
        g, c, zb = st["g"], st["c"], st["zb"]
        if 8 <= c < 12:
            A(ACT, lambda e: e.activation(out=VT[:, c - 8, g * 512:(g + 1) * 512], in_=psf(zb), func=AF.Copy),
              r=[pk(zb)], w=["vt%d_%d" % (c - 8, g)])
            return
        if c in (12, 13):
            if g == 3:
                A(DVE, lambda e: e.tensor_copy(out=UT[:, c - 12, 0:16], in_=psf(zb)[:, 496:512]), r=[pk(zb)], w=["ut%d" % (c - 12)])
            else:
                A(DVE, lambda e: e.tensor_copy(out=UT[:, c - 12, 16 + (g - 4) * 512:16 + (g - 3) * 512], in_=psf(zb)),
                  r=[pk(zb)], w=["ut%d" % (c - 12)])
            return
        if c < 4:
            gain_col, dst, dkeys = CF_GQ, QT[:, c, (g - 4) * 512:(g - 3) * 512], ["qt%d_%d" % (c, g - 4)]
        elif c < 8:
            gain_col, dst, dkeys = CF_GK, KT[:, c - 4, g * 512:(g + 1) * 512], ["kt%d_%d" % (c - 4, g)]
        else:
            gain_col, dst, dkeys = CF_GMQ, QMT[:, c - 14, (g - 4) * 512:(g - 3) * 512], ["qmt%d" % (c - 14)]
        i = st["i"]
        sq, rs = SQZ[i % 2], RS[i % 2]
        ksq, krs = "sqz%d" % (i % 2), "rs%d" % (i % 2)
        mb = 4 + i % 2
        A(PE, lambda e: e.matmul(psf(mb), lhsT=BD64, rhs=sq, start=True, stop=True), r=[ksq, "cb"], w=[pk(mb)])
        A(ACT, lambda e: e.activation(out=rs, in_=psf(mb), func=AF.Ln, bias=EPSC, scale=1.0), r=[pk(mb), "cf"], w=[krs])
        A(ACT, lambda e: e.activation(out=rs, in_=rs, func=AF.Exp, scale=-0.5), r=[krs], w=[krs])
        A(DVE, lambda e: e.scalar_tensor_tensor(out=dst, in0=psf(zb), scalar=CF[:, gain_col:gain_col + 1], in1=rs,
                                                 op0=ALU.mult, op1=ALU.mult), r=[pk(zb), krs, "cf"], w=dkeys)

    def chunks_of(g):
        k_, v_ = list(range(4, 8)), list(range(8, 12))
        ch = []
        if g >= 4:
            extra = [0, 1, 2, 3, 14, 15, 12, 13]
        elif g == 3:
            extra = [12, 13]
        else:
            extra = []
        normed = k_ + [c for c in extra if c < 4 or c >= 14]
        cheap = v_ + [c for c in extra if c in (12, 13)]
        while normed or cheap:
            if normed:
                ch.append(normed.pop(0))
            if cheap:
                ch.append(cheap.pop(0))
        return ch

    for t in range(4):
        tile_task(0, t)
    pending = None
    for g in range(8):
        ch = chunks_of(g)
        per = (len(ch) + 3) // 4
        nt = 0
        for i, c in enumerate(ch):
            st = chunk_part1(g, c)
            if pending is not None:
                chunk_part2(pending)
            pending = st
            if g + 1 < 8 and (i + 1) % per == 0 and nt < 4:
                tile_task(g + 1, nt)
                nt += 1
        while g + 1 < 8 and nt < 4:
            tile_task(g + 1, nt)
            nt += 1
    chunk_part2(pending)

    def add_dump(name, ap, keys, shape, dt):
        t = nc.dram_tensor("dump_" + name, shape, dt, kind="ExternalOutput").ap()
        dump_out[name] = t
        A(SP, lambda e: e.dma_start(out=t, in_=ap), r=keys, w=["dump_" + name], dma=True)

    vt_keys = lambda c: ["vt%d_%d" % (c, g) for g in range(8)]
    kt_keys = lambda c: ["kt%d_%d" % (c, g) for g in range(8)]
    qt_keys = lambda c: ["qt%d_%d" % (c, g) for g in range(4)]
    if "qkv" in dumps:
        add_dump("qt", QT, sum([qt_keys(c) for c in range(4)], []), [128, 4, 2048], BF16)
        add_dump("kt", KT, sum([kt_keys(c) for c in range(4)], []), [128, 4, 4096], BF16)
        add_dump("vt", VT, sum([vt_keys(c) for c in range(4)], []), [128, 4, 4096], BF16)
        add_dump("ut", UT, ["ut0", "ut1"], [128, 2, 2064], F32)
        add_dump("qmt", QMT, ["qmt0", "qmt1"], [128, 2, 2048], BF16)
    if stage <= 1:
        P.emit(nc)
        return nc, dump_out
    return build_rest(nc, P, M, D, A, CF, CB, PS, psf, psb, pk, out, xg, yg, xmid, stage, dumps, dump_out, add_dump,
                      dict(QT=QT, KT=KT, VT=VT, UT=UT, QMT=QMT, WIN=WIN, vt_keys=vt_keys, kt_keys=kt_keys, qt_keys=qt_keys,
                           norm_tile=norm_tile, head_norm=head_norm, ZT4=ZT4, XT=XT, XN=XN, ST4=ST4, SQZ=SQZ, RS=RS, win_keys=win_keys))


POOL_WINDOWS = (2, 4, 8, 16)


def prep_inputs(inp):
    f = np.float32
    x = np.ascontiguousarray(inp["x"], dtype=f)
    mem = np.ascontiguousarray(inp["mem"], dtype=f)
    cb = np.zeros((128, NCB), f)
    k = np.arange(128)[:, None]
    q = np.arange(128)[None, :]
    cb[:, CB_ID:CB_ID + 128] = np.eye(128, dtype=f)
    mprev = (k >= q).astype(f)
    mcur = (k <= q).astype(f)
    cb[:, CB_MASK:CB_MASK + 512] = np.concatenate([mprev, mcur, mprev, mcur], axis=1)
    bd = np.zeros((128, 128), f)
    bd[:64, :64] = 1.0 / 64
    bd[64:, 64:] = 1.0 / 64
    cb[:, CB_BD:CB_BD + 128] = bd
    cb[:, CB_ONES:CB_ONES + 128] = 1.0
    cb[:, CB_TRI:CB_TRI + 128] = (k < q).astype(f)
    pp = np.asarray(inp["pool_proj"], dtype=f)[0]
    for c in range(2):
        cb[0:64, CB_PBD + c * 128:CB_PBD + c * 128 + 64] = pp[2 * c]
        cb[64:128, CB_PBD + c * 128 + 64:CB_PBD + c * 128 + 128] = pp[2 * c + 1]
    w_r = np.concatenate([np.asarray(inp["w_group"], f)[0],
                          np.transpose(np.asarray(inp["w_router"], f)[0], (1, 0, 2)).reshape(1024, 64)], axis=1)
    w_r = np.ascontiguousarray(w_r)
    shared = dict(
        w_in=np.ascontiguousarray(inp["w_in"][0], dtype=f), w_out=np.ascontiguousarray(inp["w_out"][0], dtype=f),
        w_kv=np.ascontiguousarray(inp["w_mem_kv"][0], dtype=f), w_r=w_r,
        w1=np.ascontiguousarray(inp["w1"][0], dtype=f), w3=np.ascontiguousarray(inp["w3"][0], dtype=f),
        w2=np.ascontiguousarray(inp["w2"][0], dtype=f), cb=cb)
    p = np.arange(128)
    in_maps = []
    for c in range(8):
        b, half = c // 2, c % 2
        cf = np.zeros((128, NCF), f)
        cf[:, CF_GATTN:CF_GATTN + 8] = np.asarray(inp["attn_norm"], f)[0].reshape(8, 128).T
        cf[:, CF_GMEM:CF_GMEM + 8] = np.asarray(inp["mem_norm"], f)[0].reshape(8, 128).T
        cf[:, CF_GQ] = np.tile(np.asarray(inp["q_norm"], f)[0], 2)
        cf[:, CF_GK] = np.tile(np.asarray(inp["k_norm"], f)[0], 2)
        cf[:, CF_GMQ] = np.tile(np.asarray(inp["mq_norm"], f)[0], 2)
        cf[:, CF_GMK] = np.tile(np.asarray(inp["mk_norm"], f)[0], 2)
        for ch in range(2):
            w = np.array([POOL_WINDOWS[2 * ch + pi // 64] for pi in range(128)], f)
            cf[:, CF_INVW + ch] = 1.0 / w
            cf[:, CF_PSC + ch] = np.asarray(inp["pool_scale"], f)[0, ch * 128:(ch + 1) * 128]
            for t in range(16):
                pos = t + 2048 * half
                cf[:, CF_IC16 + ch * 16 + t] = 1.0 / np.minimum(pos + 1, w)
        cf[:, CF_HV] = float(half)
        cf[:, CF_EPS] = EPS
        cf[:, CF_ONES:CF_ONES + 64] = 1.0
        cf[:, CF_OFFS:CF_OFFS + 64] = (np.arange(64) * EXPERT_CAP).astype(f)[None, :]
        cf[:, CF_BIAS:CF_BIAS + 8] = np.asarray(inp["b_group"], f)[0][None, :]
        cf[:, CF_BIAS + 8:CF_BIAS + 72] = np.asarray(inp["b_router"], f)[0].reshape(64)[None, :]
        cf[:, CF_IDF:CF_IDF + 128] = np.eye(128, dtype=f)
        cf[:, CF_GFFN:CF_GFFN + 1024] = np.asarray(inp["ffn_norm"], f)[0][None, :]
        xo = x[b, half * 2048:(half + 1) * 2048]
        xh = x[b, 0:2048] if half == 1 else np.zeros((2048, 1024), f)
        m = dict(shared)
        m.update(xo=np.ascontiguousarray(xo), xh=np.ascontiguousarray(xh), mem=mem[b], cf=cf)
        in_maps.append(m)
    return in_maps


_NC_CACHE = {}


def kernel(**inputs):
    in_maps = prep_inputs(inputs)
    if "nc" not in _NC_CACHE:
        _NC_CACHE["nc"] = build_program()[0]
    res = run_bass_kernel_spmd(_NC_CACHE["nc"], in_maps, core_ids=list(range(8)))
    outs = [np.asarray(r["out"], dtype=np.float32) for r in res.results]
    return np.stack([np.concatenate([outs[2 * b], outs[2 * b + 1]], axis=0) for b in range(4)], axis=0)


def build_rest(nc, P, M, D, A, CF, CB, PS, psf, psb, pk, out, xg, yg, xmid, stage, dumps, dump_out, add_dump, S):
    QT, KT, VT, UT, QMT = S["QT"], S["KT"], S["VT"], S["UT"], S["QMT"]
    vt_keys, kt_keys, qt_keys = S["vt_keys"], S["kt_keys"], S["qt_keys"]
    IDB = CB[:, CB_ID:CB_ID + 128]
    MASK4 = CB[:, CB_MASK:CB_MASK + 512]
    ONESB = CB[:, CB_ONES:CB_ONES + 128]
    TRI = CB[:, CB_TRI:CB_TRI + 128]
    EPSC = CF[:, CF_EPS:CF_EPS + 1]
    IDF = CF[:, CF_IDF:CF_IDF + 128]
    ONES64 = CF[:, CF_ONES:CF_ONES + 64]
    HV = CF[:, CF_HV:CF_HV + 1]

    WOUT = M.alloc("wout", "wout", [128, 8, 1024], BF16)
    A(POOL, lambda e: e.dma_start(out=WOUT, in_=D["w_out"].rearrange("(k p) c -> p k c", p=128)), w=["wout"], dma=True)

    M.reset("r1")
    MIXT = M.alloc("r1", "mixt", [128, 8, 2048], BF16, keys=["mixt%d" % i for i in range(8)])
    M.reset("r2")
    TA = M.alloc("r2", "ta", [128, 2064], F32)
    TB = M.alloc("r2", "tb", [128, 2064], F32)
    PL = M.alloc("r2", "pl", [128, 2, 2048], BF16, keys=["pl0", "pl1"])
    T16 = M.alloc("r2", "t16", [128, 16], F32)
    IC = CF[:, CF_IC16:CF_IC16 + 32].rearrange("p (c t) -> p c t", c=2)
    N = 2064
    for c in range(2):
        U = UT[:, c, :]
        uk = "ut%d" % c
        A(POOL, lambda e, U=U: e.tensor_tensor(out=TA[:, 1:N], in0=U[:, 1:N], in1=U[:, 0:N - 1], op=ALU.add), r=[uk], w=["ta"])
        if c == 0:
            A(POOL, lambda e: e.tensor_tensor(out=TB[64:128, 3:N], in0=TA[64:128, 3:N], in1=TA[64:128, 1:N - 2], op=ALU.add), r=["ta"], w=["tb"])
        else:
            A(POOL, lambda e: e.tensor_tensor(out=TB[:, 3:N], in0=TA[:, 3:N], in1=TA[:, 1:N - 2], op=ALU.add), r=["ta"], w=["tb"])
            A(DVE, lambda e: e.tensor_tensor(out=TA[:, 7:N], in0=TB[:, 7:N], in1=TB[:, 3:N - 4], op=ALU.add), r=["tb"], w=["ta"])
            A(DVE, lambda e: e.tensor_tensor(out=TB[64:128, 15:N], in0=TA[64:128, 15:N], in1=TA[64:128, 7:N - 8], op=ALU.add), r=["ta"], w=["tb"])
        for (lo, hi, T, tk) in ((0, 64, TA, "ta"), (64, 128, TB, "tb")):
            A(DVE, lambda e, lo=lo, hi=hi, T=T, U=U, c=c: e.scalar_tensor_tensor(
                out=PL[lo:hi, c, :], in0=T[lo:hi, 16:N], scalar=CF[lo:hi, CF_INVW + c:CF_INVW + c + 1], in1=U[lo:hi, 16:N],
                op0=ALU.mult, op1=ALU.subtract), r=[tk, uk, "cf"], w=["pl%d" % c])
            A(DVE, lambda e, lo=lo, hi=hi, T=T, c=c: e.tensor_tensor(out=T16[lo:hi, :], in0=T[lo:hi, 16:32], in1=IC[lo:hi, c, :], op=ALU.mult),
              r=[tk, "cf"], w=["t16"])
        A(DVE, lambda e, U=U, c=c: e.tensor_tensor(out=PL[:, c, 0:16], in0=T16, in1=U[:, 16:32], op=ALU.subtract), r=["t16", uk], w=["pl%d" % c])
        for tg in range(4):
            zb = 2 + tg % 2
            A(PE, lambda e, c=c, tg=tg, zb=zb: e.matmul(psf(zb), lhsT=CB[:, CB_PBD + c * 128:CB_PBD + (c + 1) * 128],
                                                         rhs=PL[:, c, tg * 512:(tg + 1) * 512], start=True, stop=True),
              r=["pl%d" % c, "cb"], w=[pk(zb)])
            A(ACT, lambda e, c=c, tg=tg, zb=zb: e.activation(out=MIXT[:, 4 + c, tg * 512:(tg + 1) * 512], in_=psf(zb), func=AF.Copy,
                                                             scale=CF[:, CF_PSC + c:CF_PSC + c + 1]),
              r=[pk(zb), "cf"], w=["mixt%d" % (4 + c)])

    M.reset("r2")
    M.reset("ut")
    XT = [M.alloc("r2", "xt%d" % i, [128, 1024], F32) for i in range(2)]
    XN = [M.alloc("r2", "xn%d" % i, [128, 1024], BF16) for i in range(2)]
    SQZ = [M.alloc("r2", "sqz%d" % i, [128, 512], BF16) for i in range(2)]
    RS = [M.alloc("r2", "rs%d" % i, [128, 512], F32) for i in range(2)]
    ST4 = [M.alloc("r2", "st4_%d" % i, [128, 4], F32) for i in range(2)]
    MEMT = M.alloc("r2", "memt", [128, 8, 256], BF16, keys=["memt0", "memt1"])
    KMT = M.alloc("r2", "kmt", [128, 2, 256], BF16, keys=["kmt0", "kmt1"])
    VMA = M.alloc("r2", "vma", [128, 2, 4, 65], BF16)
    PTM = [M.alloc("r2", "ptm%d" % i, [128, 512], BF16) for i in range(2)]
    ACM = [M.alloc("r2", "acm%d" % i, [128, 512], F32) for i in range(2)]
    WKV = M.alloc("ut", "wkv", [128, 8, 512], BF16)
    TMPO = [M.alloc("ut", "tmpo%d" % i, [128, 2048], BF16) for i in range(2)]
    S["XT"], S["XN"], S["ST4"], S["SQZ"], S["RS"] = XT, XN, ST4, SQZ, RS
    A(POOL, lambda e: e.dma_start(out=WKV, in_=D["w_kv"].rearrange("(k p) c -> p k c", p=128)), w=["wkv"], dma=True)
    A(DVE, lambda e: e.memset(VMA[:, :, :, 64:65], 1.0), w=["vma"])

    def norm_tile(src_ap, slot, gain_col, dst, dkeys, ti):
        xt, xn, st = XT[slot], XN[slot], ST4[slot]
        kx, kn, ks = "xt%d" % slot, "xn%d" % slot, "st4_%d" % slot
        tp = ti % 2
        A(SP, lambda e: e.dma_start(out=xt, in_=src_ap), w=[kx], dma=True)
        A(ACT, lambda e: e.activation(out=xn, in_=xt, func=AF.Square, accum_out=st[:, 0:1]), r=[kx], w=[kn, ks])
        A(ACT, lambda e: e.activation(out=st[:, 1:2], in_=st[:, 0:1], func=AF.Ln, bias=EPSC, scale=1.0 / 1024), r=[ks, "cf"], w=[ks])
        A(ACT, lambda e: e.activation(out=st[:, 2:3], in_=st[:, 1:2], func=AF.Exp, scale=-0.5), r=[ks], w=[ks])
        A(ACT, lambda e: e.activation(out=xn, in_=xt, func=AF.Copy, scale=st[:, 2:3]), r=[kx, ks], w=[kn])

        def tr(e):
            for k in range(8):
                ins = e.transpose(out=psb(tp)[:, k * 128:(k + 1) * 128], in_=xn[:, k * 128:(k + 1) * 128], identity=IDB)
            return ins
        A(PE, tr, r=[kn, "cb"], w=[pk(tp)])
        g = CF[:, gain_col:gain_col + 8].unsqueeze(2).broadcast_to([128, 8, 128])
        A(DVE, lambda e: e.tensor_tensor(out=dst, in0=psb(tp).rearrange("p (k t) -> p k t", k=8), in1=g, op=ALU.mult),
          r=[pk(tp), "cf"], w=dkeys)

    def head_norm(zbank, zkey, n, gain_col, dst, dkeys, i):
        sq, rs = SQZ[i % 2], RS[i % 2]
        ksq, krs = "sqz%d" % (i % 2), "rs%d" % (i % 2)
        mb = 4 + i % 2
        BD64 = CB[:, CB_BD:CB_BD + 128]
        A(ACT, lambda e: e.activation(out=sq[:, 0:n], in_=psf(zbank)[:, 0:n], func=AF.Square), r=[zkey], w=[ksq])
        A(PE, lambda e: e.matmul(psf(mb)[:, 0:n], lhsT=BD64, rhs=sq[:, 0:n], start=True, stop=True), r=[ksq, "cb"], w=[pk(mb)])
        A(ACT, lambda e: e.activation(out=rs[:, 0:n], in_=psf(mb)[:, 0:n], func=AF.Ln, bias=EPSC, scale=1.0), r=[pk(mb), "cf"], w=[krs])
        A(ACT, lambda e: e.activation(out=rs[:, 0:n], in_=rs[:, 0:n], func=AF.Exp, scale=-0.5), r=[krs], w=[krs])
        A(DVE, lambda e: e.scalar_tensor_tensor(out=dst, in0=psf(zbank)[:, 0:n], scalar=CF[:, gain_col:gain_col + 1], in1=rs[:, 0:n],
                                                 op0=ALU.mult, op1=ALU.mult), r=[zkey, krs, "cf"], w=dkeys)

    for mt in range(2):
        norm_tile(D["mem"][mt * 128:(mt + 1) * 128, :], mt, CF_GMEM, MEMT[:, :, mt * 128:(mt + 1) * 128], ["memt%d" % mt], mt)
    mkeys = ["memt0", "memt1"]
    for c in range(2):
        zb = 2 + c

        def kproj(e, c=c, zb=zb):
            for k in range(8):
                ins = e.matmul(psf(zb)[:, 0:256], lhsT=WKV[:, k, c * 128:(c + 1) * 128], rhs=MEMT[:, k, :], start=(k == 0), stop=(k == 7))
            return ins
        A(PE, kproj, r=["wkv"] + mkeys, w=[pk(zb)])
        head_norm(zb, pk(zb), 256, CF_GMK, KMT[:, c, :], ["kmt%d" % c], c)
    for mt in range(2):
        zb = 2 + mt

        def vproj(e, mt=mt, zb=zb):
            for k in range(8):
                ins = e.matmul(psf(zb)[:, 0:256], lhsT=MEMT[:, k, mt * 128:(mt + 1) * 128], rhs=WKV[:, k, 256:512], start=(k == 0), stop=(k == 7))
            return ins
        A(PE, vproj, r=["wkv", "memt%d" % mt], w=[pk(zb)])
        A(ACT, lambda e, mt=mt, zb=zb: e.activation(out=VMA[:, mt, :, 0:64], in_=psf(zb)[:, 0:256].rearrange("p (h d) -> p h d", h=4), func=AF.Copy),
          r=[pk(zb)], w=["vma"])

    def normalize_out(acc_ap_fn, acc_keys, tmpo, tkey, bcb):
        for tg in range(4):
            a = acc_ap_fn(tg)
            A(DVE, lambda e, a=a: e.reciprocal(out=a[64:65, :], in_=a[64:65, :]), r=acc_keys, w=acc_keys)
            A(PE, lambda e, a=a: e.matmul(psf(bcb)[0:64, :], lhsT=ONES64[64:65, :], rhs=a[64:65, :], start=True, stop=True), r=acc_keys + ["cf"], w=[pk(bcb)])
            A(DVE, lambda e, a=a, tg=tg: e.tensor_tensor(out=tmpo[0:64, tg * 512:(tg + 1) * 512], in0=a[0:64, :], in1=psf(bcb)[0:64, :], op=ALU.mult),
              r=acc_keys + [pk(bcb)], w=[tkey])

    its = [(mh, tg) for mh in range(4) for tg in range(4)]

    def MF1(it):
        mh, tg = its[it]
        c, b0 = mh // 2, 64 * (mh % 2)
        sbs = (2, 3) if it % 2 == 0 else (6, 7)
        for mt in range(2):
            sb = sbs[mt]
            ptm, pkey = PTM[mt], "ptm%d" % mt
            A(PE, lambda e, mt=mt, sb=sb: e.matmul(
                psf(sb), lhsT=KMT[b0:b0 + 64, c, mt * 128:(mt + 1) * 128], rhs=QMT[b0:b0 + 64, c, tg * 512:(tg + 1) * 512], start=True, stop=True),
              r=["kmt%d" % c, "qmt%d" % c], w=[pk(sb)])
            A(ACT, lambda e, sb=sb, ptm=ptm: e.activation(out=ptm, in_=psf(sb), func=AF.Exp, scale=0.125), r=[pk(sb)], w=[pkey])

    def MF2(it):
        mh, tg = its[it]
        ob = it % 2
        for mt in range(2):
            ptm, pkey = PTM[mt], "ptm%d" % mt
            A(PE, lambda e, mt=mt, ptm=ptm: e.matmul(psf(ob)[0:65, :], lhsT=VMA[:, mt, mh, :], rhs=ptm, start=(mt == 0), stop=(mt == 1)),
              r=["vma", pkey], w=[pk(ob)])
        acm, akey = ACM[it % 2], "acm%d" % (it % 2)
        A(ACT, lambda e: e.activation(out=acm[0:65, :], in_=psf(ob)[0:65, :], func=AF.Copy), r=[pk(ob)], w=[akey])

    def MB(it):
        mh, tg = its[it]
        c, b0 = mh // 2, 64 * (mh % 2)
        tmpo, tkey = TMPO[mh % 2], "tmpo%d" % (mh % 2)
        acm, akey = ACM[it % 2], "acm%d" % (it % 2)
        bcb = 4 + it % 2
        A(ACT, lambda e: e.activation(out=acm[64:65, :], in_=acm[64:65, :], func=AF.Ln), r=[akey], w=[akey])
        A(ACT, lambda e: e.activation(out=acm[64:65, :], in_=acm[64:65, :], func=AF.Exp, scale=-1.0), r=[akey], w=[akey])
        A(PE, lambda e: e.matmul(psf(bcb)[0:64, :], lhsT=ONES64[64:65, :], rhs=acm[64:65, :], start=True, stop=True), r=[akey, "cf"], w=[pk(bcb)])
        A(DVE, lambda e: e.tensor_tensor(out=tmpo[0:64, tg * 512:(tg + 1) * 512], in0=acm[0:64, :], in1=psf(bcb)[0:64, :], op=ALU.mult),
          r=[akey, pk(bcb)], w=[tkey])
        if tg == 3:
            A(SP, lambda e: e.dma_start(out=MIXT[b0:b0 + 64, 6 + c, :], in_=tmpo[0:64, :]), r=[tkey], w=["mixt%d" % (6 + c)], dma=True)

    PTM = [M.alloc("r2", "ptm%d" % i, [128, 512], BF16) for i in range(2)] if False else PTM
    MF1(0)
    for it in range(16):
        MF2(it)
        if it + 1 < 16:
            MF1(it + 1)
        if it >= 1:
            MB(it - 1)
    MB(15)

    if "mix" in dumps and stage == 2:
        add_dump("mixt", MIXT, ["mixt%d" % i for i in range(8)], [128, 8, 2048], BF16)
    if stage <= 2:
        P.emit(nc)
        return nc, dump_out
    return build_attn(nc, P, M, D, A, CF, CB, PS, psf, psb, pk, out, xg, yg, xmid, stage, dumps, dump_out, add_dump, S, MIXT, WOUT)


def build_attn(nc, P, M, D, A, CF, CB, PS, psf, psb, pk, out, xg, yg, xmid, stage, dumps, dump_out, add_dump, S, MIXT, WOUT):
    QT, KT, VT = S["QT"], S["KT"], S["VT"]
    vt_keys, kt_keys, qt_keys = S["vt_keys"], S["kt_keys"], S["qt_keys"]
    IDB = CB[:, CB_ID:CB_ID + 128]
    MASK4 = CB[:, CB_MASK:CB_MASK + 512]
    ONES64 = CF[:, CF_ONES:CF_ONES + 64]
    HV = CF[:, CF_HV:CF_HV + 1]
    M.reset("r2"); M.reset("ut"); M.reset("qmt")
    import os as _os
    DILS = tuple(int(v) for v in _os.environ.get('ATTN_DILS', '1,4,16').split(','))
    NHP = int(_os.environ.get('ATTN_HP', '4'))
    LVL = int(_os.environ.get('ATTN_LEVEL', '9'))
    VA = [M.alloc("r2" if p < 3 else "ut", "va%d" % p, [128, 32, 2, 65], BF16) for p in range(3)]
    PT = [M.alloc("r2", "pt%d" % i, [128, 512], BF16) for i in range(4)]
    ACC = M.alloc("ut", "acc", [128, 2, 2048], F32, keys=["acc_q%d" % i for i in range(4)])
    TMPO = [M.alloc("qmt", "tmpo%d" % i, [128, 2048], BF16) for i in range(2)]
    for p, d in enumerate(DILS):
        nb = 32 // d
        A(DVE, lambda e, p=p: e.memset(VA[p][:, :, :, 64:65], 1.0), w=["va%d" % p])
        v = VA[p][:, :, :, 64:65].rearrange("p (r b) h o -> p r b (h o)", r=d)[:, :, 0:nb // 2, :]
        A(DVE, lambda e, v=v: e.tensor_scalar(out=v, in0=v, scalar1=HV, scalar2=None, op0=ALU.mult), r=["cf"], w=["va%d" % p])

    for i in range(16):
        A(SP, lambda e, i=i: e.dma_start(out=xg[i * 512:(i + 1) * 512, :].rearrange("(p r) d -> p r d", p=128), in_=S["ZT4"]),
          r=["zt"], w=["xg"], dma=True)

    units = [(hp, p, d) for hp in range(NHP) for p, d in enumerate(DILS)]
    NU = len(units)
    akeys = ["acc_q%d" % i for i in range(4)]

    def vprep_group(u, grp):
        hp, p, d = units[u]
        c, nb, va, vak = hp, 32 // d, VA[p], "va%d" % p

        def vtr(e):
            for j in range(8):
                kb = grp * 8 + j
                r, b = kb // nb, kb % nb
                s0 = d * 128 * b + r
                ins = e.transpose(out=psb(6)[:, j * 128:(j + 1) * 128], in_=VT[:, c, s0:s0 + 127 * d + 1:d], identity=IDB)
            return ins
        A(PE, vtr, r=vt_keys(c) + ["cb"], w=[pk(6)])
        src = psb(6).rearrange("p (k h d) -> p k h d", k=8, h=2)
        dst = va[:, grp * 8:grp * 8 + 8, :, 0:64]
        if grp % 2 == 0:
            A(ACT, lambda e: e.activation(out=dst, in_=src, func=AF.Copy), r=[pk(6)], w=[vak])
        else:
            A(DVE, lambda e: e.tensor_copy(out=dst, in_=src), r=[pk(6)], w=[vak])

    steps = []
    for u, (hp, p, d) in enumerate(units):
        nb = 32 // d
        for qg in range(4):
            for jp in range(2):
                blk = []
                for jj in range(2):
                    j = 2 * jp + jj
                    if d == 1:
                        r, b = 0, 16 + 4 * qg + j
                    elif d == 4:
                        r, b = j, 4 + qg
                    else:
                        r, b = 4 * qg + j, 1
                    kbc = r * nb + b
                    kc0 = d * 128 * b + r
                    blk.append((j, kbc, kbc - 1, kc0, d * 128 * (b - 1) + r, kc0 - 2048))
                steps.append(dict(u=u, hp=hp, p=p, d=d, qg=qg, jp=jp, blk=blk, n=len(steps)))
    mi = [0]

    def F1(st):
        n, c, d, blk = st["n"], st["hp"], st["d"], st["blk"]
        sset = n % 2
        pts = [(PT[2 * sset + hh], "pt%d" % (2 * sset + hh)) for hh in range(2)]
        st["pts"] = pts

        def qk(e):
            for jj, (j, kbc, kbp, kc0, kp0, q0) in enumerate(blk):
                for hh in range(2):
                    for kv, k0 in enumerate((kp0, kc0)):
                        ins = e.matmul(psf(2 * sset + hh)[:, (jj * 2 + kv) * 128:(jj * 2 + kv + 1) * 128],
                                       lhsT=KT[hh * 64:hh * 64 + 64, c, k0:k0 + 127 * d + 1:d],
                                       rhs=QT[hh * 64:hh * 64 + 64, c, q0:q0 + 127 * d + 1:d], start=True, stop=True)
            return ins
        A(PE, qk, r=kt_keys(c) + qt_keys(c), w=[pk(2 * sset), pk(2 * sset + 1)])
        for hh in range(2):
            pt, ptk = pts[hh]
            A(ACT, lambda e, sb=2 * sset + hh, pt=pt: e.activation(out=pt, in_=psf(sb), func=AF.Exp, scale=0.125), r=[pk(2 * sset + hh)], w=[ptk])
            meng = DVE if mi[0] % 2 == 0 else POOL
            mi[0] += 1
            A(meng, lambda e, pt=pt: e.tensor_tensor(out=pt, in0=pt, in1=MASK4, op=ALU.mult), r=[ptk, "cb"], w=[ptk])

    def F2(st):
        n, p, d, blk, qg, jp, pts = st["n"], st["p"], st["d"], st["blk"], st["qg"], st["jp"], st["pts"]
        va, vak = VA[p], "va%d" % p
        ob = 4 + n % 2

        def pv(e):
            for jj, (j, kbc, kbp, kc0, kp0, q0) in enumerate(blk):
                for hh in range(2):
                    pt = pts[hh][0]
                    o = psf(ob)[0:65, (hh * 2 + jj) * 128:(hh * 2 + jj + 1) * 128]
                    e.matmul(o, lhsT=va[:, kbp, hh, :], rhs=pt[:, (jj * 2) * 128:(jj * 2 + 1) * 128], start=True, stop=False)
                    ins = e.matmul(o, lhsT=va[:, kbc, hh, :], rhs=pt[:, (jj * 2 + 1) * 128:(jj * 2 + 2) * 128], start=False, stop=True)
            return ins
        A(PE, pv, r=[vak, pts[0][1], pts[1][1]], w=[pk(ob)])
        if d == 1:
            t0 = (qg * 4 + jp * 2) * 128
            dst = ACC[0:65, :, t0:t0 + 256]
            src = psf(ob)[0:65, :].rearrange("p (h t) -> p h t", h=2)
            A(ACT, lambda e: e.activation(out=dst, in_=src, func=AF.Copy), r=[pk(ob)], w=[akeys[qg]])
        else:
            if d == 4:
                dst = ACC[0:65, :, qg * 512:(qg + 1) * 512].rearrange("p h (i r) -> p h r i", r=4)[:, :, 2 * jp:2 * jp + 2, :]
                ks = [akeys[qg]]
            else:
                dst = ACC[0:65, :, :].rearrange("p h (i r) -> p h r i", r=16)[:, :, 4 * qg + 2 * jp:4 * qg + 2 * jp + 2, :]
                ks = akeys
            src = psf(ob)[0:65, :].rearrange("p (h r i) -> p h r i", h=2, r=2)
            A(DVE, lambda e: e.tensor_tensor(out=dst, in0=src, in1=dst, op=ALU.add), r=[pk(ob)] + ks, w=ks)

    tmi = [0]

    bci = [0]

    def NORM(hp):
        for hh in range(2):
            a = ACC[64:65, hh, :]
            A(ACT, lambda e, a=a: e.activation(out=a, in_=a, func=AF.Ln), r=akeys, w=akeys)
            A(ACT, lambda e, a=a: e.activation(out=a, in_=a, func=AF.Exp, scale=-1.0), r=akeys, w=akeys)
        for hh in range(2):
            tmpo, tkey = TMPO[tmi[0] % 2], "tmpo%d" % (tmi[0] % 2)
            tmi[0] += 1
            for tg in range(4):
                a = ACC[:, hh, tg * 512:(tg + 1) * 512]
                bb = 6 + bci[0] % 2
                bci[0] += 1
                A(PE, lambda e, a=a, bb=bb: e.matmul(psf(bb)[0:64, :], lhsT=ONES64[64:65, :], rhs=a[64:65, :], start=True, stop=True), r=[akeys[tg], "cf"], w=[pk(bb)])
                A(DVE, lambda e, a=a, tg=tg, tmpo=tmpo, bb=bb: e.tensor_tensor(out=tmpo[0:64, tg * 512:(tg + 1) * 512], in0=a[0:64, :], in1=psf(bb)[0:64, :], op=ALU.mult),
                  r=[akeys[tg], pk(bb)], w=[tkey])
            A(SP, lambda e, hh=hh, hp=hp, tmpo=tmpo: e.dma_start(out=MIXT[hh * 64:hh * 64 + 64, hp, :], in_=tmpo[0:64, :]), r=[tkey], w=["mixt%d" % hp], dma=True)

    for grp in range(4):
        vprep_group(0, grp)
    F1(steps[0])
    for n, st in enumerate(steps):
        if n + 1 < len(steps):
            F1(steps[n + 1])
        F2(st)
        sl = n % 8
        if sl in (1, 3, 5, 7) and st["u"] + 1 < NU:
            vprep_group(st["u"] + 1, sl // 2)
        if sl == 7 and st["p"] == len(DILS) - 1:
            NORM(st["hp"])

    if "mix" in dumps and stage == 3:
        add_dump("mixt", MIXT, ["mixt%d" % i for i in range(8)], [128, 8, 2048], BF16)
    if stage <= 3:
        P.emit(nc)
        return nc, dump_out
    return build_ffn(nc, P, M, D, A, CF, CB, PS, psf, psb, pk, out, xg, yg, xmid, stage, dumps, dump_out, add_dump, S, MIXT, WOUT)


def build_ffn(nc, P, M, D, A, CF, CB, PS, psf, psb, pk, out, xg, yg, xmid, stage, dumps, dump_out, add_dump, S, MIXT, WOUT):
    IDB = CB[:, CB_ID:CB_ID + 128]
    ONESB = CB[:, CB_ONES:CB_ONES + 128]
    TRI = CB[:, CB_TRI:CB_TRI + 128]
    EPSC = CF[:, CF_EPS:CF_EPS + 1]
    IDF = CF[:, CF_IDF:CF_IDF + 128]
    GFFN = CF[:, CF_GFFN:CF_GFFN + 1024]
    BIASR = CF[:, CF_BIAS:CF_BIAS + 72]
    OFFS = CF[:, CF_OFFS:CF_OFFS + 64]
    M.reset("kt"); M.reset("qt"); M.reset("r2")
    XT = [M.alloc("kt", "xt%d" % i, [128, 1024], F32) for i in range(2)]
    XM = [M.alloc("kt", "xm%d" % i, [128, 1024], F32, keys=["xm%d_0" % i, "xm%d_1" % i]) for i in range(2)]
    H2 = [M.alloc("kt", "h2_%d" % i, [128, 1024], F32) for i in range(2)]
    H2T = [M.alloc("kt", "h2t%d" % i, [128, 8, 128], F32, keys=["h2t%d_0" % i, "h2t%d_1" % i]) for i in range(2)]
    NHB = 12
    H2B = [M.alloc("r2", "h2b%d" % i, [128, 1024], BF16) for i in range(NHB)]
    WR = M.alloc("qt", "wr", [128, 8, 72], F32)
    AB = M.alloc("qt", "ab", [128, 16, 64], BF16, keys=["ab%d" % i for i in range(4)])
    G12 = M.alloc("qt", "g12", [128, 2, 16], F32)
    SMS = M.alloc("qt", "sms", [128, 2, 4], F32, keys=["sms0", "sms1"])
    NT = 4
    L4 = [M.alloc("r2", "l4_%d" % i, [128, NT, 72], F32, keys=["l4_%d_%d" % (i, j) for j in range(NT)]) for i in range(2)]
    RT = M.alloc("r2", "rt", [128, 1400], F32)
    DEST = [[nc.alloc_sbuf_tensor("dest%d_%d" % (a, tt), [128, 1], I32) for tt in range(16)] for a in range(2)]
    A(SP, lambda e: e.dma_start(out=WR, in_=D["w_r"].rearrange("(k p) c -> p k c", p=128)), w=["wr"], dma=True)

    _o = [0]

    def rt(n, shape=None):
        ap = RT[:, _o[0]:_o[0] + n]
        _o[0] += n
        if shape:
            ap = ap.rearrange("p (a b) -> p a b", a=shape[0]) if len(shape) == 2 else ap.rearrange("p (a b c) -> p a b c", a=shape[0], b=shape[1])
        return ap
    OHG, D8, E8, LSEL, OH1, L2, OH2 = [rt(NT * 8, (NT, 8)) for _ in range(7)]
    T64, A1F, A2F, RB = [rt(NT * 64, (NT, 8, 8)) for _ in range(4)]
    SM = rt(16 * NT, (16, NT))
    RK = "rt"

    def S1(tt):
        sl = tt % 2
        xt, xm, h2, h2b = XT[sl], XM[sl], H2[sl], H2B[tt % NHB]
        kx, km, kh, kb = "xt%d" % sl, "xm%d" % sl, "h2_%d" % sl, "h2b%d" % (tt % NHB)
        sm, ksm = SMS[:, sl, :], "sms%d" % sl
        tok = slice(tt * 128, (tt + 1) * 128)
        ob = (0, 1) if sl == 0 else (4, 5)
        A(SP, lambda e: e.dma_start(out=xt, in_=D["xo"][tok, :]), w=[kx], dma=True)
        for half in range(2):
            def oproj(e, half=half):
                for k in range(8):
                    e.matmul(psf(ob[half]), lhsT=MIXT[:, k, tok], rhs=WOUT[:, k, half * 512:(half + 1) * 512], start=(k == 0), stop=False)
                return e.matmul(psf(ob[half]), lhsT=IDF, rhs=xt[:, half * 512:(half + 1) * 512], start=False, stop=True)
            A(PE, oproj, r=["wout", kx, "cf"] + ["mixt%d" % i for i in range(8)], w=[pk(ob[half])])
            A(ACT, lambda e, half=half: e.activation(out=xm[:, half * 512:(half + 1) * 512], in_=psf(ob[half]), func=AF.Copy), r=[pk(ob[half])], w=[km + "_%d" % half])
        kms = [km + "_0", km + "_1"]
        A(SP, lambda e: e.dma_start(out=xmid[tok, :], in_=xm), r=kms, w=["xmid%d" % tt], dma=True)
        A(ACT, lambda e: e.activation(out=h2, in_=xm, func=AF.Square, accum_out=sm[:, 0:1]), r=kms, w=[kh, ksm])
        A(ACT, lambda e: e.activation(out=sm[:, 1:2], in_=sm[:, 0:1], func=AF.Ln, bias=EPSC, scale=1.0 / 1024), r=[ksm, "cf"], w=[ksm])
        A(ACT, lambda e: e.activation(out=sm[:, 2:3], in_=sm[:, 1:2], func=AF.Exp, scale=-0.5), r=[ksm], w=[ksm])

    def S1c(tt):
        sl = tt % 2
        xm, h2, h2b = XM[sl], H2[sl], H2B[tt % NHB]
        km, kh, kb = "xm%d" % sl, "h2_%d" % sl, "h2b%d" % (tt % NHB)
        sm, ksm = SMS[:, sl, :], "sms%d" % sl
        kms = [km + "_0", km + "_1"]
        A(DVE, lambda e: e.scalar_tensor_tensor(out=h2, in0=xm, scalar=sm[:, 2:3], in1=GFFN, op0=ALU.mult, op1=ALU.mult), r=kms + [ksm, "cf"], w=[kh])
        A(ACT, lambda e: e.activation(out=h2b, in_=h2, func=AF.Copy), r=[kh], w=[kb])

    def S1b(tt):
        sl = tt % 2
        h2, h2t, kh = H2[sl], H2T[sl], "h2_%d" % sl
        for hb in range(2):
            def tr(e, hb=hb):
                for k in range(4):
                    kk = hb * 4 + k
                    ins = e.transpose(out=psf(2 + hb)[:, k * 128:(k + 1) * 128], in_=h2[:, kk * 128:(kk + 1) * 128], identity=IDF)
                return ins
            A(PE, tr, r=[kh, "cf"], w=[pk(2 + hb)])
            A(ACT, lambda e, hb=hb: e.activation(out=h2t[:, 4 * hb:4 * hb + 4, :], in_=psf(2 + hb).rearrange("p (k t) -> p k t", k=4), func=AF.Copy),
              r=[pk(2 + hb)], w=["h2t%d_%d" % (sl, hb)])

        def rmm(e):
            for k in range(8):
                ins = e.matmul(psf(6)[:, 0:72], lhsT=h2t[:, k, :], rhs=WR[:, k, :], start=(k == 0), stop=(k == 7))
            return ins
        A(PE, rmm, r=["h2t%d_0" % sl, "h2t%d_1" % sl, "wr"], w=[pk(6)])
        b, i = tt // NT, tt % NT
        A(DVE, lambda e: e.tensor_tensor(out=L4[b % 2][:, i, :], in0=psf(6)[:, 0:72], in1=BIASR, op=ALU.add), r=[pk(6), "cf"], w=["l4_%d_%d" % (b % 2, i)])

    V = lambda fn, r=(), w=(): A(DVE, fn, r=list(r) + [RK], w=list(w) + [RK])
    bc3 = lambda ap: ap.unsqueeze(2).broadcast_to([128, NT, 8])
    bcE = lambda ap: ap.unsqueeze(3).broadcast_to([128, NT, 8, 8])
    bcG = lambda ap: ap.unsqueeze(2).broadcast_to([128, NT, 8, 8])
    R_ = lambda i: SM[:, i, :]

    def S2B(b):
        l4 = L4[b % 2]
        lk = ["l4_%d_%d" % (b % 2, j) for j in range(NT)]
        LG_ = l4[:, :, 0:8]
        LE_ = l4[:, :, 8:72].rearrange("p t (g x) -> p t g x", g=8)
        V(lambda e: e.tensor_reduce(out=R_(0), in_=LG_, axis=AX.X, op=ALU.max), r=lk)
        V(lambda e: e.tensor_tensor(out=OHG, in0=LG_, in1=bc3(R_(0)), op=ALU.is_equal), r=lk)
        V(lambda e: e.tensor_tensor(out=D8, in0=LG_, in1=bc3(R_(0)), op=ALU.subtract), r=lk)
        A(ACT, lambda e: e.activation(out=E8, in_=D8, func=AF.Exp), r=[RK], w=[RK])
        V(lambda e: e.tensor_reduce(out=R_(1), in_=E8, axis=AX.X, op=ALU.add))
        V(lambda e: e.reciprocal(out=R_(2), in_=R_(1)))
        V(lambda e: e.tensor_tensor(out=T64, in0=LE_, in1=bcE(OHG), op=ALU.mult), r=lk)
        V(lambda e: e.tensor_reduce(out=LSEL, in_=T64.rearrange("p t g x -> p t x g"), axis=AX.X, op=ALU.add))
        V(lambda e: e.tensor_reduce(out=R_(3), in_=LSEL, axis=AX.X, op=ALU.max))
        V(lambda e: e.tensor_tensor(out=OH1, in0=LSEL, in1=bc3(R_(3)), op=ALU.is_equal))
        V(lambda e: e.scalar_tensor_tensor(out=L2, in0=OH1, scalar=-1e30, in1=LSEL, op0=ALU.mult, op1=ALU.add))
        V(lambda e: e.tensor_reduce(out=R_(4), in_=L2, axis=AX.X, op=ALU.max))
        V(lambda e: e.tensor_tensor(out=OH2, in0=L2, in1=bc3(R_(4)), op=ALU.is_equal))
        V(lambda e: e.tensor_tensor(out=R_(5), in0=R_(4), in1=R_(3), op=ALU.subtract))
        A(ACT, lambda e: e.activation(out=R_(6), in_=R_(5), func=AF.Exp), r=[RK], w=[RK])
        V(lambda e: e.tensor_tensor(out=A1F, in0=bcE(OHG), in1=bcG(OH1), op=ALU.mult))
        V(lambda e: e.tensor_tensor(out=A2F, in0=bcE(OHG), in1=bcG(OH2), op=ALU.mult))
        V(lambda e: e.tensor_tensor(out=AB[:, b * NT:(b + 1) * NT, :].rearrange("p t (g x) -> p t g x", g=8), in0=A1F, in1=A2F, op=ALU.add), w=["ab%d" % b])
        V(lambda e: e.tensor_scalar(out=R_(7), in0=R_(6), scalar1=1.0, scalar2=None, op0=ALU.add))
        V(lambda e: e.reciprocal(out=R_(8), in_=R_(7)))
        V(lambda e: e.tensor_tensor(out=G12[:, 0, b * NT:(b + 1) * NT], in0=R_(2), in1=R_(8), op=ALU.mult), w=["g12"])
        V(lambda e: e.tensor_tensor(out=G12[:, 1, b * NT:(b + 1) * NT], in0=R_(2), in1=G12[:, 0, b * NT:(b + 1) * NT], op=ALU.subtract), r=["g12"], w=["g12"])

    def S3B(b):
        def rank(e):
            for i in range(NT):
                tt = b * NT + i
                o = psf(7)[:, i * 64:(i + 1) * 64]
                for t2 in range(tt):
                    e.matmul(o, lhsT=ONESB, rhs=AB[:, t2, :], start=(t2 == 0), stop=False)
                ins = e.matmul(o, lhsT=TRI, rhs=AB[:, tt, :], start=(tt == 0), stop=True)
            return ins
        A(PE, rank, r=["ab%d" % i for i in range(b + 1)] + ["cb"], w=[pk(7)])
        V(lambda e: e.scalar_tensor_tensor(out=RB.rearrange("p t g x -> p t (g x)"), in0=psf(7)[:, 0:NT * 64].rearrange("p (t x) -> p t x", t=NT), scalar=float(EXPERT_CAP - 1),
                                           in1=OFFS.unsqueeze(1).broadcast_to([128, NT, 64]), op0=ALU.min, op1=ALU.add), r=[pk(7), "cf"])
        for a, AF_ in enumerate((A1F, A2F)):
            V(lambda e, AF_=AF_: e.tensor_tensor(out=T64, in0=AF_, in1=RB, op=ALU.mult))
            V(lambda e, a=a: e.tensor_reduce(out=R_(9 + a), in_=T64.rearrange("p t g x -> p t (g x)"), axis=AX.X, op=ALU.add))
            for i in range(NT):
                tt = b * NT + i
                dk = "dest%d_%d" % (a, tt)
                V(lambda e, a=a, i=i, tt=tt: e.tensor_copy(out=DEST[a][tt][:, :], in_=R_(9 + a)[:, i:i + 1]), w=[dk])
        for i in range(NT):
            tt = b * NT + i
            h2b, kb = H2B[tt % NHB], "h2b%d" % (tt % NHB)
            for a in range(2):
                dk = "dest%d_%d" % (a, tt)
                A(POOL, lambda e, a=a, tt=tt, h2b=h2b: e.indirect_dma_start(
                    out=xg, out_offset=bass.IndirectOffsetOnAxis(ap=DEST[a][tt][:, 0:1], axis=0), in_=h2b, in_offset=None,
                    bounds_check=None, oob_is_err=False), r=[dk, kb, "xg"], w=["xgs%d_%d" % (a, tt)], dma=True)

    S1(0)
    S1c(0)
    S1(1)
    S1b(0)
    S1c(1)
    for tt in range(16):
        if tt + 2 < 16:
            S1(tt + 2)
        if tt + 1 < 16:
            S1b(tt + 1)
        if tt + 2 < 16:
            S1c(tt + 2)
        if tt % NT == 1 and tt >= NT:
            S2B(tt // NT - 1)
        if tt % NT == 2 and tt >= NT:
            S3B(tt // NT - 1)
    S2B(3)
    S3B(3)

    if "route" in dumps:
        add_dump("g12", G12, ["g12"], [128, 2, 16], F32)
        for a in range(2):
            for tt in (0, 15):
                add_dump("dest%d_%d" % (a, tt), DEST[a][tt][:, :], ["dest%d_%d" % (a, tt)], [128, 1], I32)
        add_dump("xm", XM[1], ["xm1_0", "xm1_1"], [128, 1024], F32)
    if stage <= 4:
        P.emit(nc)
        return nc, dump_out

    M.reset("vt"); M.reset("r2"); M.reset("ut"); M.reset("qmt"); M.reset("r1")
    NW = 3
    W1 = [M.alloc("vt" if i == 0 else "r2", "w1_%d" % i, [128, 8, 512], BF16) for i in range(NW)]
    W3 = [M.alloc("vt" if i == 0 else "r2", "w3_%d" % i, [128, 8, 512], BF16) for i in range(NW)]
    W2 = [M.alloc("vt" if i == 0 else "ut", "w2_%d" % i, [128, 4, 1024], BF16) for i in range(NW)]
    XE = [M.alloc("vt", "xe%d" % i, [128, 1024], BF16) for i in range(2)]
    XET = [M.alloc("vt", "xet%d" % i, [128, 8, 128], BF16) for i in range(2)]
    SA = M.alloc("qmt", "sa", [128, 512], F32)
    HM = M.alloc("qmt", "hm", [128, 512], BF16)
    HMT = M.alloc("qmt", "hmt", [128, 4, 128], BF16)
    YE = [M.alloc("r1", "ye%d" % i, [128, 1024], BF16) for i in range(2)]

    def wload(e_):
        i = e_ % NW
        A(POOL, lambda e, i=i, e_=e_: e.dma_start(out=W1[i], in_=D["w1"][e_].rearrange("(k p) f -> p k f", p=128)), w=["w1_%d" % i], dma=True)
        A(POOL, lambda e, i=i, e_=e_: e.dma_start(out=W3[i], in_=D["w3"][e_].rearrange("(k p) f -> p k f", p=128)), w=["w3_%d" % i], dma=True)
        A(POOL, lambda e, i=i, e_=e_: e.dma_start(out=W2[i], in_=D["w2"][e_].rearrange("(k p) f -> p k f", p=128)), w=["w2_%d" % i], dma=True)

    NEXP = 64
    for e_ in range(min(NW - 1, NEXP)):
        wload(e_)
    for e_ in range(NEXP):
        if e_ + NW - 1 < NEXP:
            wload(e_ + NW - 1)
        i = e_ % NW
        s2 = e_ % 2
        xe, xet, ye = XE[s2], XET[s2], YE[s2]
        kxe, kxt, kye = "xe%d" % s2, "xet%d" % s2, "ye%d" % s2
        tb = 0 if s2 == 0 else 6
        ab = 1 if s2 == 0 else 7
        A(SP, lambda e, xe=xe, e_=e_: e.dma_start(out=xe, in_=xg[e_ * 128:(e_ + 1) * 128, :]), r=["xg"] + ["xgs%d_%d" % (a, t) for a in range(2) for t in range(16)], w=[kxe], dma=True)

        def trx(e, xe=xe, tb=tb):
            for k in range(8):
                ins = e.transpose(out=psb(tb)[:, k * 128:(k + 1) * 128], in_=xe[:, k * 128:(k + 1) * 128], identity=IDB)
            return ins
        A(PE, trx, r=[kxe, "cb"], w=[pk(tb)])
        A(ACT, lambda e, xet=xet, tb=tb: e.activation(out=xet, in_=psb(tb).rearrange("p (k t) -> p k t", k=8), func=AF.Copy), r=[pk(tb)], w=[kxt])

        def up(e, W, bank, xet=xet):
            for k in range(8):
                ins = e.matmul(psf(bank), lhsT=xet[:, k, :], rhs=W[:, k, :], start=(k == 0), stop=(k == 7))
            return ins
        A(PE, lambda e, i=i, ab=ab, up=up: up(e, W1[i], ab), r=[kxt, "w1_%d" % i], w=[pk(ab)])
        A(PE, lambda e, i=i, up=up: up(e, W3[i], 2), r=[kxt, "w3_%d" % i], w=[pk(2)])
        A(ACT, lambda e, ab=ab: e.activation(out=SA, in_=psf(ab), func=AF.Silu), r=[pk(ab)], w=["sa"])
        A(DVE, lambda e: e.tensor_tensor(out=HM, in0=SA, in1=psf(2), op=ALU.mult), r=["sa", pk(2)], w=["hm"])

        def trh(e):
            for k in range(4):
                ins = e.transpose(out=psb(3)[:, k * 128:(k + 1) * 128], in_=HM[:, k * 128:(k + 1) * 128], identity=IDB)
            return ins
        A(PE, trh, r=["hm", "cb"], w=[pk(3)])
        A(DVE, lambda e: e.tensor_copy(out=HMT, in_=psb(3)[:, 0:512].rearrange("p (k t) -> p k t", k=4)), r=[pk(3)], w=["hmt"])
        for half in range(2):
            def dn(e, i=i, half=half):
                for k in range(4):
                    ins = e.matmul(psf(4 + half), lhsT=HMT[:, k, :], rhs=W2[i][:, k, half * 512:(half + 1) * 512], start=(k == 0), stop=(k == 3))
                return ins
            A(PE, dn, r=["hmt", "w2_%d" % i], w=[pk(4 + half)])
        A(ACT, lambda e, ye=ye: e.activation(out=ye[:, 0:512], in_=psf(4), func=AF.Copy), r=[pk(4)], w=[kye + "a"])
        A(DVE, lambda e, ye=ye: e.tensor_copy(out=ye[:, 512:1024], in_=psf(5)), r=[pk(5)], w=[kye + "b"])
        A(SP, lambda e, ye=ye, e_=e_: e.dma_start(out=yg[e_ * 128:(e_ + 1) * 128, :], in_=ye), r=[kye + "a", kye + "b"], w=["yg%d" % e_], dma=True)

    if stage <= 5:
        P.emit(nc)
        return nc, dump_out

    M.reset("kt"); M.reset("vt")
    NC_ = 4
    Y1 = [M.alloc("kt", "y1_%d" % i, [128, 1024], BF16) for i in range(NC_)]
    Y2 = [M.alloc("kt", "y2_%d" % i, [128, 1024], BF16) for i in range(NC_)]
    XM2 = [M.alloc("vt", "xm2_%d" % i, [128, 1024], F32) for i in range(NC_)]
    OT_ = [M.alloc("vt", "ot_%d" % i, [128, 1024], F32) for i in range(NC_)]
    for tt in range(16):
        sl = tt % NC_
        tok = slice(tt * 128, (tt + 1) * 128)
        for a, Y in enumerate((Y1, Y2)):
            A(POOL, lambda e, a=a, tt=tt, y=Y[sl]: e.indirect_dma_start(
                out=y, out_offset=None, in_=yg, in_offset=bass.IndirectOffsetOnAxis(ap=DEST[a][tt][:, 0:1], axis=0),
                bounds_check=None, oob_is_err=False), r=["dest%d_%d" % (a, tt)] + ["yg%d" % i for i in range(64)], w=["y%d_%d" % (a + 1, sl)], dma=True)
        A(SP, lambda e, sl=sl, tok=tok: e.dma_start(out=XM2[sl], in_=xmid[tok, :]), r=["xmid%d" % tt], w=["xm2_%d" % sl], dma=True)
        A(DVE, lambda e, sl=sl, tt=tt: e.scalar_tensor_tensor(out=OT_[sl], in0=Y1[sl], scalar=G12[:, 0, tt:tt + 1], in1=XM2[sl], op0=ALU.mult, op1=ALU.add),
          r=["y1_%d" % sl, "xm2_%d" % sl, "g12"], w=["ot_%d" % sl])
        A(DVE, lambda e, sl=sl, tt=tt: e.scalar_tensor_tensor(out=OT_[sl], in0=Y2[sl], scalar=G12[:, 1, tt:tt + 1], in1=OT_[sl], op0=ALU.mult, op1=ALU.add),
          r=["y2_%d" % sl, "ot_%d" % sl, "g12"], w=["ot_%d" % sl])
        A(SP, lambda e, sl=sl, tok=tok: e.dma_start(out=out[tok, :], in_=OT_[sl]), r=["ot_%d" % sl], w=["out"], dma=True)
    if "moe" in dumps:
        allxg = ["xg"] + ["xgs%d_%d" % (a, t) for a in range(2) for t in range(16)]
        for ee in (0, 37):
            add_dump("xg%d" % ee, xg[ee * 128:(ee + 1) * 128, :], allxg, [128, 1024], BF16)
            add_dump("yg%d" % ee, yg[ee * 128:(ee + 1) * 128, :], ["yg%d" % ee], [128, 1024], BF16)
        add_dump("g12", G12, ["g12"], [128, 2, 16], F32)
        for a in range(2):
            for tt in range(16):
                add_dump("dest%d_%d" % (a, tt), DEST[a][tt][:, :], ["dest%d_%d" % (a, tt)], [128, 1], I32)
        add_dump("xmid", xmid, ["xmid%d" % t for t in range(16)], [2048, 1024], F32)
    P.emit(nc)
    return nc, dump_out
```

```python
import numpy as np
import concourse.bass as bass
import concourse.mybir as mybir
from concourse.bass_utils import run_bass_kernel_spmd

F32 = mybir.dt.float32
BF16 = mybir.dt.bfloat16
I32 = mybir.dt.int32
AF = mybir.ActivationFunctionType
ALU = mybir.AluOpType
AX = mybir.AxisListType

PE, ACT, DVE, POOL, SP = "pe", "act", "dve", "pool", "sp"
ENGS = (PE, ACT, DVE, POOL, SP)
RING = 12


class Op:
    __slots__ = ("eng", "fn", "deps", "dma", "need", "seq", "di", "name")

    def __init__(self, eng, fn, dma, name):
        self.eng, self.fn, self.dma, self.name = eng, fn, dma, name
        self.deps, self.need, self.seq, self.di = [], False, 0, -1


class Prog:
    def __init__(self):
        self.ops = {e: [] for e in ENGS}
        self.lastw = {}
        self.readers = {}

    def add(self, eng, fn, r=(), w=(), dma=False, name=""):
        op = Op(eng, fn, dma, name)
        deps = {}
        for k in r:
            lw = self.lastw.get(k)
            if lw is not None:
                deps[id(lw)] = (lw, True)
        for k in w:
            lw = self.lastw.get(k)
            if lw is not None:
                deps[id(lw)] = (lw, True)
            for rd in self.readers.get(k, ()):
                if id(rd) not in deps:
                    deps[id(rd)] = (rd, False)
        for k in r:
            self.readers.setdefault(k, []).append(op)
        for k in w:
            self.lastw[k] = op
            self.readers[k] = []
        op.deps = [d for d in deps.values() if d[0] is not op]
        self.ops[eng].append(op)
        return op

    @staticmethod
    def _needs_wait(o, d, true_dep):
        if d.dma:
            return True
        if d.eng != o.eng:
            return True
        if o.eng == PE:
            return False
        if o.dma:
            return True
        return true_dep

    def emit(self, nc):
        for e in ENGS:
            for o in self.ops[e]:
                for d, t in o.deps:
                    if self._needs_wait(o, d, t):
                        d.need = True
        for e in ENGS:
            c = 0
            n = 0
            for o in self.ops[e]:
                if o.dma:
                    o.di = n
                    n += 1
                elif o.need:
                    c += 1
                    o.seq = c
        from contextlib import ExitStack

        with ExitStack() as st:
            esem = {e: st.enter_context(nc.semaphore("c_" + e)) for e in (PE, ACT, DVE, POOL)}
            rsem = {
                e: [st.enter_context(nc.semaphore("r_%s%d" % (e, i))) for i in range(RING)]
                for e in (SP, POOL, ACT)
            }
            block = st.enter_context(nc.Block())

            def run(ename, eng):
                seen = {}

                def wait(sem, val):
                    if seen.get(sem.num, 0) < val:
                        eng.wait_ge(sem, val)
                        seen[sem.num] = val

                ndma = 0
                for o in self.ops[ename]:
                    for d, t in o.deps:
                        if not self._needs_wait(o, d, t):
                            continue
                        if d.dma:
                            wait(rsem[d.eng][d.di % RING], 16 * (d.di // RING + 1))
                        else:
                            wait(esem[d.eng], d.seq)
                    if o.dma:
                        if o.di >= RING:
                            wait(rsem[ename][o.di % RING], 16 * (o.di // RING))
                        ins = o.fn(eng)
                        ins.then_inc(rsem[ename][o.di % RING], 16)
                        ndma = o.di + 1
                    else:
                        ins = o.fn(eng)
                        if o.need:
                            ins.then_inc(esem[ename], 1)
                for i in range(max(0, ndma - RING), ndma):
                    wait(rsem[ename][i % RING], 16 * (i // RING + 1))

            block.tensor(lambda eng: run(PE, eng))
            block.scalar(lambda eng: run(ACT, eng))
            block.vector(lambda eng: run(DVE, eng))
            block.gpsimd(lambda eng: run(POOL, eng))
            block.sync(lambda eng: run(SP, eng))


class Mem:
    def __init__(self, nc, P, total_bytes):
        self.P = P
        self.t = nc.alloc_sbuf_tensor("arena", [128, total_bytes // 4], F32)
        self.regs = {}
        self.top = 0
        self.total = total_bytes

    def region(self, name, size):
        size = (size + 63) // 64 * 64
        assert self.top + size <= self.total, (name, self.top, size)
        self.regs[name] = dict(off=self.top, size=size, cur=0, keys=[], old=[])
        self.top += size

    def reset(self, name):
        r = self.regs[name]
        r["old"] = r["old"] + r["keys"]
        r["keys"] = []
        r["cur"] = 0

    def alloc(self, reg, key, shape, dt, keys=None):
        r = self.regs[reg]
        isz = 2 if dt == BF16 else 4
        n = 1
        for s in shape[1:]:
            n *= s
        nb = (n * isz + 63) // 64 * 64
        assert r["cur"] + nb <= r["size"], (reg, key, r["cur"], nb, r["size"])
        off = r["off"] + r["cur"]
        r["cur"] += nb
        keys = list(keys) if keys else [key]
        r["keys"].extend(keys)
        olds = []
        for ok in r["old"]:
            lw = self.P.lastw.get(ok)
            if lw is not None:
                olds.append(lw)
            olds.extend(self.P.readers.get(ok, ()))
        if olds:
            for kk in keys:
                self.P.readers.setdefault(kk, []).extend(olds)
        ap = self.t[:, off // 4:(off + n * isz + 3) // 4]
        if dt == BF16:
            ap = ap.bitcast(BF16)
        ap = ap[:, 0:n]
        if len(shape) == 3:
            ap = ap.rearrange("p (a b) -> p a b", a=shape[1])
        elif len(shape) == 4:
            ap = ap.rearrange("p (a b c) -> p a b c", a=shape[1], b=shape[2])
        return ap


NCF = 1416
NCB = 1280
CF_GATTN, CF_GMEM, CF_GQ, CF_GK, CF_GMQ, CF_GMK = 0, 8, 16, 17, 18, 19
CF_INVW, CF_PSC, CF_IC16, CF_HV, CF_EPS = 20, 22, 24, 56, 57
CF_ONES, CF_OFFS, CF_BIAS, CF_IDF, CF_GFFN = 64, 128, 192, 264, 392
CB_ID, CB_MASK, CB_BD, CB_ONES, CB_TRI, CB_PBD = 0, 128, 640, 768, 896, 1024
EPS = 1e-6
EXPERT_CAP = 128


def build_program(stage=99, dumps=()):
    nc = bass.Bass("TRN2", target_bir_lowering=False)
    P = Prog()
    D = {}

    def din(name, shape, dt=F32):
        D[name] = nc.dram_tensor(name, shape, dt, kind="ExternalInput").ap()

    din("xo", [2048, 1024]); din("xh", [2048, 1024]); din("mem", [256, 1024])
    din("w_in", [1024, 2048]); din("w_out", [1024, 1024]); din("w_kv", [1024, 512])
    din("w_r", [1024, 72]); din("cf", [128, NCF]); din("cb", [128, NCB])
    if stage >= 5:
        din("w1", [64, 1024, 512]); din("w3", [64, 1024, 512]); din("w2", [64, 512, 1024])
    out = nc.dram_tensor("out", [2048, 1024], F32, kind="ExternalOutput").ap()
    xg = nc.dram_tensor("xg", [64 * EXPERT_CAP, 1024], BF16, kind="Internal").ap()
    yg = nc.dram_tensor("yg", [64 * EXPERT_CAP, 1024], BF16, kind="Internal").ap()
    xmid = nc.dram_tensor("xmid", [2048, 1024], F32, kind="Internal").ap()
    dump_out = {}

    M = Mem(nc, P, 203 * 1024)
    M.region("const", 12 * 1024)
    M.region("wout", 16 * 1024)
    M.region("r1", 32 * 1024)
    M.region("qt", 16 * 1024)
    M.region("kt", 32 * 1024)
    M.region("vt", 32 * 1024)
    M.region("r2", 36 * 1024)
    M.region("ut", 16640)
    M.region("qmt", 8 * 1024)
    PS = [nc.alloc_psum_tensor("ps%d" % i, [128, 512], F32) for i in range(8)]

    def psf(i):
        return PS[i][:, :]

    def psb(i):
        return PS[i][:, :].bitcast(BF16)

    def pk(i):
        return "ps%d" % i

    A = P.add
    CF = M.alloc("const", "cf", [128, NCF], F32)
    CB = M.alloc("const", "cb", [128, NCB], BF16)
    ZT = M.alloc("const", "zt", [128, 1024], BF16)
    IDB = CB[:, CB_ID:CB_ID + 128]
    MASK4 = CB[:, CB_MASK:CB_MASK + 512]
    BD64 = CB[:, CB_BD:CB_BD + 128]
    ONESB = CB[:, CB_ONES:CB_ONES + 128]
    TRI = CB[:, CB_TRI:CB_TRI + 128]
    EPSC = CF[:, CF_EPS:CF_EPS + 1]
    IDF = CF[:, CF_IDF:CF_IDF + 128]
    ONES64 = CF[:, CF_ONES:CF_ONES + 64]

    A(SP, lambda e: e.dma_start(out=CF, in_=D["cf"]), w=["cf"], dma=True)
    A(POOL, lambda e: e.dma_start(out=CB, in_=D["cb"]), w=["cb"], dma=True)
    WIN = M.alloc("r1", "win", [128, 8, 2048], BF16, keys=["win%d" % i for i in range(4)])
    win_keys = []
    for i in range(4):
        key = "win%d" % i
        win_keys.append(key)
        A(POOL, lambda e, i=i: e.dma_start(
            out=WIN[:, 2 * i:2 * i + 2, :],
            in_=D["w_in"][256 * i:256 * i + 256, :].rearrange("(k p) c -> p k c", p=128)),
          w=[key], dma=True)
    A(DVE, lambda e: e.memset(ZT, 0.0), w=["zt"])
    ZT4 = ZT.unsqueeze(1).broadcast_to([128, 4, 1024])

    QT = M.alloc("qt", "qt", [128, 4, 2048], BF16, keys=["qt%d_%d" % (c, g) for c in range(4) for g in range(4)])
    KT = M.alloc("kt", "kt", [128, 4, 4096], BF16, keys=["kt%d_%d" % (c, g) for c in range(4) for g in range(8)])
    VT = M.alloc("vt", "vt", [128, 4, 4096], BF16, keys=["vt%d_%d" % (c, g) for c in range(4) for g in range(8)])
    UT = M.alloc("ut", "ut", [128, 2, 2064], F32, keys=["ut0", "ut1"])
    QMT = M.alloc("qmt", "qmt", [128, 2, 2048], BF16, keys=["qmt0", "qmt1"])
    XT = [M.alloc("r2", "xt%d" % i, [128, 1024], F32) for i in range(2)]
    XN = [M.alloc("r2", "xn%d" % i, [128, 1024], BF16) for i in range(2)]
    HTG = [M.alloc("r2", "htg%d" % i, [128, 8, 512], BF16, keys=["htg%d_%d" % (i, t) for t in range(4)]) for i in range(2)]
    SQZ = [M.alloc("r2", "sqz%d" % i, [128, 512], BF16) for i in range(2)]
    RS = [M.alloc("r2", "rs%d" % i, [128, 512], F32) for i in range(2)]
    ST4 = [M.alloc("r2", "st4_%d" % i, [128, 4], F32) for i in range(2)]

    def norm_front(src_ap, slot):
        xt, xn, st = XT[slot], XN[slot], ST4[slot]
        kx, kn, ks = "xt%d" % slot, "xn%d" % slot, "st4_%d" % slot
        A(SP, lambda e: e.dma_start(out=xt, in_=src_ap), w=[kx], dma=True)
        A(ACT, lambda e: e.activation(out=xn, in_=xt, func=AF.Square, accum_out=st[:, 0:1]), r=[kx], w=[kn, ks])
        A(ACT, lambda e: e.activation(out=st[:, 1:2], in_=st[:, 0:1], func=AF.Ln, bias=EPSC, scale=1.0 / 1024), r=[ks, "cf"], w=[ks])
        A(ACT, lambda e: e.activation(out=st[:, 2:3], in_=st[:, 1:2], func=AF.Exp, scale=-0.5), r=[ks], w=[ks])
        A(DVE, lambda e: e.tensor_scalar(out=xn, in0=xt, scalar1=st[:, 2:3], scalar2=None, op0=ALU.mult), r=[kx, ks], w=[kn])

    def norm_back(slot, gain_col, dst_fn, dkeys, ti):
        xn, kn = XN[slot], "xn%d" % slot
        tp = ti % 2

        def tr(e):
            for k in range(8):
                ins = e.transpose(out=psb(tp)[:, k * 128:(k + 1) * 128], in_=xn[:, k * 128:(k + 1) * 128], identity=IDB)
            return ins
        A(PE, tr, r=[kn, "cb"], w=[pk(tp)])
        g = CF[:, gain_col:gain_col + 8].unsqueeze(2).broadcast_to([128, 8, 128])
        A(DVE, lambda e: e.tensor_tensor(out=dst_fn(), in0=psb(tp).rearrange("p (k t) -> p k t", k=8), in1=g, op=ALU.mult),
          r=[pk(tp), "cf"], w=dkeys)

    def norm_tile(src_ap, slot, gain_col, dst_fn, dkeys, ti):
        norm_front(src_ap, slot)
        norm_back(slot, gain_col, dst_fn, dkeys, ti)

    def head_norm(zbank, zkey, n, gain_col, dst, dkeys, i):
        sq, rs = SQZ[i % 2], RS[i % 2]
        ksq, krs = "sqz%d" % (i % 2), "rs%d" % (i % 2)
        mb = 4 + i % 2
        A(ACT, lambda e: e.activation(out=sq[:, 0:n], in_=psf(zbank)[:, 0:n], func=AF.Square), r=[zkey], w=[ksq])
        A(PE, lambda e: e.matmul(psf(mb)[:, 0:n], lhsT=BD64, rhs=sq[:, 0:n], start=True, stop=True), r=[ksq, "cb"], w=[pk(mb)])
        A(ACT, lambda e: e.activation(out=rs[:, 0:n], in_=psf(mb)[:, 0:n], func=AF.Ln, bias=EPSC, scale=1.0), r=[pk(mb), "cf"], w=[krs])
        A(ACT, lambda e: e.activation(out=rs[:, 0:n], in_=rs[:, 0:n], func=AF.Exp, scale=-0.5), r=[krs], w=[krs])
        A(DVE, lambda e: e.scalar_tensor_tensor(out=dst, in0=psf(zbank)[:, 0:n], scalar=CF[:, gain_col:gain_col + 1], in1=rs[:, 0:n],
                                                 op0=ALU.mult, op1=ALU.mult), r=[zkey, krs, "cf"], w=dkeys)

    cnt = dict(ti=0, zi=0, hn=0)

    def tile_front(g, t):
        src = (D["xh"] if g < 4 else D["xo"])[((g % 4) * 4 + t) * 128:((g % 4) * 4 + t + 1) * 128, :]
        norm_front(src, (g * 4 + t) % 2)

    def tile_back(g, t):
        hb = HTG[g % 2]
        ti = g * 4 + t
        norm_back(ti % 2, CF_GATTN, lambda: hb[:, :, t * 128:(t + 1) * 128], ["htg%d_%d" % (g % 2, t)], ti)

    def tile_task(g, t):
        tile_front(g, t)
        tile_back(g, t)

    ZB = (2, 3, 6, 7)

    def chunk_part1(g, c):
        hb = HTG[g % 2]
        hkeys = ["htg%d_%d" % (g % 2, t) for t in range(4)]
        zb = ZB[cnt["zi"] % 4]
        cnt["zi"] += 1

        def proj(e):
            for k in range(8):
                ins = e.matmul(psf(zb), lhsT=WIN[:, k, c * 128:(c + 1) * 128], rhs=hb[:, k, :], start=(k == 0), stop=(k == 7))
            return ins
        A(PE, proj, r=win_keys + hkeys, w=[pk(zb)])
        st = dict(g=g, c=c, zb=zb)
        if c < 8 or c >= 14:
            i = cnt["hn"]
            cnt["hn"] += 1
            st["i"] = i
            sq, ksq = SQZ[i % 2], "sqz%d" % (i % 2)
            A(ACT, lambda e: e.activation(out=sq, in_=psf(zb), func=AF.Square), r=[pk(zb)], w=[ksq])
        return st

    def chunk_part2(st):
        g, c, zb = st["g"], st["c"], st["zb"]
        if 8 <= c < 12:
            A(DVE, lambda e: e.tensor_copy(out=VT[:, c - 8, g * 512:(g + 1) * 512], in_=psf(zb)),
              r=[pk(zb)], w=["vt%d_%d" % (c - 8, g)])
            return
        if c in (12, 13):
            if g == 3:
                A(DVE, lambda e: e.tensor_copy(out=UT[:, c - 12, 0:16], in_=psf(zb)[:, 496:512]), r=[pk(zb)], w=["ut%d" % (c - 12)])
            else:
                A(DVE, lambda e: e.tensor_copy(out=UT[:, c - 12, 16 + (g - 4) * 512:16 + (g - 3) * 512], in_=psf(zb)),
                  r=[pk(zb)], w=["ut%d" % (c - 12)])
            return
        if c < 4:
            gain_col, dst, dkeys = CF_GQ, QT[:, c, (g - 4) * 512:(g - 3) * 512], ["qt%d_%d" % (c, g - 4)]
        elif c < 8:
            gain_col, dst, dkeys = CF_GK, KT[:, c - 4, g * 512:(g + 1) * 512], ["kt%d_%d" % (c - 4, g)]
        else:
            gain_col, dst, dkeys = CF_GMQ, QMT[:, c - 14, (g - 4) * 512:(g - 3) * 512], ["qmt%d" % (c - 14)]
        i = st["i"]
        sq, rs = SQZ[i % 2], RS[i % 2]
        ksq, krs = "sqz%d" % (i % 2), "rs%d" % (i % 2)
        mb = 4 + i % 2
        A(PE, lambda e: e.matmul(psf(mb), lhsT=BD64, rhs=sq, start=True, stop=True), r=[ksq, "cb"], w=[pk(mb)])
        A(ACT, lambda e: e.activation(out=rs, in_=psf(mb), func=AF.Ln, bias=EPSC, scale=1.0), r=[pk(mb), "cf"], w=[krs])
        A(ACT, lambda e: e.activation(out=rs, in_=rs, func=AF.Exp, scale=-0.5), r=[krs], w=[krs])
        A(DVE, lambda e: e.scalar_tensor_tensor(out=dst, in0=psf(zb), scalar=CF[:, gain_col:gain_col + 1], in1=rs,
                                                 op0=ALU.mult, op1=ALU.mult), r=[pk(zb), krs, "cf"], w=dkeys)

    def chunks_of(g):
        k_, v_ = list(range(4, 8)), list(range(8, 12))
        ch = []
        if g >= 4:
            extra = [0, 1, 2, 3, 14, 15, 12, 13]
        elif g == 3:
            extra = [12, 13]
        else:
            extra = []
        normed = k_ + [c for c in extra if c < 4 or c >= 14]
        cheap = v_ + [c for c in extra if c in (12, 13)]
        while normed or cheap:
            if normed:
                ch.append(normed.pop(0))
            if cheap:
                ch.append(cheap.pop(0))
        return ch

    tile_front(0, 0)
    for t in range(4):
        if t + 1 < 4:
            tile_front(0, t + 1)
        tile_back(0, t)
    pending = None
    for g in range(8):
        ch = chunks_of(g)
        pts = [max(1, (len(ch) * (j + 1)) // 5) for j in range(5)]
        nt = 0
        for i, c in enumerate(ch):
            st = chunk_part1(g, c)
            if pending is not None:
                chunk_part2(pending)
            pending = st
            while g + 1 < 8 and nt < 5 and (i + 1) >= pts[nt]:
                if nt < 4:
                    tile_front(g + 1, nt)
                if nt >= 1:
                    tile_back(g + 1, nt - 1)
                nt += 1
        while g + 1 < 8 and nt < 5:
            if nt < 4:
                tile_front(g + 1, nt)
            if nt >= 1:
                tile_back(g + 1, nt - 1)
            nt += 1
    chunk_part2(pending)

    def add_dump(name, ap, keys, shape, dt):
        t = nc.dram_tensor("dump_" + name, shape, dt, kind="ExternalOutput").ap()
        dump_out[name] = t
        A(SP, lambda e: e.dma_start(out=t, in_=ap), r=keys, w=["dump_" + name], dma=True)

    vt_keys = lambda c: ["vt%d_%d" % (c, g) for g in range(8)]
    kt_keys = lambda c: ["kt%d_%d" % (c, g) for g in range(8)]
    qt_keys = lambda c: ["qt%d_%d" % (c, g) for g in range(4)]
    if "qkv" in dumps:
        add_dump("qt", QT, sum([qt_keys(c) for c in range(4)], []), [128, 4, 2048], BF16)
        add_dump("kt", KT, sum([kt_keys(c) for c in range(4)], []), [128, 4, 4096], BF16)
        add_dump("vt", VT, sum([vt_keys(c) for c in range(4)], []), [128, 4, 4096], BF16)
        add_dump("ut", UT, ["ut0", "ut1"], [128, 2, 2064], F32)
        add_dump("qmt", QMT, ["qmt0", "qmt1"], [128, 2, 2048], BF16)
    if stage <= 1:
        P.emit(nc)
        return nc, dump_out
    return build_rest(nc, P, M, D, A, CF, CB, PS, psf, psb, pk, out, xg, yg, xmid, stage, dumps, dump_out, add_dump,
                      dict(QT=QT, KT=KT, VT=VT, UT=UT, QMT=QMT, WIN=WIN, vt_keys=vt_keys, kt_keys=kt_keys, qt_keys=qt_keys,
                           norm_tile=norm_tile, head_norm=head_norm, ZT4=ZT4, XT=XT, XN=XN, ST4=ST4, SQZ=SQZ, RS=RS, win_keys=win_keys))


POOL_WINDOWS = (2, 4, 8, 16)


def prep_inputs(inp):
    f = np.float32
    x = np.ascontiguousarray(inp["x"], dtype=f)
    mem = np.ascontiguousarray(inp["mem"], dtype=f)
    cb = np.zeros((128, NCB), f)
    k = np.arange(128)[:, None]
    q = np.arange(128)[None, :]
    cb[:, CB_ID:CB_ID + 128] = np.eye(128, dtype=f)
    mprev = (k >= q).astype(f)
    mcur = (k <= q).astype(f)
    cb[:, CB_MASK:CB_MASK + 512] = np.concatenate([mprev, mcur, mprev, mcur], axis=1)
    bd = np.zeros((128, 128), f)
    bd[:64, :64] = 1.0 / 64
    bd[64:, 64:] = 1.0 / 64
    cb[:, CB_BD:CB_BD + 128] = bd
    cb[:, CB_ONES:CB_ONES + 128] = 1.0
    cb[:, CB_TRI:CB_TRI + 128] = (k < q).astype(f)
    pp = np.asarray(inp["pool_proj"], dtype=f)[0]
    for c in range(2):
        cb[0:64, CB_PBD + c * 128:CB_PBD + c * 128 + 64] = pp[2 * c]
        cb[64:128, CB_PBD + c * 128 + 64:CB_PBD + c * 128 + 128] = pp[2 * c + 1]
    w_r = np.concatenate([np.asarray(inp["w_group"], f)[0],
                          np.transpose(np.asarray(inp["w_router"], f)[0], (1, 0, 2)).reshape(1024, 64)], axis=1)
    w_r = np.ascontiguousarray(w_r)
    shared = dict(
        w_in=np.ascontiguousarray(inp["w_in"][0], dtype=f), w_out=np.ascontiguousarray(inp["w_out"][0], dtype=f),
        w_kv=np.ascontiguousarray(inp["w_mem_kv"][0], dtype=f), w_r=w_r,
        w1=np.ascontiguousarray(inp["w1"][0], dtype=f), w3=np.ascontiguousarray(inp["w3"][0], dtype=f),
        w2=np.ascontiguousarray(inp["w2"][0], dtype=f), cb=cb)
    p = np.arange(128)
    in_maps = []
    for c in range(8):
        b, half = c // 2, c % 2
        cf = np.zeros((128, NCF), f)
        cf[:, CF_GATTN:CF_GATTN + 8] = np.asarray(inp["attn_norm"], f)[0].reshape(8, 128).T
        cf[:, CF_GMEM:CF_GMEM + 8] = np.asarray(inp["mem_norm"], f)[0].reshape(8, 128).T
        cf[:, CF_GQ] = np.tile(np.asarray(inp["q_norm"], f)[0], 2)
        cf[:, CF_GK] = np.tile(np.asarray(inp["k_norm"], f)[0], 2)
        cf[:, CF_GMQ] = np.tile(np.asarray(inp["mq_norm"], f)[0], 2)
        cf[:, CF_GMK] = np.tile(np.asarray(inp["mk_norm"], f)[0], 2)
        for ch in range(2):
            w = np.array([POOL_WINDOWS[2 * ch + pi // 64] for pi in range(128)], f)
            cf[:, CF_INVW + ch] = 1.0 / w
            cf[:, CF_PSC + ch] = np.asarray(inp["pool_scale"], f)[0, ch * 128:(ch + 1) * 128]
            for t in range(16):
                pos = t + 2048 * half
                cf[:, CF_IC16 + ch * 16 + t] = 1.0 / np.minimum(pos + 1, w)
        cf[:, CF_HV] = float(half)
        cf[:, CF_EPS] = EPS
        cf[:, CF_ONES:CF_ONES + 64] = 1.0
        cf[:, CF_OFFS:CF_OFFS + 64] = (np.arange(64) * EXPERT_CAP).astype(f)[None, :]
        cf[:, CF_BIAS:CF_BIAS + 8] = np.asarray(inp["b_group"], f)[0][None, :]
        cf[:, CF_BIAS + 8:CF_BIAS + 72] = np.asarray(inp["b_router"], f)[0].reshape(64)[None, :]
        cf[:, CF_IDF:CF_IDF + 128] = np.eye(128, dtype=f)
        cf[:, CF_GFFN:CF_GFFN + 1024] = np.asarray(inp["ffn_norm"], f)[0][None, :]
        xo = x[b, half * 2048:(half + 1) * 2048]
        xh = x[b, 0:2048] if half == 1 else np.zeros((2048, 1024), f)
        m = dict(shared)
        m.update(xo=np.ascontiguousarray(xo), xh=np.ascontiguousarray(xh), mem=mem[b], cf=cf)
        in_maps.append(m)
    return in_maps


_NC_CACHE = {}


def kernel(**inputs):
    in_maps = prep_inputs(inputs)
    if "nc" not in _NC_CACHE:
        _NC_CACHE["nc"] = build_program()[0]
    res = run_bass_kernel_spmd(_NC_CACHE["nc"], in_maps, core_ids=list(range(8)))
    outs = [np.asarray(r["out"], dtype=np.float32) for r in res.results]
    return np.stack([np.concatenate([outs[2 * b], outs[2 * b + 1]], axis=0) for b in range(4)], axis=0)


def build_rest(nc, P, M, D, A, CF, CB, PS, psf, psb, pk, out, xg, yg, xmid, stage, dumps, dump_out, add_dump, S):
    QT, KT, VT, UT, QMT = S["QT"], S["KT"], S["VT"], S["UT"], S["QMT"]
    vt_keys, kt_keys, qt_keys = S["vt_keys"], S["kt_keys"], S["qt_keys"]
    IDB = CB[:, CB_ID:CB_ID + 128]
    MASK4 = CB[:, CB_MASK:CB_MASK + 512]
    ONESB = CB[:, CB_ONES:CB_ONES + 128]
    TRI = CB[:, CB_TRI:CB_TRI + 128]
    EPSC = CF[:, CF_EPS:CF_EPS + 1]
    IDF = CF[:, CF_IDF:CF_IDF + 128]
    ONES64 = CF[:, CF_ONES:CF_ONES + 64]
    HV = CF[:, CF_HV:CF_HV + 1]

    WOUT = M.alloc("wout", "wout", [128, 8, 1024], BF16)
    A(POOL, lambda e: e.dma_start(out=WOUT, in_=D["w_out"].rearrange("(k p) c -> p k c", p=128)), w=["wout"], dma=True)

    M.reset("r1")
    MIXT = M.alloc("r1", "mixt", [128, 8, 2048], BF16, keys=["mixt%d" % i for i in range(8)])
    M.reset("r2")
    TA = M.alloc("r2", "ta", [128, 2064], F32)
    TB = M.alloc("r2", "tb", [128, 2064], F32)
    PL = M.alloc("r2", "pl", [128, 2, 2048], BF16, keys=["pl0", "pl1"])
    T16 = M.alloc("r2", "t16", [128, 16], F32)
    IC = CF[:, CF_IC16:CF_IC16 + 32].rearrange("p (c t) -> p c t", c=2)
    N = 2064
    for c in range(2):
        U = UT[:, c, :]
        uk = "ut%d" % c
        A(POOL, lambda e, U=U: e.tensor_tensor(out=TA[:, 1:N], in0=U[:, 1:N], in1=U[:, 0:N - 1], op=ALU.add), r=[uk], w=["ta"])
        if c == 0:
            A(POOL, lambda e: e.tensor_tensor(out=TB[64:128, 3:N], in0=TA[64:128, 3:N], in1=TA[64:128, 1:N - 2], op=ALU.add), r=["ta"], w=["tb"])
        else:
            A(POOL, lambda e: e.tensor_tensor(out=TB[:, 3:N], in0=TA[:, 3:N], in1=TA[:, 1:N - 2], op=ALU.add), r=["ta"], w=["tb"])
            A(DVE, lambda e: e.tensor_tensor(out=TA[:, 7:N], in0=TB[:, 7:N], in1=TB[:, 3:N - 4], op=ALU.add), r=["tb"], w=["ta"])
            A(DVE, lambda e: e.tensor_tensor(out=TB[64:128, 15:N], in0=TA[64:128, 15:N], in1=TA[64:128, 7:N - 8], op=ALU.add), r=["ta"], w=["tb"])
        for (lo, hi, T, tk) in ((0, 64, TA, "ta"), (64, 128, TB, "tb")):
            A(DVE, lambda e, lo=lo, hi=hi, T=T, U=U, c=c: e.scalar_tensor_tensor(
                out=PL[lo:hi, c, :], in0=T[lo:hi, 16:N], scalar=CF[lo:hi, CF_INVW + c:CF_INVW + c + 1], in1=U[lo:hi, 16:N],
                op0=ALU.mult, op1=ALU.subtract), r=[tk, uk, "cf"], w=["pl%d" % c])
            A(DVE, lambda e, lo=lo, hi=hi, T=T, c=c: e.tensor_tensor(out=T16[lo:hi, :], in0=T[lo:hi, 16:32], in1=IC[lo:hi, c, :], op=ALU.mult),
              r=[tk, "cf"], w=["t16"])
        A(DVE, lambda e, U=U, c=c: e.tensor_tensor(out=PL[:, c, 0:16], in0=T16, in1=U[:, 16:32], op=ALU.subtract), r=["t16", uk], w=["pl%d" % c])
        for tg in range(4):
            zb = 2 + tg % 2
            A(PE, lambda e, c=c, tg=tg, zb=zb: e.matmul(psf(zb), lhsT=CB[:, CB_PBD + c * 128:CB_PBD + (c + 1) * 128],
                                                         rhs=PL[:, c, tg * 512:(tg + 1) * 512], start=True, stop=True),
              r=["pl%d" % c, "cb"], w=[pk(zb)])
            A(ACT, lambda e, c=c, tg=tg, zb=zb: e.activation(out=MIXT[:, 4 + c, tg * 512:(tg + 1) * 512], in_=psf(zb), func=AF.Copy,
                                                             scale=CF[:, CF_PSC + c:CF_PSC + c + 1]),
              r=[pk(zb), "cf"], w=["mixt%d" % (4 + c)])

    M.reset("r2")
    M.reset("ut")
    XT = [M.alloc("r2", "xt%d" % i, [128, 1024], F32) for i in range(2)]
    XN = [M.alloc("r2", "xn%d" % i, [128, 1024], BF16) for i in range(2)]
    SQZ = [M.alloc("r2", "sqz%d" % i, [128, 512], BF16) for i in range(2)]
    RS = [M.alloc("r2", "rs%d" % i, [128, 512], F32) for i in range(2)]
    ST4 = [M.alloc("r2", "st4_%d" % i, [128, 4], F32) for i in range(2)]
    MEMT = M.alloc("r2", "memt", [128, 8, 256], BF16, keys=["memt0", "memt1"])
    KMT = M.alloc("r2", "kmt", [128, 2, 256], BF16, keys=["kmt0", "kmt1"])
    VMA = M.alloc("r2", "vma", [128, 2, 4, 65], BF16)
    PTM = [M.alloc("r2", "ptm%d" % i, [128, 512], BF16) for i in range(2)]
    ACM = [M.alloc("r2", "acm%d" % i, [128, 512], F32) for i in range(2)]
    WKV = M.alloc("ut", "wkv", [128, 8, 512], BF16)
    TMPO = [M.alloc("ut", "tmpo%d" % i, [128, 2048], BF16) for i in range(2)]
    S["XT"], S["XN"], S["ST4"], S["SQZ"], S["RS"] = XT, XN, ST4, SQZ, RS
    A(POOL, lambda e: e.dma_start(out=WKV, in_=D["w_kv"].rearrange("(k p) c -> p k c", p=128)), w=["wkv"], dma=True)
    A(DVE, lambda e: e.memset(VMA[:, :, :, 64:65], 1.0), w=["vma"])

    def norm_tile(src_ap, slot, gain_col, dst, dkeys, ti):
        xt, xn, st = XT[slot], XN[slot], ST4[slot]
        kx, kn, ks = "xt%d" % slot, "xn%d" % slot, "st4_%d" % slot
        tp = ti % 2
        A(SP, lambda e: e.dma_start(out=xt, in_=src_ap), w=[kx], dma=True)
        A(ACT, lambda e: e.activation(out=xn, in_=xt, func=AF.Square, accum_out=st[:, 0:1]), r=[kx], w=[kn, ks])
        A(ACT, lambda e: e.activation(out=st[:, 1:2], in_=st[:, 0:1], func=AF.Ln, bias=EPSC, scale=1.0 / 1024), r=[ks, "cf"], w=[ks])
        A(ACT, lambda e: e.activation(out=st[:, 2:3], in_=st[:, 1:2], func=AF.Exp, scale=-0.5), r=[ks], w=[ks])
        A(ACT, lambda e: e.activation(out=xn, in_=xt, func=AF.Copy, scale=st[:, 2:3]), r=[kx, ks], w=[kn])

        def tr(e):
            for k in range(8):
                ins = e.transpose(out=psb(tp)[:, k * 128:(k + 1) * 128], in_=xn[:, k * 128:(k + 1) * 128], identity=IDB)
            return ins
        A(PE, tr, r=[kn, "cb"], w=[pk(tp)])
        g = CF[:, gain_col:gain_col + 8].unsqueeze(2).broadcast_to([128, 8, 128])
        A(DVE, lambda e: e.tensor_tensor(out=dst, in0=psb(tp).rearrange("p (k t) -> p k t", k=8), in1=g, op=ALU.mult),
          r=[pk(tp), "cf"], w=dkeys)

    def head_norm(zbank, zkey, n, gain_col, dst, dkeys, i):
        sq, rs = SQZ[i % 2], RS[i % 2]
        ksq, krs = "sqz%d" % (i % 2), "rs%d" % (i % 2)
        mb = 4 + i % 2
        BD64 = CB[:, CB_BD:CB_BD + 128]
        A(ACT, lambda e: e.activation(out=sq[:, 0:n], in_=psf(zbank)[:, 0:n], func=AF.Square), r=[zkey], w=[ksq])
        A(PE, lambda e: e.matmul(psf(mb)[:, 0:n], lhsT=BD64, rhs=sq[:, 0:n], start=True, stop=True), r=[ksq, "cb"], w=[pk(mb)])
        A(ACT, lambda e: e.activation(out=rs[:, 0:n], in_=psf(mb)[:, 0:n], func=AF.Ln, bias=EPSC, scale=1.0), r=[pk(mb), "cf"], w=[krs])
        A(ACT, lambda e: e.activation(out=rs[:, 0:n], in_=rs[:, 0:n], func=AF.Exp, scale=-0.5), r=[krs], w=[krs])
        A(DVE, lambda e: e.scalar_tensor_tensor(out=dst, in0=psf(zbank)[:, 0:n], scalar=CF[:, gain_col:gain_col + 1], in1=rs[:, 0:n],
                                                 op0=ALU.mult, op1=ALU.mult), r=[zkey, krs, "cf"], w=dkeys)

    for mt in range(2):
        norm_tile(D["mem"][mt * 128:(mt + 1) * 128, :], mt, CF_GMEM, MEMT[:, :, mt * 128:(mt + 1) * 128], ["memt%d" % mt], mt)
    mkeys = ["memt0", "memt1"]
    for c in range(2):
        zb = 2 + c

        def kproj(e, c=c, zb=zb):
            for k in range(8):
                ins = e.matmul(psf(zb)[:, 0:256], lhsT=WKV[:, k, c * 128:(c + 1) * 128], rhs=MEMT[:, k, :], start=(k == 0), stop=(k == 7))
            return ins
        A(PE, kproj, r=["wkv"] + mkeys, w=[pk(zb)])
        head_norm(zb, pk(zb), 256, CF_GMK, KMT[:, c, :], ["kmt%d" % c], c)
    for mt in range(2):
        zb = 2 + mt

        def vproj(e, mt=mt, zb=zb):
            for k in range(8):
                ins = e.matmul(psf(zb)[:, 0:256], lhsT=MEMT[:, k, mt * 128:(mt + 1) * 128], rhs=WKV[:, k, 256:512], start=(k == 0), stop=(k == 7))
            return ins
        A(PE, vproj, r=["wkv", "memt%d" % mt], w=[pk(zb)])
        A(ACT, lambda e, mt=mt, zb=zb: e.activation(out=VMA[:, mt, :, 0:64], in_=psf(zb)[:, 0:256].rearrange("p (h d) -> p h d", h=4), func=AF.Copy),
          r=[pk(zb)], w=["vma"])

    def normalize_out(acc_ap_fn, acc_keys, tmpo, tkey, bcb):
        for tg in range(4):
            a = acc_ap_fn(tg)
            A(DVE, lambda e, a=a: e.reciprocal(out=a[64:65, :], in_=a[64:65, :]), r=acc_keys, w=acc_keys)
            A(PE, lambda e, a=a: e.matmul(psf(bcb)[0:64, :], lhsT=ONES64[64:65, :], rhs=a[64:65, :], start=True, stop=True), r=acc_keys + ["cf"], w=[pk(bcb)])
            A(DVE, lambda e, a=a, tg=tg: e.tensor_tensor(out=tmpo[0:64, tg * 512:(tg + 1) * 512], in0=a[0:64, :], in1=psf(bcb)[0:64, :], op=ALU.mult),
              r=acc_keys + [pk(bcb)], w=[tkey])

    its = [(mh, tg) for mh in range(4) for tg in range(4)]

    def MF1(it):
        mh, tg = its[it]
        c, b0 = mh // 2, 64 * (mh % 2)
        sbs = (2, 3) if it % 2 == 0 else (6, 7)
        for mt in range(2):
            sb = sbs[mt]
            ptm, pkey = PTM[mt], "ptm%d" % mt
            A(PE, lambda e, mt=mt, sb=sb: e.matmul(
                psf(sb), lhsT=KMT[b0:b0 + 64, c, mt * 128:(mt + 1) * 128], rhs=QMT[b0:b0 + 64, c, tg * 512:(tg + 1) * 512], start=True, stop=True),
              r=["kmt%d" % c, "qmt%d" % c], w=[pk(sb)])
            A(ACT, lambda e, sb=sb, ptm=ptm: e.activation(out=ptm, in_=psf(sb), func=AF.Exp, scale=0.125), r=[pk(sb)], w=[pkey])

    def MF2(it):
        mh, tg = its[it]
        ob = it % 2
        for mt in range(2):
            ptm, pkey = PTM[mt], "ptm%d" % mt
            A(PE, lambda e, mt=mt, ptm=ptm: e.matmul(psf(ob)[0:65, :], lhsT=VMA[:, mt, mh, :], rhs=ptm, start=(mt == 0), stop=(mt == 1)),
              r=["vma", pkey], w=[pk(ob)])
        acm, akey = ACM[it % 2], "acm%d" % (it % 2)
        A(ACT, lambda e: e.activation(out=acm[0:65, :], in_=psf(ob)[0:65, :], func=AF.Copy), r=[pk(ob)], w=[akey])

    def MB(it):
        mh, tg = its[it]
        c, b0 = mh // 2, 64 * (mh % 2)
        tmpo, tkey = TMPO[mh % 2], "tmpo%d" % (mh % 2)
        acm, akey = ACM[it % 2], "acm%d" % (it % 2)
        bcb = 4 + it % 2
        A(ACT, lambda e: e.activation(out=acm[64:65, :], in_=acm[64:65, :], func=AF.Ln), r=[akey], w=[akey])
        A(ACT, lambda e: e.activation(out=acm[64:65, :], in_=acm[64:65, :], func=AF.Exp, scale=-1.0), r=[akey], w=[akey])
        A(PE, lambda e: e.matmul(psf(bcb)[0:64, :], lhsT=ONES64[64:65, :], rhs=acm[64:65, :], start=True, stop=True), r=[akey, "cf"], w=[pk(bcb)])
        A(DVE, lambda e: e.tensor_tensor(out=tmpo[0:64, tg * 512:(tg + 1) * 512], in0=acm[0:64, :], in1=psf(bcb)[0:64, :], op=ALU.mult),
          r=[akey, pk(bcb)], w=[tkey])
        if tg == 3:
            A(SP, lambda e: e.dma_start(out=MIXT[b0:b0 + 64, 6 + c, :], in_=tmpo[0:64, :]), r=[tkey], w=["mixt%d" % (6 + c)], dma=True)

    PTM = [M.alloc("r2", "ptm%d" % i, [128, 512], BF16) for i in range(2)] if False else PTM
    MF1(0)
    for it in range(16):
        MF2(it)
        if it + 1 < 16:
            MF1(it + 1)
        if it >= 1:
            MB(it - 1)
    MB(15)

    if "mix" in dumps and stage == 2:
        add_dump("mixt", MIXT, ["mixt%d" % i for i in range(8)], [128, 8, 2048], BF16)
    if stage <= 2:
        P.emit(nc)
        return nc, dump_out
    return build_attn(nc, P, M, D, A, CF, CB, PS, psf, psb, pk, out, xg, yg, xmid, stage, dumps, dump_out, add_dump, S, MIXT, WOUT)


def build_attn(nc, P, M, D, A, CF, CB, PS, psf, psb, pk, out, xg, yg, xmid, stage, dumps, dump_out, add_dump, S, MIXT, WOUT):
    QT, KT, VT = S["QT"], S["KT"], S["VT"]
    vt_keys, kt_keys, qt_keys = S["vt_keys"], S["kt_keys"], S["qt_keys"]
    IDB = CB[:, CB_ID:CB_ID + 128]
    MASK4 = CB[:, CB_MASK:CB_MASK + 512]
    ONES64 = CF[:, CF_ONES:CF_ONES + 64]
    HV = CF[:, CF_HV:CF_HV + 1]
    M.reset("r2"); M.reset("ut"); M.reset("qmt")
    import os as _os
    DILS = tuple(int(v) for v in _os.environ.get('ATTN_DILS', '1,4,16').split(','))
    NHP = int(_os.environ.get('ATTN_HP', '4'))
    LVL = int(_os.environ.get('ATTN_LEVEL', '9'))
    VA = [M.alloc("r2" if p < 3 else "ut", "va%d" % p, [128, 32, 2, 65], BF16) for p in range(3)]
    PT = [M.alloc("r2", "pt%d" % i, [128, 512], BF16) for i in range(4)]
    ACC = M.alloc("ut", "acc", [128, 2, 2048], F32, keys=["acc_q%d" % i for i in range(4)])
    TMPO = [M.alloc("qmt", "tmpo%d" % i, [128, 2048], BF16) for i in range(2)]
    for p, d in enumerate(DILS):
        nb = 32 // d
        A(DVE, lambda e, p=p: e.memset(VA[p][:, :, :, 64:65], 1.0), w=["va%d" % p])
        v = VA[p][:, :, :, 64:65].rearrange("p (r b) h o -> p r b (h o)", r=d)[:, :, 0:nb // 2, :]
        A(DVE, lambda e, v=v: e.tensor_scalar(out=v, in0=v, scalar1=HV, scalar2=None, op0=ALU.mult), r=["cf"], w=["va%d" % p])

    for i in range(16):
        A(SP, lambda e, i=i: e.dma_start(out=xg[i * 512:(i + 1) * 512, :].rearrange("(p r) d -> p r d", p=128), in_=S["ZT4"]),
          r=["zt"], w=["xg"], dma=True)

    units = [(hp, p, d) for hp in range(NHP) for p, d in enumerate(DILS)]
    NU = len(units)
    akeys = ["acc_q%d" % i for i in range(4)]

    def vprep_group(u, grp):
        hp, p, d = units[u]
        c, nb, va, vak = hp, 32 // d, VA[p], "va%d" % p

        def vtr(e):
            for j in range(8):
                kb = grp * 8 + j
                r, b = kb // nb, kb % nb
                s0 = d * 128 * b + r
                ins = e.transpose(out=psb(6)[:, j * 128:(j + 1) * 128], in_=VT[:, c, s0:s0 + 127 * d + 1:d], identity=IDB)
            return ins
        A(PE, vtr, r=vt_keys(c) + ["cb"], w=[pk(6)])
        src = psb(6).rearrange("p (k h d) -> p k h d", k=8, h=2)
        dst = va[:, grp * 8:grp * 8 + 8, :, 0:64]
        if grp % 2 == 0:
            A(ACT, lambda e: e.activation(out=dst, in_=src, func=AF.Copy), r=[pk(6)], w=[vak])
        else:
            A(DVE, lambda e: e.tensor_copy(out=dst, in_=src), r=[pk(6)], w=[vak])

    steps = []
    for u, (hp, p, d) in enumerate(units):
        nb = 32 // d
        for qg in range(4):
            for jp in range(2):
                blk = []
                for jj in range(2):
                    j = 2 * jp + jj
                    if d == 1:
                        r, b = 0, 16 + 4 * qg + j
                    elif d == 4:
                        r, b = j, 4 + qg
                    else:
                        r, b = 4 * qg + j, 1
                    kbc = r * nb + b
                    kc0 = d * 128 * b + r
                    blk.append((j, kbc, kbc - 1, kc0, d * 128 * (b - 1) + r, kc0 - 2048))
                steps.append(dict(u=u, hp=hp, p=p, d=d, qg=qg, jp=jp, blk=blk, n=len(steps)))
    mi = [0]

    def F1(st):
        n, c, d, blk = st["n"], st["hp"], st["d"], st["blk"]
        sset = n % 2
        pts = [(PT[2 * sset + hh], "pt%d" % (2 * sset + hh)) for hh in range(2)]
        st["pts"] = pts

        def qk(e):
            for jj, (j, kbc, kbp, kc0, kp0, q0) in enumerate(blk):
                for hh in range(2):
                    for kv, k0 in enumerate((kp0, kc0)):
                        ins = e.matmul(psf(2 * sset + hh)[:, (jj * 2 + kv) * 128:(jj * 2 + kv + 1) * 128],
                                       lhsT=KT[hh * 64:hh * 64 + 64, c, k0:k0 + 127 * d + 1:d],
                                       rhs=QT[hh * 64:hh * 64 + 64, c, q0:q0 + 127 * d + 1:d], start=True, stop=True)
            return ins
        A(PE, qk, r=kt_keys(c) + qt_keys(c), w=[pk(2 * sset), pk(2 * sset + 1)])
        for hh in range(2):
            pt, ptk = pts[hh]
            A(ACT, lambda e, sb=2 * sset + hh, pt=pt: e.activation(out=pt, in_=psf(sb), func=AF.Exp, scale=0.125), r=[pk(2 * sset + hh)], w=[ptk])
            meng = DVE if mi[0] % 2 == 0 else POOL
            mi[0] += 1
            A(meng, lambda e, pt=pt: e.tensor_tensor(out=pt, in0=pt, in1=MASK4, op=ALU.mult), r=[ptk, "cb"], w=[ptk])

    def F2(st):
        n, p, d, blk, qg, jp, pts = st["n"], st["p"], st["d"], st["blk"], st["qg"], st["jp"], st["pts"]
        va, vak = VA[p], "va%d" % p
        ob = 4 + n % 2

        def pv(e):
            for jj, (j, kbc, kbp, kc0, kp0, q0) in enumerate(blk):
                for hh in range(2):
                    pt = pts[hh][0]
                    o = psf(ob)[0:65, (hh * 2 + jj) * 128:(hh * 2 + jj + 1) * 128]
                    e.matmul(o, lhsT=va[:, kbp, hh, :], rhs=pt[:, (jj * 2) * 128:(jj * 2 + 1) * 128], start=True, stop=False)
                    ins = e.matmul(o, lhsT=va[:, kbc, hh, :], rhs=pt[:, (jj * 2 + 1) * 128:(jj * 2 + 2) * 128], start=False, stop=True)
            return ins
        A(PE, pv, r=[vak, pts[0][1], pts[1][1]], w=[pk(ob)])
        if d == 1:
            t0 = (qg * 4 + jp * 2) * 128
            dst = ACC[0:65, :, t0:t0 + 256]
            src = psf(ob)[0:65, :].rearrange("p (h t) -> p h t", h=2)
            A(ACT, lambda e: e.activation(out=dst, in_=src, func=AF.Copy), r=[pk(ob)], w=[akeys[qg]])
        else:
            if d == 4:
                dst = ACC[0:65, :, qg * 512:(qg + 1) * 512].rearrange("p h (i r) -> p h r i", r=4)[:, :, 2 * jp:2 * jp + 2, :]
                ks = [akeys[qg]]
            else:
                dst = ACC[0:65, :, :].rearrange("p h (i r) -> p h r i", r=16)[:, :, 4 * qg + 2 * jp:4 * qg + 2 * jp + 2, :]
                ks = akeys
            src = psf(ob)[0:65, :].rearrange("p (h r i) -> p h r i", h=2, r=2)
            A(DVE, lambda e: e.tensor_tensor(out=dst, in0=src, in1=dst, op=ALU.add), r=[pk(ob)] + ks, w=ks)

    tmi = [0]

    bci = [0]

    def NORM(hp):
        for hh in range(2):
            a = ACC[64:65, hh, :]
            A(ACT, lambda e, a=a: e.activation(out=a, in_=a, func=AF.Ln), r=akeys, w=akeys)
            A(ACT, lambda e, a=a: e.activation(out=a, in_=a, func=AF.Exp, scale=-1.0), r=akeys, w=akeys)
        for hh in range(2):
            tmpo, tkey = TMPO[tmi[0] % 2], "tmpo%d" % (tmi[0] % 2)
            tmi[0] += 1
            for tg in range(4):
                a = ACC[:, hh, tg * 512:(tg + 1) * 512]
                bb = 6 + bci[0] % 2
                bci[0] += 1
                A(PE, lambda e, a=a, bb=bb: e.matmul(psf(bb)[0:64, :], lhsT=ONES64[64:65, :], rhs=a[64:65, :], start=True, stop=True), r=[akeys[tg], "cf"], w=[pk(bb)])
                A(DVE, lambda e, a=a, tg=tg, tmpo=tmpo, bb=bb: e.tensor_tensor(out=tmpo[0:64, tg * 512:(tg + 1) * 512], in0=a[0:64, :], in1=psf(bb)[0:64, :], op=ALU.mult),
                  r=[akeys[tg], pk(bb)], w=[tkey])
            A(SP, lambda e, hh=hh, hp=hp, tmpo=tmpo: e.dma_start(out=MIXT[hh * 64:hh * 64 + 64, hp, :], in_=tmpo[0:64, :]), r=[tkey], w=["mixt%d" % hp], dma=True)

    for grp in range(4):
        vprep_group(0, grp)
    F1(steps[0])
    for n, st in enumerate(steps):
        if n + 1 < len(steps):
            F1(steps[n + 1])
        F2(st)
        sl = n % 8
        if sl in (1, 3, 5, 7) and st["u"] + 1 < NU:
            vprep_group(st["u"] + 1, sl // 2)
        if sl == 7 and st["p"] == len(DILS) - 1:
            NORM(st["hp"])

    if "mix" in dumps and stage == 3:
        add_dump("mixt", MIXT, ["mixt%d" % i for i in range(8)], [128, 8, 2048], BF16)
    if stage <= 3:
        P.emit(nc)
        return nc, dump_out
    return build_ffn(nc, P, M, D, A, CF, CB, PS, psf, psb, pk, out, xg, yg, xmid, stage, dumps, dump_out, add_dump, S, MIXT, WOUT)


def build_ffn(nc, P, M, D, A, CF, CB, PS, psf, psb, pk, out, xg, yg, xmid, stage, dumps, dump_out, add_dump, S, MIXT, WOUT):
    IDB = CB[:, CB_ID:CB_ID + 128]
    ONESB = CB[:, CB_ONES:CB_ONES + 128]
    TRI = CB[:, CB_TRI:CB_TRI + 128]
    EPSC = CF[:, CF_EPS:CF_EPS + 1]
    IDF = CF[:, CF_IDF:CF_IDF + 128]
    GFFN = CF[:, CF_GFFN:CF_GFFN + 1024]
    BIASR = CF[:, CF_BIAS:CF_BIAS + 72]
    OFFS = CF[:, CF_OFFS:CF_OFFS + 64]
    M.reset("kt"); M.reset("qt"); M.reset("r2")
    XT = [M.alloc("kt", "xt%d" % i, [128, 1024], F32) for i in range(2)]
    XM = [M.alloc("kt", "xm%d" % i, [128, 1024], F32, keys=["xm%d_0" % i, "xm%d_1" % i]) for i in range(2)]
    H2 = [M.alloc("kt", "h2_%d" % i, [128, 1024], F32) for i in range(2)]
    H2T = [M.alloc("kt", "h2t%d" % i, [128, 8, 128], F32, keys=["h2t%d_0" % i, "h2t%d_1" % i]) for i in range(2)]
    NHB = 12
    H2B = [M.alloc("r2", "h2b%d" % i, [128, 1024], BF16) for i in range(NHB)]
    WR = M.alloc("qt", "wr", [128, 8, 72], F32)
    AB = M.alloc("qt", "ab", [128, 16, 64], BF16, keys=["ab%d" % i for i in range(4)])
    G12 = M.alloc("qt", "g12", [128, 2, 16], F32)
    SMS = M.alloc("qt", "sms", [128, 2, 4], F32, keys=["sms0", "sms1"])
    NT = 4
    L4 = [M.alloc("r2", "l4_%d" % i, [128, NT, 72], F32, keys=["l4_%d_%d" % (i, j) for j in range(NT)]) for i in range(2)]
    RT = M.alloc("r2", "rt", [128, 1400], F32)
    DEST = [[nc.alloc_sbuf_tensor("dest%d_%d" % (a, tt), [128, 1], I32) for tt in range(16)] for a in range(2)]
    A(SP, lambda e: e.dma_start(out=WR, in_=D["w_r"].rearrange("(k p) c -> p k c", p=128)), w=["wr"], dma=True)

    _o = [0]

    def rt(n, shape=None):
        ap = RT[:, _o[0]:_o[0] + n]
        _o[0] += n
        if shape:
            ap = ap.rearrange("p (a b) -> p a b", a=shape[0]) if len(shape) == 2 else ap.rearrange("p (a b c) -> p a b c", a=shape[0], b=shape[1])
        return ap
    OHG, D8, E8, LSEL, OH1, L2, OH2 = [rt(NT * 8, (NT, 8)) for _ in range(7)]
    T64, A1F, A2F, RB = [rt(NT * 64, (NT, 8, 8)) for _ in range(4)]
    SM = rt(16 * NT, (16, NT))
    RK = "rt"

    def S1(tt):
        sl = tt % 2
        xt, xm, h2, h2b = XT[sl], XM[sl], H2[sl], H2B[tt % NHB]
        kx, km, kh, kb = "xt%d" % sl, "xm%d" % sl, "h2_%d" % sl, "h2b%d" % (tt % NHB)
        sm, ksm = SMS[:, sl, :], "sms%d" % sl
        tok = slice(tt * 128, (tt + 1) * 128)
        ob = (0, 1) if sl == 0 else (4, 5)
        A(SP, lambda e: e.dma_start(out=xt, in_=D["xo"][tok, :]), w=[kx], dma=True)
        for half in range(2):
            def oproj(e, half=half):
                for k in range(8):
                    e.matmul(psf(ob[half]), lhsT=MIXT[:, k, tok], rhs=WOUT[:, k, half * 512:(half + 1) * 512], start=(k == 0), stop=False)
                return e.matmul(psf(ob[half]), lhsT=IDF, rhs=xt[:, half * 512:(half + 1) * 512], start=False, stop=True)
            A(PE, oproj, r=["wout", kx, "cf"] + ["mixt%d" % i for i in range(8)], w=[pk(ob[half])])
            A(ACT, lambda e, half=half: e.activation(out=xm[:, half * 512:(half + 1) * 512], in_=psf(ob[half]), func=AF.Copy), r=[pk(ob[half])], w=[km + "_%d" % half])
        kms = [km + "_0", km + "_1"]
        A(SP, lambda e: e.dma_start(out=xmid[tok, :], in_=xm), r=kms, w=["xmid%d" % tt], dma=True)
        A(ACT, lambda e: e.activation(out=h2, in_=xm, func=AF.Square, accum_out=sm[:, 0:1]), r=kms, w=[kh, ksm])
        A(ACT, lambda e: e.activation(out=sm[:, 1:2], in_=sm[:, 0:1], func=AF.Ln, bias=EPSC, scale=1.0 / 1024), r=[ksm, "cf"], w=[ksm])
        A(ACT, lambda e: e.activation(out=sm[:, 2:3], in_=sm[:, 1:2], func=AF.Exp, scale=-0.5), r=[ksm], w=[ksm])

    def S1c(tt):
        sl = tt % 2
        xm, h2, h2b = XM[sl], H2[sl], H2B[tt % NHB]
        km, kh, kb = "xm%d" % sl, "h2_%d" % sl, "h2b%d" % (tt % NHB)
        sm, ksm = SMS[:, sl, :], "sms%d" % sl
        kms = [km + "_0", km + "_1"]
        A(DVE, lambda e: e.scalar_tensor_tensor(out=h2, in0=xm, scalar=sm[:, 2:3], in1=GFFN, op0=ALU.mult, op1=ALU.mult), r=kms + [ksm, "cf"], w=[kh])
        A(ACT, lambda e: e.activation(out=h2b, in_=h2, func=AF.Copy), r=[kh], w=[kb])

    def S1b(tt):
        sl = tt % 2
        h2, h2t, kh = H2[sl], H2T[sl], "h2_%d" % sl
        for hb in range(2):
            def tr(e, hb=hb):
                for k in range(4):
                    kk = hb * 4 + k
                    ins = e.transpose(out=psf(2 + hb)[:, k * 128:(k + 1) * 128], in_=h2[:, kk * 128:(kk + 1) * 128], identity=IDF)
                return ins
            A(PE, tr, r=[kh, "cf"], w=[pk(2 + hb)])
            A(ACT, lambda e, hb=hb: e.activation(out=h2t[:, 4 * hb:4 * hb + 4, :], in_=psf(2 + hb).rearrange("p (k t) -> p k t", k=4), func=AF.Copy),
              r=[pk(2 + hb)], w=["h2t%d_%d" % (sl, hb)])

        def rmm(e):
            for k in range(8):
                ins = e.matmul(psf(6)[:, 0:72], lhsT=h2t[:, k, :], rhs=WR[:, k, :], start=(k == 0), stop=(k == 7))
            return ins
        A(PE, rmm, r=["h2t%d_0" % sl, "h2t%d_1" % sl, "wr"], w=[pk(6)])
        b, i = tt // NT, tt % NT
        A(DVE, lambda e: e.tensor_tensor(out=L4[b % 2][:, i, :], in0=psf(6)[:, 0:72], in1=BIASR, op=ALU.add), r=[pk(6), "cf"], w=["l4_%d_%d" % (b % 2, i)])

    V = lambda fn, r=(), w=(): A(DVE, fn, r=list(r) + [RK], w=list(w) + [RK])
    bc3 = lambda ap: ap.unsqueeze(2).broadcast_to([128, NT, 8])
    bcE = lambda ap: ap.unsqueeze(3).broadcast_to([128, NT, 8, 8])
    bcG = lambda ap: ap.unsqueeze(2).broadcast_to([128, NT, 8, 8])
    R_ = lambda i: SM[:, i, :]

    def S2B(b):
        l4 = L4[b % 2]
        lk = ["l4_%d_%d" % (b % 2, j) for j in range(NT)]
        LG_ = l4[:, :, 0:8]
        LE_ = l4[:, :, 8:72].rearrange("p t (g x) -> p t g x", g=8)
        V(lambda e: e.tensor_reduce(out=R_(0), in_=LG_, axis=AX.X, op=ALU.max), r=lk)
        V(lambda e: e.tensor_tensor(out=OHG, in0=LG_, in1=bc3(R_(0)), op=ALU.is_equal), r=lk)
        V(lambda e: e.tensor_tensor(out=D8, in0=LG_, in1=bc3(R_(0)), op=ALU.subtract), r=lk)
        A(ACT, lambda e: e.activation(out=E8, in_=D8, func=AF.Exp), r=[RK], w=[RK])
        V(lambda e: e.tensor_reduce(out=R_(1), in_=E8, axis=AX.X, op=ALU.add))
        V(lambda e: e.reciprocal(out=R_(2), in_=R_(1)))
        V(lambda e: e.tensor_tensor(out=T64, in0=LE_, in1=bcE(OHG), op=ALU.mult), r=lk)
        V(lambda e: e.tensor_reduce(out=LSEL, in_=T64.rearrange("p t g x -> p t x g"), axis=AX.X, op=ALU.add))
        V(lambda e: e.tensor_reduce(out=R_(3), in_=LSEL, axis=AX.X, op=ALU.max))
        V(lambda e: e.tensor_tensor(out=OH1, in0=LSEL, in1=bc3(R_(3)), op=ALU.is_equal))
        V(lambda e: e.scalar_tensor_tensor(out=L2, in0=OH1, scalar=-1e30, in1=LSEL, op0=ALU.mult, op1=ALU.add))
        V(lambda e: e.tensor_reduce(out=R_(4), in_=L2, axis=AX.X, op=ALU.max))
        V(lambda e: e.tensor_tensor(out=OH2, in0=L2, in1=bc3(R_(4)), op=ALU.is_equal))
        V(lambda e: e.tensor_tensor(out=R_(5), in0=R_(4), in1=R_(3), op=ALU.subtract))
        A(ACT, lambda e: e.activation(out=R_(6), in_=R_(5), func=AF.Exp), r=[RK], w=[RK])
        V(lambda e: e.tensor_tensor(out=A1F, in0=bcE(OHG), in1=bcG(OH1), op=ALU.mult))
        V(lambda e: e.tensor_tensor(out=A2F, in0=bcE(OHG), in1=bcG(OH2), op=ALU.mult))
        V(lambda e: e.tensor_tensor(out=AB[:, b * NT:(b + 1) * NT, :].rearrange("p t (g x) -> p t g x", g=8), in0=A1F, in1=A2F, op=ALU.add), w=["ab%d" % b])
        V(lambda e: e.tensor_scalar(out=R_(7), in0=R_(6), scalar1=1.0, scalar2=None, op0=ALU.add))
        V(lambda e: e.reciprocal(out=R_(8), in_=R_(7)))
        V(lambda e: e.tensor_tensor(out=G12[:, 0, b * NT:(b + 1) * NT], in0=R_(2), in1=R_(8), op=ALU.mult), w=["g12"])
        V(lambda e: e.tensor_tensor(out=G12[:, 1, b * NT:(b + 1) * NT], in0=R_(2), in1=G12[:, 0, b * NT:(b + 1) * NT], op=ALU.subtract), r=["g12"], w=["g12"])

    def S3B(b):
        def rank(e):
            for i in range(NT):
                tt = b * NT + i
                o = psf(7)[:, i * 64:(i + 1) * 64]
                for t2 in range(tt):
                    e.matmul(o, lhsT=ONESB, rhs=AB[:, t2, :], start=(t2 == 0), stop=False)
                ins = e.matmul(o, lhsT=TRI, rhs=AB[:, tt, :], start=(tt == 0), stop=True)
            return ins
        A(PE, rank, r=["ab%d" % i for i in range(b + 1)] + ["cb"], w=[pk(7)])
        V(lambda e: e.scalar_tensor_tensor(out=RB.rearrange("p t g x -> p t (g x)"), in0=psf(7)[:, 0:NT * 64].rearrange("p (t x) -> p t x", t=NT), scalar=float(EXPERT_CAP - 1),
                                           in1=OFFS.unsqueeze(1).broadcast_to([128, NT, 64]), op0=ALU.min, op1=ALU.add), r=[pk(7), "cf"])
        for a, AF_ in enumerate((A1F, A2F)):
            V(lambda e, AF_=AF_: e.tensor_tensor(out=T64, in0=AF_, in1=RB, op=ALU.mult))
            V(lambda e, a=a: e.tensor_reduce(out=R_(9 + a), in_=T64.rearrange("p t g x -> p t (g x)"), axis=AX.X, op=ALU.add))
            for i in range(NT):
                tt = b * NT + i
                dk = "dest%d_%d" % (a, tt)
                V(lambda e, a=a, i=i, tt=tt: e.tensor_copy(out=DEST[a][tt][:, :], in_=R_(9 + a)[:, i:i + 1]), w=[dk])
        for i in range(NT):
            tt = b * NT + i
            h2b, kb = H2B[tt % NHB], "h2b%d" % (tt % NHB)
            for a in range(2):
                dk = "dest%d_%d" % (a, tt)
                A(POOL, lambda e, a=a, tt=tt, h2b=h2b: e.indirect_dma_start(
                    out=xg, out_offset=bass.IndirectOffsetOnAxis(ap=DEST[a][tt][:, 0:1], axis=0), in_=h2b, in_offset=None,
                    bounds_check=None, oob_is_err=False), r=[dk, kb, "xg"], w=["xgs%d_%d" % (a, tt)], dma=True)

    S1(0)
    S1c(0)
    S1(1)
    S1b(0)
    S1c(1)
    for tt in range(16):
        if tt + 2 < 16:
            S1(tt + 2)
        if tt + 1 < 16:
            S1b(tt + 1)
        if tt + 2 < 16:
            S1c(tt + 2)
        if tt % NT == 1 and tt >= NT:
            S2B(tt // NT - 1)
        if tt % NT == 2 and tt >= NT:
            S3B(tt // NT - 1)
    S2B(3)
    S3B(3)

    if "route" in dumps:
        add_dump("g12", G12, ["g12"], [128, 2, 16], F32)
        for a in range(2):
            for tt in (0, 15):
                add_dump("dest%d_%d" % (a, tt), DEST[a][tt][:, :], ["dest%d_%d" % (a, tt)], [128, 1], I32)
        add_dump("xm", XM[1], ["xm1_0", "xm1_1"], [128, 1024], F32)
    if stage <= 4:
        P.emit(nc)
        return nc, dump_out

    M.reset("vt"); M.reset("r2"); M.reset("ut"); M.reset("qmt"); M.reset("r1")
    NW = 3
    W1 = [M.alloc("vt" if i == 0 else "r2", "w1_%d" % i, [128, 8, 512], BF16) for i in range(NW)]
    W3 = [M.alloc("vt" if i == 0 else "r2", "w3_%d" % i, [128, 8, 512], BF16) for i in range(NW)]
    W2 = [M.alloc("vt" if i == 0 else "ut", "w2_%d" % i, [128, 4, 1024], BF16) for i in range(NW)]
    XE = [M.alloc("vt", "xe%d" % i, [128, 1024], BF16) for i in range(2)]
    XET = [M.alloc("vt", "xet%d" % i, [128, 8, 128], BF16) for i in range(2)]
    SA = M.alloc("qmt", "sa", [128, 512], F32)
    HM = M.alloc("qmt", "hm", [128, 512], BF16)
    HMT = M.alloc("qmt", "hmt", [128, 4, 128], BF16)
    YE = [M.alloc("r1", "ye%d" % i, [128, 1024], BF16) for i in range(2)]

    def wload(e_):
        i = e_ % NW
        A(POOL, lambda e, i=i, e_=e_: e.dma_start(out=W1[i], in_=D["w1"][e_].rearrange("(k p) f -> p k f", p=128)), w=["w1_%d" % i], dma=True)
        A(POOL, lambda e, i=i, e_=e_: e.dma_start(out=W3[i], in_=D["w3"][e_].rearrange("(k p) f -> p k f", p=128)), w=["w3_%d" % i], dma=True)
        A(POOL, lambda e, i=i, e_=e_: e.dma_start(out=W2[i], in_=D["w2"][e_].rearrange("(k p) f -> p k f", p=128)), w=["w2_%d" % i], dma=True)

    NEXP = 64
    for e_ in range(min(NW - 1, NEXP)):
        wload(e_)
    for e_ in range(NEXP):
        if e_ + NW - 1 < NEXP:
            wload(e_ + NW - 1)
        i = e_ % NW
        s2 = e_ % 2
        xe, xet, ye = XE[s2], XET[s2], YE[s2]
        kxe, kxt, kye = "xe%d" % s2, "xet%d" % s2, "ye%d" % s2
        tb = 0 if s2 == 0 else 6
        ab = 1 if s2 == 0 else 7
        A(SP, lambda e, xe=xe, e_=e_: e.dma_start(out=xe, in_=xg[e_ * 128:(e_ + 1) * 128, :]), r=["xg"] + ["xgs%d_%d" % (a, t) for a in range(2) for t in range(16)], w=[kxe], dma=True)

        def trx(e, xe=xe, tb=tb):
            for k in range(8):
                ins = e.transpose(out=psb(tb)[:, k * 128:(k + 1) * 128], in_=xe[:, k * 128:(k + 1) * 128], identity=IDB)
            return ins
        A(PE, trx, r=[kxe, "cb"], w=[pk(tb)])
        A(ACT, lambda e, xet=xet, tb=tb: e.activation(out=xet, in_=psb(tb).rearrange("p (k t) -> p k t", k=8), func=AF.Copy), r=[pk(tb)], w=[kxt])

        def up(e, W, bank, xet=xet):
            for k in range(8):
                ins = e.matmul(psf(bank), lhsT=xet[:, k, :], rhs=W[:, k, :], start=(k == 0), stop=(k == 7))
            return ins
        A(PE, lambda e, i=i, ab=ab, up=up: up(e, W1[i], ab), r=[kxt, "w1_%d" % i], w=[pk(ab)])
        A(PE, lambda e, i=i, up=up: up(e, W3[i], 2), r=[kxt, "w3_%d" % i], w=[pk(2)])
        A(ACT, lambda e, ab=ab: e.activation(out=SA, in_=psf(ab), func=AF.Silu), r=[pk(ab)], w=["sa"])
        A(DVE, lambda e: e.tensor_tensor(out=HM, in0=SA, in1=psf(2), op=ALU.mult), r=["sa", pk(2)], w=["hm"])

        def trh(e):
            for k in range(4):
                ins = e.transpose(out=psb(3)[:, k * 128:(k + 1) * 128], in_=HM[:, k * 128:(k + 1) * 128], identity=IDB)
            return ins
        A(PE, trh, r=["hm", "cb"], w=[pk(3)])
        A(DVE, lambda e: e.tensor_copy(out=HMT, in_=psb(3)[:, 0:512].rearrange("p (k t) -> p k t", k=4)), r=[pk(3)], w=["hmt"])
        for half in range(2):
            def dn(e, i=i, half=half):
                for k in range(4):
                    ins = e.matmul(psf(4 + half), lhsT=HMT[:, k, :], rhs=W2[i][:, k, half * 512:(half + 1) * 512], start=(k == 0), stop=(k == 3))
                return ins
            A(PE, dn, r=["hmt", "w2_%d" % i], w=[pk(4 + half)])
        A(ACT, lambda e, ye=ye: e.activation(out=ye[:, 0:512], in_=psf(4), func=AF.Copy), r=[pk(4)], w=[kye + "a"])
        A(DVE, lambda e, ye=ye: e.tensor_copy(out=ye[:, 512:1024], in_=psf(5)), r=[pk(5)], w=[kye + "b"])
        A(SP, lambda e, ye=ye, e_=e_: e.dma_start(out=yg[e_ * 128:(e_ + 1) * 128, :], in_=ye), r=[kye + "a", kye + "b"], w=["yg%d" % e_], dma=True)

    if stage <= 5:
        P.emit(nc)
        return nc, dump_out

    M.reset("kt"); M.reset("vt")
    NC_ = 4
    Y1 = [M.alloc("kt", "y1_%d" % i, [128, 1024], BF16) for i in range(NC_)]
    Y2 = [M.alloc("kt", "y2_%d" % i, [128, 1024], BF16) for i in range(NC_)]
    XM2 = [M.alloc("vt", "xm2_%d" % i, [128, 1024], F32) for i in range(NC_)]
    OT_ = [M.alloc("vt", "ot_%d" % i, [128, 1024], F32) for i in range(NC_)]
    for tt in range(16):
        sl = tt % NC_
        tok = slice(tt * 128, (tt + 1) * 128)
        for a, Y in enumerate((Y1, Y2)):
            A(POOL, lambda e, a=a, tt=tt, y=Y[sl]: e.indirect_dma_start(
                out=y, out_offset=None, in_=yg, in_offset=bass.IndirectOffsetOnAxis(ap=DEST[a][tt][:, 0:1], axis=0),
                bounds_check=None, oob_is_err=False), r=["dest%d_%d" % (a, tt)] + ["yg%d" % i for i in range(64)], w=["y%d_%d" % (a + 1, sl)], dma=True)
        A(SP, lambda e, sl=sl, tok=tok: e.dma_start(out=XM2[sl], in_=xmid[tok, :]), r=["xmid%d" % tt], w=["xm2_%d" % sl], dma=True)
        A(DVE, lambda e, sl=sl, tt=tt: e.scalar_tensor_tensor(out=OT_[sl], in0=Y1[sl], scalar=G12[:, 0, tt:tt + 1], in1=XM2[sl], op0=ALU.mult, op1=ALU.add),
          r=["y1_%d" % sl, "xm2_%d" % sl, "g12"], w=["ot_%d" % sl])
        A(DVE, lambda e, sl=sl, tt=tt: e.scalar_tensor_tensor(out=OT_[sl], in0=Y2[sl], scalar=G12[:, 1, tt:tt + 1], in1=OT_[sl], op0=ALU.mult, op1=ALU.add),
          r=["y2_%d" % sl, "ot_%d" % sl, "g12"], w=["ot_%d" % sl])
        A(SP, lambda e, sl=sl, tok=tok: e.dma_start(out=out[tok, :], in_=OT_[sl]), r=["ot_%d" % sl], w=["out"], dma=True)
    if "moe" in dumps:
        allxg = ["xg"] + ["xgs%d_%d" % (a, t) for a in range(2) for t in range(16)]
        for ee in (0, 37):
            add_dump("xg%d" % ee, xg[ee * 128:(ee + 1) * 128, :], allxg, [128, 1024], BF16)
            add_dump("yg%d" % ee, yg[ee * 128:(ee + 1) * 128, :], ["yg%d" % ee], [128, 1024], BF16)
        add_dump("g12", G12, ["g12"], [128, 2, 16], F32)
        for a in range(2):
            for tt in range(16):
                add_dump("dest%d_%d" % (a, tt), DEST[a][tt][:, :], ["dest%d_%d" % (a, tt)], [128, 1], I32)
        add_dump("xmid", xmid, ["xmid%d" % t for t in range(16)], [2048, 1024], F32)
    P.emit(nc)
    return nc, dump_out
```

```python
import numpy as np
import concourse.bass as bass
import concourse.mybir as mybir
from concourse.bass_utils import run_bass_kernel_spmd

F32 = mybir.dt.float32
BF16 = mybir.dt.bfloat16
I32 = mybir.dt.int32
AF = mybir.ActivationFunctionType
ALU = mybir.AluOpType
AX = mybir.AxisListType

PE, ACT, DVE, POOL, SP = "pe", "act", "dve", "pool", "sp"
ENGS = (PE, ACT, DVE, POOL, SP)
RING = 12


class Op:
    __slots__ = ("eng", "fn", "deps", "dma", "need", "seq", "di", "name")

    def __init__(self, eng, fn, dma, name):
        self.eng, self.fn, self.dma, self.name = eng, fn, dma, name
        self.deps, self.need, self.seq, self.di = [], False, 0, -1


class Prog:
    def __init__(self):
        self.ops = {e: [] for e in ENGS}
        self.lastw = {}
        self.readers = {}

    def add(self, eng, fn, r=(), w=(), dma=False, name=""):
        op = Op(eng, fn, dma, name)
        deps = {}
        for k in r:
            lw = self.lastw.get(k)
            if lw is not None:
                deps[id(lw)] = (lw, True)
        for k in w:
            lw = self.lastw.get(k)
            if lw is not None:
                deps[id(lw)] = (lw, True)
            for rd in self.readers.get(k, ()):
                if id(rd) not in deps:
                    deps[id(rd)] = (rd, False)
        for k in r:
            self.readers.setdefault(k, []).append(op)
        for k in w:
            self.lastw[k] = op
            self.readers[k] = []
        op.deps = [d for d in deps.values() if d[0] is not op]
        self.ops[eng].append(op)
        return op

    @staticmethod
    def _needs_wait(o, d, true_dep):
        if d.dma:
            return True
        if d.eng != o.eng:
            return True
        if o.eng == PE:
            return False
        if o.dma:
            return True
        return true_dep

    def emit(self, nc):
        for e in ENGS:
            for o in self.ops[e]:
                for d, t in o.deps:
                    if self._needs_wait(o, d, t):
                        d.need = True
        for e in ENGS:
            c = 0
            n = 0
            for o in self.ops[e]:
                if o.dma:
                    o.di = n
                    n += 1
                elif o.need:
                    c += 1
                    o.seq = c
        from contextlib import ExitStack

        with ExitStack() as st:
            esem = {e: st.enter_context(nc.semaphore("c_" + e)) for e in (PE, ACT, DVE, POOL)}
            rsem = {
                e: [st.enter_context(nc.semaphore("r_%s%d" % (e, i))) for i in range(RING)]
                for e in (SP, POOL, ACT)
            }
            block = st.enter_context(nc.Block())

            def run(ename, eng):
                seen = {}

                def wait(sem, val):
                    if seen.get(sem.num, 0) < val:
                        eng.wait_ge(sem, val)
                        seen[sem.num] = val

                ndma = 0
                for o in self.ops[ename]:
                    for d, t in o.deps:
                        if not self._needs_wait(o, d, t):
                            continue
                        if d.dma:
                            wait(rsem[d.eng][d.di % RING], 16 * (d.di // RING + 1))
                        else:
                            wait(esem[d.eng], d.seq)
                    if o.dma:
                        if o.di >= RING:
                            wait(rsem[ename][o.di % RING], 16 * (o.di // RING))
                        ins = o.fn(eng)
                        ins.then_inc(rsem[ename][o.di % RING], 16)
                        ndma = o.di + 1
                    else:
                        ins = o.fn(eng)
                        if o.need:
                            ins.then_inc(esem[ename], 1)
                for i in range(max(0, ndma - RING), ndma):
                    wait(rsem[ename][i % RING], 16 * (i // RING + 1))

            block.tensor(lambda eng: run(PE, eng))
            block.scalar(lambda eng: run(ACT, eng))
            block.vector(lambda eng: run(DVE, eng))
            block.gpsimd(lambda eng: run(POOL, eng))
            block.sync(lambda eng: run(SP, eng))


class Mem:
    def __init__(self, nc, P, total_bytes):
        self.P = P
        self.t = nc.alloc_sbuf_tensor("arena", [128, total_bytes // 4], F32)
        self.regs = {}
        self.top = 0
        self.total = total_bytes

    def region(self, name, size):
        size = (size + 63) // 64 * 64
        assert self.top + size <= self.total, (name, self.top, size)
        self.regs[name] = dict(off=self.top, size=size, cur=0, keys=[], old=[])
        self.top += size

    def reset(self, name):
        r = self.regs[name]
        r["old"] = r["old"] + r["keys"]
        r["keys"] = []
        r["cur"] = 0

    def alloc(self, reg, key, shape, dt, keys=None):
        r = self.regs[reg]
        isz = 2 if dt == BF16 else 4
        n = 1
        for s in shape[1:]:
            n *= s
        nb = (n * isz + 63) // 64 * 64
        assert r["cur"] + nb <= r["size"], (reg, key, r["cur"], nb, r["size"])
        off = r["off"] + r["cur"]
        r["cur"] += nb
        keys = list(keys) if keys else [key]
        r["keys"].extend(keys)
        olds = []
        for ok in r["old"]:
            lw = self.P.lastw.get(ok)
            if lw is not None:
                olds.append(lw)
            olds.extend(self.P.readers.get(ok, ()))
        if olds:
            for kk in keys:
                self.P.readers.setdefault(kk, []).extend(olds)
        ap = self.t[:, off // 4:(off + n * isz + 3) // 4]
        if dt == BF16:
            ap = ap.bitcast(BF16)
        ap = ap[:, 0:n]
        if len(shape) == 3:
            ap = ap.rearrange("p (a b) -> p a b", a=shape[1])
        elif len(shape) == 4:
            ap = ap.rearrange("p (a b c) -> p a b c", a=shape[1], b=shape[2])
        return ap


NCF = 1416
NCB = 1280
CF_GATTN, CF_GMEM, CF_GQ, CF_GK, CF_GMQ, CF_GMK = 0, 8, 16, 17, 18, 19
CF_INVW, CF_PSC, CF_IC16, CF_HV, CF_EPS = 20, 22, 24, 56, 57
CF_ONES, CF_OFFS, CF_BIAS, CF_IDF, CF_GFFN = 64, 128, 192, 264, 392
CB_ID, CB_MASK, CB_BD, CB_ONES, CB_TRI, CB_PBD = 0, 128, 640, 768, 896, 1024
EPS = 1e-6
EXPERT_CAP = 128


def build_program(stage=99, dumps=()):
    nc = bass.Bass("TRN2", target_bir_lowering=False)
    P = Prog()
    D = {}

    def din(name, shape, dt=F32):
        D[name] = nc.dram_tensor(name, shape, dt, kind="ExternalInput").ap()

    din("xo", [2048, 1024]); din("xh", [2048, 1024]); din("mem", [256, 1024])
    din("w_in", [1024, 2048]); din("w_out", [1024, 1024]); din("w_kv", [1024, 512])
    din("w_r", [1024, 72]); din("cf", [128, NCF]); din("cb", [128, NCB])
    if stage >= 5:
        din("w1", [64, 1024, 512]); din("w3", [64, 1024, 512]); din("w2", [64, 512, 1024])
    out = nc.dram_tensor("out", [2048, 1024], F32, kind="ExternalOutput").ap()
    xg = nc.dram_tensor("xg", [64 * EXPERT_CAP, 1024], BF16, kind="Internal").ap()
    yg = nc.dram_tensor("yg", [64 * EXPERT_CAP, 1024], BF16, kind="Internal").ap()
    xmid = nc.dram_tensor("xmid", [2048, 1024], F32, kind="Internal").ap()
    dump_out = {}

    M = Mem(nc, P, 205 * 1024)
    M.region("const", 14 * 1024)
    M.region("wout", 16 * 1024)
    M.region("r1", 32 * 1024)
    M.region("qt", 16 * 1024)
    M.region("kt", 32 * 1024)
    M.region("vt", 32 * 1024)
    M.region("r2", 36 * 1024)
    M.region("ut", 16640)
    M.region("qmt", 8 * 1024)
    PS = [nc.alloc_psum_tensor("ps%d" % i, [128, 512], F32) for i in range(8)]

    def psf(i):
        return PS[i][:, :]

    def psb(i):
        return PS[i][:, :].bitcast(BF16)

    def pk(i):
        return "ps%d" % i

    A = P.add
    CF = M.alloc("const", "cf", [128, NCF], F32)
    CB = M.alloc("const", "cb", [128, NCB], BF16)
    ZT = M.alloc("const", "zt", [128, 1024], BF16)
    IDB = CB[:, CB_ID:CB_ID + 128]
    MASK4 = CB[:, CB_MASK:CB_MASK + 512]
    BD64 = CB[:, CB_BD:CB_BD + 128]
    ONESB = CB[:, CB_ONES:CB_ONES + 128]
    TRI = CB[:, CB_TRI:CB_TRI + 128]
    EPSC = CF[:, CF_EPS:CF_EPS + 1]
    IDF = CF[:, CF_IDF:CF_IDF + 128]
    ONES64 = CF[:, CF_ONES:CF_ONES + 64]

    A(SP, lambda e: e.dma_start(out=CF, in_=D["cf"]), w=["cf"], dma=True)
    A(POOL, lambda e: e.dma_start(out=CB, in_=D["cb"]), w=["cb"], dma=True)
    WKV = M.alloc("qt", "wkv", [128, 8, 512], BF16)
    MEMT = M.alloc("qt", "memt", [128, 8, 256], BF16, keys=["memt0", "memt1"])
    KMT = M.alloc("const", "kmt", [128, 2, 256], BF16, keys=["kmt0", "kmt1"])
    VMA = M.alloc("const", "vma", [128, 2, 4, 65], BF16)
    A(POOL, lambda e: e.dma_start(out=WKV, in_=D["w_kv"].rearrange("(k p) c -> p k c", p=128)), w=["wkv"], dma=True)
    WIN = M.alloc("r1", "win", [128, 8, 2048], BF16, keys=["win%d" % i for i in range(4)])
    win_keys = []
    for i in range(4):
        key = "win%d" % i
        win_keys.append(key)
        A(POOL, lambda e, i=i: e.dma_start(
            out=WIN[:, 2 * i:2 * i + 2, :],
            in_=D["w_in"][256 * i:256 * i + 256, :].rearrange("(k p) c -> p k c", p=128)),
          w=[key], dma=True)
    A(DVE, lambda e: e.memset(ZT, 0.0), w=["zt"])
    ZT4 = ZT.unsqueeze(1).broadcast_to([128, 4, 1024])

    KT = M.alloc("kt", "kt", [128, 4, 4096], BF16, keys=["kt%d_%d" % (c, g) for c in range(4) for g in range(8)])
    VT = M.alloc("vt", "vt", [128, 4, 4096], BF16, keys=["vt%d_%d" % (c, g) for c in range(4) for g in range(8)])
    UT = M.alloc("ut", "ut", [128, 2, 2064], F32, keys=["ut0", "ut1"])
    QMT = M.alloc("qmt", "qmt", [128, 2, 2048], BF16, keys=["qmt0", "qmt1"])
    XT = [M.alloc("r2", "xt%d" % i, [128, 1024], F32) for i in range(2)]
    XN = [M.alloc("r2", "xn%d" % i, [128, 1024], BF16) for i in range(2)]
    HTG = [M.alloc("r2", "htg%d" % i, [128, 8, 512], BF16, keys=["htg%d_%d" % (i, t) for t in range(4)]) for i in range(2)]
    SQZ = [M.alloc("r2", "sqz%d" % i, [128, 512], BF16) for i in range(2)]
    RS = [M.alloc("r2", "rs%d" % i, [128, 512], F32) for i in range(2)]
    ST4 = [M.alloc("r2", "st4_%d" % i, [128, 4], F32) for i in range(2)]

    def norm_front(src_ap, slot):
        xt, xn, st = XT[slot], XN[slot], ST4[slot]
        kx, kn, ks = "xt%d" % slot, "xn%d" % slot, "st4_%d" % slot
        A(SP, lambda e: e.dma_start(out=xt, in_=src_ap), w=[kx], dma=True)
        A(ACT, lambda e: e.activation(out=xn, in_=xt, func=AF.Square, accum_out=st[:, 0:1]), r=[kx], w=[kn, ks])
        A(ACT, lambda e: e.activation(out=st[:, 1:2], in_=st[:, 0:1], func=AF.Ln, bias=EPSC, scale=1.0 / 1024), r=[ks, "cf"], w=[ks])
        A(ACT, lambda e: e.activation(out=st[:, 2:3], in_=st[:, 1:2], func=AF.Exp, scale=-0.5), r=[ks], w=[ks])
        A(DVE, lambda e: e.tensor_scalar(out=xn, in0=xt, scalar1=st[:, 2:3], scalar2=None, op0=ALU.mult), r=[kx, ks], w=[kn])

    def norm_back(slot, gain_col, dst_fn, dkeys, ti):
        xn, kn = XN[slot], "xn%d" % slot
        tp = ti % 2

        def tr(e):
            for k in range(8):
                ins = e.transpose(out=psb(tp)[:, k * 128:(k + 1) * 128], in_=xn[:, k * 128:(k + 1) * 128], identity=IDB)
            return ins
        A(PE, tr, r=[kn, "cb"], w=[pk(tp)])
        g = CF[:, gain_col:gain_col + 8].unsqueeze(2).broadcast_to([128, 8, 128])
        A(DVE, lambda e: e.tensor_tensor(out=dst_fn(), in0=psb(tp).rearrange("p (k t) -> p k t", k=8), in1=g, op=ALU.mult),
          r=[pk(tp), "cf"], w=dkeys)

    def norm_tile(src_ap, slot, gain_col, dst_fn, dkeys, ti):
        norm_front(src_ap, slot)
        norm_back(slot, gain_col, dst_fn, dkeys, ti)

    def head_norm(zbank, zkey, n, gain_col, dst, dkeys, i):
        sq, rs = SQZ[i % 2], RS[i % 2]
        ksq, krs = "sqz%d" % (i % 2), "rs%d" % (i % 2)
        mb = 4 + i % 2
        A(ACT, lambda e: e.activation(out=sq[:, 0:n], in_=psf(zbank)[:, 0:n], func=AF.Square), r=[zkey], w=[ksq])
        A(PE, lambda e: e.matmul(psf(mb)[:, 0:n], lhsT=BD64, rhs=sq[:, 0:n], start=True, stop=True), r=[ksq, "cb"], w=[pk(mb)])
        A(ACT, lambda e: e.activation(out=rs[:, 0:n], in_=psf(mb)[:, 0:n], func=AF.Ln, bias=EPSC, scale=1.0), r=[pk(mb), "cf"], w=[krs])
        A(ACT, lambda e: e.activation(out=rs[:, 0:n], in_=rs[:, 0:n], func=AF.Exp, scale=-0.5), r=[krs], w=[krs])
        A(DVE, lambda e: e.scalar_tensor_tensor(out=dst, in0=psf(zbank)[:, 0:n], scalar=CF[:, gain_col:gain_col + 1], in1=rs[:, 0:n],
                                                 op0=ALU.mult, op1=ALU.mult), r=[zkey, krs, "cf"], w=dkeys)

    A(DVE, lambda e: e.memset(VMA[:, :, :, 64:65], 1.0), w=["vma"])
    for mt in range(2):
        norm_tile(D["mem"][mt * 128:(mt + 1) * 128, :], mt, CF_GMEM, lambda mt=mt: MEMT[:, :, mt * 128:(mt + 1) * 128], ["memt%d" % mt], mt)
    for c in range(2):
        zb = 2 + c

        def kproj(e, c=c, zb=zb):
            for k in range(8):
                ins = e.matmul(psf(zb)[:, 0:256], lhsT=WKV[:, k, c * 128:(c + 1) * 128], rhs=MEMT[:, k, :], start=(k == 0), stop=(k == 7))
            return ins
        A(PE, kproj, r=["wkv", "memt0", "memt1"], w=[pk(zb)])
        head_norm(zb, pk(zb), 256, CF_GMK, KMT[:, c, :], ["kmt%d" % c], c)
    for mt in range(2):
        zb = 6 + mt

        def vproj(e, mt=mt, zb=zb):
            for k in range(8):
                ins = e.matmul(psf(zb)[:, 0:256], lhsT=MEMT[:, k, mt * 128:(mt + 1) * 128], rhs=WKV[:, k, 256:512], start=(k == 0), stop=(k == 7))
            return ins
        A(PE, vproj, r=["wkv", "memt%d" % mt], w=[pk(zb)])
        A(ACT, lambda e, mt=mt, zb=zb: e.activation(out=VMA[:, mt, :, 0:64], in_=psf(zb)[:, 0:256].rearrange("p (h d) -> p h d", h=4), func=AF.Copy),
          r=[pk(zb)], w=["vma"])
    M.reset("qt")
    QT = M.alloc("qt", "qt", [128, 4, 2048], BF16, keys=["qt%d_%d" % (c, g) for c in range(4) for g in range(4)])

    cnt = dict(ti=0, zi=0, hn=0)

    def tile_front(g, t):
        src = (D["xh"] if g < 4 else D["xo"])[((g % 4) * 4 + t) * 128:((g % 4) * 4 + t + 1) * 128, :]
        norm_front(src, (g * 4 + t) % 2)

    def tile_back(g, t):
        hb = HTG[g % 2]
        ti = g * 4 + t
        norm_back(ti % 2, CF_GATTN, lambda: hb[:, :, t * 128:(t + 1) * 128], ["htg%d_%d" % (g % 2, t)], ti)

    def tile_task(g, t):
        tile_front(g, t)
        tile_back(g, t)

    ZB = (2, 3, 6, 7)

    def chunk_part1(g, c):
        hb = HTG[g % 2]
        hkeys = ["htg%d_%d" % (g % 2, t) for t in range(4)]
        zb = ZB[cnt["zi"] % 4]
        cnt["zi"] += 1

        def proj(e):
            for k in range(8):
                ins = e.matmul(psf(zb), lhsT=WIN[:, k, c * 128:(c + 1) * 128], rhs=hb[:, k, :], start=(k == 0), stop=(k == 7))
            return ins
        A(PE, proj, r=win_keys + hkeys, w=[pk(zb)])
        st = dict(g=g, c=c, zb=zb)
        if c < 8 or c >= 14:
            i = cnt["hn"]
            cnt["hn"] += 1
            st["i"] = i
            sq, ksq = SQZ[i % 2], "sqz%d" % (i % 2)
            A(ACT, lambda e: e.activation(out=sq, in_=psf(zb), func=AF.Square), r=[pk(zb)], w=[ksq])
        return st

    def chunk_part2(st):
        g, c, zb = st["g"], st["c"], st["zb"]
        if 8 <= c < 12:
            A(DVE, lambda e: e.tensor_copy(out=VT[:, c - 8, g * 512:(g + 1) * 512], in_=psf(zb)),
              r=[pk(zb)], w=["vt%d_%d" % (c - 8, g)])
            return
        if c in (12, 13):
            if g == 3:
                A(DVE, lambda e: e.tensor_copy(out=UT[:, c - 12, 0:16], in_=psf(zb)[:, 496:512]), r=[pk(zb)], w=["ut%d" % (c - 12)])
            else:
                A(DVE, lambda e: e.tensor_copy(out=UT[:, c - 12, 16 + (g - 4) * 512:16 + (g - 3) * 512], in_=psf(zb)),
                  r=[pk(zb)], w=["ut%d" % (c - 12)])
            return
        if c < 4:
            gain_col, dst, dkeys = CF_GQ, QT[:, c, (g - 4) * 512:(g - 3) * 512], ["qt%d_%d" % (c, g - 4)]
        elif c < 8:
            gain_col, dst, dkeys = CF_GK, KT[:, c - 4, g * 512:(g + 1) * 512], ["kt%d_%d" % (c - 4, g)]
        else:
            gain_col, dst, dkeys = CF_GMQ, QMT[:, c - 14, (g - 4) * 512:(g - 3) * 512], ["qmt%d" % (c - 14)]
        i = st["i"]
        sq, rs = SQZ[i % 2], RS[i % 2]
        ksq, krs = "sqz%d" % (i % 2), "rs%d" % (i % 2)
        mb = 4 + i % 2
        A(PE, lambda e: e.matmul(psf(mb), lhsT=BD64, rhs=sq, start=True, stop=True), r=[ksq, "cb"], w=[pk(mb)])
        A(ACT, lambda e: e.activation(out=rs, in_=psf(mb), func=AF.Ln, bias=EPSC, scale=1.0), r=[pk(mb), "cf"], w=[krs])
        A(ACT, lambda e: e.activation(out=rs, in_=rs, func=AF.Exp, scale=-0.5), r=[krs], w=[krs])
        A(DVE, lambda e: e.scalar_tensor_tensor(out=dst, in0=psf(zb), scalar=CF[:, gain_col:gain_col + 1], in1=rs,
                                                 op0=ALU.mult, op1=ALU.mult), r=[pk(zb), krs, "cf"], w=dkeys)

    def chunks_of(g):
        k_, v_ = list(range(4, 8)), list(range(8, 12))
        ch = []
        if g >= 4:
            extra = [0, 1, 2, 3, 14, 15, 12, 13]
        elif g == 3:
            extra = [12, 13]
        else:
            extra = []
        normed = k_ + [c for c in extra if c < 4 or c >= 14]
        cheap = v_ + [c for c in extra if c in (12, 13)]
        while normed or cheap:
            if normed:
                ch.append(normed.pop(0))
            if cheap:
                ch.append(cheap.pop(0))
        return ch

    tile_front(0, 0)
    for t in range(4):
        if t + 1 < 4:
            tile_front(0, t + 1)
        tile_back(0, t)
    pending = None
    for g in range(8):
        ch = chunks_of(g)
        pts = [max(1, (len(ch) * (j + 1)) // 5) for j in range(5)]
        nt = 0
        for i, c in enumerate(ch):
            st = chunk_part1(g, c)
            if pending is not None:
                chunk_part2(pending)
            pending = st
            while g + 1 < 8 and nt < 5 and (i + 1) >= pts[nt]:
                if nt < 4:
                    tile_front(g + 1, nt)
                if nt >= 1:
                    tile_back(g + 1, nt - 1)
                nt += 1
        while g + 1 < 8 and nt < 5:
            if nt < 4:
                tile_front(g + 1, nt)
            if nt >= 1:
                tile_back(g + 1, nt - 1)
            nt += 1
    chunk_part2(pending)

    def add_dump(name, ap, keys, shape, dt):
        t = nc.dram_tensor("dump_" + name, shape, dt, kind="ExternalOutput").ap()
        dump_out[name] = t
        A(SP, lambda e: e.dma_start(out=t, in_=ap), r=keys, w=["dump_" + name], dma=True)

    vt_keys = lambda c: ["vt%d_%d" % (c, g) for g in range(8)]
    kt_keys = lambda c: ["kt%d_%d" % (c, g) for g in range(8)]
    qt_keys = lambda c: ["qt%d_%d" % (c, g) for g in range(4)]
    if "qkv" in dumps:
        add_dump("qt", QT, sum([qt_keys(c) for c in range(4)], []), [128, 4, 2048], BF16)
        add_dump("kt", KT, sum([kt_keys(c) for c in range(4)], []), [128, 4, 4096], BF16)
        add_dump("vt", VT, sum([vt_keys(c) for c in range(4)], []), [128, 4, 4096], BF16)
        add_dump("ut", UT, ["ut0", "ut1"], [128, 2, 2064], F32)
        add_dump("qmt", QMT, ["qmt0", "qmt1"], [128, 2, 2048], BF16)
    if stage <= 1:
        P.emit(nc)
        return nc, dump_out
    return build_rest(nc, P, M, D, A, CF, CB, PS, psf, psb, pk, out, xg, yg, xmid, stage, dumps, dump_out, add_dump,
                      dict(QT=QT, KT=KT, VT=VT, UT=UT, QMT=QMT, WIN=WIN, vt_keys=vt_keys, kt_keys=kt_keys, qt_keys=qt_keys,
                           norm_tile=norm_tile, head_norm=head_norm, ZT4=ZT4, KMT=KMT, VMA=VMA, XT=XT, XN=XN, ST4=ST4, SQZ=SQZ, RS=RS, win_keys=win_keys))


POOL_WINDOWS = (2, 4, 8, 16)


def prep_inputs(inp):
    f = np.float32
    x = np.ascontiguousarray(inp["x"], dtype=f)
    mem = np.ascontiguousarray(inp["mem"], dtype=f)
    cb = np.zeros((128, NCB), f)
    k = np.arange(128)[:, None]
    q = np.arange(128)[None, :]
    cb[:, CB_ID:CB_ID + 128] = np.eye(128, dtype=f)
    mprev = (k >= q).astype(f)
    mcur = (k <= q).astype(f)
    cb[:, CB_MASK:CB_MASK + 512] = np.concatenate([mprev, mcur, mprev, mcur], axis=1)
    bd = np.zeros((128, 128), f)
    bd[:64, :64] = 1.0 / 64
    bd[64:, 64:] = 1.0 / 64
    cb[:, CB_BD:CB_BD + 128] = bd
    cb[:, CB_ONES:CB_ONES + 128] = 1.0
    cb[:, CB_TRI:CB_TRI + 128] = (k < q).astype(f)
    pp = np.asarray(inp["pool_proj"], dtype=f)[0]
    for c in range(2):
        cb[0:64, CB_PBD + c * 128:CB_PBD + c * 128 + 64] = pp[2 * c]
        cb[64:128, CB_PBD + c * 128 + 64:CB_PBD + c * 128 + 128] = pp[2 * c + 1]
    w_r = np.concatenate([np.asarray(inp["w_group"], f)[0],
                          np.transpose(np.asarray(inp["w_router"], f)[0], (1, 0, 2)).reshape(1024, 64)], axis=1)
    w_r = np.ascontiguousarray(w_r)
    shared = dict(
        w_in=np.ascontiguousarray(inp["w_in"][0], dtype=f), w_out=np.ascontiguousarray(inp["w_out"][0], dtype=f),
        w_kv=np.ascontiguousarray(inp["w_mem_kv"][0], dtype=f), w_r=w_r,
        w1=np.ascontiguousarray(inp["w1"][0], dtype=f), w3=np.ascontiguousarray(inp["w3"][0], dtype=f),
        w2=np.ascontiguousarray(inp["w2"][0], dtype=f), cb=cb)
    p = np.arange(128)
    in_maps = []
    for c in range(8):
        b, half = c // 2, c % 2
        cf = np.zeros((128, NCF), f)
        cf[:, CF_GATTN:CF_GATTN + 8] = np.asarray(inp["attn_norm"], f)[0].reshape(8, 128).T
        cf[:, CF_GMEM:CF_GMEM + 8] = np.asarray(inp["mem_norm"], f)[0].reshape(8, 128).T
        cf[:, CF_GQ] = np.tile(np.asarray(inp["q_norm"], f)[0], 2)
        cf[:, CF_GK] = np.tile(np.asarray(inp["k_norm"], f)[0], 2)
        cf[:, CF_GMQ] = np.tile(np.asarray(inp["mq_norm"], f)[0], 2)
        cf[:, CF_GMK] = np.tile(np.asarray(inp["mk_norm"], f)[0], 2)
        for ch in range(2):
            w = np.array([POOL_WINDOWS[2 * ch + pi // 64] for pi in range(128)], f)
            cf[:, CF_INVW + ch] = 1.0 / w
            cf[:, CF_PSC + ch] = np.asarray(inp["pool_scale"], f)[0, ch * 128:(ch + 1) * 128]
            for t in range(16):
                pos = t + 2048 * half
                cf[:, CF_IC16 + ch * 16 + t] = 1.0 / np.minimum(pos + 1, w)
        cf[:, CF_HV] = float(half)
        cf[:, CF_EPS] = EPS
        cf[:, CF_ONES:CF_ONES + 64] = 1.0
        cf[:, CF_OFFS:CF_OFFS + 64] = (np.arange(64) * EXPERT_CAP).astype(f)[None, :]
        cf[:, CF_BIAS:CF_BIAS + 8] = np.asarray(inp["b_group"], f)[0][None, :]
        cf[:, CF_BIAS + 8:CF_BIAS + 72] = np.asarray(inp["b_router"], f)[0].reshape(64)[None, :]
        cf[:, CF_IDF:CF_IDF + 128] = np.eye(128, dtype=f)
        cf[:, CF_GFFN:CF_GFFN + 1024] = np.asarray(inp["ffn_norm"], f)[0][None, :]
        xo = x[b, half * 2048:(half + 1) * 2048]
        xh = x[b, 0:2048] if half == 1 else np.zeros((2048, 1024), f)
        m = dict(shared)
        m.update(xo=np.ascontiguousarray(xo), xh=np.ascontiguousarray(xh), mem=mem[b], cf=cf)
        in_maps.append(m)
    return in_maps


_NC_CACHE = {}


def kernel(**inputs):
    in_maps = prep_inputs(inputs)
    if "nc" not in _NC_CACHE:
        _NC_CACHE["nc"] = build_program()[0]
    res = run_bass_kernel_spmd(_NC_CACHE["nc"], in_maps, core_ids=list(range(8)))
    outs = [np.asarray(r["out"], dtype=np.float32) for r in res.results]
    return np.stack([np.concatenate([outs[2 * b], outs[2 * b + 1]], axis=0) for b in range(4)], axis=0)


def build_rest(nc, P, M, D, A, CF, CB, PS, psf, psb, pk, out, xg, yg, xmid, stage, dumps, dump_out, add_dump, S):
    QT, KT, VT, UT, QMT = S["QT"], S["KT"], S["VT"], S["UT"], S["QMT"]
    vt_keys, kt_keys, qt_keys = S["vt_keys"], S["kt_keys"], S["qt_keys"]
    IDB = CB[:, CB_ID:CB_ID + 128]
    MASK4 = CB[:, CB_MASK:CB_MASK + 512]
    ONESB = CB[:, CB_ONES:CB_ONES + 128]
    TRI = CB[:, CB_TRI:CB_TRI + 128]
    EPSC = CF[:, CF_EPS:CF_EPS + 1]
    IDF = CF[:, CF_IDF:CF_IDF + 128]
    ONES64 = CF[:, CF_ONES:CF_ONES + 64]
    HV = CF[:, CF_HV:CF_HV + 1]

    WOUT = M.alloc("wout", "wout", [128, 8, 1024], BF16)
    A(POOL, lambda e: e.dma_start(out=WOUT, in_=D["w_out"].rearrange("(k p) c -> p k c", p=128)), w=["wout"], dma=True)

    M.reset("r1")
    MIXT = M.alloc("r1", "mixt", [128, 8, 2048], BF16, keys=["mixt%d" % i for i in range(8)])
    M.reset("r2")
    TA = M.alloc("r2", "ta", [128, 2064], F32)
    TB = M.alloc("r2", "tb", [128, 2064], F32)
    PL = M.alloc("r2", "pl", [128, 2, 2048], BF16, keys=["pl0", "pl1"])
    T16 = M.alloc("r2", "t16", [128, 16], F32)
    IC = CF[:, CF_IC16:CF_IC16 + 32].rearrange("p (c t) -> p c t", c=2)
    N = 2064
    for c in range(2):
        U = UT[:, c, :]
        uk = "ut%d" % c
        A(POOL, lambda e, U=U: e.tensor_tensor(out=TA[:, 1:N], in0=U[:, 1:N], in1=U[:, 0:N - 1], op=ALU.add), r=[uk], w=["ta"])
        if c == 0:
            A(POOL, lambda e: e.tensor_tensor(out=TB[64:128, 3:N], in0=TA[64:128, 3:N], in1=TA[64:128, 1:N - 2], op=ALU.add), r=["ta"], w=["tb"])
        else:
            A(POOL, lambda e: e.tensor_tensor(out=TB[:, 3:N], in0=TA[:, 3:N], in1=TA[:, 1:N - 2], op=ALU.add), r=["ta"], w=["tb"])
            A(DVE, lambda e: e.tensor_tensor(out=TA[:, 7:N], in0=TB[:, 7:N], in1=TB[:, 3:N - 4], op=ALU.add), r=["tb"], w=["ta"])
            A(DVE, lambda e: e.tensor_tensor(out=TB[64:128, 15:N], in0=TA[64:128, 15:N], in1=TA[64:128, 7:N - 8], op=ALU.add), r=["ta"], w=["tb"])
        for (lo, hi, T, tk) in ((0, 64, TA, "ta"), (64, 128, TB, "tb")):
            A(DVE, lambda e, lo=lo, hi=hi, T=T, U=U, c=c: e.scalar_tensor_tensor(
                out=PL[lo:hi, c, :], in0=T[lo:hi, 16:N], scalar=CF[lo:hi, CF_INVW + c:CF_INVW + c + 1], in1=U[lo:hi, 16:N],
                op0=ALU.mult, op1=ALU.subtract), r=[tk, uk, "cf"], w=["pl%d" % c])
            A(DVE, lambda e, lo=lo, hi=hi, T=T, c=c: e.tensor_tensor(out=T16[lo:hi, :], in0=T[lo:hi, 16:32], in1=IC[lo:hi, c, :], op=ALU.mult),
              r=[tk, "cf"], w=["t16"])
        A(DVE, lambda e, U=U, c=c: e.tensor_tensor(out=PL[:, c, 0:16], in0=T16, in1=U[:, 16:32], op=ALU.subtract), r=["t16", uk], w=["pl%d" % c])
        for tg in range(4):
            zb = 2 + tg % 2
            A(PE, lambda e, c=c, tg=tg, zb=zb: e.matmul(psf(zb), lhsT=CB[:, CB_PBD + c * 128:CB_PBD + (c + 1) * 128],
                                                         rhs=PL[:, c, tg * 512:(tg + 1) * 512], start=True, stop=True),
              r=["pl%d" % c, "cb"], w=[pk(zb)])
            A(ACT, lambda e, c=c, tg=tg, zb=zb: e.activation(out=MIXT[:, 4 + c, tg * 512:(tg + 1) * 512], in_=psf(zb), func=AF.Copy,
                                                             scale=CF[:, CF_PSC + c:CF_PSC + c + 1]),
              r=[pk(zb), "cf"], w=["mixt%d" % (4 + c)])

    M.reset("r2")
    M.reset("ut")
    XT = [M.alloc("r2", "xt%d" % i, [128, 1024], F32) for i in range(2)]
    XN = [M.alloc("r2", "xn%d" % i, [128, 1024], BF16) for i in range(2)]
    SQZ = [M.alloc("r2", "sqz%d" % i, [128, 512], BF16) for i in range(2)]
    RS = [M.alloc("r2", "rs%d" % i, [128, 512], F32) for i in range(2)]
    ST4 = [M.alloc("r2", "st4_%d" % i, [128, 4], F32) for i in range(2)]
    KMT, VMA = S["KMT"], S["VMA"]
    PTM = [M.alloc("r2", "ptm%d" % i, [128, 512], BF16) for i in range(2)]
    ACM = [M.alloc("r2", "acm%d" % i, [128, 512], F32) for i in range(2)]
    TMPO = [M.alloc("ut", "tmpo%d" % i, [128, 2048], BF16) for i in range(2)]
    S["XT"], S["XN"], S["ST4"], S["SQZ"], S["RS"] = XT, XN, ST4, SQZ, RS

    def norm_tile(src_ap, slot, gain_col, dst, dkeys, ti):
        xt, xn, st = XT[slot], XN[slot], ST4[slot]
        kx, kn, ks = "xt%d" % slot, "xn%d" % slot, "st4_%d" % slot
        tp = ti % 2
        A(SP, lambda e: e.dma_start(out=xt, in_=src_ap), w=[kx], dma=True)
        A(ACT, lambda e: e.activation(out=xn, in_=xt, func=AF.Square, accum_out=st[:, 0:1]), r=[kx], w=[kn, ks])
        A(ACT, lambda e: e.activation(out=st[:, 1:2], in_=st[:, 0:1], func=AF.Ln, bias=EPSC, scale=1.0 / 1024), r=[ks, "cf"], w=[ks])
        A(ACT, lambda e: e.activation(out=st[:, 2:3], in_=st[:, 1:2], func=AF.Exp, scale=-0.5), r=[ks], w=[ks])
        A(ACT, lambda e: e.activation(out=xn, in_=xt, func=AF.Copy, scale=st[:, 2:3]), r=[kx, ks], w=[kn])

        def tr(e):
            for k in range(8):
                ins = e.transpose(out=psb(tp)[:, k * 128:(k + 1) * 128], in_=xn[:, k * 128:(k + 1) * 128], identity=IDB)
            return ins
        A(PE, tr, r=[kn, "cb"], w=[pk(tp)])
        g = CF[:, gain_col:gain_col + 8].unsqueeze(2).broadcast_to([128, 8, 128])
        A(DVE, lambda e: e.tensor_tensor(out=dst, in0=psb(tp).rearrange("p (k t) -> p k t", k=8), in1=g, op=ALU.mult),
          r=[pk(tp), "cf"], w=dkeys)

    def head_norm(zbank, zkey, n, gain_col, dst, dkeys, i):
        sq, rs = SQZ[i % 2], RS[i % 2]
        ksq, krs = "sqz%d" % (i % 2), "rs%d" % (i % 2)
        mb = 4 + i % 2
        BD64 = CB[:, CB_BD:CB_BD + 128]
        A(ACT, lambda e: e.activation(out=sq[:, 0:n], in_=psf(zbank)[:, 0:n], func=AF.Square), r=[zkey], w=[ksq])
        A(PE, lambda e: e.matmul(psf(mb)[:, 0:n], lhsT=BD64, rhs=sq[:, 0:n], start=True, stop=True), r=[ksq, "cb"], w=[pk(mb)])
        A(ACT, lambda e: e.activation(out=rs[:, 0:n], in_=psf(mb)[:, 0:n], func=AF.Ln, bias=EPSC, scale=1.0), r=[pk(mb), "cf"], w=[krs])
        A(ACT, lambda e: e.activation(out=rs[:, 0:n], in_=rs[:, 0:n], func=AF.Exp, scale=-0.5), r=[krs], w=[krs])
        A(DVE, lambda e: e.scalar_tensor_tensor(out=dst, in0=psf(zbank)[:, 0:n], scalar=CF[:, gain_col:gain_col + 1], in1=rs[:, 0:n],
                                                 op0=ALU.mult, op1=ALU.mult), r=[zkey, krs, "cf"], w=dkeys)

    def normalize_out(acc_ap_fn, acc_keys, tmpo, tkey, bcb):
        for tg in range(4):
            a = acc_ap_fn(tg)
            A(DVE, lambda e, a=a: e.reciprocal(out=a[64:65, :], in_=a[64:65, :]), r=acc_keys, w=acc_keys)
            A(PE, lambda e, a=a: e.matmul(psf(bcb)[0:64, :], lhsT=ONES64[64:65, :], rhs=a[64:65, :], start=True, stop=True), r=acc_keys + ["cf"], w=[pk(bcb)])
            A(DVE, lambda e, a=a, tg=tg: e.tensor_tensor(out=tmpo[0:64, tg * 512:(tg + 1) * 512], in0=a[0:64, :], in1=psf(bcb)[0:64, :], op=ALU.mult),
              r=acc_keys + [pk(bcb)], w=[tkey])

    its = [(mh, tg) for mh in range(4) for tg in range(4)]

    def MF1(it):
        mh, tg = its[it]
        c, b0 = mh // 2, 64 * (mh % 2)
        sbs = (2, 3) if it % 2 == 0 else (6, 7)
        for mt in range(2):
            sb = sbs[mt]
            ptm, pkey = PTM[mt], "ptm%d" % mt
            A(PE, lambda e, mt=mt, sb=sb: e.matmul(
                psf(sb), lhsT=KMT[b0:b0 + 64, c, mt * 128:(mt + 1) * 128], rhs=QMT[b0:b0 + 64, c, tg * 512:(tg + 1) * 512], start=True, stop=True),
              r=["kmt%d" % c, "qmt%d" % c], w=[pk(sb)])
            A(ACT, lambda e, sb=sb, ptm=ptm: e.activation(out=ptm, in_=psf(sb), func=AF.Exp, scale=0.125), r=[pk(sb)], w=[pkey])

    def MF2(it):
        mh, tg = its[it]
        ob = it % 2
        for mt in range(2):
            ptm, pkey = PTM[mt], "ptm%d" % mt
            A(PE, lambda e, mt=mt, ptm=ptm: e.matmul(psf(ob)[0:65, :], lhsT=VMA[:, mt, mh, :], rhs=ptm, start=(mt == 0), stop=(mt == 1)),
              r=["vma", pkey], w=[pk(ob)])
        acm, akey = ACM[it % 2], "acm%d" % (it % 2)
        A(ACT, lambda e: e.activation(out=acm[0:65, :], in_=psf(ob)[0:65, :], func=AF.Copy), r=[pk(ob)], w=[akey])

    def MB(it):
        mh, tg = its[it]
        c, b0 = mh // 2, 64 * (mh % 2)
        tmpo, tkey = TMPO[mh % 2], "tmpo%d" % (mh % 2)
        acm, akey = ACM[it % 2], "acm%d" % (it % 2)
        bcb = 4 + it % 2
        A(ACT, lambda e: e.activation(out=acm[64:65, :], in_=acm[64:65, :], func=AF.Ln), r=[akey], w=[akey])
        A(ACT, lambda e: e.activation(out=acm[64:65, :], in_=acm[64:65, :], func=AF.Exp, scale=-1.0), r=[akey], w=[akey])
        A(PE, lambda e: e.matmul(psf(bcb)[0:64, :], lhsT=ONES64[64:65, :], rhs=acm[64:65, :], start=True, stop=True), r=[akey, "cf"], w=[pk(bcb)])
        A(DVE, lambda e: e.tensor_tensor(out=tmpo[0:64, tg * 512:(tg + 1) * 512], in0=acm[0:64, :], in1=psf(bcb)[0:64, :], op=ALU.mult),
          r=[akey, pk(bcb)], w=[tkey])
        if tg == 3:
            A(SP, lambda e: e.dma_start(out=MIXT[b0:b0 + 64, 6 + c, :], in_=tmpo[0:64, :]), r=[tkey], w=["mixt%d" % (6 + c)], dma=True)

    PTM = [M.alloc("r2", "ptm%d" % i, [128, 512], BF16) for i in range(2)] if False else PTM
    MF1(0)
    for it in range(16):
        MF2(it)
        if it + 1 < 16:
            MF1(it + 1)
        if it >= 1:
            MB(it - 1)
    MB(15)

    if "mix" in dumps and stage == 2:
        add_dump("mixt", MIXT, ["mixt%d" % i for i in range(8)], [128, 8, 2048], BF16)
    if stage <= 2:
        P.emit(nc)
        return nc, dump_out
    return build_attn(nc, P, M, D, A, CF, CB, PS, psf, psb, pk, out, xg, yg, xmid, stage, dumps, dump_out, add_dump, S, MIXT, WOUT)


def build_attn(nc, P, M, D, A, CF, CB, PS, psf, psb, pk, out, xg, yg, xmid, stage, dumps, dump_out, add_dump, S, MIXT, WOUT):
    QT, KT, VT = S["QT"], S["KT"], S["VT"]
    vt_keys, kt_keys, qt_keys = S["vt_keys"], S["kt_keys"], S["qt_keys"]
    IDB = CB[:, CB_ID:CB_ID + 128]
    MASK4 = CB[:, CB_MASK:CB_MASK + 512]
    ONES64 = CF[:, CF_ONES:CF_ONES + 64]
    HV = CF[:, CF_HV:CF_HV + 1]
    M.reset("r2"); M.reset("ut"); M.reset("qmt")
    import os as _os
    DILS = tuple(int(v) for v in _os.environ.get('ATTN_DILS', '1,4,16').split(','))
    NHP = int(_os.environ.get('ATTN_HP', '4'))
    LVL = int(_os.environ.get('ATTN_LEVEL', '9'))
    VA = [M.alloc("r2" if p < 3 else "ut", "va%d" % p, [128, 32, 2, 65], BF16) for p in range(3)]
    PT = [M.alloc("r2", "pt%d" % i, [128, 512], BF16) for i in range(4)]
    ACC = M.alloc("ut", "acc", [128, 2, 2048], F32, keys=["acc_q%d" % i for i in range(4)])
    TMPO = [M.alloc("qmt", "tmpo%d" % i, [128, 2048], BF16) for i in range(2)]
    for p, d in enumerate(DILS):
        nb = 32 // d
        A(DVE, lambda e, p=p: e.memset(VA[p][:, :, :, 64:65], 1.0), w=["va%d" % p])
        v = VA[p][:, :, :, 64:65].rearrange("p (r b) h o -> p r b (h o)", r=d)[:, :, 0:nb // 2, :]
        A(DVE, lambda e, v=v: e.tensor_scalar(out=v, in0=v, scalar1=HV, scalar2=None, op0=ALU.mult), r=["cf"], w=["va%d" % p])

    for i in range(16):
        A(SP, lambda e, i=i: e.dma_start(out=xg[i * 512:(i + 1) * 512, :].rearrange("(p r) d -> p r d", p=128), in_=S["ZT4"]),
          r=["zt"], w=["xg"], dma=True)

    units = [(hp, p, d) for hp in range(NHP) for p, d in enumerate(DILS)]
    NU = len(units)
    akeys = ["acc_q%d" % i for i in range(4)]

    def vprep_group(u, grp):
        hp, p, d = units[u]
        c, nb, va, vak = hp, 32 // d, VA[p], "va%d" % p

        def vtr(e):
            for j in range(8):
                kb = grp * 8 + j
                r, b = kb // nb, kb % nb
                s0 = d * 128 * b + r
                ins = e.transpose(out=psb(6)[:, j * 128:(j + 1) * 128], in_=VT[:, c, s0:s0 + 127 * d + 1:d], identity=IDB)
            return ins
        A(PE, vtr, r=vt_keys(c) + ["cb"], w=[pk(6)])
        src = psb(6).rearrange("p (k h d) -> p k h d", k=8, h=2)
        dst = va[:, grp * 8:grp * 8 + 8, :, 0:64]
        if grp % 2 == 0:
            A(ACT, lambda e: e.activation(out=dst, in_=src, func=AF.Copy), r=[pk(6)], w=[vak])
        else:
            A(DVE, lambda e: e.tensor_copy(out=dst, in_=src), r=[pk(6)], w=[vak])

    steps = []
    for u, (hp, p, d) in enumerate(units):
        nb = 32 // d
        for qg in range(4):
            for jp in range(2):
                blk = []
                for jj in range(2):
                    j = 2 * jp + jj
                    if d == 1:
                        r, b = 0, 16 + 4 * qg + j
                    elif d == 4:
                        r, b = j, 4 + qg
                    else:
                        r, b = 4 * qg + j, 1
                    kbc = r * nb + b
                    kc0 = d * 128 * b + r
                    blk.append((j, kbc, kbc - 1, kc0, d * 128 * (b - 1) + r, kc0 - 2048))
                steps.append(dict(u=u, hp=hp, p=p, d=d, qg=qg, jp=jp, blk=blk, n=len(steps)))
    mi = [0]

    def F1(st):
        n, c, d, blk = st["n"], st["hp"], st["d"], st["blk"]
        sset = n % 2
        pts = [(PT[2 * sset + hh], "pt%d" % (2 * sset + hh)) for hh in range(2)]
        st["pts"] = pts

        def qk(e):
            for jj, (j, kbc, kbp, kc0, kp0, q0) in enumerate(blk):
                for hh in range(2):
                    for kv, k0 in enumerate((kp0, kc0)):
                        ins = e.matmul(psf(2 * sset + hh)[:, (jj * 2 + kv) * 128:(jj * 2 + kv + 1) * 128],
                                       lhsT=KT[hh * 64:hh * 64 + 64, c, k0:k0 + 127 * d + 1:d],
                                       rhs=QT[hh * 64:hh * 64 + 64, c, q0:q0 + 127 * d + 1:d], start=True, stop=True)
            return ins
        A(PE, qk, r=kt_keys(c) + qt_keys(c), w=[pk(2 * sset), pk(2 * sset + 1)])
        for hh in range(2):
            pt, ptk = pts[hh]
            A(ACT, lambda e, sb=2 * sset + hh, pt=pt: e.activation(out=pt, in_=psf(sb), func=AF.Exp, scale=0.125), r=[pk(2 * sset + hh)], w=[ptk])
            meng = DVE if mi[0] % 2 == 0 else POOL
            mi[0] += 1
            A(meng, lambda e, pt=pt: e.tensor_tensor(out=pt, in0=pt, in1=MASK4, op=ALU.mult), r=[ptk, "cb"], w=[ptk])

    def F2(st):
        n, p, d, blk, qg, jp, pts = st["n"], st["p"], st["d"], st["blk"], st["qg"], st["jp"], st["pts"]
        va, vak = VA[p], "va%d" % p
        ob = 4 + n % 2

        def pv(e):
            for jj, (j, kbc, kbp, kc0, kp0, q0) in enumerate(blk):
                for hh in range(2):
                    pt = pts[hh][0]
                    o = psf(ob)[0:65, (hh * 2 + jj) * 128:(hh * 2 + jj + 1) * 128]
                    e.matmul(o, lhsT=va[:, kbp, hh, :], rhs=pt[:, (jj * 2) * 128:(jj * 2 + 1) * 128], start=True, stop=False)
                    ins = e.matmul(o, lhsT=va[:, kbc, hh, :], rhs=pt[:, (jj * 2 + 1) * 128:(jj * 2 + 2) * 128], start=False, stop=True)
            return ins
        A(PE, pv, r=[vak, pts[0][1], pts[1][1]], w=[pk(ob)])
        if d == 1:
            t0 = (qg * 4 + jp * 2) * 128
            dst = ACC[0:65, :, t0:t0 + 256]
            src = psf(ob)[0:65, :].rearrange("p (h t) -> p h t", h=2)
            A(ACT, lambda e: e.activation(out=dst, in_=src, func=AF.Copy), r=[pk(ob)], w=[akeys[qg]])
        else:
            if d == 4:
                dst = ACC[0:65, :, qg * 512:(qg + 1) * 512].rearrange("p h (i r) -> p h r i", r=4)[:, :, 2 * jp:2 * jp + 2, :]
                ks = [akeys[qg]]
            else:
                dst = ACC[0:65, :, :].rearrange("p h (i r) -> p h r i", r=16)[:, :, 4 * qg + 2 * jp:4 * qg + 2 * jp + 2, :]
                ks = akeys
            src = psf(ob)[0:65, :].rearrange("p (h r i) -> p h r i", h=2, r=2)
            A(DVE, lambda e: e.tensor_tensor(out=dst, in0=src, in1=dst, op=ALU.add), r=[pk(ob)] + ks, w=ks)

    tmi = [0]

    bci = [0]

    def NORM(hp):
        for hh in range(2):
            a = ACC[64:65, hh, :]
            A(ACT, lambda e, a=a: e.activation(out=a, in_=a, func=AF.Ln), r=akeys, w=akeys)
            A(ACT, lambda e, a=a: e.activation(out=a, in_=a, func=AF.Exp, scale=-1.0), r=akeys, w=akeys)
        for hh in range(2):
            tmpo, tkey = TMPO[tmi[0] % 2], "tmpo%d" % (tmi[0] % 2)
            tmi[0] += 1
            for tg in range(4):
                a = ACC[:, hh, tg * 512:(tg + 1) * 512]
                bb = 6 + bci[0] % 2
                bci[0] += 1
                A(PE, lambda e, a=a, bb=bb: e.matmul(psf(bb)[0:64, :], lhsT=ONES64[64:65, :], rhs=a[64:65, :], start=True, stop=True), r=[akeys[tg], "cf"], w=[pk(bb)])
                A(DVE, lambda e, a=a, tg=tg, tmpo=tmpo, bb=bb: e.tensor_tensor(out=tmpo[0:64, tg * 512:(tg + 1) * 512], in0=a[0:64, :], in1=psf(bb)[0:64, :], op=ALU.mult),
                  r=[akeys[tg], pk(bb)], w=[tkey])
            A(SP, lambda e, hh=hh, hp=hp, tmpo=tmpo: e.dma_start(out=MIXT[hh * 64:hh * 64 + 64, hp, :], in_=tmpo[0:64, :]), r=[tkey], w=["mixt%d" % hp], dma=True)

    for grp in range(4):
        vprep_group(0, grp)
    F1(steps[0])
    for n, st in enumerate(steps):
        if n + 1 < len(steps):
            F1(steps[n + 1])
        F2(st)
        sl = n % 8
        if sl in (1, 3, 5, 7) and st["u"] + 1 < NU:
            vprep_group(st["u"] + 1, sl // 2)
        if sl == 7 and st["p"] == len(DILS) - 1:
            NORM(st["hp"])

    if "mix" in dumps and stage == 3:
        add_dump("mixt", MIXT, ["mixt%d" % i for i in range(8)], [128, 8, 2048], BF16)
    if stage <= 3:
        P.emit(nc)
        return nc, dump_out
    return build_ffn(nc, P, M, D, A, CF, CB, PS, psf, psb, pk, out, xg, yg, xmid, stage, dumps, dump_out, add_dump, S, MIXT, WOUT)


def build_ffn(nc, P, M, D, A, CF, CB, PS, psf, psb, pk, out, xg, yg, xmid, stage, dumps, dump_out, add_dump, S, MIXT, WOUT):
    IDB = CB[:, CB_ID:CB_ID + 128]
    ONESB = CB[:, CB_ONES:CB_ONES + 128]
    TRI = CB[:, CB_TRI:CB_TRI + 128]
    EPSC = CF[:, CF_EPS:CF_EPS + 1]
    IDF = CF[:, CF_IDF:CF_IDF + 128]
    GFFN = CF[:, CF_GFFN:CF_GFFN + 1024]
    BIASR = CF[:, CF_BIAS:CF_BIAS + 72]
    OFFS = CF[:, CF_OFFS:CF_OFFS + 64]
    M.reset("kt"); M.reset("qt"); M.reset("r2")
    XT = [M.alloc("kt", "xt%d" % i, [128, 1024], F32) for i in range(2)]
    XM = [M.alloc("kt", "xm%d" % i, [128, 1024], F32, keys=["xm%d_0" % i, "xm%d_1" % i]) for i in range(2)]
    H2 = [M.alloc("kt", "h2_%d" % i, [128, 1024], F32) for i in range(2)]
    H2T = [M.alloc("kt", "h2t%d" % i, [128, 8, 128], F32, keys=["h2t%d_0" % i, "h2t%d_1" % i]) for i in range(2)]
    NHB = 12
    H2B = [M.alloc("r2", "h2b%d" % i, [128, 1024], BF16) for i in range(NHB)]
    WR = M.alloc("qt", "wr", [128, 8, 72], F32)
    AB = M.alloc("qt", "ab", [128, 16, 64], BF16, keys=["ab%d" % i for i in range(4)])
    G12 = M.alloc("qt", "g12", [128, 2, 16], F32)
    SMS = M.alloc("qt", "sms", [128, 2, 4], F32, keys=["sms0", "sms1"])
    NT = 4
    L4 = [M.alloc("r2", "l4_%d" % i, [128, NT, 72], F32, keys=["l4_%d_%d" % (i, j) for j in range(NT)]) for i in range(2)]
    RT = M.alloc("r2", "rt", [128, 1400], F32)
    DEST = [[nc.alloc_sbuf_tensor("dest%d_%d" % (a, tt), [128, 1], I32) for tt in range(16)] for a in range(2)]
    A(SP, lambda e: e.dma_start(out=WR, in_=D["w_r"].rearrange("(k p) c -> p k c", p=128)), w=["wr"], dma=True)

    _o = [0]

    def rt(n, shape=None):
        ap = RT[:, _o[0]:_o[0] + n]
        _o[0] += n
        if shape:
            ap = ap.rearrange("p (a b) -> p a b", a=shape[0]) if len(shape) == 2 else ap.rearrange("p (a b c) -> p a b c", a=shape[0], b=shape[1])
        return ap
    OHG, D8, E8, LSEL, OH1, L2, OH2 = [rt(NT * 8, (NT, 8)) for _ in range(7)]
    T64, A1F, A2F, RB = [rt(NT * 64, (NT, 8, 8)) for _ in range(4)]
    SM = rt(16 * NT, (16, NT))
    RK = "rt"

    def S1(tt):
        sl = tt % 2
        xt, xm, h2, h2b = XT[sl], XM[sl], H2[sl], H2B[tt % NHB]
        kx, km, kh, kb = "xt%d" % sl, "xm%d" % sl, "h2_%d" % sl, "h2b%d" % (tt % NHB)
        sm, ksm = SMS[:, sl, :], "sms%d" % sl
        tok = slice(tt * 128, (tt + 1) * 128)
        ob = (0, 1) if sl == 0 else (4, 5)
        A(SP, lambda e: e.dma_start(out=xt, in_=D["xo"][tok, :]), w=[kx], dma=True)
        for half in range(2):
            def oproj(e, half=half):
                for k in range(8):
                    e.matmul(psf(ob[half]), lhsT=MIXT[:, k, tok], rhs=WOUT[:, k, half * 512:(half + 1) * 512], start=(k == 0), stop=False)
                return e.matmul(psf(ob[half]), lhsT=IDF, rhs=xt[:, half * 512:(half + 1) * 512], start=False, stop=True)
            A(PE, oproj, r=["wout", kx, "cf"] + ["mixt%d" % i for i in range(8)], w=[pk(ob[half])])
            A(ACT, lambda e, half=half: e.activation(out=xm[:, half * 512:(half + 1) * 512], in_=psf(ob[half]), func=AF.Copy), r=[pk(ob[half])], w=[km + "_%d" % half])
        kms = [km + "_0", km + "_1"]
        A(SP, lambda e: e.dma_start(out=xmid[tok, :], in_=xm), r=kms, w=["xmid%d" % tt], dma=True)
        A(ACT, lambda e: e.activation(out=h2, in_=xm, func=AF.Square, accum_out=sm[:, 0:1]), r=kms, w=[kh, ksm])
        A(ACT, lambda e: e.activation(out=sm[:, 1:2], in_=sm[:, 0:1], func=AF.Ln, bias=EPSC, scale=1.0 / 1024), r=[ksm, "cf"], w=[ksm])
        A(ACT, lambda e: e.activation(out=sm[:, 2:3], in_=sm[:, 1:2], func=AF.Exp, scale=-0.5), r=[ksm], w=[ksm])

    def S1c(tt):
        sl = tt % 2
        xm, h2, h2b = XM[sl], H2[sl], H2B[tt % NHB]
        km, kh, kb = "xm%d" % sl, "h2_%d" % sl, "h2b%d" % (tt % NHB)
        sm, ksm = SMS[:, sl, :], "sms%d" % sl
        kms = [km + "_0", km + "_1"]
        A(DVE, lambda e: e.scalar_tensor_tensor(out=h2, in0=xm, scalar=sm[:, 2:3], in1=GFFN, op0=ALU.mult, op1=ALU.mult), r=kms + [ksm, "cf"], w=[kh])
        A(ACT, lambda e: e.activation(out=h2b, in_=h2, func=AF.Copy), r=[kh], w=[kb])

    def S1b(tt):
        sl = tt % 2
        h2, h2t, kh = H2[sl], H2T[sl], "h2_%d" % sl
        for hb in range(2):
            def tr(e, hb=hb):
                for k in range(4):
                    kk = hb * 4 + k
                    ins = e.transpose(out=psf(2 + hb)[:, k * 128:(k + 1) * 128], in_=h2[:, kk * 128:(kk + 1) * 128], identity=IDF)
                return ins
            A(PE, tr, r=[kh, "cf"], w=[pk(2 + hb)])
            A(ACT, lambda e, hb=hb: e.activation(out=h2t[:, 4 * hb:4 * hb + 4, :], in_=psf(2 + hb).rearrange("p (k t) -> p k t", k=4), func=AF.Copy),
              r=[pk(2 + hb)], w=["h2t%d_%d" % (sl, hb)])

        def rmm(e):
            for k in range(8):
                ins = e.matmul(psf(6)[:, 0:72], lhsT=h2t[:, k, :], rhs=WR[:, k, :], start=(k == 0), stop=(k == 7))
            return ins
        A(PE, rmm, r=["h2t%d_0" % sl, "h2t%d_1" % sl, "wr"], w=[pk(6)])
        b, i = tt // NT, tt % NT
        A(DVE, lambda e: e.tensor_tensor(out=L4[b % 2][:, i, :], in0=psf(6)[:, 0:72], in1=BIASR, op=ALU.add), r=[pk(6), "cf"], w=["l4_%d_%d" % (b % 2, i)])

    V = lambda fn, r=(), w=(): A(DVE, fn, r=list(r) + [RK], w=list(w) + [RK])
    bc3 = lambda ap: ap.unsqueeze(2).broadcast_to([128, NT, 8])
    bcE = lambda ap: ap.unsqueeze(3).broadcast_to([128, NT, 8, 8])
    bcG = lambda ap: ap.unsqueeze(2).broadcast_to([128, NT, 8, 8])
    R_ = lambda i: SM[:, i, :]

    def S2B(b):
        l4 = L4[b % 2]
        lk = ["l4_%d_%d" % (b % 2, j) for j in range(NT)]
        LG_ = l4[:, :, 0:8]
        LE_ = l4[:, :, 8:72].rearrange("p t (g x) -> p t g x", g=8)
        V(lambda e: e.tensor_reduce(out=R_(0), in_=LG_, axis=AX.X, op=ALU.max), r=lk)
        V(lambda e: e.tensor_tensor(out=OHG, in0=LG_, in1=bc3(R_(0)), op=ALU.is_equal), r=lk)
        V(lambda e: e.tensor_tensor(out=D8, in0=LG_, in1=bc3(R_(0)), op=ALU.subtract), r=lk)
        A(ACT, lambda e: e.activation(out=E8, in_=D8, func=AF.Exp), r=[RK], w=[RK])
        V(lambda e: e.tensor_reduce(out=R_(1), in_=E8, axis=AX.X, op=ALU.add))
        V(lambda e: e.reciprocal(out=R_(2), in_=R_(1)))
        V(lambda e: e.tensor_tensor(out=T64, in0=LE_, in1=bcE(OHG), op=ALU.mult), r=lk)
        V(lambda e: e.tensor_reduce(out=LSEL, in_=T64.rearrange("p t g x -> p t x g"), axis=AX.X, op=ALU.add))
        V(lambda e: e.tensor_reduce(out=R_(3), in_=LSEL, axis=AX.X, op=ALU.max))
        V(lambda e: e.tensor_tensor(out=OH1, in0=LSEL, in1=bc3(R_(3)), op=ALU.is_equal))
        V(lambda e: e.scalar_tensor_tensor(out=L2, in0=OH1, scalar=-1e30, in1=LSEL, op0=ALU.mult, op1=ALU.add))
        V(lambda e: e.tensor_reduce(out=R_(4), in_=L2, axis=AX.X, op=ALU.max))
        V(lambda e: e.tensor_tensor(out=OH2, in0=L2, in1=bc3(R_(4)), op=ALU.is_equal))
        V(lambda e: e.tensor_tensor(out=R_(5), in0=R_(4), in1=R_(3), op=ALU.subtract))
        A(ACT, lambda e: e.activation(out=R_(6), in_=R_(5), func=AF.Exp), r=[RK], w=[RK])
        V(lambda e: e.tensor_tensor(out=A1F, in0=bcE(OHG), in1=bcG(OH1), op=ALU.mult))
        V(lambda e: e.tensor_tensor(out=A2F, in0=bcE(OHG), in1=bcG(OH2), op=ALU.mult))
        V(lambda e: e.tensor_tensor(out=AB[:, b * NT:(b + 1) * NT, :].rearrange("p t (g x) -> p t g x", g=8), in0=A1F, in1=A2F, op=ALU.add), w=["ab%d" % b])
        V(lambda e: e.tensor_scalar(out=R_(7), in0=R_(6), scalar1=1.0, scalar2=None, op0=ALU.add))
        V(lambda e: e.reciprocal(out=R_(8), in_=R_(7)))
        V(lambda e: e.tensor_tensor(out=G12[:, 0, b * NT:(b + 1) * NT], in0=R_(2), in1=R_(8), op=ALU.mult), w=["g12"])
        V(lambda e: e.tensor_tensor(out=G12[:, 1, b * NT:(b + 1) * NT], in0=R_(2), in1=G12[:, 0, b * NT:(b + 1) * NT], op=ALU.subtract), r=["g12"], w=["g12"])

    def S3B(b):
        def rank(e):
            for i in range(NT):
                tt = b * NT + i
                o = psf(7)[:, i * 64:(i + 1) * 64]
                for t2 in range(tt):
                    e.matmul(o, lhsT=ONESB, rhs=AB[:, t2, :], start=(t2 == 0), stop=False)
                ins = e.matmul(o, lhsT=TRI, rhs=AB[:, tt, :], start=(tt == 0), stop=True)
            return ins
        A(PE, rank, r=["ab%d" % i for i in range(b + 1)] + ["cb"], w=[pk(7)])
        V(lambda e: e.scalar_tensor_tensor(out=RB.rearrange("p t g x -> p t (g x)"), in0=psf(7)[:, 0:NT * 64].rearrange("p (t x) -> p t x", t=NT), scalar=float(EXPERT_CAP - 1),
                                           in1=OFFS.unsqueeze(1).broadcast_to([128, NT, 64]), op0=ALU.min, op1=ALU.add), r=[pk(7), "cf"])
        for a, AF_ in enumerate((A1F, A2F)):
            V(lambda e, AF_=AF_: e.tensor_tensor(out=T64, in0=AF_, in1=RB, op=ALU.mult))
            V(lambda e, a=a: e.tensor_reduce(out=R_(9 + a), in_=T64.rearrange("p t g x -> p t (g x)"), axis=AX.X, op=ALU.add))
            for i in range(NT):
                tt = b * NT + i
                dk = "dest%d_%d" % (a, tt)
                V(lambda e, a=a, i=i, tt=tt: e.tensor_copy(out=DEST[a][tt][:, :], in_=R_(9 + a)[:, i:i + 1]), w=[dk])
        for i in range(NT):
            tt = b * NT + i
            h2b, kb = H2B[tt % NHB], "h2b%d" % (tt % NHB)
            for a in range(2):
                dk = "dest%d_%d" % (a, tt)
                A(POOL, lambda e, a=a, tt=tt, h2b=h2b: e.indirect_dma_start(
                    out=xg, out_offset=bass.IndirectOffsetOnAxis(ap=DEST[a][tt][:, 0:1], axis=0), in_=h2b, in_offset=None,
                    bounds_check=None, oob_is_err=False), r=[dk, kb, "xg"], w=["xgs%d_%d" % (a, tt)], dma=True)

    S1(0)
    S1c(0)
    S1(1)
    S1b(0)
    S1c(1)
    for tt in range(16):
        if tt + 2 < 16:
            S1(tt + 2)
        if tt + 1 < 16:
            S1b(tt + 1)
        if tt + 2 < 16:
            S1c(tt + 2)
        if tt % NT == 1 and tt >= NT:
            S2B(tt // NT - 1)
        if tt % NT == 2 and tt >= NT:
            S3B(tt // NT - 1)
    S2B(3)
    S3B(3)

    if "route" in dumps:
        add_dump("g12", G12, ["g12"], [128, 2, 16], F32)
        for a in range(2):
            for tt in (0, 15):
                add_dump("dest%d_%d" % (a, tt), DEST[a][tt][:, :], ["dest%d_%d" % (a, tt)], [128, 1], I32)
        add_dump("xm", XM[1], ["xm1_0", "xm1_1"], [128, 1024], F32)
    if stage <= 4:
        P.emit(nc)
        return nc, dump_out

    M.reset("vt"); M.reset("r2"); M.reset("ut"); M.reset("qmt"); M.reset("r1")
    NW = 3
    W1 = [M.alloc("vt" if i == 0 else "r2", "w1_%d" % i, [128, 8, 512], BF16) for i in range(NW)]
    W3 = [M.alloc("vt" if i == 0 else "r2", "w3_%d" % i, [128, 8, 512], BF16) for i in range(NW)]
    W2 = [M.alloc("vt" if i == 0 else "ut", "w2_%d" % i, [128, 4, 1024], BF16) for i in range(NW)]
    XE = [M.alloc("vt", "xe%d" % i, [128, 1024], BF16) for i in range(2)]
    XET = [M.alloc("vt", "xet%d" % i, [128, 8, 128], BF16) for i in range(2)]
    SA = M.alloc("qmt", "sa", [128, 512], F32)
    HM = M.alloc("qmt", "hm", [128, 512], BF16)
    HMT = M.alloc("qmt", "hmt", [128, 4, 128], BF16)
    YE = [M.alloc("r1", "ye%d" % i, [128, 1024], BF16) for i in range(2)]

    def wload(e_):
        i = e_ % NW
        A(POOL, lambda e, i=i, e_=e_: e.dma_start(out=W1[i], in_=D["w1"][e_].rearrange("(k p) f -> p k f", p=128)), w=["w1_%d" % i], dma=True)
        A(POOL, lambda e, i=i, e_=e_: e.dma_start(out=W3[i], in_=D["w3"][e_].rearrange("(k p) f -> p k f", p=128)), w=["w3_%d" % i], dma=True)
        A(POOL, lambda e, i=i, e_=e_: e.dma_start(out=W2[i], in_=D["w2"][e_].rearrange("(k p) f -> p k f", p=128)), w=["w2_%d" % i], dma=True)

    NEXP = 64
    for e_ in range(min(NW - 1, NEXP)):
        wload(e_)
    for e_ in range(NEXP):
        if e_ + NW - 1 < NEXP:
            wload(e_ + NW - 1)
        i = e_ % NW
        s2 = e_ % 2
        xe, xet, ye = XE[s2], XET[s2], YE[s2]
        kxe, kxt, kye = "xe%d" % s2, "xet%d" % s2, "ye%d" % s2
        tb = 0 if s2 == 0 else 6
        ab = 1 if s2 == 0 else 7
        A(SP, lambda e, xe=xe, e_=e_: e.dma_start(out=xe, in_=xg[e_ * 128:(e_ + 1) * 128, :]), r=["xg"] + ["xgs%d_%d" % (a, t) for a in range(2) for t in range(16)], w=[kxe], dma=True)

        def trx(e, xe=xe, tb=tb):
            for k in range(8):
                ins = e.transpose(out=psb(tb)[:, k * 128:(k + 1) * 128], in_=xe[:, k * 128:(k + 1) * 128], identity=IDB)
            return ins
        A(PE, trx, r=[kxe, "cb"], w=[pk(tb)])
        A(ACT, lambda e, xet=xet, tb=tb: e.activation(out=xet, in_=psb(tb).rearrange("p (k t) -> p k t", k=8), func=AF.Copy), r=[pk(tb)], w=[kxt])

        def up(e, W, bank, xet=xet):
            for k in range(8):
                ins = e.matmul(psf(bank), lhsT=xet[:, k, :], rhs=W[:, k, :], start=(k == 0), stop=(k == 7))
            return ins
        A(PE, lambda e, i=i, ab=ab, up=up: up(e, W1[i], ab), r=[kxt, "w1_%d" % i], w=[pk(ab)])
        A(PE, lambda e, i=i, up=up: up(e, W3[i], 2), r=[kxt, "w3_%d" % i], w=[pk(2)])
        A(ACT, lambda e, ab=ab: e.activation(out=SA, in_=psf(ab), func=AF.Silu), r=[pk(ab)], w=["sa"])
        A(DVE, lambda e: e.tensor_tensor(out=HM, in0=SA, in1=psf(2), op=ALU.mult), r=["sa", pk(2)], w=["hm"])

        def trh(e):
            for k in range(4):
                ins = e.transpose(out=psb(3)[:, k * 128:(k + 1) * 128], in_=HM[:, k * 128:(k + 1) * 128], identity=IDB)
            return ins
        A(PE, trh, r=["hm", "cb"], w=[pk(3)])
        A(DVE, lambda e: e.tensor_copy(out=HMT, in_=psb(3)[:, 0:512].rearrange("p (k t) -> p k t", k=4)), r=[pk(3)], w=["hmt"])
        for half in range(2):
            def dn(e, i=i, half=half):
                for k in range(4):
                    ins = e.matmul(psf(4 + half), lhsT=HMT[:, k, :], rhs=W2[i][:, k, half * 512:(half + 1) * 512], start=(k == 0), stop=(k == 3))
                return ins
            A(PE, dn, r=["hmt", "w2_%d" % i], w=[pk(4 + half)])
        A(ACT, lambda e, ye=ye: e.activation(out=ye[:, 0:512], in_=psf(4), func=AF.Copy), r=[pk(4)], w=[kye + "a"])
        A(DVE, lambda e, ye=ye: e.tensor_copy(out=ye[:, 512:1024], in_=psf(5)), r=[pk(5)], w=[kye + "b"])
        A(SP, lambda e, ye=ye, e_=e_: e.dma_start(out=yg[e_ * 128:(e_ + 1) * 128, :], in_=ye), r=[kye + "a", kye + "b"], w=["yg%d" % e_], dma=True)

    if stage <= 5:
        P.emit(nc)
        return nc, dump_out

    M.reset("kt"); M.reset("vt")
    NC_ = 4
    Y1 = [M.alloc("kt", "y1_%d" % i, [128, 1024], BF16) for i in range(NC_)]
    Y2 = [M.alloc("kt", "y2_%d" % i, [128, 1024], BF16) for i in range(NC_)]
    XM2 = [M.alloc("vt", "xm2_%d" % i, [128, 1024], F32) for i in range(NC_)]
    OT_ = [M.alloc("vt", "ot_%d" % i, [128, 1024], F32) for i in range(NC_)]
    for tt in range(16):
        sl = tt % NC_
        tok = slice(tt * 128, (tt + 1) * 128)
        for a, Y in enumerate((Y1, Y2)):
            A(POOL, lambda e, a=a, tt=tt, y=Y[sl]: e.indirect_dma_start(
                out=y, out_offset=None, in_=yg, in_offset=bass.IndirectOffsetOnAxis(ap=DEST[a][tt][:, 0:1], axis=0),
                bounds_check=None, oob_is_err=False), r=["dest%d_%d" % (a, tt)] + ["yg%d" % i for i in range(64)], w=["y%d_%d" % (a + 1, sl)], dma=True)
        A(SP, lambda e, sl=sl, tok=tok: e.dma_start(out=XM2[sl], in_=xmid[tok, :]), r=["xmid%d" % tt], w=["xm2_%d" % sl], dma=True)
        A(DVE, lambda e, sl=sl, tt=tt: e.scalar_tensor_tensor(out=OT_[sl], in0=Y1[sl], scalar=G12[:, 0, tt:tt + 1], in1=XM2[sl], op0=ALU.mult, op1=ALU.add),
          r=["y1_%d" % sl, "xm2_%d" % sl, "g12"], w=["ot_%d" % sl])
        A(DVE, lambda e, sl=sl, tt=tt: e.scalar_tensor_tensor(out=OT_[sl], in0=Y2[sl], scalar=G12[:, 1, tt:tt + 1], in1=OT_[sl], op0=ALU.mult, op1=ALU.add),
          r=["y2_%d" % sl, "ot_%d" % sl, "g12"], w=["ot_%d" % sl])
        A(SP, lambda e, sl=sl, tok=tok: e.dma_start(out=out[tok, :], in_=OT_[sl]), r=["ot_%d" % sl], w=["out"], dma=True)
    if "moe" in dumps:
        allxg = ["xg"] + ["xgs%d_%d" % (a, t) for a in range(2) for t in range(16)]
        for ee in (0, 37):
            add_dump("xg%d" % ee, xg[ee * 128:(ee + 1) * 128, :], allxg, [128, 1024], BF16)
            add_dump("yg%d" % ee, yg[ee * 128:(ee + 1) * 128, :], ["yg%d" % ee], [128, 1024], BF16)
        add_dump("g12", G12, ["g12"], [128, 2, 16], F32)
        for a in range(2):
            for tt in range(16):
                add_dump("dest%d_%d" % (a, tt), DEST[a][tt][:, :], ["dest%d_%d" % (a, tt)], [128, 1], I32)
        add_dump("xmid", xmid, ["xmid%d" % t for t in range(16)], [2048, 1024], F32)
    P.emit(nc)
    return nc, dump_out
```
